# Optimizing a Trainium2 kernel written in Bass

```python
import jax, jax.numpy as jnp
from jax import lax
import numpy as np

D_MODEL = 2048
BATCH = 32
SEQ = 256
DEPTH = 1
DEC_BATCH = 4
DEC_SEQ = 2048
PAST_LEN = 256

GRID_W = 64
HEAD_DIM = 128
AXIS_DIM = HEAD_DIM // 2
N_HEADS_GLOB = 8
N_KV_GLOB = 2
G_GLOB = N_HEADS_GLOB // N_KV_GLOB
N_HEADS_WIN = 8
N_KV_WIN = 2
G_WIN = N_HEADS_WIN // N_KV_WIN
MIX_WIDTH = (N_HEADS_GLOB + N_HEADS_WIN) * HEAD_DIM
QG_W = N_HEADS_GLOB * HEAD_DIM
KG_W = N_KV_GLOB * HEAD_DIM
QW_W = N_HEADS_WIN * HEAD_DIM
KW_W = N_KV_WIN * HEAD_DIM
QKV_WIDTH = QG_W + 2 * KG_W + QW_W + 2 * KW_W
QKV_SPLITS = (QG_W, QG_W + KG_W, QG_W + 2 * KG_W, QG_W + 2 * KG_W + QW_W, QG_W + 2 * KG_W + QW_W + KW_W)
WINDOW = 128
Q_BLOCK = 128
ROPE_THETA = 10000.0
NORM_EPS = 1e-6
N_EXPERTS = 64
N_EXPERT_GROUPS = 8
EXPERTS_PER_GROUP = N_EXPERTS // N_EXPERT_GROUPS
TOPK_GROUPS = 4
TOP_K = 8
D_EXPERT = 512
D_SHARED = 512
ROUTED_SCALE = 2.5
EXPERT_BLOCK = 128

kernel_name = "hybrid_dit_prefix_ctx_step"


def rms_norm(x, gain):
    xf = x.astype(jnp.float32)
    r = lax.rsqrt(jnp.mean(xf * xf, axis=-1, keepdims=True) + NORM_EPS)
    return (xf * r * gain.astype(jnp.float32)).astype(x.dtype)


def adaln_params(cond, w_ada, b_ada):
    mod = jax.nn.silu(cond) @ w_ada + b_ada
    return jnp.split(mod[:, None, :], 6, axis=-1)


def pre_modulate(x, gain, shift, scale):
    return rms_norm(x, gain) * (1 + scale) + shift


def axial_rope_tables(n_tokens):
    rows = n_tokens // GRID_W
    row = jnp.repeat(jnp.arange(rows, dtype=jnp.float32), GRID_W)
    col = jnp.tile(jnp.arange(GRID_W, dtype=jnp.float32), rows)
    inv_freq = ROPE_THETA ** (-jnp.arange(0, AXIS_DIM, 2, dtype=jnp.float32) / AXIS_DIM)
    ang_r = row[:, None] * inv_freq
    ang_c = col[:, None] * inv_freq
    ang = jnp.concatenate([ang_r, ang_r, ang_c, ang_c], axis=-1)
    return jnp.cos(ang), jnp.sin(ang)


def _rotate_half(h):
    a, b = jnp.split(h, 2, axis=-1)
    return jnp.concatenate([-b, a], axis=-1)


def apply_axial_rope(x, cos, sin):
    shape = (1, x.shape[1]) + (1,) * (x.ndim - 3) + (HEAD_DIM,)
    xf = x.astype(jnp.float32)
    xr, xc = jnp.split(xf, 2, axis=-1)
    rot = jnp.concatenate([_rotate_half(xr), _rotate_half(xc)], axis=-1)
    return (xf * cos.reshape(shape) + rot * sin.reshape(shape)).astype(x.dtype)


def project_heads(h, w_in, q_gain, k_gain):
    B, S, _ = h.shape
    proj = h @ w_in
    qg, kg, vg, qw, kw, vw = jnp.split(proj, QKV_SPLITS, axis=-1)
    qg = rms_norm(qg.reshape(B, S, N_KV_GLOB, G_GLOB, HEAD_DIM), q_gain)
    kg = rms_norm(kg.reshape(B, S, N_KV_GLOB, HEAD_DIM), k_gain)
    vg = vg.reshape(B, S, N_KV_GLOB, HEAD_DIM)
    qw = qw.reshape(B, S, N_KV_WIN, G_WIN, HEAD_DIM)
    kw = kw.reshape(B, S, N_KV_WIN, HEAD_DIM)
    vw = vw.reshape(B, S, N_KV_WIN, HEAD_DIM)
    return qg, kg, vg, qw, kw, vw


def attend(q, k, v, mask, sink):
    B, Q, KV, G, HD = q.shape
    s = jnp.einsum('bqngd,bknd->bngqk', q.astype(jnp.float32), k.astype(jnp.float32)) * (HEAD_DIM ** -0.5)
    if mask is not None:
        s = jnp.where(mask, s, -jnp.inf)
    if sink is not None:
        sk = jnp.broadcast_to(sink.astype(jnp.float32).reshape(1, KV, G, 1, 1), s.shape[:-1] + (1,))
        p = jax.nn.softmax(jnp.concatenate([sk, s], axis=-1), axis=-1)[..., 1:]
    else:
        p = jax.nn.softmax(s, axis=-1)
    o = jnp.einsum('bngqk,bknd->bqngd', p, v.astype(jnp.float32))
    return o.astype(q.dtype)


def dense_attention(q, k, v, sink):
    B, S, KV, G, HD = q.shape
    nb = S // Q_BLOCK
    qb = jnp.moveaxis(q.reshape(B, nb, Q_BLOCK, KV, G, HD), 1, 0)
    out = lax.map(lambda qi: attend(qi, k, v, None, sink), qb)
    return jnp.moveaxis(out, 0, 1).reshape(B, S, KV, G, HD)


def window_attention(q, k, v, k_ctx, v_ctx, sink):
    B, S, KV, G, HD = q.shape
    nb = S // Q_BLOCK
    pad = ((0, 0), (Q_BLOCK, Q_BLOCK), (0, 0), (0, 0))
    kp = jnp.pad(k, pad)
    vp = jnp.pad(v, pad)
    r = jnp.arange(Q_BLOCK)[:, None]
    j = jnp.arange(3 * Q_BLOCK)[None, :]
    rel_ok = jnp.abs(j - Q_BLOCK - r) <= WINDOW
    ctx_ok = jnp.ones((Q_BLOCK, k_ctx.shape[1]), dtype=bool)

    def block(i):
        qi = lax.dynamic_slice_in_dim(q, i * Q_BLOCK, Q_BLOCK, axis=1)
        ki = lax.dynamic_slice_in_dim(kp, i * Q_BLOCK, 3 * Q_BLOCK, axis=1)
        vi = lax.dynamic_slice_in_dim(vp, i * Q_BLOCK, 3 * Q_BLOCK, axis=1)
        kpos = i * Q_BLOCK - Q_BLOCK + j
        band_ok = rel_ok & (kpos >= 0) & (kpos < S)
        mask = jnp.concatenate([ctx_ok, band_ok], axis=1)
        return attend(qi, jnp.concatenate([k_ctx, ki], axis=1), jnp.concatenate([v_ctx, vi], axis=1), mask, sink)

    out = lax.map(block, jnp.arange(nb))
    return jnp.moveaxis(out, 0, 1).reshape(B, S, KV, G, HD)


def merge_heads(o_glob, o_win, w_out):
    B, S = o_glob.shape[:2]
    o = jnp.concatenate([o_glob.reshape(B, S, -1), o_win.reshape(B, S, -1)], axis=-1)
    return o @ w_out


def swiglu(x, w_gate, w_up, w_down):
    return (jax.nn.silu(x @ w_gate) * (x @ w_up)) @ w_down


def routed_experts(xf, topi, gates, w_gate_e, w_up_e, w_down_e):
    T, D = xf.shape
    A = T * TOP_K
    n_blocks = -(-(A + N_EXPERTS * (EXPERT_BLOCK - 1)) // EXPERT_BLOCK)
    n_rows = n_blocks * EXPERT_BLOCK
    e_flat = topi.reshape(A)
    tok_flat = jnp.arange(A, dtype=jnp.int32) // TOP_K
    order = jnp.argsort(e_flat)
    e_sorted = e_flat[order]
    counts = jnp.bincount(e_flat, length=N_EXPERTS)
    padded = (counts + EXPERT_BLOCK - 1) // EXPERT_BLOCK * EXPERT_BLOCK
    pad_end = jnp.cumsum(padded)
    pad_start = pad_end - padded
    start = jnp.cumsum(counts) - counts
    dest = pad_start[e_sorted] + jnp.arange(A, dtype=jnp.int32) - start[e_sorted]
    row_tok = jnp.full((n_rows,), T, dtype=jnp.int32).at[dest].set(tok_flat[order])
    row_gate = jnp.zeros((n_rows,), jnp.float32).at[dest].set(gates.reshape(A)[order])
    block_expert = jnp.minimum(
        jnp.searchsorted(pad_end, jnp.arange(n_blocks, dtype=jnp.int32) * EXPERT_BLOCK, side='right'),
        N_EXPERTS - 1)
    x_pad = jnp.concatenate([xf, jnp.zeros((1, D), xf.dtype)], axis=0)

    def block(args):
        toks, e = args
        return swiglu(x_pad[toks], w_gate_e[e], w_up_e[e], w_down_e[e])

    yb = lax.map(block, (row_tok.reshape(n_blocks, EXPERT_BLOCK), block_expert))
    y = jax.ops.segment_sum(yb.reshape(n_rows, D).astype(jnp.float32) * row_gate[:, None], row_tok,
                            num_segments=T + 1)[:T]
    return y.astype(xf.dtype)


def moe_ffn(h, w_router, router_bias, w_gate_e, w_up_e, w_down_e, w_gate_s, w_up_s, w_down_s):
    B, S, D = h.shape
    T = B * S
    xf = h.reshape(T, D)
    scores = jax.nn.sigmoid((xf @ w_router).astype(jnp.float32))
    sel = scores + router_bias.astype(jnp.float32)
    grp_score = lax.top_k(sel.reshape(T, N_EXPERT_GROUPS, EXPERTS_PER_GROUP), 2)[0].sum(-1)
    _, top_g = lax.top_k(grp_score, TOPK_GROUPS)
    gmask = jnp.any(top_g[..., None] == jnp.arange(N_EXPERT_GROUPS), axis=1)
    emask = jnp.repeat(gmask, EXPERTS_PER_GROUP, axis=1)
    _, topi = lax.top_k(jnp.where(emask, sel, -jnp.inf), TOP_K)
    w = jnp.take_along_axis(scores, topi, axis=1)
    w = w / jnp.sum(w, axis=-1, keepdims=True) * ROUTED_SCALE
    y = routed_experts(xf, topi, w, w_gate_e, w_up_e, w_down_e) + swiglu(xf, w_gate_s, w_up_s, w_down_s)
    return y.reshape(B, S, D)


def setup_inputs(seed: int = 0) -> dict:
    key = jax.random.key(seed)
    ks = jax.random.split(key, 32)
    f32 = jnp.float32
    D = D_MODEL
    nrm = lambda k, shape, s: jax.random.normal(k, shape, f32) * s
    gain = lambda k, shape: 1.0 + 0.1 * jax.random.normal(k, shape, f32)
    return {
        "x_prompt": nrm(ks[0], (BATCH, SEQ, D), 1.0),
        "x_sample": nrm(ks[1], (DEC_BATCH, DEC_SEQ, D), 1.0),
        "cache_glob_k": nrm(ks[2], (DEC_BATCH, DEPTH, PAST_LEN, N_KV_GLOB, HEAD_DIM), 1.0),
        "cache_glob_v": nrm(ks[3], (DEC_BATCH, DEPTH, PAST_LEN, N_KV_GLOB, HEAD_DIM), 1.0),
        "cache_win_k": nrm(ks[4], (DEC_BATCH, DEPTH, PAST_LEN, N_KV_WIN, HEAD_DIM), 1.0),
        "cache_win_v": nrm(ks[5], (DEC_BATCH, DEPTH, PAST_LEN, N_KV_WIN, HEAD_DIM), 1.0),
        "c": nrm(ks[6], (DEC_BATCH, D), 1.0),
        "c_ctx": nrm(ks[7], (D,), 1.0),
        "w_ada": nrm(ks[8], (DEPTH, D, 6 * D), 0.5 * D ** -0.5),
        "b_ada": nrm(ks[9], (DEPTH, 6 * D), 0.02),
        "attn_pre_g": gain(ks[10], (DEPTH, D)),
        "attn_post_g": gain(ks[11], (DEPTH, D)),
        "w_in": nrm(ks[12], (DEPTH, D, QKV_WIDTH), D ** -0.5),
        "q_norm_g": gain(ks[13], (DEPTH, HEAD_DIM)),
        "k_norm_g": gain(ks[14], (DEPTH, HEAD_DIM)),
        "sink_logit": nrm(ks[15], (DEPTH, N_HEADS_WIN), 0.5),
        "w_out": nrm(ks[16], (DEPTH, MIX_WIDTH, D), MIX_WIDTH ** -0.5),
        "ffn_pre_g": gain(ks[17], (DEPTH, D)),
        "ffn_post_g": gain(ks[18], (DEPTH, D)),
        "w_router": nrm(ks[19], (DEPTH, D, N_EXPERTS), D ** -0.5),
        "router_bias": nrm(ks[20], (DEPTH, N_EXPERTS), 0.01),
        "w_gate_e": nrm(ks[21], (DEPTH, N_EXPERTS, D, D_EXPERT), D ** -0.5),
        "w_up_e": nrm(ks[22], (DEPTH, N_EXPERTS, D, D_EXPERT), D ** -0.5),
        "w_down_e": nrm(ks[23], (DEPTH, N_EXPERTS, D_EXPERT, D), D_EXPERT ** -0.5),
        "w_gate_s": nrm(ks[24], (DEPTH, D, D_SHARED), D ** -0.5),
        "w_up_s": nrm(ks[25], (DEPTH, D, D_SHARED), D ** -0.5),
        "w_down_s": nrm(ks[26], (DEPTH, D_SHARED, D), D_SHARED ** -0.5),
    }


def reference(x_prompt, x_sample, cache_glob_k, cache_glob_v, cache_win_k, cache_win_v, c, c_ctx,
              w_ada, b_ada, attn_pre_g, attn_post_g, w_in, q_norm_g, k_norm_g, sink_logit, w_out,
              ffn_pre_g, ffn_post_g, w_router, router_bias, w_gate_e, w_up_e, w_down_e,
              w_gate_s, w_up_s, w_down_s):
    cos, sin = axial_rope_tables(x_sample.shape[1])
    y_p = x_prompt
    y_s = x_sample
    glob_k, glob_v, win_k, win_v = [], [], [], []
    for l in range(DEPTH):
        moe_w = (w_router[l], router_bias[l], w_gate_e[l], w_up_e[l], w_down_e[l],
                 w_gate_s[l], w_up_s[l], w_down_s[l])
        sh_a, sc_a, g_a, sh_f, sc_f, g_f = adaln_params(c_ctx[None, :], w_ada[l], b_ada[l])
        h = pre_modulate(y_p, attn_pre_g[l], sh_a, sc_a)
        qg, kg, vg, qw, kw, vw = project_heads(h, w_in[l], q_norm_g[l], k_norm_g[l])
        o = merge_heads(dense_attention(qg, kg, vg, None),
                        dense_attention(qw, kw, vw, sink_logit[l]), w_out[l])
        y_p = y_p + g_a * rms_norm(o, attn_post_g[l])
        glob_k.append(kg)
        glob_v.append(vg)
        win_k.append(kw)
        win_v.append(vw)
        h = pre_modulate(y_p, ffn_pre_g[l], sh_f, sc_f)
        y_p = y_p + g_f * rms_norm(moe_ffn(h, *moe_w), ffn_post_g[l])
        sh_a, sc_a, g_a, sh_f, sc_f, g_f = adaln_params(c, w_ada[l], b_ada[l])
        h = pre_modulate(y_s, attn_pre_g[l], sh_a, sc_a)
        qg, kg, vg, qw, kw, vw = project_heads(h, w_in[l], q_norm_g[l], k_norm_g[l])
        qg = apply_axial_rope(qg, cos, sin)
        kg = apply_axial_rope(kg, cos, sin)
        qw = apply_axial_rope(qw, cos, sin)
        kw = apply_axial_rope(kw, cos, sin)
        o_g = dense_attention(qg, jnp.concatenate([cache_glob_k[:, l], kg], axis=1),
                              jnp.concatenate([cache_glob_v[:, l], vg], axis=1), None)
        o_w = window_attention(qw, kw, vw, cache_win_k[:, l], cache_win_v[:, l], sink_logit[l])
        y_s = y_s + g_a * rms_norm(merge_heads(o_g, o_w, w_out[l]), attn_post_g[l])
        h = pre_modulate(y_s, ffn_pre_g[l], sh_f, sc_f)
        y_s = y_s + g_f * rms_norm(moe_ffn(h, *moe_w), ffn_post_g[l])
    new_glob_k = jnp.stack(glob_k, axis=1)
    new_glob_v = jnp.stack(glob_v, axis=1)
    new_win_k = jnp.stack(win_k, axis=1)
    new_win_v = jnp.stack(win_v, axis=1)
    return (y_p, y_s, new_glob_k, new_glob_v, new_win_k, new_win_v)
```

```python
import numpy as np
from contextlib import ExitStack
import concourse.bass as bass
import concourse.mybir as mybir
from concourse.bass_utils import run_bass_kernel_spmd

F32 = mybir.dt.float32
BF16 = mybir.dt.bfloat16
I32 = mybir.dt.int32
U32 = mybir.dt.uint32
AF = mybir.ActivationFunctionType
ALU = mybir.AluOpType
AX = mybir.AxisListType

D = 2048
NCORES = 8
EPS = 1e-6
HD = 128
SCALE = HD ** -0.5
NE = 64
CAP = 1024
NSLOT = NE * CAP + 2048
STAGE = 99
DEBUG = False
SKIP_INPUTS = set()
INPUT_NAMES = []


class _Eng:
    def __init__(self, key):
        self.key = key
        self.sem = None
        self.count = 0
        self.thunks = []
        self.waited = {}


class Res:
    __slots__ = ("name", "w", "r")

    def __init__(self, name):
        self.name = name
        self.w = None
        self.r = []


class Sched:
    def __init__(self, nc, n_dma_sems=24):
        self.nc = nc
        self.eng = {k: _Eng(k) for k in ("tensor", "vector", "scalar", "gpsimd", "sync")}
        self.n_dma_sems = n_dma_sems
        self.dma_sems = {}
        self.dma_rr = {}
        self.sems = {}
        self.phase_id = 0

    def alloc_sems(self, stack):
        for k, e in self.eng.items():
            e.sem = stack.enter_context(self.nc.semaphore("s_" + k))
            self.sems[("e", k)] = e.sem
        for q in ("sync", "gpsimd"):
            lst = []
            for i in range(self.n_dma_sems):
                s = stack.enter_context(self.nc.semaphore(f"d_{q}_{i}"))
                self.sems[("d", q, i)] = s
                lst.append([("d", q, i), 0])
            self.dma_sems[q] = lst
            self.dma_rr[q] = 0

    def _deps(self, reads, writes):
        deps = []
        for r in reads:
            if r.w is not None:
                deps.append(r.w)
        for w in writes:
            if w.w is not None:
                deps.append(w.w)
            deps.extend(w.r)
        return deps

    def _waits(self, e, deps, skip_self=False):
        need = {}
        for src, val in deps:
            if skip_self and src == ("e", e.key):
                continue
            if e.waited.get(src, 0) >= val:
                continue
            if need.get(src, 0) < val:
                need[src] = val
        for src, val in need.items():
            e.waited[src] = val
        return list(need.items())

    def op(self, engine, fn, reads=(), writes=(), signal=True):
        e = self.eng[engine]
        waits = self._waits(e, self._deps(reads, writes), skip_self=(engine == "tensor"))
        if signal:
            e.count += 1
            tok = (("e", engine), e.count)
            for r in reads:
                r.r.append(tok)
            for w in writes:
                w.w = tok
                w.r = []
        sems = self.sems

        def thunk(h, waits=waits, fn=fn, signal=signal, sem=e.sem):
            for src, val in waits:
                h.wait_ge(sems[src], val)
            ins = fn(h)
            if signal:
                ins.then_inc(sem, 1)
        e.thunks.append(thunk)

    def dma(self, queue, fn, reads=(), writes=()):
        e = self.eng[queue]
        lst = self.dma_sems[queue]
        i = self.dma_rr[queue]
        self.dma_rr[queue] = (i + 1) % len(lst)
        slot = lst[i]
        deps = self._deps(reads, writes)
        if slot[1] > 0:
            deps.append((slot[0], slot[1]))
        waits = self._waits(e, deps)
        slot[1] += 16
        tok = (slot[0], slot[1])
        for r in reads:
            r.r.append(tok)
        for w in writes:
            w.w = tok
            w.r = []
        sems = self.sems

        def thunk(h, waits=waits, fn=fn, sem=sems[slot[0]]):
            for src, val in waits:
                h.wait_ge(sems[src], val)
            fn(h).then_inc(sem, 16)
        e.thunks.append(thunk)

    def final_wait(self, engine, resources):
        e = self.eng[engine]
        deps = [r.w for r in resources if r.w is not None]
        waits = self._waits(e, deps)
        sems = self.sems

        def thunk(h, waits=waits):
            for src, val in waits:
                h.wait_ge(sems[src], val)
        e.thunks.append(thunk)

    def drain(self):
        e = self.eng["sync"]
        deps = []
        for q, lst in self.dma_sems.items():
            for slot in lst:
                if slot[1] > 0:
                    deps.append((slot[0], slot[1]))
        for k, e2 in self.eng.items():
            if e2.count > 0 and k != "sync":
                deps.append((("e", k), e2.count))
        waits = self._waits(e, deps)
        sems = self.sems

        def thunk(h, waits=waits):
            for src, val in waits:
                h.wait_ge(sems[src], val)
        e.thunks.append(thunk)

    def emit(self):
        self.drain()
        self.phase_id = getattr(self, "phase_id", 0) + 1
        with self.nc.Block() as block:
            for k, e in self.eng.items():
                if not e.thunks:
                    continue

                def body(h, thunks=list(e.thunks)):
                    for t in thunks:
                        t(h)
                getattr(block, k)(body)
        for e in self.eng.values():
            e.thunks = []


def build():
    nc = bass.Bass("TRN2", target_bir_lowering=False)

    def din(name, shape, dt=F32):
        if name in SKIP_INPUTS:
            return None
        INPUT_NAMES.append(name)
        return nc.dram_tensor(name, list(shape), dt, kind="ExternalInput").ap()

    def dout(name, shape, dt=F32):
        return nc.dram_tensor(name, list(shape), dt, kind="ExternalOutput").ap()

    def dscr(name, shape, dt=F32):
        kind = "ExternalOutput" if (DEBUG and name in ("ysp", "dbg_dst", "dbg_gate", "dbg_moe", "dbg_sc")) else "Internal"
        return nc.dram_tensor(name, list(shape), dt, kind=kind).ap()

    xc = din("xc", [1024, D])
    xl = din("xl", [2048, D])
    ropec = din("ropec", [2048, 128])
    ropes = din("ropes", [2048, 128])
    cache = din("cache", [4, 256, 256])
    cond = din("cond", [2, D])
    w_ada = din("w_ada", [D, 6 * D])
    b_ada = din("b_ada", [1, 6 * D])
    gains = din("gains", [4, D])
    w_in = din("w_in", [D, 3072])
    qkg = din("qkg", [2, 128])
    sink = din("sink", [1, 8])
    w_out = din("w_out", [D, D])
    w_router = din("w_router", [D, NE])
    rbias = din("rbias", [1, NE])
    wge = din("wge", [NE, D, 512])
    wue = din("wue", [NE, D, 512])
    wde = din("wde", [NE, 512, D])
    wgs = din("wgs", [D, 512])
    wus = din("wus", [D, 512])
    wds = din("wds", [512, D])
    consts = din("consts", [128, 7 * 128])

    yc = dout("yc", [1024, D])
    yl = dout("yl", [1024, D])
    ngk = dout("ngk", [1024, 256])
    ngv = dout("ngv", [1024, 256])
    nwk = dout("nwk", [1024, 256])
    nwv = dout("nwv", [1024, 256])

    modsp = dscr("modsp", [12, 128, D])

    R = {}

    def res(name):
        if name not in R:
            R[name] = Res(name)
        return R[name]

    with ExitStack() as st:
        s = Sched(nc)
        s.alloc_sems(st)
        st.enter_context(nc.allow_non_contiguous_dma(reason="small strided loads"))
        st.enter_context(nc.allow_low_precision(reason="bf16 matmul operands"))

        def sb(name, shape, dt=F32):
            return st.enter_context(nc.sbuf_tensor(name, list(shape), dt))

        def ps(name, shape, dt=F32):
            return st.enter_context(nc.psum_tensor(name, list(shape), dt))

        pp = [ps(f"pp{i}", [128, 512], F32) for i in range(6)]
        pt = [ps(f"pt{i}", [128, 1024], BF16) for i in range(2)]
        r_pp = [res(f"pp{i}") for i in range(6)]
        r_pt = [res(f"pt{i}") for i in range(2)]

        cf = sb("cf", [128, 7 * 128], F32)
        cb = sb("cb", [128, 7 * 128], BF16)
        s.dma("sync", lambda h: h.dma_start(out=cf[:], in_=consts[:, :]), writes=[res("cf")])
        s.op("vector", lambda h: h.tensor_copy(out=cb[:], in_=cf[:]), reads=[res("cf")], writes=[res("cb")])
        identb = cb[:, 0:128]
        onesb = cb[:, 256:384]

        ph0 = ExitStack()

        def sb0(name, shape, dt=F32):
            return ph0.enter_context(nc.sbuf_tensor(name, list(shape), dt))
        condT = sb0("condT", [128, 16, 2], F32)
        crep = sb0("crep", [128, 2, 16, 128], BF16)
        gbc = sb0("gbc", [128, 4, D], F32)
        for p in range(2):
            s.dma("sync", lambda h, p=p: h.dma_start(
                out=condT[:, :, p], in_=cond[p:p + 1, :].rearrange("r (c p) -> p (r c)", p=128)),
                writes=[res("condT")])
        s.dma("sync", lambda h: h.dma_start(out=gbc[:], in_=gains.partition_broadcast(128)), writes=[res("gbc")])
        for p in range(2):
            s.op("scalar", lambda h, p=p: h.activation(
                out=crep[:, p, :, :], in_=condT[:, :, p:p + 1].broadcast_to([128, 16, 128]), func=AF.Silu),
                reads=[res("condT")], writes=[res("crep")])
        wa = [sb0(f"wa{i}", [128, 16, 512], BF16) for i in range(2)]
        bb = [sb0(f"bb{i}", [128, 512], F32) for i in range(2)]
        mt = [sb0(f"mt{i}", [128, 512], F32) for i in range(4)]
        gain_of = {1: 0, 2: 1, 4: 2, 5: 3}
        n_mt = 0
        for j in range(24):
            which, cc = j // 4, j % 4
            b = j % 2
            s.dma("gpsimd", lambda h, b=b, j=j: h.dma_start(
                out=wa[b][:], in_=w_ada[:, j * 512:(j + 1) * 512].rearrange("(c p) n -> p c n", p=128)),
                writes=[res(f"wa{b}")])
            s.dma("sync", lambda h, b=b, j=j: h.dma_start(
                out=bb[b][:], in_=b_ada[:, j * 512:(j + 1) * 512].partition_broadcast(128)),
                writes=[res(f"bb{b}")])
            for p in range(2):
                pi = (j * 2 + p) % 6
                for k in range(16):
                    s.op("tensor", lambda h, p=p, k=k, b=b, pi=pi: h.matmul(
                        pp[pi][:], lhsT=crep[:, p, k, :], rhs=wa[b][:, k, :], start=(k == 0), stop=(k == 15)),
                        reads=[res("crep"), res(f"wa{b}")], writes=[r_pp[pi]], signal=(k == 15))
                m = mt[n_mt % 4]
                rm = res(f"mt{n_mt % 4}")
                n_mt += 1
                if which in (0, 3):
                    s.op("vector", lambda h, m=m, pi=pi, b=b: h.tensor_tensor(
                        out=m[:], in0=pp[pi][:], in1=bb[b][:], op=ALU.add),
                        reads=[r_pp[pi], res(f"bb{b}")], writes=[rm])
                else:
                    gsl = gbc[:, gain_of[which], cc * 512:(cc + 1) * 512]
                    s.op("vector", lambda h, m=m, pi=pi, b=b: h.tensor_tensor(
                        out=m[:], in0=pp[pi][:], in1=bb[b][:], op=ALU.add),
                        reads=[r_pp[pi], res(f"bb{b}")], writes=[rm])
                    add1 = 1.0 if which in (1, 4) else 0.0
                    s.op("vector", lambda h, m=m, gsl=gsl, add1=add1: h.scalar_tensor_tensor(
                        out=m[:], in0=m[:], scalar=add1, in1=gsl, op0=ALU.add, op1=ALU.mult),
                        reads=[rm, res("gbc")], writes=[rm])
                idx = which * 2 + p
                s.dma("sync", lambda h, m=m, idx=idx, cc=cc: h.dma_start(
                    out=modsp[idx, :, cc * 512:(cc + 1) * 512], in_=m[:]),
                    reads=[rm])

        s.emit()
        ph0.close()

        qsp = [dscr("qsp0", [1024, 2560], BF16), dscr("qsp1", [2048, 2560], BF16)]
        vsp = [dscr("vsp0", [1024, 512], BF16), dscr("vsp1", [2048, 512], BF16)]
        ysp = dscr("ysp", [2048, D])
        xbuf = dscr("xbuf", [NE * CAP, D], BF16)
        xbuf_s = dscr("xbuf_s", [2048, D], BF16)
        obuf_s = dscr("obuf_s", [2048, D], BF16)
        obuf = dscr("obuf", [NE * CAP, D], BF16)
        xin = [xc, xl]
        youts = [yc, yl]
        identf = cf[:, 0:128]
        onesf = cf[:, 256:384]
        ustrb = cb[:, 128:256]
        _bc = {}

        def bc_reg(h):
            if _bc.get("phase") != s.phase_id:
                _bc["r"] = h.to_reg(NE * CAP - 1)
                _bc["phase"] = s.phase_id
            return _bc["r"]

        mx = sb("mx", [128, 2, 4], F32)
        negm = sb("negm", [128, 2, 2], F32)
        Mall = sb("Mall", [128, 16, NE], BF16)
        dstall = sb("dstall", [128, 16, 8], I32)
        gall = sb("gall", [128, 16, 8], F32)
        sinkb = sb("sinkb", [128, 8], F32)
        g10 = sb("g10", [128, 10, 128], F32)
        rbb = sb("rbb", [128, NE], F32)
        s.op("vector", lambda h: h.memset(mx[:], 0.0), writes=[res("mx")])
        s.dma("sync", lambda h: h.dma_start(out=sinkb[:], in_=sink.partition_broadcast(128)), writes=[res("sinkb")])
        s.dma("sync", lambda h: h.dma_start(out=rbb[:], in_=rbias.partition_broadcast(128)), writes=[res("rbb")])
        for hh in range(10):
            s.dma("sync", lambda h, hh=hh: h.dma_start(
                out=g10[:, hh, :], in_=qkg[(0 if hh < 8 else 1):(1 if hh < 8 else 2), :].partition_broadcast(128)),
                writes=[res("g10")])

        def rstd_chain(ssq_ap, rs_ap, n, r_in, r_out):
            s.op("vector", lambda h: h.tensor_scalar(out=rs_ap, in0=ssq_ap, scalar1=1.0 / n, scalar2=EPS,
                                                     op0=ALU.mult, op1=ALU.add), reads=[r_in], writes=[r_out])
            s.op("scalar", lambda h: h.activation(out=rs_ap, in_=rs_ap, func=AF.Sqrt), reads=[r_out], writes=[r_out])
            s.op("vector", lambda h: h.reciprocal(out=rs_ap, in_=rs_ap), reads=[r_out], writes=[r_out])

        def transpose16(src_bf, r_src, dst, r_dst, dst_slices):
            for half in range(2):
                for c in range(8):
                    cc = half * 8 + c
                    s.op("tensor", lambda h, half=half, c=c, cc=cc: h.transpose(
                        out=pt[half][:, c * 128:(c + 1) * 128], in_=src_bf[:, cc * 128:(cc + 1) * 128], identity=identb),
                        reads=[r_src, res("cb")], writes=[r_pt[half]], signal=(c == 7))
                eng = "scalar" if half == 0 else "vector"
                o = dst_slices(half)
                i_ = pt[half][:, :].rearrange("p (c t) -> p c t", t=128)
                if eng == "scalar":
                    s.op("scalar", lambda h, o=o, i_=i_: h.copy(out=o, in_=i_), reads=[r_pt[half]], writes=[r_dst])
                else:
                    s.op("vector", lambda h, o=o, i_=i_: h.tensor_copy(out=o, in_=i_), reads=[r_pt[half]], writes=[r_dst])

        def qkv_phase(p):
            nt = 8 if p == 0 else 16
            with ExitStack() as ph:
                def sbp(name, shape, dt=F32):
                    return ph.enter_context(nc.sbuf_tensor(f"{name}_{p}", list(shape), dt))
                win = sbp("win", [128, 16, 3072], BF16)
                A1 = sbp("A1", [128, D]); B1 = sbp("B1", [128, D])
                xt = [sbp(f"xt{i}", [128, D]) for i in range(2)]
                hb = sbp("hb", [128, D], BF16)
                hT = sbp("hT", [128, 16, 128], BF16)
                qkall = sbp("qkall", [128, 3072])
                qk20 = sbp("qk20", [128, 20, 128])
                t1 = sbp("t1", [128, 20, 128]); t2 = sbp("t2", [128, 20, 128])
                qkb = sbp("qkb", [128, 20, 128], BF16)
                vb = sbp("vb", [128, 512], BF16)
                rc = sbp("rc", [128, 128]); rs_ = sbp("rs", [128, 128])
                st1 = sbp("st1", [128, 8]); st10 = sbp("st10", [128, 10]); st20 = sbp("st20", [128, 20]); g4 = sbp("g4", [128, 4])
                for n in range(6):
                    s.dma("gpsimd", lambda h, n=n: h.dma_start(
                        out=win[:, :, n * 512:(n + 1) * 512],
                        in_=w_in[:, n * 512:(n + 1) * 512].rearrange("(c p) n -> p c n", p=128)), writes=[res("win")])
                s.dma("sync", lambda h: h.dma_start(out=A1[:], in_=modsp[2 + p]), writes=[res("A1")])
                s.dma("sync", lambda h: h.dma_start(out=B1[:], in_=modsp[0 + p]), writes=[res("B1")])
                for i in range(nt):
                    own = i < 8
                    b = i % 2
                    rx = res(f"xt{b}")
                    s.dma("sync", lambda h, b=b, i=i: h.dma_start(out=xt[b][:], in_=xin[p][i * 128:(i + 1) * 128, :]), writes=[rx])
                    s.op("scalar", lambda h, b=b: h.activation(out=t1[:].rearrange("p a b -> p (a b)")[:, 0:D], in_=xt[b][:],
                                                               func=AF.Square, accum_out=st1[:, 0:1]),
                         reads=[rx], writes=[res("t1"), res("st1")])
                    rstd_chain(st1[:, 0:1], st1[:, 1:2], D, res("st1"), res("st1b"))
                    t1f = t1[:].rearrange("p a b -> p (a b)")[:, 0:D]
                    s.op("vector", lambda h, b=b: h.scalar_tensor_tensor(out=t1f, in0=xt[b][:], scalar=st1[:, 1:2], in1=A1[:],
                                                                         op0=ALU.mult, op1=ALU.mult),
                         reads=[rx, res("st1b"), res("A1")], writes=[res("t1")])
                    s.op("gpsimd", lambda h: h.tensor_tensor(out=hb[:], in0=t1f, in1=B1[:], op=ALU.add),
                         reads=[res("t1"), res("B1")], writes=[res("hb")])
                    transpose16(hb, res("hb"), hT, res("hT"), lambda half: hT[:, half * 8:(half + 1) * 8, :])
                    chunks = range(6) if own else (2, 5)
                    for n in chunks:
                        for k in range(16):
                            s.op("tensor", lambda h, n=n, k=k: h.matmul(pp[n][:], lhsT=hT[:, k, :], rhs=win[:, k, n * 512:(n + 1) * 512],
                                                                        start=(k == 0), stop=(k == 15)),
                                 reads=[res("hT"), res("win")], writes=[r_pp[n]], signal=(k == 15))
                        eng = "scalar" if n % 2 == 0 else "vector"
                        if eng == "scalar":
                            s.op("scalar", lambda h, n=n: h.copy(out=qkall[:, n * 512:(n + 1) * 512], in_=pp[n][:]),
                                 reads=[r_pp[n]], writes=[res("qkall")])
                        else:
                            s.op("vector", lambda h, n=n: h.tensor_copy(out=qkall[:, n * 512:(n + 1) * 512], in_=pp[n][:]),
                                 reads=[r_pp[n]], writes=[res("qkall")])
                    h0 = 0 if own else 8
                    nn = 10 - h0
                    src_n = qkall[:, h0 * 128:1280].rearrange("p (a b) -> p a b", b=128)
                    s.op("scalar", lambda h, src_n=src_n, h0=h0: h.activation(out=t2[:, h0:10, :], in_=src_n, func=AF.Square),
                         reads=[res("qkall")], writes=[res("t2")])
                    s.op("vector", lambda h, h0=h0: h.tensor_reduce(out=st10[:, h0:10], in_=t2[:, h0:10, :], axis=AX.X, op=ALU.add),
                         reads=[res("t2")], writes=[res("st10")])
                    rstd_chain(st10[:, h0:10], st10[:, h0:10], 128, res("st10"), res("st10"))
                    s.op("vector", lambda h, src_n=src_n, h0=h0, nn=nn: h.tensor_tensor(
                        out=qk20[:, h0:10, :], in0=src_n, in1=st10[:, h0:10].unsqueeze(2).broadcast_to([128, nn, 128]), op=ALU.mult),
                        reads=[res("qkall"), res("st10")], writes=[res("qk20")])
                    s.op("vector", lambda h, h0=h0: h.tensor_tensor(out=qk20[:, h0:10, :], in0=qk20[:, h0:10, :], in1=g10[:, h0:10, :], op=ALU.mult),
                         reads=[res("qk20"), res("g10")], writes=[res("qk20")])
                    w0 = 10 if own else 18
                    c0 = 1536 + (w0 - 10) * 128
                    s.op("scalar", lambda h, w0=w0, c0=c0: h.copy(out=qk20[:, w0:20, :], in_=qkall[:, c0:2816].rearrange("p (a b) -> p a b", b=128)),
                         reads=[res("qkall")], writes=[res("qk20")])
                    rows = slice(i * 128, (i + 1) * 128)
                    if p == 0:
                        for (dst, src_ap) in ((ngk, qk20[:, 8:10, :].rearrange("p a b -> p (a b)")), (ngv, qkall[:, 1280:1536]),
                                              (nwk, qkall[:, 2560:2816]), (nwv, qkall[:, 2816:3072])):
                            s.dma("sync", lambda h, dst=dst, src_ap=src_ap, rows=rows: h.dma_start(out=dst[rows, :], in_=src_ap),
                                  reads=[res("qk20"), res("qkall")])
                        s.op("scalar", lambda h: h.copy(out=qkb[:], in_=qk20[:]), reads=[res("qk20")], writes=[res("qkb")])
                    else:
                        s.dma("sync", lambda h, rows=rows: h.dma_start(out=rc[:], in_=ropec[rows, :]), writes=[res("rc")])
                        s.dma("sync", lambda h, rows=rows: h.dma_start(out=rs_[:], in_=ropes[rows, :]), writes=[res("rs")])
                        groups = [(0, 20)] if own else [(8, 10), (18, 20)]
                        for (a, bnd) in groups:
                            nh = bnd - a
                            s.op("vector", lambda h, a=a, bnd=bnd, nh=nh: h.tensor_tensor(
                                out=t1[:, a:bnd, :], in0=qk20[:, a:bnd, :], in1=rc[:].unsqueeze(1).broadcast_to([128, nh, 128]), op=ALU.mult),
                                reads=[res("qk20"), res("rc")], writes=[res("t1")])
                            for pr in range(2):
                                for hf in range(2):
                                    o_ = t2[:, a:bnd, pr * 64 + hf * 32: pr * 64 + hf * 32 + 32]
                                    i_ = qk20[:, a:bnd, pr * 64 + (1 - hf) * 32: pr * 64 + (1 - hf) * 32 + 32]
                                    sn = rs_[:, pr * 64 + hf * 32: pr * 64 + hf * 32 + 32].unsqueeze(1).broadcast_to([128, nh, 32])
                                    s.op("gpsimd", lambda h, o_=o_, i_=i_, sn=sn: h.tensor_tensor(out=o_, in0=i_, in1=sn, op=ALU.mult),
                                         reads=[res("qk20"), res("rs")], writes=[res("t2")])
                            s.op("vector", lambda h, a=a, bnd=bnd: h.tensor_tensor(out=qkb[:, a:bnd, :], in0=t1[:, a:bnd, :], in1=t2[:, a:bnd, :], op=ALU.add),
                                 reads=[res("t1"), res("t2")], writes=[res("qkb")])
                    hs = [(0, 20)] if own else [(8, 10), (18, 20)]
                    for (a, bnd) in hs:
                        s.op("scalar", lambda h, a=a, bnd=bnd: h.activation(out=t1[:, a:bnd, :], in_=qkb[:, a:bnd, :], func=AF.Square),
                             reads=[res("qkb")], writes=[res("t1")])
                        s.op("vector", lambda h, a=a, bnd=bnd: h.tensor_reduce(out=st20[:, a:bnd], in_=t1[:, a:bnd, :], axis=AX.X, op=ALU.add),
                             reads=[res("t1")], writes=[res("st20")])
                    grp = [(0, 0, 8), (1, 8, 10), (2, 10, 18), (3, 18, 20)] if own else [(1, 8, 10), (3, 18, 20)]
                    for (gi_, a, bnd) in grp:
                        s.op("vector", lambda h, gi_=gi_, a=a, bnd=bnd: h.tensor_reduce(out=g4[:, gi_:gi_ + 1], in_=st20[:, a:bnd], axis=AX.X, op=ALU.max),
                             reads=[res("st20")], writes=[res("g4")])
                        s.op("vector", lambda h, gi_=gi_: h.tensor_tensor(out=mx[:, p, gi_:gi_ + 1], in0=mx[:, p, gi_:gi_ + 1], in1=g4[:, gi_:gi_ + 1], op=ALU.max),
                             reads=[res("g4"), res("mx")], writes=[res("mx")])
                    s.op("scalar", lambda h: h.copy(out=vb[:, 0:256], in_=qkall[:, 1280:1536]), reads=[res("qkall")], writes=[res("vb")])
                    s.op("scalar", lambda h: h.copy(out=vb[:, 256:512], in_=qkall[:, 2816:3072]), reads=[res("qkall")], writes=[res("vb")])
                    s.dma("sync", lambda h, rows=rows: h.dma_start(out=qsp[p][rows, :], in_=qkb[:].rearrange("p a b -> p (a b)")),
                          reads=[res("qkb")])
                    s.dma("sync", lambda h, rows=rows: h.dma_start(out=vsp[p][rows, :], in_=vb[:]),
                          reads=[res("vb")])
                s.emit()

        def attn_phase(p, OT):
            nq_t = 8
            nloc = 8 if p == 0 else 16
            nkt = 8 if p == 0 else 18
            koff = 0 if p == 0 else 2
            with ExitStack() as ph:
                def sbp(name, shape, dt=F32):
                    return ph.enter_context(nc.sbuf_tensor(f"{name}_a{p}", list(shape), dt))
                QT = [sbp("QTg", [128, 8, 1024], BF16), sbp("QTw", [128, 8, 1024], BF16)]
                KT = [sbp("KTg", [128, 2, nkt * 128], BF16), sbp("KTw", [128, 2, nkt * 128], BF16)]
                V = sbp("V", [128, nkt, 512], BF16)
                qt = [sbp(f"qt{i}", [128, 2560], BF16) for i in range(2)]
                pb = [sbp(f"pb{i}", [128, 512], BF16) for i in range(4)]
                rec = [sbp(f"rec{i}", [128, 512]) for i in range(2)]
                SE = sbp("SE", [128, 8])
                m4 = sbp("m4", [128, 4]); dg = sbp("dg", [128, 4]); mb4 = sbp("mb4", [128, 4])
                cft = sbp("cft", [128, 512]); cbt = sbp("cbt", [128, 512], BF16); st4 = sbp("st4", [128, 4])
                if p == 1:
                    for kt in range(2):
                        for which, kind in ((0, 0), (2, 1)):
                            s.dma("sync", lambda h, kt=kt, which=which: h.dma_start(out=cft[:, 0:256], in_=cache[which, kt * 128:(kt + 1) * 128, :]),
                                  writes=[res("cft")])
                            s.op("vector", lambda h: h.tensor_copy(out=cbt[:, 0:256], in_=cft[:, 0:256]), reads=[res("cft")], writes=[res("cbt")])
                            s.op("scalar", lambda h: h.activation(out=cft[:, 256:512], in_=cbt[:, 0:256], func=AF.Square),
                                 reads=[res("cbt")], writes=[res("cft2")])
                            s.op("vector", lambda h: h.tensor_reduce(out=st4[:, 0:2], in_=cft[:, 256:512].rearrange("p (a b) -> p a b", b=128), axis=AX.X, op=ALU.add),
                                 reads=[res("cft2")], writes=[res("st4")])
                            s.op("vector", lambda h: h.tensor_reduce(out=st4[:, 2:3], in_=st4[:, 0:2], axis=AX.X, op=ALU.max),
                                 reads=[res("st4")], writes=[res("st4")])
                            col = 1 if kind == 0 else 3
                            s.op("vector", lambda h, col=col: h.tensor_tensor(out=mx[:, 1, col:col + 1], in0=mx[:, 1, col:col + 1], in1=st4[:, 2:3], op=ALU.max),
                                 reads=[res("st4"), res("mx")], writes=[res("mx")])
                            for n in range(2):
                                s.op("tensor", lambda h, n=n: h.transpose(out=pt[0][:, n * 128:(n + 1) * 128], in_=cbt[:, n * 128:(n + 1) * 128], identity=identb),
                                     reads=[res("cbt"), res("cb")], writes=[r_pt[0]], signal=(n == 1))
                            s.op("vector", lambda h, kt=kt, kind=kind: h.tensor_copy(
                                out=KT[kind][:, :, kt * 128:(kt + 1) * 128], in_=pt[0][:, 0:256].rearrange("p (a b) -> p a b", b=128)),
                                reads=[r_pt[0]], writes=[res(f"KT{kind}")])
                        for which, off in ((1, 0), (3, 256)):
                            s.dma("sync", lambda h, kt=kt, which=which: h.dma_start(out=cft[:, 0:256], in_=cache[which, kt * 128:(kt + 1) * 128, :]),
                                  writes=[res("cft")])
                            s.op("vector", lambda h, kt=kt, off=off: h.tensor_copy(out=V[:, kt, off:off + 256], in_=cft[:, 0:256]),
                                 reads=[res("cft")], writes=[res("V")])
                for i in range(nloc):
                    b = i % 2
                    rows = slice(i * 128, (i + 1) * 128)
                    s.dma("sync", lambda h, b=b, rows=rows: h.dma_start(out=qt[b][:], in_=qsp[p][rows, :]),
                          writes=[res(f"qt{b}")])
                    s.dma("sync", lambda h, i=i, rows=rows: h.dma_start(out=V[:, koff + i, :], in_=vsp[p][rows, :]),
                          writes=[res("V")])
                    if i < 8:
                        for kind in range(2):
                            c0 = 0 if kind == 0 else 1280
                            for hh in range(8):
                                s.op("tensor", lambda h, b=b, hh=hh, c0=c0, kind=kind: h.transpose(
                                    out=pt[kind][:, hh * 128:(hh + 1) * 128], in_=qt[b][:, c0 + hh * 128:c0 + (hh + 1) * 128], identity=identb),
                                    reads=[res(f"qt{b}"), res("cb")], writes=[r_pt[kind]], signal=(hh == 7))
                            o_ = QT[kind][:, :, i * 128:(i + 1) * 128]
                            i_ = pt[kind][:, :].rearrange("p (a b) -> p a b", b=128)
                            if kind == 0:
                                s.op("scalar", lambda h, o_=o_, i_=i_: h.copy(out=o_, in_=i_), reads=[r_pt[kind]], writes=[res(f"QT{kind}")])
                            else:
                                s.op("vector", lambda h, o_=o_, i_=i_: h.tensor_copy(out=o_, in_=i_), reads=[r_pt[kind]], writes=[res(f"QT{kind}")])
                    for kind in range(2):
                        c0 = 1024 if kind == 0 else 2304
                        for n in range(2):
                            s.op("tensor", lambda h, b=b, n=n, c0=c0, kind=kind: h.transpose(
                                out=pt[kind][:, n * 128:(n + 1) * 128], in_=qt[b][:, c0 + n * 128:c0 + (n + 1) * 128], identity=identb),
                                reads=[res(f"qt{b}"), res("cb")], writes=[r_pt[kind]], signal=(n == 1))
                        kk = koff + i
                        s.op("vector", lambda h, kk=kk, kind=kind: h.tensor_copy(
                            out=KT[kind][:, :, kk * 128:(kk + 1) * 128], in_=pt[kind][:, 0:256].rearrange("p (a b) -> p a b", b=128)),
                            reads=[r_pt[kind]], writes=[res(f"KT{kind}")])
                s.op("tensor", lambda h: h.transpose(out=pp[0][0:4, 0:128], in_=mx[:, p, :], identity=identf),
                     reads=[res("mx"), res("cf")], writes=[r_pp[0]])
                s.op("vector", lambda h: h.tensor_reduce(out=m4[0:4, 0:1], in_=pp[0][0:4, 0:128], axis=AX.X, op=ALU.max),
                     reads=[r_pp[0]], writes=[res("m4")])
                s.op("vector", lambda h: h.tensor_scalar(out=dg[0:4, 0:4], in0=identf[0:4, 0:4], scalar1=m4[0:4, 0:1], scalar2=None, op0=ALU.mult),
                     reads=[res("m4"), res("cf")], writes=[res("dg")])
                s.op("tensor", lambda h: h.matmul(pp[1][:, 0:4], lhsT=onesf[0:4, 0:128], rhs=dg[0:4, 0:4], start=True, stop=True),
                     reads=[res("dg"), res("cf")], writes=[r_pp[1]])
                s.op("vector", lambda h: h.tensor_copy(out=mb4[:], in_=pp[1][:, 0:4]), reads=[r_pp[1]], writes=[res("mb4")])
                for kind in range(2):
                    s.op("vector", lambda h, kind=kind: h.tensor_tensor(out=negm[:, p, kind:kind + 1], in0=mb4[:, 2 * kind:2 * kind + 1],
                                                                        in1=mb4[:, 2 * kind + 1:2 * kind + 2], op=ALU.mult),
                         reads=[res("mb4")], writes=[res("negm")])
                s.op("scalar", lambda h: h.activation(out=negm[:, p, :], in_=negm[:, p, :], func=AF.Sqrt), reads=[res("negm")], writes=[res("negm")])
                s.op("vector", lambda h: h.tensor_scalar(out=negm[:, p, :], in0=negm[:, p, :], scalar1=-SCALE, scalar2=None, op0=ALU.mult),
                     reads=[res("negm")], writes=[res("negm")])
                s.op("scalar", lambda h: h.activation(out=SE[:], in_=sinkb[:], func=AF.Exp, bias=negm[:, p, 1:2], scale=1.0),
                     reads=[res("negm"), res("sinkb")], writes=[res("SE")])

                jobs = []
                if p == 0:
                    for sq in range(4):
                        for kind in range(2):
                            for n in range(2):
                                for qc in range(2):
                                    h0 = 4 * n + 2 * qc
                                    q_ap = QT[kind][:, h0:h0 + 2, sq * 256:(sq + 1) * 256]
                                    keys = [(sq * 2 + kt, None) for kt in range(2)]
                                    o_ap = OT[:, kind * 8 + h0:kind * 8 + h0 + 2, sq * 256:(sq + 1) * 256]
                                    jobs.append((kind, n, q_ap, keys, o_ap, (h0, 2, 256)))
                else:
                    for n in range(2):
                        for qb in range(8):
                            q_ap = QT[0][:, 4 * n:4 * n + 4, qb * 128:(qb + 1) * 128]
                            o_ap = OT[:, 4 * n:4 * n + 4, qb * 128:(qb + 1) * 128]
                            jobs.append((0, n, q_ap, [(kt, None) for kt in range(18)], o_ap, (4 * n, 4, 128)))
                    for n in range(2):
                        for qb in range(8):
                            q_ap = QT[1][:, 4 * n:4 * n + 4, qb * 128:(qb + 1) * 128]
                            o_ap = OT[:, 8 + 4 * n:8 + 4 * n + 4, qb * 128:(qb + 1) * 128]
                            prev = (2 + qb - 1, 384) if qb > 0 else (2 + 8, 640)
                            nxt = (2 + qb + 1, 512) if qb < 7 else (2 + 8, 768)
                            keys = [(0, None), (1, None), prev, (2 + qb, None), nxt]
                            jobs.append((1, n, q_ap, keys, o_ap, (4 * n, 4, 128)))
                npb = 0
                for ji, (kind, n, q_ap, keys, o_ap, (h0, nh, nqq)) in enumerate(jobs):
                    po, psm = 2 + (ji % 2), 4 + (ji % 2)
                    for ki, (kt, moff) in enumerate(keys):
                        sbank = ki % 2
                        s.op("tensor", lambda h, sbank=sbank, kind=kind, n=n, kt=kt, q_ap=q_ap: h.matmul(
                            pp[sbank][:], lhsT=KT[kind][:, n, kt * 128:(kt + 1) * 128], rhs=q_ap, start=True, stop=True),
                            reads=[res(f"KT{kind}"), res(f"QT{kind}")], writes=[r_pp[sbank]])
                        pi = npb % 4
                        npb += 1
                        s.op("scalar", lambda h, pi=pi, sbank=sbank, kind=kind: h.activation(
                            out=pb[pi][:], in_=pp[sbank][:], func=AF.Exp, bias=negm[:, p, kind:kind + 1], scale=SCALE),
                            reads=[r_pp[sbank], res("negm")], writes=[res(f"pb{pi}")])
                        if moff is not None:
                            mk = cb[:, moff:moff + 128].unsqueeze(1).broadcast_to([128, 4, 128])
                            s.op("gpsimd", lambda h, pi=pi, mk=mk: h.tensor_tensor(
                                out=pb[pi][:].rearrange("p (a b) -> p a b", b=128), in0=pb[pi][:].rearrange("p (a b) -> p a b", b=128), in1=mk, op=ALU.mult),
                                reads=[res(f"pb{pi}"), res("cb")], writes=[res(f"pb{pi}")])
                        vs = V[:, kt, kind * 256 + n * 128: kind * 256 + (n + 1) * 128]
                        last = ki == len(keys) - 1
                        s.op("tensor", lambda h, po=po, vs=vs, pi=pi, ki=ki, last=last: h.matmul(
                            pp[po][:], lhsT=vs, rhs=pb[pi][:], start=(ki == 0), stop=last),
                            reads=[res("V"), res(f"pb{pi}")], writes=[r_pp[po]], signal=last)
                        s.op("tensor", lambda h, psm=psm, pi=pi, ki=ki, last=last: h.matmul(
                            pp[psm][:], lhsT=onesb, rhs=pb[pi][:], start=(ki == 0), stop=last),
                            reads=[res("cb"), res(f"pb{pi}")], writes=[r_pp[psm]], signal=True)
                    rb = ji % 2
                    if kind == 1:
                        se = SE[:, h0:h0 + nh].unsqueeze(2).broadcast_to([128, nh, nqq])
                        s.op("vector", lambda h, rb=rb, psm=psm, se=se, nqq=nqq: h.tensor_tensor(
                            out=rec[rb][:].rearrange("p (a b) -> p a b", b=nqq), in0=pp[psm][:].rearrange("p (a b) -> p a b", b=nqq), in1=se, op=ALU.add),
                            reads=[r_pp[psm], res("SE")], writes=[res(f"rec{rb}")])
                        s.op("vector", lambda h, rb=rb: h.reciprocal(out=rec[rb][:], in_=rec[rb][:]), reads=[res(f"rec{rb}")], writes=[res(f"rec{rb}")])
                    else:
                        s.op("vector", lambda h, rb=rb, psm=psm: h.reciprocal(out=rec[rb][:], in_=pp[psm][:]), reads=[r_pp[psm]], writes=[res(f"rec{rb}")])
                    s.op("vector", lambda h, rb=rb, po=po, o_ap=o_ap, nqq=nqq: h.tensor_tensor(
                        out=o_ap, in0=pp[po][:].rearrange("p (a b) -> p a b", b=nqq), in1=rec[rb][:].rearrange("p (a b) -> p a b", b=nqq), op=ALU.mult),
                        reads=[r_pp[po], res(f"rec{rb}")], writes=[res("OT")])
                s.emit()

        def post_phase(p, OT):
            with ExitStack() as ph:
                def sbp(name, shape, dt=F32):
                    return ph.enter_context(nc.sbuf_tensor(f"{name}_p{p}", list(shape), dt))
                wo = sbp("wo", [128, 16, D], BF16)
                wr = sbp("wr", [128, 16, NE], BF16)
                G1 = sbp("G1", [128, D]); A2 = sbp("A2", [128, D]); B2 = sbp("B2", [128, D])
                xt = sbp("xt", [128, D]); yt = sbp("yt", [128, D]); tf = sbp("tf", [128, D])
                h2b = sbp("h2b", [128, D], BF16); h2T = sbp("h2T", [128, 16, 128], BF16)
                st = sbp("st", [128, 8])
                sc = sbp("sc", [128, NE]); sel = sbp("sel", [128, NE]); srt = sbp("srt", [128, 8, 8]); gs = sbp("gs", [128, 8])
                gs8 = sbp("gs8", [128, 8]); gm = sbp("gm", [128, 8]); gneg = sbp("gneg", [128, 8]); selm = sbp("selm", [128, NE])
                top8 = sbp("top8", [128, 8]); Mf = sbp("Mf", [128, NE]); wsel = sbp("wsel", [128, NE]); den = sbp("den", [128, 2])
                posf = sbp("posf", [128, NE]); vv = sbp("vv", [128, NE]); d8 = sbp("d8", [128, 8]); oh = sbp("oh", [128, NE])
                g8 = sbp("g8", [128, 8]); eoff = sbp("eoff", [128, NE])
                for n in range(4):
                    s.dma("gpsimd", lambda h, n=n: h.dma_start(out=wo[:, :, n * 512:(n + 1) * 512],
                                                              in_=w_out[:, n * 512:(n + 1) * 512].rearrange("(c p) n -> p c n", p=128)), writes=[res("wo")])
                s.dma("gpsimd", lambda h: h.dma_start(out=wr[:], in_=w_router.rearrange("(c p) n -> p c n", p=128)), writes=[res("wr")])
                s.dma("sync", lambda h: h.dma_start(out=G1[:], in_=modsp[4 + p]), writes=[res("G1")])
                s.dma("sync", lambda h: h.dma_start(out=A2[:], in_=modsp[8 + p]), writes=[res("A2")])
                s.dma("sync", lambda h: h.dma_start(out=B2[:], in_=modsp[6 + p]), writes=[res("B2")])
                s.op("gpsimd", lambda h: h.iota(eoff[:], pattern=[[CAP, NE]], base=1, channel_multiplier=0, allow_small_or_imprecise_dtypes=True),
                     writes=[res("eoff")])
                for i in range(8):
                    gi = p * 8 + i
                    rows = slice(i * 128, (i + 1) * 128)
                    grow = slice(gi * 128, (gi + 1) * 128)
                    for n in range(4):
                        for mh in range(16):
                            s.op("tensor", lambda h, n=n, mh=mh, i=i: h.matmul(pp[n][:], lhsT=OT[:, mh, i * 128:(i + 1) * 128], rhs=wo[:, mh, n * 512:(n + 1) * 512],
                                                                              start=(mh == 0), stop=(mh == 15)),
                                 reads=[res("OT"), res("wo")], writes=[r_pp[n]], signal=(mh == 15))
                        s.op("scalar", lambda h, n=n: h.activation(out=tf[:, n * 512:(n + 1) * 512], in_=pp[n][:], func=AF.Square, accum_out=st[:, n:n + 1]),
                             reads=[r_pp[n]], writes=[res("tf"), res("st")])
                    s.op("vector", lambda h: h.tensor_reduce(out=st[:, 4:5], in_=st[:, 0:4], axis=AX.X, op=ALU.add), reads=[res("st")], writes=[res("st")])
                    rstd_chain(st[:, 4:5], st[:, 5:6], D, res("st"), res("st"))
                    s.dma("sync", lambda h, rows=rows: h.dma_start(out=xt[:], in_=xin[p][rows, :]), writes=[res("xt")])
                    for n in range(4):
                        cs = slice(n * 512, (n + 1) * 512)
                        s.op("vector", lambda h, n=n, cs=cs: h.scalar_tensor_tensor(out=tf[:, cs], in0=pp[n][:], scalar=st[:, 5:6], in1=G1[:, cs],
                                                                                    op0=ALU.mult, op1=ALU.mult),
                             reads=[r_pp[n], res("st"), res("G1")], writes=[res("tf")])
                    s.op("gpsimd", lambda h: h.tensor_tensor(out=yt[:], in0=tf[:], in1=xt[:], op=ALU.add), reads=[res("tf"), res("xt")], writes=[res("yt")])
                    s.dma("sync", lambda h, grow=grow: h.dma_start(out=ysp[grow, :], in_=yt[:]), reads=[res("yt")])
                    s.op("scalar", lambda h: h.activation(out=tf[:], in_=yt[:], func=AF.Square, accum_out=st[:, 6:7]),
                         reads=[res("yt")], writes=[res("tf"), res("st")])
                    rstd_chain(st[:, 6:7], st[:, 7:8], D, res("st"), res("st"))
                    s.op("vector", lambda h: h.scalar_tensor_tensor(out=tf[:], in0=yt[:], scalar=st[:, 7:8], in1=A2[:], op0=ALU.mult, op1=ALU.mult),
                         reads=[res("yt"), res("st"), res("A2")], writes=[res("tf")])
                    s.op("gpsimd", lambda h: h.tensor_tensor(out=h2b[:], in0=tf[:], in1=B2[:], op=ALU.add), reads=[res("tf"), res("B2")], writes=[res("h2b")])
                    s.dma("sync", lambda h, gi=gi: h.dma_start(out=xbuf_s[gi * 128:(gi + 1) * 128, :], in_=h2b[:]),
                          reads=[res("h2b")])
                    transpose16(h2b, res("h2b"), h2T, res("h2T"), lambda half: h2T[:, half * 8:(half + 1) * 8, :])
                    for k in range(16):
                        s.op("tensor", lambda h, k=k: h.matmul(pp[4][:, 0:NE], lhsT=h2T[:, k, :], rhs=wr[:, k, :], start=(k == 0), stop=(k == 15)),
                             reads=[res("h2T"), res("wr")], writes=[r_pp[4]], signal=(k == 15))
                    s.op("scalar", lambda h: h.activation(out=sc[:], in_=pp[4][:, 0:NE], func=AF.Sigmoid), reads=[r_pp[4]], writes=[res("sc")])
                    s.op("vector", lambda h: h.tensor_tensor(out=sel[:], in0=sc[:], in1=rbb[:], op=ALU.add), reads=[res("sc"), res("rbb")], writes=[res("sel")])
                    for g in range(8):
                        s.op("vector", lambda h, g=g: h.max(out=srt[:, g, :], in_=sel[:, g * 8:(g + 1) * 8]), reads=[res("sel")], writes=[res("srt")])
                    s.op("vector", lambda h: h.tensor_tensor(out=gs[:], in0=srt[:, :, 0], in1=srt[:, :, 1], op=ALU.add), reads=[res("srt")], writes=[res("gs")])
                    s.op("vector", lambda h: h.max(out=gs8[:], in_=gs[:]), reads=[res("gs")], writes=[res("gs8")])
                    s.op("vector", lambda h: h.tensor_scalar(out=gm[:], in0=gs[:], scalar1=gs8[:, 3:4], scalar2=None, op0=ALU.is_ge),
                         reads=[res("gs"), res("gs8")], writes=[res("gm")])
                    s.op("vector", lambda h: h.tensor_scalar(out=gneg[:], in0=gm[:], scalar1=-1.0, scalar2=1e9, op0=ALU.add, op1=ALU.mult),
                         reads=[res("gm")], writes=[res("gneg")])
                    for g in range(8):
                        s.op("vector", lambda h, g=g: h.tensor_scalar(out=selm[:, g * 8:(g + 1) * 8], in0=sel[:, g * 8:(g + 1) * 8],
                                                                      scalar1=gm[:, g:g + 1], scalar2=gneg[:, g:g + 1], op0=ALU.mult, op1=ALU.add),
                             reads=[res("sel"), res("gm"), res("gneg")], writes=[res("selm")])
                    s.op("vector", lambda h: h.max(out=top8[:], in_=selm[:]), reads=[res("selm")], writes=[res("top8")])
                    s.op("vector", lambda h: h.tensor_scalar(out=Mf[:], in0=selm[:], scalar1=top8[:, 7:8], scalar2=None, op0=ALU.is_ge),
                         reads=[res("selm"), res("top8")], writes=[res("Mf")])
                    s.op("vector", lambda h, gi=gi: h.tensor_copy(out=Mall[:, gi, :], in_=Mf[:]), reads=[res("Mf")], writes=[res("Mall")])
                    s.op("vector", lambda h: h.tensor_tensor(out=wsel[:], in0=sc[:], in1=Mf[:], op=ALU.mult), reads=[res("sc"), res("Mf")], writes=[res("wsel")])
                    s.op("vector", lambda h: h.tensor_reduce(out=den[:, 0:1], in_=wsel[:], axis=AX.X, op=ALU.add), reads=[res("wsel")], writes=[res("den")])
                    s.op("vector", lambda h: h.reciprocal(out=den[:, 1:2], in_=den[:, 0:1]), reads=[res("den")], writes=[res("den")])
                    s.op("vector", lambda h: h.tensor_scalar(out=wsel[:], in0=wsel[:], scalar1=den[:, 1:2], scalar2=2.5, op0=ALU.mult, op1=ALU.mult),
                         reads=[res("wsel"), res("den")], writes=[res("wsel")])
                    s.op("tensor", lambda h, gi=gi: h.matmul(pp[5][:, 0:NE], lhsT=ustrb, rhs=Mall[:, gi, :], start=True, stop=(gi == 0)),
                         reads=[res("Mall"), res("cb")], writes=[r_pp[5]], signal=(gi == 0))
                    for j in range(gi):
                        s.op("tensor", lambda h, j=j, gi=gi: h.matmul(pp[5][:, 0:NE], lhsT=onesb, rhs=Mall[:, j, :], start=False, stop=(j == gi - 1)),
                             reads=[res("Mall"), res("cb")], writes=[r_pp[5]], signal=(j == gi - 1))
                    s.op("vector", lambda h: h.tensor_scalar(out=posf[:], in0=pp[5][:, 0:NE], scalar1=float(CAP - 1), scalar2=None, op0=ALU.min),
                         reads=[r_pp[5]], writes=[res("posf")])
                    s.op("vector", lambda h: h.tensor_tensor(out=vv[:], in0=posf[:], in1=eoff[:], op=ALU.add), reads=[res("posf"), res("eoff")], writes=[res("vv")])
                    s.op("vector", lambda h: h.tensor_tensor(out=vv[:], in0=vv[:], in1=Mf[:], op=ALU.mult), reads=[res("vv"), res("Mf")], writes=[res("vv")])
                    s.op("vector", lambda h: h.max(out=d8[:], in_=vv[:]), reads=[res("vv")], writes=[res("d8")])
                    for k in range(8):
                        s.op("vector", lambda h, k=k: h.tensor_scalar(out=oh[:], in0=vv[:], scalar1=d8[:, k:k + 1], scalar2=None, op0=ALU.is_equal),
                             reads=[res("vv"), res("d8")], writes=[res("oh")])
                        s.op("vector", lambda h, k=k: h.tensor_tensor(out=oh[:], in0=oh[:], in1=wsel[:], op=ALU.mult), reads=[res("oh"), res("wsel")], writes=[res("oh")])
                        s.op("vector", lambda h, k=k: h.tensor_reduce(out=g8[:, k:k + 1], in_=oh[:], axis=AX.X, op=ALU.add), reads=[res("oh")], writes=[res("g8")])
                    s.op("vector", lambda h: h.tensor_scalar(out=d8[:], in0=d8[:], scalar1=-1.0, scalar2=None, op0=ALU.add), reads=[res("d8")], writes=[res("d8")])
                    s.op("vector", lambda h, gi=gi: h.tensor_copy(out=dstall[:, gi, :], in_=d8[:]), reads=[res("d8")], writes=[res("dstall")])
                    s.op("vector", lambda h, gi=gi: h.tensor_copy(out=gall[:, gi, :], in_=g8[:]), reads=[res("g8")], writes=[res("gall")])
                    if DEBUG:
                        s.dma("sync", lambda h, gi=gi: h.dma_start(out=dbg_sc[gi], in_=sc[:]), reads=[res("sc")])
                    for k in range(8):
                        s.dma("gpsimd", lambda h, k=k, gi=gi: h.indirect_dma_start(
                            out=xbuf[:, :], out_offset=bass.IndirectOffsetOnAxis(ap=dstall[:, gi, k:k + 1], axis=0),
                            in_=h2b[:], in_offset=None, bounds_check=None),
                            reads=[res("h2b"), res("dstall")])
                s.emit()

        def expert_phase():
            with ExitStack() as ph:
                def sbp(name, shape, dt=F32):
                    return ph.enter_context(nc.sbuf_tensor(f"{name}_e", list(shape), dt))
                wg = [sbp(f"wg{i}", [128, 16, 512], BF16) for i in range(2)]
                wu = [sbp(f"wu{i}", [128, 16, 512], BF16) for i in range(2)]
                wd = [sbp(f"wd{i}", [128, 4, D], BF16) for i in range(2)]
                xg = [sbp(f"xg{i}", [128, D], BF16) for i in range(2)]
                xT = sbp("xT", [128, 16, 512], BF16)
                sg = [sbp(f"sg{i}", [128, 512]) for i in range(2)]
                act = sbp("act", [128, 4, 512], BF16)
                ob = [sbp(f"ob{i}", [128, D], BF16) for i in range(2)]
                passes = [(e, e * CAP + q * 512) for e in range(NE) for q in range(CAP // 512)] + [(NE, q * 512) for q in range(4)]
                loaded = -1
                nx = 0
                nob = 0
                for (e, r0) in passes:
                    b = e % 2
                    if e != loaded:
                        loaded = e
                        gsrc = wge[e] if e < NE else wgs
                        usrc = wue[e] if e < NE else wus
                        dsrc = wde[e] if e < NE else wds
                        s.dma("gpsimd", lambda h, b=b, gsrc=gsrc: h.dma_start(out=wg[b][:], in_=gsrc.rearrange("(c p) n -> p c n", p=128)), writes=[res(f"wg{b}")])
                        s.dma("gpsimd", lambda h, b=b, usrc=usrc: h.dma_start(out=wu[b][:], in_=usrc.rearrange("(c p) n -> p c n", p=128)), writes=[res(f"wu{b}")])
                        s.dma("gpsimd", lambda h, b=b, dsrc=dsrc: h.dma_start(out=wd[b][:], in_=dsrc.rearrange("(c p) n -> p c n", p=128)), writes=[res(f"wd{b}")])
                    for sbk in range(4):
                        xb_ = nx % 2
                        nx += 1
                        s.dma("sync", lambda h, xb_=xb_, r0=r0, sbk=sbk, e=e: h.dma_start(out=xg[xb_][:], in_=(xbuf if e < NE else xbuf_s)[r0 + sbk * 128:r0 + (sbk + 1) * 128, :]),
                              writes=[res(f"xg{xb_}")])
                        transpose16(xg[xb_], res(f"xg{xb_}"), xT, res("xT"),
                                    lambda half, sbk=sbk: xT[:, half * 8:(half + 1) * 8, sbk * 128:(sbk + 1) * 128])
                    for fc in range(4):
                        gb, ub = fc % 2, 2 + fc % 2
                        for k in range(16):
                            s.op("tensor", lambda h, gb=gb, b=b, k=k, fc=fc: h.matmul(pp[gb][:], lhsT=wg[b][:, k, fc * 128:(fc + 1) * 128], rhs=xT[:, k, :],
                                                                                      start=(k == 0), stop=(k == 15)),
                                 reads=[res(f"wg{b}"), res("xT")], writes=[r_pp[gb]], signal=(k == 15))
                        for k in range(16):
                            s.op("tensor", lambda h, ub=ub, b=b, k=k, fc=fc: h.matmul(pp[ub][:], lhsT=wu[b][:, k, fc * 128:(fc + 1) * 128], rhs=xT[:, k, :],
                                                                                      start=(k == 0), stop=(k == 15)),
                                 reads=[res(f"wu{b}"), res("xT")], writes=[r_pp[ub]], signal=(k == 15))
                        s.op("scalar", lambda h, gb=gb, fc=fc: h.activation(out=sg[fc % 2][:], in_=pp[gb][:], func=AF.Silu),
                             reads=[r_pp[gb]], writes=[res(f"sg{fc % 2}")])
                        s.op("vector", lambda h, ub=ub, fc=fc: h.tensor_tensor(out=act[:, fc, :], in0=pp[ub][:], in1=sg[fc % 2][:], op=ALU.mult),
                             reads=[r_pp[ub], res(f"sg{fc % 2}")], writes=[res("act")])
                    for sbk in range(4):
                        ob_ = nob % 2
                        nob += 1
                        for n in range(4):
                            bank = 4 + n % 2
                            for fc in range(4):
                                s.op("tensor", lambda h, bank=bank, fc=fc, sbk=sbk, n=n, b=b: h.matmul(
                                    pp[bank][:], lhsT=act[:, fc, sbk * 128:(sbk + 1) * 128], rhs=wd[b][:, fc, n * 512:(n + 1) * 512],
                                    start=(fc == 0), stop=(fc == 3)),
                                    reads=[res("act"), res(f"wd{b}")], writes=[r_pp[bank]], signal=(fc == 3))
                            if n % 2 == 0:
                                s.op("scalar", lambda h, bank=bank, ob_=ob_, n=n, sbk=sbk: h.copy(
                                    out=ob[ob_][:, n * 512:(n + 1) * 512], in_=pp[bank][:]),
                                    reads=[r_pp[bank]], writes=[res(f"ob{ob_}")])
                            else:
                                s.op("vector", lambda h, bank=bank, ob_=ob_, n=n, sbk=sbk: h.tensor_copy(
                                    out=ob[ob_][:, n * 512:(n + 1) * 512], in_=pp[bank][:]),
                                    reads=[r_pp[bank]], writes=[res(f"ob{ob_}")])
                        s.dma("sync", lambda h, ob_=ob_, r0=r0, sbk=sbk, e=e: h.dma_start(out=(obuf if e < NE else obuf_s)[r0 + sbk * 128:r0 + (sbk + 1) * 128, :], in_=ob[ob_][:]),
                              reads=[res(f"ob{ob_}")])
                s.emit()

        def combine_phase():
            with ExitStack() as ph:
                def sbp(name, shape, dt=F32):
                    return ph.enter_context(nc.sbuf_tensor(f"{name}_c", list(shape), dt))
                gk = [sbp(f"gk{i}", [128, D], BF16) for i in range(9)]
                accA = sbp("accA", [128, D]); accB = sbp("accB", [128, D])
                yt = sbp("yt", [128, D]); G2 = [sbp("G2a", [128, D]), sbp("G2b", [128, D])]
                st = sbp("st", [128, 4])
                for k in range(9):
                    s.op("gpsimd", lambda h, k=k: h.memset(gk[k][:], 0.0), writes=[res(f"gk{k}")])
                for p in range(2):
                    s.dma("sync", lambda h, p=p: h.dma_start(out=G2[p][:], in_=modsp[10 + p]), writes=[res(f"G2{p}")])
                for gi in range(16):
                    p, i = gi // 8, gi % 8
                    for k in range(8):
                        s.dma("gpsimd", lambda h, k=k, gi=gi: h.indirect_dma_start(
                            out=gk[k][:], out_offset=None, in_=obuf[:, :],
                            in_offset=bass.IndirectOffsetOnAxis(ap=dstall[:, gi, k:k + 1], axis=0),
                            bounds_check=None),
                            reads=[res("dstall")], writes=[res(f"gk{k}")])
                    s.dma("sync", lambda h, gi=gi: h.dma_start(out=gk[8][:], in_=obuf_s[gi * 128:(gi + 1) * 128, :]),
                          writes=[res("gk8")])
                    s.dma("sync", lambda h, gi=gi: h.dma_start(out=yt[:], in_=ysp[gi * 128:(gi + 1) * 128, :]), writes=[res("ytc")])
                    s.op("vector", lambda h, gi=gi: h.scalar_tensor_tensor(out=accA[:], in0=gk[0][:], scalar=gall[:, gi, 0:1], in1=gk[8][:], op0=ALU.mult, op1=ALU.add),
                         reads=[res("gk8"), res("gk0"), res("gall")], writes=[res("accA")])
                    for k in range(1, 8):
                        s.op("vector", lambda h, k=k, gi=gi: h.scalar_tensor_tensor(out=accA[:], in0=gk[k][:], scalar=gall[:, gi, k:k + 1], in1=accA[:], op0=ALU.mult, op1=ALU.add),
                             reads=[res("accA"), res(f"gk{k}"), res("gall")], writes=[res("accA")])
                    if DEBUG:
                        s.dma("sync", lambda h, gi=gi: h.dma_start(out=dbg_moe[gi * 128:(gi + 1) * 128, :], in_=accA[:]), reads=[res("accA")])
                    s.op("scalar", lambda h: h.activation(out=accB[:], in_=accA[:], func=AF.Square, accum_out=st[:, 0:1]),
                         reads=[res("accA")], writes=[res("accB"), res("stc")])
                    rstd_chain(st[:, 0:1], st[:, 1:2], D, res("stc"), res("stc"))
                    s.op("vector", lambda h, p=p: h.scalar_tensor_tensor(out=accB[:], in0=accA[:], scalar=st[:, 1:2], in1=G2[p][:], op0=ALU.mult, op1=ALU.mult),
                         reads=[res("accA"), res("stc"), res(f"G2{p}")], writes=[res("accB")])
                    s.op("gpsimd", lambda h: h.tensor_tensor(out=accB[:], in0=accB[:], in1=yt[:], op=ALU.add), reads=[res("accB"), res("ytc")], writes=[res("accB")])
                    s.dma("sync", lambda h, p=p, i=i: h.dma_start(out=youts[p][i * 128:(i + 1) * 128, :], in_=accB[:]),
                          reads=[res("accB")])
                s.emit()

        if DEBUG:
            dbg_dst = dscr("dbg_dst", [128, 16 * 8], I32)
            dbg_gate = dscr("dbg_gate", [128, 16 * 8])
            dbg_moe = dscr("dbg_moe", [2048, D])
            dbg_sc = dscr("dbg_sc", [16, 128, NE])
        for p in range(2):
            if STAGE >= 1:
                qkv_phase(p)
            if STAGE >= 2:
                with nc.sbuf_tensor(f"OT{p}", [128, 16, 1024], BF16) as OT:
                    attn_phase(p, OT)
                    if STAGE >= 3:
                        post_phase(p, OT)
        if STAGE >= 8:
            expert_phase()
            combine_phase()
        if DEBUG and STAGE >= 3:
            s.dma("sync", lambda h: h.dma_start(out=dbg_dst[:, :], in_=dstall[:].rearrange("p a b -> p (a b)")), reads=[res("dstall")])
            s.dma("sync", lambda h: h.dma_start(out=dbg_gate[:, :], in_=gall[:].rearrange("p a b -> p (a b)")), reads=[res("gall")])
        s.final_wait("sync", list(R.values()))
        s.emit()
    return nc


def _rope_tables():
    n = 2048
    rows = n // 64
    row = np.repeat(np.arange(rows, dtype=np.float32), 64)
    col = np.tile(np.arange(64, dtype=np.float32), rows)
    inv_freq = (10000.0 ** (-np.arange(0, 64, 2, dtype=np.float32) / 64)).astype(np.float32)
    ang_r = row[:, None] * inv_freq
    ang_c = col[:, None] * inv_freq
    ang = np.concatenate([ang_r, ang_r, ang_c, ang_c], axis=-1).astype(np.float32)
    cos = np.cos(ang).astype(np.float32)
    sin = np.sin(ang).astype(np.float32)
    sgn = np.ones(128, np.float32)
    sgn[0:32] = -1.0
    sgn[64:96] = -1.0
    return cos, sin * sgn[None, :]


def _consts(half):
    c = np.zeros((128, 7 * 128), np.float32)
    j = np.arange(128)[:, None]
    r = np.arange(128)[None, :]
    c[:, 0:128] = np.eye(128, dtype=np.float32)
    c[:, 128:256] = (j < r).astype(np.float32)
    c[:, 256:384] = 1.0
    band_prev = (j >= r).astype(np.float32)
    band_next = (j <= r).astype(np.float32)
    c[:, 384:512] = band_prev
    c[:, 512:640] = band_next
    c[:, 640:768] = band_prev if half == 1 else 0.0
    c[:, 768:896] = band_next if half == 0 else 0.0
    return c


def _local_order(half):
    own = np.arange(half * 1024, (half + 1) * 1024)
    if half == 0:
        other = np.arange(1024, 2048)
    else:
        other = np.concatenate([np.arange(896, 1024), np.arange(0, 896)])
    return np.concatenate([own, other])


_NC_CACHE = {}


def kernel(x_prompt, x_sample, cache_glob_k, cache_glob_v, cache_win_k, cache_win_v, c, c_ctx,
           w_ada, b_ada, attn_pre_g, attn_post_g, w_in, q_norm_g, k_norm_g, sink_logit, w_out,
           ffn_pre_g, ffn_post_g, w_router, router_bias, w_gate_e, w_up_e, w_down_e,
           w_gate_s, w_up_s, w_down_s):
    f = lambda a: np.ascontiguousarray(np.asarray(a, dtype=np.float32))
    x_prompt, x_sample = f(x_prompt), f(x_sample)
    cos, sins = _rope_tables()
    shared = {
        "w_ada": f(w_ada)[0], "b_ada": f(b_ada)[0][None, :],
        "gains": np.stack([f(attn_pre_g)[0], f(attn_post_g)[0], f(ffn_pre_g)[0], f(ffn_post_g)[0]]),
        "w_in": f(w_in)[0], "qkg": np.stack([f(q_norm_g)[0], f(k_norm_g)[0]]),
        "sink": f(sink_logit)[0][None, :], "w_out": f(w_out)[0], "w_router": f(w_router)[0],
        "rbias": f(router_bias)[0][None, :], "wge": f(w_gate_e)[0], "wue": f(w_up_e)[0], "wde": f(w_down_e)[0],
        "wgs": f(w_gate_s)[0], "wus": f(w_up_s)[0], "wds": f(w_down_s)[0],
    }
    caches = [f(cache_glob_k), f(cache_glob_v), f(cache_win_k), f(cache_win_v)]
    in_maps = []
    for core in range(NCORES):
        b, half = core // 2, core % 2
        order = _local_order(half)
        m = dict(shared)
        m["xc"] = x_prompt[4 * core:4 * core + 4].reshape(1024, D)
        m["xl"] = np.ascontiguousarray(x_sample[b][order])
        m["ropec"] = np.ascontiguousarray(cos[order])
        m["ropes"] = np.ascontiguousarray(sins[order])
        m["cache"] = np.stack([cc[b, 0].reshape(256, 256) for cc in caches])
        m["cond"] = np.stack([f(c_ctx), f(c)[b]])
        m["consts"] = _consts(half)
        in_maps.append(m)
    if "nc" not in _NC_CACHE:
        del INPUT_NAMES[:]
        _NC_CACHE["nc"] = build()
    nc = _NC_CACHE["nc"]
    in_maps = [{k: v for k, v in m.items() if k in INPUT_NAMES} for m in in_maps]
    r = run_bass_kernel_spmd(nc, in_maps, core_ids=list(range(NCORES))).results
    if DEBUG:
        _NC_CACHE["raw"] = r
    y_p = np.concatenate([r[i]["yc"].reshape(4, 256, D) for i in range(NCORES)], axis=0)
    y_s = np.stack([np.concatenate([r[2 * b]["yl"], r[2 * b + 1]["yl"]], axis=0) for b in range(4)])
    def kv(name):
        return np.concatenate([r[i][name].reshape(4, 1, 256, 2, 128) for i in range(NCORES)], axis=0)
    return (y_p.astype(np.float32), y_s.astype(np.float32), kv("ngk"), kv("ngv"), kv("nwk"), kv("nwv"))
```

```python
import numpy as np
from contextlib import ExitStack
import concourse.bass as bass
import concourse.mybir as mybir
from concourse.bass_utils import run_bass_kernel_spmd

F32 = mybir.dt.float32
BF16 = mybir.dt.bfloat16
I32 = mybir.dt.int32
U32 = mybir.dt.uint32
AF = mybir.ActivationFunctionType
ALU = mybir.AluOpType
AX = mybir.AxisListType

D = 2048
NCORES = 8
EPS = 1e-6
HD = 128
SCALE = HD ** -0.5
NE = 64
CAP = 1024
NSLOT = NE * CAP + 2048
STAGE = 99
DEBUG = False
SKIP_INPUTS = set()
INPUT_NAMES = []


class _Eng:
    def __init__(self, key):
        self.key = key
        self.sem = None
        self.count = 0
        self.thunks = []
        self.waited = {}


class Res:
    __slots__ = ("name", "w", "r")

    def __init__(self, name):
        self.name = name
        self.w = None
        self.r = []


class Sched:
    def __init__(self, nc, n_dma_sems=24):
        self.nc = nc
        self.eng = {k: _Eng(k) for k in ("tensor", "vector", "scalar", "gpsimd", "sync")}
        self.n_dma_sems = n_dma_sems
        self.dma_sems = {}
        self.dma_rr = {}
        self.sems = {}
        self.phase_id = 0

    def alloc_sems(self, stack):
        for k, e in self.eng.items():
            e.sem = stack.enter_context(self.nc.semaphore("s_" + k))
            self.sems[("e", k)] = e.sem
        for q in ("sync", "gpsimd"):
            lst = []
            for i in range(self.n_dma_sems):
                s = stack.enter_context(self.nc.semaphore(f"d_{q}_{i}"))
                self.sems[("d", q, i)] = s
                lst.append([("d", q, i), 0])
            self.dma_sems[q] = lst
            self.dma_rr[q] = 0

    def _deps(self, reads, writes):
        deps = []
        for r in reads:
            if r.w is not None:
                deps.append(r.w)
        for w in writes:
            if w.w is not None:
                deps.append(w.w)
            deps.extend(w.r)
        return deps

    def _waits(self, e, deps, skip_self=False):
        need = {}
        for src, val in deps:
            if skip_self and src == ("e", e.key):
                continue
            if e.waited.get(src, 0) >= val:
                continue
            if need.get(src, 0) < val:
                need[src] = val
        for src, val in need.items():
            e.waited[src] = val
        return list(need.items())

    def op(self, engine, fn, reads=(), writes=(), signal=True):
        e = self.eng[engine]
        waits = self._waits(e, self._deps(reads, writes), skip_self=(engine == "tensor"))
        if signal:
            e.count += 1
            tok = (("e", engine), e.count)
            for r in reads:
                r.r.append(tok)
            for w in writes:
                w.w = tok
                w.r = []
        sems = self.sems

        def thunk(h, waits=waits, fn=fn, signal=signal, sem=e.sem):
            for src, val in waits:
                h.wait_ge(sems[src], val)
            ins = fn(h)
            if signal:
                ins.then_inc(sem, 1)
        e.thunks.append(thunk)

    def dma(self, queue, fn, reads=(), writes=()):
        e = self.eng[queue]
        lst = self.dma_sems[queue]
        i = self.dma_rr[queue]
        self.dma_rr[queue] = (i + 1) % len(lst)
        slot = lst[i]
        deps = self._deps(reads, writes)
        if slot[1] > 0:
            deps.append((slot[0], slot[1]))
        waits = self._waits(e, deps)
        slot[1] += 16
        tok = (slot[0], slot[1])
        for r in reads:
            r.r.append(tok)
        for w in writes:
            w.w = tok
            w.r = []
        sems = self.sems

        def thunk(h, waits=waits, fn=fn, sem=sems[slot[0]]):
            for src, val in waits:
                h.wait_ge(sems[src], val)
            fn(h).then_inc(sem, 16)
        e.thunks.append(thunk)

    def final_wait(self, engine, resources):
        e = self.eng[engine]
        deps = [r.w for r in resources if r.w is not None]
        waits = self._waits(e, deps)
        sems = self.sems

        def thunk(h, waits=waits):
            for src, val in waits:
                h.wait_ge(sems[src], val)
        e.thunks.append(thunk)

    def drain(self):
        e = self.eng["sync"]
        deps = []
        for q, lst in self.dma_sems.items():
            for slot in lst:
                if slot[1] > 0:
                    deps.append((slot[0], slot[1]))
        for k, e2 in self.eng.items():
            if e2.count > 0 and k != "sync":
                deps.append((("e", k), e2.count))
        waits = self._waits(e, deps)
        sems = self.sems

        def thunk(h, waits=waits):
            for src, val in waits:
                h.wait_ge(sems[src], val)
        e.thunks.append(thunk)

    def emit(self):
        self.drain()
        self.phase_id = getattr(self, "phase_id", 0) + 1
        with self.nc.Block() as block:
            for k, e in self.eng.items():
                if not e.thunks:
                    continue

                def body(h, thunks=list(e.thunks)):
                    for t in thunks:
                        t(h)
                getattr(block, k)(body)
        for e in self.eng.values():
            e.thunks = []


def build():
    nc = bass.Bass("TRN2", target_bir_lowering=False)

    def din(name, shape, dt=F32):
        if name in SKIP_INPUTS:
            return None
        INPUT_NAMES.append(name)
        return nc.dram_tensor(name, list(shape), dt, kind="ExternalInput").ap()

    def dout(name, shape, dt=F32):
        return nc.dram_tensor(name, list(shape), dt, kind="ExternalOutput").ap()

    def dscr(name, shape, dt=F32):
        kind = "ExternalOutput" if (DEBUG and name in ("ysp", "dbg_dst", "dbg_gate", "dbg_moe", "dbg_sc")) else "Internal"
        return nc.dram_tensor(name, list(shape), dt, kind=kind).ap()

    xc = din("xc", [1024, D])
    xl = din("xl", [2048, D])
    ropec = din("ropec", [2048, 128])
    ropes = din("ropes", [2048, 128])
    cache = din("cache", [4, 256, 256])
    cond = din("cond", [2, D])
    w_ada = din("w_ada", [D, 6 * D])
    b_ada = din("b_ada", [1, 6 * D])
    gains = din("gains", [4, D])
    w_in = din("w_in", [D, 3072])
    qkg = din("qkg", [2, 128])
    sink = din("sink", [1, 8])
    w_out = din("w_out", [D, D])
    w_router = din("w_router", [D, NE])
    rbias = din("rbias", [1, NE])
    wge = din("wge", [NE, D, 512])
    wue = din("wue", [NE, D, 512])
    wde = din("wde", [NE, 512, D])
    wgs = din("wgs", [D, 512])
    wus = din("wus", [D, 512])
    wds = din("wds", [512, D])
    consts = din("consts", [128, 7 * 128])

    yc = dout("yc", [1024, D])
    yl = dout("yl", [1024, D])
    ngk = dout("ngk", [1024, 256])
    ngv = dout("ngv", [1024, 256])
    nwk = dout("nwk", [1024, 256])
    nwv = dout("nwv", [1024, 256])

    modsp = dscr("modsp", [12, 128, D])

    R = {}

    def res(name):
        if name not in R:
            R[name] = Res(name)
        return R[name]

    with ExitStack() as st:
        s = Sched(nc)
        s.alloc_sems(st)
        st.enter_context(nc.allow_non_contiguous_dma(reason="small strided loads"))
        st.enter_context(nc.allow_low_precision(reason="bf16 matmul operands"))

        def sb(name, shape, dt=F32):
            return st.enter_context(nc.sbuf_tensor(name, list(shape), dt))

        def ps(name, shape, dt=F32):
            return st.enter_context(nc.psum_tensor(name, list(shape), dt))

        pp = [ps(f"pp{i}", [128, 512], F32) for i in range(6)]
        pt = [ps(f"pt{i}", [128, 1024], BF16) for i in range(2)]
        r_pp = [res(f"pp{i}") for i in range(6)]
        r_pt = [res(f"pt{i}") for i in range(2)]

        cf = sb("cf", [128, 7 * 128], F32)
        cb = sb("cb", [128, 7 * 128], BF16)
        s.dma("sync", lambda h: h.dma_start(out=cf[:], in_=consts[:, :]), writes=[res("cf")])
        s.op("vector", lambda h: h.tensor_copy(out=cb[:], in_=cf[:]), reads=[res("cf")], writes=[res("cb")])
        identb = cb[:, 0:128]
        onesb = cb[:, 256:384]

        ph0 = ExitStack()

        def sb0(name, shape, dt=F32):
            return ph0.enter_context(nc.sbuf_tensor(name, list(shape), dt))
        condT = sb0("condT", [128, 16, 2], F32)
        crep = sb0("crep", [128, 2, 16, 128], BF16)
        gbc = sb0("gbc", [128, 4, D], F32)
        for p in range(2):
            s.dma("sync", lambda h, p=p: h.dma_start(
                out=condT[:, :, p], in_=cond[p:p + 1, :].rearrange("r (c p) -> p (r c)", p=128)),
                writes=[res("condT")])
        s.dma("sync", lambda h: h.dma_start(out=gbc[:], in_=gains.partition_broadcast(128)), writes=[res("gbc")])
        for p in range(2):
            s.op("scalar", lambda h, p=p: h.activation(
                out=crep[:, p, :, :], in_=condT[:, :, p:p + 1].broadcast_to([128, 16, 128]), func=AF.Silu),
                reads=[res("condT")], writes=[res("crep")])
        wa = [sb0(f"wa{i}", [128, 16, 512], BF16) for i in range(2)]
        bb = [sb0(f"bb{i}", [128, 512], F32) for i in range(2)]
        mt = [sb0(f"mt{i}", [128, 512], F32) for i in range(4)]
        gain_of = {1: 0, 2: 1, 4: 2, 5: 3}
        n_mt = 0
        for j in range(24):
            which, cc = j // 4, j % 4
            b = j % 2
            s.dma("gpsimd", lambda h, b=b, j=j: h.dma_start(
                out=wa[b][:], in_=w_ada[:, j * 512:(j + 1) * 512].rearrange("(c p) n -> p c n", p=128)),
                writes=[res(f"wa{b}")])
            s.dma("sync", lambda h, b=b, j=j: h.dma_start(
                out=bb[b][:], in_=b_ada[:, j * 512:(j + 1) * 512].partition_broadcast(128)),
                writes=[res(f"bb{b}")])
            for p in range(2):
                pi = (j * 2 + p) % 6
                for k in range(16):
                    s.op("tensor", lambda h, p=p, k=k, b=b, pi=pi: h.matmul(
                        pp[pi][:], lhsT=crep[:, p, k, :], rhs=wa[b][:, k, :], start=(k == 0), stop=(k == 15)),
                        reads=[res("crep"), res(f"wa{b}")], writes=[r_pp[pi]], signal=(k == 15))
                m = mt[n_mt % 4]
                rm = res(f"mt{n_mt % 4}")
                n_mt += 1
                if which in (0, 3):
                    s.op("vector", lambda h, m=m, pi=pi, b=b: h.tensor_tensor(
                        out=m[:], in0=pp[pi][:], in1=bb[b][:], op=ALU.add),
                        reads=[r_pp[pi], res(f"bb{b}")], writes=[rm])
                else:
                    gsl = gbc[:, gain_of[which], cc * 512:(cc + 1) * 512]
                    s.op("vector", lambda h, m=m, pi=pi, b=b: h.tensor_tensor(
                        out=m[:], in0=pp[pi][:], in1=bb[b][:], op=ALU.add),
                        reads=[r_pp[pi], res(f"bb{b}")], writes=[rm])
                    add1 = 1.0 if which in (1, 4) else 0.0
                    s.op("vector", lambda h, m=m, gsl=gsl, add1=add1: h.scalar_tensor_tensor(
                        out=m[:], in0=m[:], scalar=add1, in1=gsl, op0=ALU.add, op1=ALU.mult),
                        reads=[rm, res("gbc")], writes=[rm])
                idx = which * 2 + p
                s.dma("sync", lambda h, m=m, idx=idx, cc=cc: h.dma_start(
                    out=modsp[idx, :, cc * 512:(cc + 1) * 512], in_=m[:]),
                    reads=[rm])

        s.emit()
        ph0.close()

        qsp = [dscr("qsp0", [1024, 2560], BF16), dscr("qsp1", [2048, 2560], BF16)]
        vsp = [dscr("vsp0", [1024, 512], BF16), dscr("vsp1", [2048, 512], BF16)]
        ysp = dscr("ysp", [2048, D])
        xbuf = dscr("xbuf", [NE * CAP, D], BF16)
        xbuf_s = dscr("xbuf_s", [2048, D], BF16)
        obuf_s = dscr("obuf_s", [2048, D], BF16)
        obuf = dscr("obuf", [NE * CAP, D], BF16)
        xin = [xc, xl]
        youts = [yc, yl]
        identf = cf[:, 0:128]
        onesf = cf[:, 256:384]
        ustrb = cb[:, 128:256]
        _bc = {}

        def bc_reg(h):
            if _bc.get("phase") != s.phase_id:
                _bc["r"] = h.to_reg(NE * CAP - 1)
                _bc["phase"] = s.phase_id
            return _bc["r"]

        mx = sb("mx", [128, 2, 4], F32)
        negm = sb("negm", [128, 2, 2], F32)
        Mall = sb("Mall", [128, 16, NE], BF16)
        dstall = sb("dstall", [128, 16, 8], I32)
        gall = sb("gall", [128, 16, 8], F32)
        sinkb = sb("sinkb", [128, 8], F32)
        g10 = sb("g10", [128, 10, 128], F32)
        rbb = sb("rbb", [128, NE], F32)
        s.op("vector", lambda h: h.memset(mx[:], 0.0), writes=[res("mx")])
        s.dma("sync", lambda h: h.dma_start(out=sinkb[:], in_=sink.partition_broadcast(128)), writes=[res("sinkb")])
        s.dma("sync", lambda h: h.dma_start(out=rbb[:], in_=rbias.partition_broadcast(128)), writes=[res("rbb")])
        for hh in range(10):
            s.dma("sync", lambda h, hh=hh: h.dma_start(
                out=g10[:, hh, :], in_=qkg[(0 if hh < 8 else 1):(1 if hh < 8 else 2), :].partition_broadcast(128)),
                writes=[res("g10")])

        def rstd_chain(ssq_ap, rs_ap, n, r_in, r_out):
            s.op("vector", lambda h: h.tensor_scalar(out=rs_ap, in0=ssq_ap, scalar1=1.0 / n, scalar2=EPS,
                                                     op0=ALU.mult, op1=ALU.add), reads=[r_in], writes=[r_out])
            s.op("scalar", lambda h: h.activation(out=rs_ap, in_=rs_ap, func=AF.Sqrt), reads=[r_out], writes=[r_out])
            s.op("vector", lambda h: h.reciprocal(out=rs_ap, in_=rs_ap), reads=[r_out], writes=[r_out])

        def transpose16(src_bf, r_src, dst, r_dst, dst_slices):
            for half in range(2):
                for c in range(8):
                    cc = half * 8 + c
                    s.op("tensor", lambda h, half=half, c=c, cc=cc: h.transpose(
                        out=pt[half][:, c * 128:(c + 1) * 128], in_=src_bf[:, cc * 128:(cc + 1) * 128], identity=identb),
                        reads=[r_src, res("cb")], writes=[r_pt[half]], signal=(c == 7))
                eng = "scalar" if half == 0 else "vector"
                o = dst_slices(half)
                i_ = pt[half][:, :].rearrange("p (c t) -> p c t", t=128)
                if eng == "scalar":
                    s.op("scalar", lambda h, o=o, i_=i_: h.copy(out=o, in_=i_), reads=[r_pt[half]], writes=[r_dst])
                else:
                    s.op("vector", lambda h, o=o, i_=i_: h.tensor_copy(out=o, in_=i_), reads=[r_pt[half]], writes=[r_dst])

        def qkv_phase(p):
            nt = 8 if p == 0 else 16
            with ExitStack() as ph:
                def sbp(name, shape, dt=F32):
                    return ph.enter_context(nc.sbuf_tensor(f"{name}_{p}", list(shape), dt))
                win = sbp("win", [128, 16, 3072], BF16)
                A1 = sbp("A1", [128, D]); B1 = sbp("B1", [128, D])
                xt = [sbp(f"xt{i}", [128, D]) for i in range(2)]
                hb = sbp("hb", [128, D], BF16)
                hT = sbp("hT", [128, 16, 128], BF16)
                qkall = sbp("qkall", [128, 3072])
                qk20 = sbp("qk20", [128, 20, 128])
                t1 = sbp("t1", [128, 20, 128]); t2 = sbp("t2", [128, 20, 128])
                qkb = sbp("qkb", [128, 20, 128], BF16)
                vb = sbp("vb", [128, 512], BF16)
                rc = sbp("rc", [128, 128]); rs_ = sbp("rs", [128, 128])
                st1 = sbp("st1", [128, 8]); st10 = sbp("st10", [128, 10]); st20 = sbp("st20", [128, 20]); g4 = sbp("g4", [128, 4])
                for n in range(6):
                    s.dma("gpsimd", lambda h, n=n: h.dma_start(
                        out=win[:, :, n * 512:(n + 1) * 512],
                        in_=w_in[:, n * 512:(n + 1) * 512].rearrange("(c p) n -> p c n", p=128)), writes=[res("win")])
                s.dma("sync", lambda h: h.dma_start(out=A1[:], in_=modsp[2 + p]), writes=[res("A1")])
                s.dma("sync", lambda h: h.dma_start(out=B1[:], in_=modsp[0 + p]), writes=[res("B1")])
                for i in range(nt):
                    own = i < 8
                    b = i % 2
                    rx = res(f"xt{b}")
                    s.dma("sync", lambda h, b=b, i=i: h.dma_start(out=xt[b][:], in_=xin[p][i * 128:(i + 1) * 128, :]), writes=[rx])
                    s.op("scalar", lambda h, b=b: h.activation(out=t1[:].rearrange("p a b -> p (a b)")[:, 0:D], in_=xt[b][:],
                                                               func=AF.Square, accum_out=st1[:, 0:1]),
                         reads=[rx], writes=[res("t1"), res("st1")])
                    rstd_chain(st1[:, 0:1], st1[:, 1:2], D, res("st1"), res("st1b"))
                    t1f = t1[:].rearrange("p a b -> p (a b)")[:, 0:D]
                    s.op("vector", lambda h, b=b: h.scalar_tensor_tensor(out=t1f, in0=xt[b][:], scalar=st1[:, 1:2], in1=A1[:],
                                                                         op0=ALU.mult, op1=ALU.mult),
                         reads=[rx, res("st1b"), res("A1")], writes=[res("t1")])
                    s.op("gpsimd", lambda h: h.tensor_tensor(out=hb[:], in0=t1f, in1=B1[:], op=ALU.add),
                         reads=[res("t1"), res("B1")], writes=[res("hb")])
                    transpose16(hb, res("hb"), hT, res("hT"), lambda half: hT[:, half * 8:(half + 1) * 8, :])
                    chunks = range(6) if own else (2, 5)
                    for n in chunks:
                        for k in range(16):
                            s.op("tensor", lambda h, n=n, k=k: h.matmul(pp[n][:], lhsT=hT[:, k, :], rhs=win[:, k, n * 512:(n + 1) * 512],
                                                                        start=(k == 0), stop=(k == 15)),
                                 reads=[res("hT"), res("win")], writes=[r_pp[n]], signal=(k == 15))
                        eng = "scalar" if n % 2 == 0 else "vector"
                        if eng == "scalar":
                            s.op("scalar", lambda h, n=n: h.copy(out=qkall[:, n * 512:(n + 1) * 512], in_=pp[n][:]),
                                 reads=[r_pp[n]], writes=[res("qkall")])
                        else:
                            s.op("vector", lambda h, n=n: h.tensor_copy(out=qkall[:, n * 512:(n + 1) * 512], in_=pp[n][:]),
                                 reads=[r_pp[n]], writes=[res("qkall")])
                    h0 = 0 if own else 8
                    nn = 10 - h0
                    src_n = qkall[:, h0 * 128:1280].rearrange("p (a b) -> p a b", b=128)
                    s.op("scalar", lambda h, src_n=src_n, h0=h0: h.activation(out=t2[:, h0:10, :], in_=src_n, func=AF.Square),
                         reads=[res("qkall")], writes=[res("t2")])
                    s.op("vector", lambda h, h0=h0: h.tensor_reduce(out=st10[:, h0:10], in_=t2[:, h0:10, :], axis=AX.X, op=ALU.add),
                         reads=[res("t2")], writes=[res("st10")])
                    rstd_chain(st10[:, h0:10], st10[:, h0:10], 128, res("st10"), res("st10"))
                    s.op("vector", lambda h, src_n=src_n, h0=h0, nn=nn: h.tensor_tensor(
                        out=qk20[:, h0:10, :], in0=src_n, in1=st10[:, h0:10].unsqueeze(2).broadcast_to([128, nn, 128]), op=ALU.mult),
                        reads=[res("qkall"), res("st10")], writes=[res("qk20")])
                    s.op("vector", lambda h, h0=h0: h.tensor_tensor(out=qk20[:, h0:10, :], in0=qk20[:, h0:10, :], in1=g10[:, h0:10, :], op=ALU.mult),
                         reads=[res("qk20"), res("g10")], writes=[res("qk20")])
                    w0 = 10 if own else 18
                    c0 = 1536 + (w0 - 10) * 128
                    s.op("scalar", lambda h, w0=w0, c0=c0: h.copy(out=qk20[:, w0:20, :], in_=qkall[:, c0:2816].rearrange("p (a b) -> p a b", b=128)),
                         reads=[res("qkall")], writes=[res("qk20")])
                    rows = slice(i * 128, (i + 1) * 128)
                    if p == 0:
                        for (dst, src_ap) in ((ngk, qk20[:, 8:10, :].rearrange("p a b -> p (a b)")), (ngv, qkall[:, 1280:1536]),
                                              (nwk, qkall[:, 2560:2816]), (nwv, qkall[:, 2816:3072])):
                            s.dma("sync", lambda h, dst=dst, src_ap=src_ap, rows=rows: h.dma_start(out=dst[rows, :], in_=src_ap),
                                  reads=[res("qk20"), res("qkall")])
                        s.op("scalar", lambda h: h.copy(out=qkb[:], in_=qk20[:]), reads=[res("qk20")], writes=[res("qkb")])
                    else:
                        s.dma("sync", lambda h, rows=rows: h.dma_start(out=rc[:], in_=ropec[rows, :]), writes=[res("rc")])
                        s.dma("sync", lambda h, rows=rows: h.dma_start(out=rs_[:], in_=ropes[rows, :]), writes=[res("rs")])
                        groups = [(0, 20)] if own else [(8, 10), (18, 20)]
                        for (a, bnd) in groups:
                            nh = bnd - a
                            s.op("vector", lambda h, a=a, bnd=bnd, nh=nh: h.tensor_tensor(
                                out=t1[:, a:bnd, :], in0=qk20[:, a:bnd, :], in1=rc[:].unsqueeze(1).broadcast_to([128, nh, 128]), op=ALU.mult),
                                reads=[res("qk20"), res("rc")], writes=[res("t1")])
                            for pr in range(2):
                                for hf in range(2):
                                    o_ = t2[:, a:bnd, pr * 64 + hf * 32: pr * 64 + hf * 32 + 32]
                                    i_ = qk20[:, a:bnd, pr * 64 + (1 - hf) * 32: pr * 64 + (1 - hf) * 32 + 32]
                                    sn = rs_[:, pr * 64 + hf * 32: pr * 64 + hf * 32 + 32].unsqueeze(1).broadcast_to([128, nh, 32])
                                    s.op("gpsimd", lambda h, o_=o_, i_=i_, sn=sn: h.tensor_tensor(out=o_, in0=i_, in1=sn, op=ALU.mult),
                                         reads=[res("qk20"), res("rs")], writes=[res("t2")])
                            s.op("vector", lambda h, a=a, bnd=bnd: h.tensor_tensor(out=qkb[:, a:bnd, :], in0=t1[:, a:bnd, :], in1=t2[:, a:bnd, :], op=ALU.add),
                                 reads=[res("t1"), res("t2")], writes=[res("qkb")])
                    hs = [(0, 20)] if own else [(8, 10), (18, 20)]
                    for (a, bnd) in hs:
                        s.op("scalar", lambda h, a=a, bnd=bnd: h.activation(out=t1[:, a:bnd, :], in_=qkb[:, a:bnd, :], func=AF.Square),
                             reads=[res("qkb")], writes=[res("t1")])
                        s.op("vector", lambda h, a=a, bnd=bnd: h.tensor_reduce(out=st20[:, a:bnd], in_=t1[:, a:bnd, :], axis=AX.X, op=ALU.add),
                             reads=[res("t1")], writes=[res("st20")])
                    grp = [(0, 0, 8), (1, 8, 10), (2, 10, 18), (3, 18, 20)] if own else [(1, 8, 10), (3, 18, 20)]
                    for (gi_, a, bnd) in grp:
                        s.op("vector", lambda h, gi_=gi_, a=a, bnd=bnd: h.tensor_reduce(out=g4[:, gi_:gi_ + 1], in_=st20[:, a:bnd], axis=AX.X, op=ALU.max),
                             reads=[res("st20")], writes=[res("g4")])
                        s.op("vector", lambda h, gi_=gi_: h.tensor_tensor(out=mx[:, p, gi_:gi_ + 1], in0=mx[:, p, gi_:gi_ + 1], in1=g4[:, gi_:gi_ + 1], op=ALU.max),
                             reads=[res("g4"), res("mx")], writes=[res("mx")])
                    s.op("scalar", lambda h: h.copy(out=vb[:, 0:256], in_=qkall[:, 1280:1536]), reads=[res("qkall")], writes=[res("vb")])
                    s.op("scalar", lambda h: h.copy(out=vb[:, 256:512], in_=qkall[:, 2816:3072]), reads=[res("qkall")], writes=[res("vb")])
                    s.dma("sync", lambda h, rows=rows: h.dma_start(out=qsp[p][rows, :], in_=qkb[:].rearrange("p a b -> p (a b)")),
                          reads=[res("qkb")])
                    s.dma("sync", lambda h, rows=rows: h.dma_start(out=vsp[p][rows, :], in_=vb[:]),
                          reads=[res("vb")])
                s.emit()

        def attn_phase(p, OT):
            nq_t = 8
            nloc = 8 if p == 0 else 16
            nkt = 8 if p == 0 else 18
            koff = 0 if p == 0 else 2
            with ExitStack() as ph:
                def sbp(name, shape, dt=F32):
                    return ph.enter_context(nc.sbuf_tensor(f"{name}_a{p}", list(shape), dt))
                QT = [sbp("QTg", [128, 8, 1024], BF16), sbp("QTw", [128, 8, 1024], BF16)]
                KT = [sbp("KTg", [128, 2, nkt * 128], BF16), sbp("KTw", [128, 2, nkt * 128], BF16)]
                V = sbp("V", [128, nkt, 512], BF16)
                qt = [sbp(f"qt{i}", [128, 2560], BF16) for i in range(2)]
                pb = [sbp(f"pb{i}", [128, 512], BF16) for i in range(4)]
                rec = [sbp(f"rec{i}", [128, 512]) for i in range(2)]
                SE = sbp("SE", [128, 8])
                m4 = sbp("m4", [128, 4]); dg = sbp("dg", [128, 4]); mb4 = sbp("mb4", [128, 4])
                cft = sbp("cft", [128, 512]); cbt = sbp("cbt", [128, 512], BF16); st4 = sbp("st4", [128, 4])
                if p == 1:
                    for kt in range(2):
                        for which, kind in ((0, 0), (2, 1)):
                            s.dma("sync", lambda h, kt=kt, which=which: h.dma_start(out=cft[:, 0:256], in_=cache[which, kt * 128:(kt + 1) * 128, :]),
                                  writes=[res("cft")])
                            s.op("vector", lambda h: h.tensor_copy(out=cbt[:, 0:256], in_=cft[:, 0:256]), reads=[res("cft")], writes=[res("cbt")])
                            s.op("scalar", lambda h: h.activation(out=cft[:, 256:512], in_=cbt[:, 0:256], func=AF.Square),
                                 reads=[res("cbt")], writes=[res("cft2")])
                            s.op("vector", lambda h: h.tensor_reduce(out=st4[:, 0:2], in_=cft[:, 256:512].rearrange("p (a b) -> p a b", b=128), axis=AX.X, op=ALU.add),
                                 reads=[res("cft2")], writes=[res("st4")])
                            s.op("vector", lambda h: h.tensor_reduce(out=st4[:, 2:3], in_=st4[:, 0:2], axis=AX.X, op=ALU.max),
                                 reads=[res("st4")], writes=[res("st4")])
                            col = 1 if kind == 0 else 3
                            s.op("vector", lambda h, col=col: h.tensor_tensor(out=mx[:, 1, col:col + 1], in0=mx[:, 1, col:col + 1], in1=st4[:, 2:3], op=ALU.max),
                                 reads=[res("st4"), res("mx")], writes=[res("mx")])
                            for n in range(2):
                                s.op("tensor", lambda h, n=n: h.transpose(out=pt[0][:, n * 128:(n + 1) * 128], in_=cbt[:, n * 128:(n + 1) * 128], identity=identb),
                                     reads=[res("cbt"), res("cb")], writes=[r_pt[0]], signal=(n == 1))
                            s.op("vector", lambda h, kt=kt, kind=kind: h.tensor_copy(
                                out=KT[kind][:, :, kt * 128:(kt + 1) * 128], in_=pt[0][:, 0:256].rearrange("p (a b) -> p a b", b=128)),
                                reads=[r_pt[0]], writes=[res(f"KT{kind}")])
                        for which, off in ((1, 0), (3, 256)):
                            s.dma("sync", lambda h, kt=kt, which=which: h.dma_start(out=cft[:, 0:256], in_=cache[which, kt * 128:(kt + 1) * 128, :]),
                                  writes=[res("cft")])
                            s.op("vector", lambda h, kt=kt, off=off: h.tensor_copy(out=V[:, kt, off:off + 256], in_=cft[:, 0:256]),
                                 reads=[res("cft")], writes=[res("V")])
                for i in range(nloc):
                    b = i % 2
                    rows = slice(i * 128, (i + 1) * 128)
                    s.dma("sync", lambda h, b=b, rows=rows: h.dma_start(out=qt[b][:], in_=qsp[p][rows, :]),
                          writes=[res(f"qt{b}")])
                    s.dma("sync", lambda h, i=i, rows=rows: h.dma_start(out=V[:, koff + i, :], in_=vsp[p][rows, :]),
                          writes=[res("V")])
                    if i < 8:
                        for kind in range(2):
                            c0 = 0 if kind == 0 else 1280
                            for hh in range(8):
                                s.op("tensor", lambda h, b=b, hh=hh, c0=c0, kind=kind: h.transpose(
                                    out=pt[kind][:, hh * 128:(hh + 1) * 128], in_=qt[b][:, c0 + hh * 128:c0 + (hh + 1) * 128], identity=identb),
                                    reads=[res(f"qt{b}"), res("cb")], writes=[r_pt[kind]], signal=(hh == 7))
                            o_ = QT[kind][:, :, i * 128:(i + 1) * 128]
                            i_ = pt[kind][:, :].rearrange("p (a b) -> p a b", b=128)
                            if kind == 0:
                                s.op("scalar", lambda h, o_=o_, i_=i_: h.copy(out=o_, in_=i_), reads=[r_pt[kind]], writes=[res(f"QT{kind}")])
                            else:
                                s.op("vector", lambda h, o_=o_, i_=i_: h.tensor_copy(out=o_, in_=i_), reads=[r_pt[kind]], writes=[res(f"QT{kind}")])
                    for kind in range(2):
                        c0 = 1024 if kind == 0 else 2304
                        for n in range(2):
                            s.op("tensor", lambda h, b=b, n=n, c0=c0, kind=kind: h.transpose(
                                out=pt[kind][:, n * 128:(n + 1) * 128], in_=qt[b][:, c0 + n * 128:c0 + (n + 1) * 128], identity=identb),
                                reads=[res(f"qt{b}"), res("cb")], writes=[r_pt[kind]], signal=(n == 1))
                        kk = koff + i
                        s.op("vector", lambda h, kk=kk, kind=kind: h.tensor_copy(
                            out=KT[kind][:, :, kk * 128:(kk + 1) * 128], in_=pt[kind][:, 0:256].rearrange("p (a b) -> p a b", b=128)),
                            reads=[r_pt[kind]], writes=[res(f"KT{kind}")])
                s.op("tensor", lambda h: h.transpose(out=pp[0][0:4, 0:128], in_=mx[:, p, :], identity=identf),
                     reads=[res("mx"), res("cf")], writes=[r_pp[0]])
                s.op("vector", lambda h: h.tensor_reduce(out=m4[0:4, 0:1], in_=pp[0][0:4, 0:128], axis=AX.X, op=ALU.max),
                     reads=[r_pp[0]], writes=[res("m4")])
                s.op("vector", lambda h: h.tensor_scalar(out=dg[0:4, 0:4], in0=identf[0:4, 0:4], scalar1=m4[0:4, 0:1], scalar2=None, op0=ALU.mult),
                     reads=[res("m4"), res("cf")], writes=[res("dg")])
                s.op("tensor", lambda h: h.matmul(pp[1][:, 0:4], lhsT=onesf[0:4, 0:128], rhs=dg[0:4, 0:4], start=True, stop=True),
                     reads=[res("dg"), res("cf")], writes=[r_pp[1]])
                s.op("vector", lambda h: h.tensor_copy(out=mb4[:], in_=pp[1][:, 0:4]), reads=[r_pp[1]], writes=[res("mb4")])
                for kind in range(2):
                    s.op("vector", lambda h, kind=kind: h.tensor_tensor(out=negm[:, p, kind:kind + 1], in0=mb4[:, 2 * kind:2 * kind + 1],
                                                                        in1=mb4[:, 2 * kind + 1:2 * kind + 2], op=ALU.mult),
                         reads=[res("mb4")], writes=[res("negm")])
                s.op("scalar", lambda h: h.activation(out=negm[:, p, :], in_=negm[:, p, :], func=AF.Sqrt), reads=[res("negm")], writes=[res("negm")])
                s.op("vector", lambda h: h.tensor_scalar(out=negm[:, p, :], in0=negm[:, p, :], scalar1=-SCALE, scalar2=None, op0=ALU.mult),
                     reads=[res("negm")], writes=[res("negm")])
                s.op("scalar", lambda h: h.activation(out=SE[:], in_=sinkb[:], func=AF.Exp, bias=negm[:, p, 1:2], scale=1.0),
                     reads=[res("negm"), res("sinkb")], writes=[res("SE")])

                jobs = []
                if p == 0:
                    for sq in range(4):
                        for kind in range(2):
                            for n in range(2):
                                for qc in range(2):
                                    h0 = 4 * n + 2 * qc
                                    q_ap = QT[kind][:, h0:h0 + 2, sq * 256:(sq + 1) * 256]
                                    keys = [(sq * 2 + kt, None) for kt in range(2)]
                                    o_ap = OT[:, kind * 8 + h0:kind * 8 + h0 + 2, sq * 256:(sq + 1) * 256]
                                    jobs.append((kind, n, q_ap, keys, o_ap, (h0, 2, 256)))
                else:
                    for n in range(2):
                        for qb in range(8):
                            q_ap = QT[0][:, 4 * n:4 * n + 4, qb * 128:(qb + 1) * 128]
                            o_ap = OT[:, 4 * n:4 * n + 4, qb * 128:(qb + 1) * 128]
                            jobs.append((0, n, q_ap, [(kt, None) for kt in range(18)], o_ap, (4 * n, 4, 128)))
                    for n in range(2):
                        for qb in range(8):
                            q_ap = QT[1][:, 4 * n:4 * n + 4, qb * 128:(qb + 1) * 128]
                            o_ap = OT[:, 8 + 4 * n:8 + 4 * n + 4, qb * 128:(qb + 1) * 128]
                            prev = (2 + qb - 1, 384) if qb > 0 else (2 + 8, 640)
                            nxt = (2 + qb + 1, 512) if qb < 7 else (2 + 8, 768)
                            keys = [(0, None), (1, None), prev, (2 + qb, None), nxt]
                            jobs.append((1, n, q_ap, keys, o_ap, (4 * n, 4, 128)))
                npb = 0
                for ji, (kind, n, q_ap, keys, o_ap, (h0, nh, nqq)) in enumerate(jobs):
                    po, psm = 2 + (ji % 2), 4 + (ji % 2)
                    def emit_S(ki):
                        kt = keys[ki][0]
                        sbank = ki % 2
                        s.op("tensor", lambda h, sbank=sbank, kind=kind, n=n, kt=kt, q_ap=q_ap: h.matmul(
                            pp[sbank][:], lhsT=KT[kind][:, n, kt * 128:(kt + 1) * 128], rhs=q_ap, start=True, stop=True),
                            reads=[res(f"KT{kind}"), res(f"QT{kind}")], writes=[r_pp[sbank]])
                    emit_S(0)
                    for ki, (kt, moff) in enumerate(keys):
                        sbank = ki % 2
                        pi = npb % 4
                        npb += 1
                        s.op("scalar", lambda h, pi=pi, sbank=sbank, kind=kind: h.activation(
                            out=pb[pi][:], in_=pp[sbank][:], func=AF.Exp, bias=negm[:, p, kind:kind + 1], scale=SCALE),
                            reads=[r_pp[sbank], res("negm")], writes=[res(f"pb{pi}")])
                        if ki + 1 < len(keys):
                            emit_S(ki + 1)
                        if moff is not None:
                            mk = cb[:, moff:moff + 128].unsqueeze(1).broadcast_to([128, 4, 128])
                            s.op("gpsimd", lambda h, pi=pi, mk=mk: h.tensor_tensor(
                                out=pb[pi][:].rearrange("p (a b) -> p a b", b=128), in0=pb[pi][:].rearrange("p (a b) -> p a b", b=128), in1=mk, op=ALU.mult),
                                reads=[res(f"pb{pi}"), res("cb")], writes=[res(f"pb{pi}")])
                        vs = V[:, kt, kind * 256 + n * 128: kind * 256 + (n + 1) * 128]
                        last = ki == len(keys) - 1
                        s.op("tensor", lambda h, po=po, vs=vs, pi=pi, ki=ki, last=last: h.matmul(
                            pp[po][:], lhsT=vs, rhs=pb[pi][:], start=(ki == 0), stop=last),
                            reads=[res("V"), res(f"pb{pi}")], writes=[r_pp[po]], signal=last)
                        s.op("tensor", lambda h, psm=psm, pi=pi, ki=ki, last=last: h.matmul(
                            pp[psm][:], lhsT=onesb, rhs=pb[pi][:], start=(ki == 0), stop=last),
                            reads=[res("cb"), res(f"pb{pi}")], writes=[r_pp[psm]], signal=True)
                    rb = ji % 2
                    if kind == 1:
                        se = SE[:, h0:h0 + nh].unsqueeze(2).broadcast_to([128, nh, nqq])
                        s.op("vector", lambda h, rb=rb, psm=psm, se=se, nqq=nqq: h.tensor_tensor(
                            out=rec[rb][:].rearrange("p (a b) -> p a b", b=nqq), in0=pp[psm][:].rearrange("p (a b) -> p a b", b=nqq), in1=se, op=ALU.add),
                            reads=[r_pp[psm], res("SE")], writes=[res(f"rec{rb}")])
                        s.op("vector", lambda h, rb=rb: h.reciprocal(out=rec[rb][:], in_=rec[rb][:]), reads=[res(f"rec{rb}")], writes=[res(f"rec{rb}")])
                    else:
                        s.op("vector", lambda h, rb=rb, psm=psm: h.reciprocal(out=rec[rb][:], in_=pp[psm][:]), reads=[r_pp[psm]], writes=[res(f"rec{rb}")])
                    s.op("vector", lambda h, rb=rb, po=po, o_ap=o_ap, nqq=nqq: h.tensor_tensor(
                        out=o_ap, in0=pp[po][:].rearrange("p (a b) -> p a b", b=nqq), in1=rec[rb][:].rearrange("p (a b) -> p a b", b=nqq), op=ALU.mult),
                        reads=[r_pp[po], res(f"rec{rb}")], writes=[res("OT")])
                s.emit()

        def post_phase(p, OT):
            with ExitStack() as ph:
                def sbp(name, shape, dt=F32):
                    return ph.enter_context(nc.sbuf_tensor(f"{name}_p{p}", list(shape), dt))
                wo = sbp("wo", [128, 16, D], BF16)
                wr = sbp("wr", [128, 16, NE], BF16)
                G1 = sbp("G1", [128, D]); A2 = sbp("A2", [128, D]); B2 = sbp("B2", [128, D])
                xt = sbp("xt", [128, D]); yt = sbp("yt", [128, D]); tf = sbp("tf", [128, D])
                h2b = sbp("h2b", [128, D], BF16); h2T = sbp("h2T", [128, 16, 128], BF16)
                st = sbp("st", [128, 8])
                sc = sbp("sc", [128, NE]); sel = sbp("sel", [128, NE]); srt = sbp("srt", [128, 8, 8]); gs = sbp("gs", [128, 8])
                gs8 = sbp("gs8", [128, 8]); gm = sbp("gm", [128, 8]); gneg = sbp("gneg", [128, 8]); selm = sbp("selm", [128, NE])
                top8 = sbp("top8", [128, 8]); Mf = sbp("Mf", [128, NE]); wsel = sbp("wsel", [128, NE]); den = sbp("den", [128, 2])
                posf = sbp("posf", [128, NE]); vv = sbp("vv", [128, NE]); d8 = sbp("d8", [128, 8]); oh = sbp("oh", [128, NE])
                g8 = sbp("g8", [128, 8]); eoff = sbp("eoff", [128, NE])
                for n in range(4):
                    s.dma("gpsimd", lambda h, n=n: h.dma_start(out=wo[:, :, n * 512:(n + 1) * 512],
                                                              in_=w_out[:, n * 512:(n + 1) * 512].rearrange("(c p) n -> p c n", p=128)), writes=[res("wo")])
                s.dma("gpsimd", lambda h: h.dma_start(out=wr[:], in_=w_router.rearrange("(c p) n -> p c n", p=128)), writes=[res("wr")])
                s.dma("sync", lambda h: h.dma_start(out=G1[:], in_=modsp[4 + p]), writes=[res("G1")])
                s.dma("sync", lambda h: h.dma_start(out=A2[:], in_=modsp[8 + p]), writes=[res("A2")])
                s.dma("sync", lambda h: h.dma_start(out=B2[:], in_=modsp[6 + p]), writes=[res("B2")])
                s.op("gpsimd", lambda h: h.iota(eoff[:], pattern=[[CAP, NE]], base=1, channel_multiplier=0, allow_small_or_imprecise_dtypes=True),
                     writes=[res("eoff")])
                for i in range(8):
                    gi = p * 8 + i
                    rows = slice(i * 128, (i + 1) * 128)
                    grow = slice(gi * 128, (gi + 1) * 128)
                    for n in range(4):
                        for mh in range(16):
                            s.op("tensor", lambda h, n=n, mh=mh, i=i: h.matmul(pp[n][:], lhsT=OT[:, mh, i * 128:(i + 1) * 128], rhs=wo[:, mh, n * 512:(n + 1) * 512],
                                                                              start=(mh == 0), stop=(mh == 15)),
                                 reads=[res("OT"), res("wo")], writes=[r_pp[n]], signal=(mh == 15))
                        s.op("scalar", lambda h, n=n: h.activation(out=tf[:, n * 512:(n + 1) * 512], in_=pp[n][:], func=AF.Square, accum_out=st[:, n:n + 1]),
                             reads=[r_pp[n]], writes=[res("tf"), res("st")])
                    s.op("vector", lambda h: h.tensor_reduce(out=st[:, 4:5], in_=st[:, 0:4], axis=AX.X, op=ALU.add), reads=[res("st")], writes=[res("st")])
                    rstd_chain(st[:, 4:5], st[:, 5:6], D, res("st"), res("st"))
                    s.dma("sync", lambda h, rows=rows: h.dma_start(out=xt[:], in_=xin[p][rows, :]), writes=[res("xt")])
                    for n in range(4):
                        cs = slice(n * 512, (n + 1) * 512)
                        s.op("vector", lambda h, n=n, cs=cs: h.scalar_tensor_tensor(out=tf[:, cs], in0=pp[n][:], scalar=st[:, 5:6], in1=G1[:, cs],
                                                                                    op0=ALU.mult, op1=ALU.mult),
                             reads=[r_pp[n], res("st"), res("G1")], writes=[res("tf")])
                    s.op("gpsimd", lambda h: h.tensor_tensor(out=yt[:], in0=tf[:], in1=xt[:], op=ALU.add), reads=[res("tf"), res("xt")], writes=[res("yt")])
                    s.dma("sync", lambda h, grow=grow: h.dma_start(out=ysp[grow, :], in_=yt[:]), reads=[res("yt")])
                    s.op("scalar", lambda h: h.activation(out=tf[:], in_=yt[:], func=AF.Square, accum_out=st[:, 6:7]),
                         reads=[res("yt")], writes=[res("tf"), res("st")])
                    rstd_chain(st[:, 6:7], st[:, 7:8], D, res("st"), res("st"))
                    s.op("vector", lambda h: h.scalar_tensor_tensor(out=tf[:], in0=yt[:], scalar=st[:, 7:8], in1=A2[:], op0=ALU.mult, op1=ALU.mult),
                         reads=[res("yt"), res("st"), res("A2")], writes=[res("tf")])
                    s.op("gpsimd", lambda h: h.tensor_tensor(out=h2b[:], in0=tf[:], in1=B2[:], op=ALU.add), reads=[res("tf"), res("B2")], writes=[res("h2b")])
                    s.dma("sync", lambda h, gi=gi: h.dma_start(out=xbuf_s[gi * 128:(gi + 1) * 128, :], in_=h2b[:]),
                          reads=[res("h2b")])
                    transpose16(h2b, res("h2b"), h2T, res("h2T"), lambda half: h2T[:, half * 8:(half + 1) * 8, :])
                    for k in range(16):
                        s.op("tensor", lambda h, k=k: h.matmul(pp[4][:, 0:NE], lhsT=h2T[:, k, :], rhs=wr[:, k, :], start=(k == 0), stop=(k == 15)),
                             reads=[res("h2T"), res("wr")], writes=[r_pp[4]], signal=(k == 15))
                    s.op("scalar", lambda h: h.activation(out=sc[:], in_=pp[4][:, 0:NE], func=AF.Sigmoid), reads=[r_pp[4]], writes=[res("sc")])
                    s.op("vector", lambda h: h.tensor_tensor(out=sel[:], in0=sc[:], in1=rbb[:], op=ALU.add), reads=[res("sc"), res("rbb")], writes=[res("sel")])
                    for g in range(8):
                        s.op("vector", lambda h, g=g: h.max(out=srt[:, g, :], in_=sel[:, g * 8:(g + 1) * 8]), reads=[res("sel")], writes=[res("srt")])
                    s.op("vector", lambda h: h.tensor_tensor(out=gs[:], in0=srt[:, :, 0], in1=srt[:, :, 1], op=ALU.add), reads=[res("srt")], writes=[res("gs")])
                    s.op("vector", lambda h: h.max(out=gs8[:], in_=gs[:]), reads=[res("gs")], writes=[res("gs8")])
                    s.op("vector", lambda h: h.tensor_scalar(out=gm[:], in0=gs[:], scalar1=gs8[:, 3:4], scalar2=None, op0=ALU.is_ge),
                         reads=[res("gs"), res("gs8")], writes=[res("gm")])
                    s.op("vector", lambda h: h.tensor_scalar(out=gneg[:], in0=gm[:], scalar1=-1.0, scalar2=1e9, op0=ALU.add, op1=ALU.mult),
                         reads=[res("gm")], writes=[res("gneg")])
                    for g in range(8):
                        s.op("vector", lambda h, g=g: h.tensor_scalar(out=selm[:, g * 8:(g + 1) * 8], in0=sel[:, g * 8:(g + 1) * 8],
                                                                      scalar1=gm[:, g:g + 1], scalar2=gneg[:, g:g + 1], op0=ALU.mult, op1=ALU.add),
                             reads=[res("sel"), res("gm"), res("gneg")], writes=[res("selm")])
                    s.op("vector", lambda h: h.max(out=top8[:], in_=selm[:]), reads=[res("selm")], writes=[res("top8")])
                    s.op("vector", lambda h: h.tensor_scalar(out=Mf[:], in0=selm[:], scalar1=top8[:, 7:8], scalar2=None, op0=ALU.is_ge),
                         reads=[res("selm"), res("top8")], writes=[res("Mf")])
                    s.op("vector", lambda h, gi=gi: h.tensor_copy(out=Mall[:, gi, :], in_=Mf[:]), reads=[res("Mf")], writes=[res("Mall")])
                    s.op("vector", lambda h: h.tensor_tensor(out=wsel[:], in0=sc[:], in1=Mf[:], op=ALU.mult), reads=[res("sc"), res("Mf")], writes=[res("wsel")])
                    s.op("vector", lambda h: h.tensor_reduce(out=den[:, 0:1], in_=wsel[:], axis=AX.X, op=ALU.add), reads=[res("wsel")], writes=[res("den")])
                    s.op("vector", lambda h: h.reciprocal(out=den[:, 1:2], in_=den[:, 0:1]), reads=[res("den")], writes=[res("den")])
                    s.op("vector", lambda h: h.tensor_scalar(out=wsel[:], in0=wsel[:], scalar1=den[:, 1:2], scalar2=2.5, op0=ALU.mult, op1=ALU.mult),
                         reads=[res("wsel"), res("den")], writes=[res("wsel")])
                    s.op("tensor", lambda h, gi=gi: h.matmul(pp[5][:, 0:NE], lhsT=ustrb, rhs=Mall[:, gi, :], start=True, stop=(gi == 0)),
                         reads=[res("Mall"), res("cb")], writes=[r_pp[5]], signal=(gi == 0))
                    for j in range(gi):
                        s.op("tensor", lambda h, j=j, gi=gi: h.matmul(pp[5][:, 0:NE], lhsT=onesb, rhs=Mall[:, j, :], start=False, stop=(j == gi - 1)),
                             reads=[res("Mall"), res("cb")], writes=[r_pp[5]], signal=(j == gi - 1))
                    s.op("vector", lambda h: h.tensor_scalar(out=posf[:], in0=pp[5][:, 0:NE], scalar1=float(CAP - 1), scalar2=None, op0=ALU.min),
                         reads=[r_pp[5]], writes=[res("posf")])
                    s.op("vector", lambda h: h.tensor_tensor(out=vv[:], in0=posf[:], in1=eoff[:], op=ALU.add), reads=[res("posf"), res("eoff")], writes=[res("vv")])
                    s.op("vector", lambda h: h.tensor_tensor(out=vv[:], in0=vv[:], in1=Mf[:], op=ALU.mult), reads=[res("vv"), res("Mf")], writes=[res("vv")])
                    s.op("vector", lambda h: h.max(out=d8[:], in_=vv[:]), reads=[res("vv")], writes=[res("d8")])
                    for k in range(8):
                        s.op("vector", lambda h, k=k: h.tensor_scalar(out=oh[:], in0=vv[:], scalar1=d8[:, k:k + 1], scalar2=None, op0=ALU.is_equal),
                             reads=[res("vv"), res("d8")], writes=[res("oh")])
                        s.op("vector", lambda h, k=k: h.tensor_tensor(out=oh[:], in0=oh[:], in1=wsel[:], op=ALU.mult), reads=[res("oh"), res("wsel")], writes=[res("oh")])
                        s.op("vector", lambda h, k=k: h.tensor_reduce(out=g8[:, k:k + 1], in_=oh[:], axis=AX.X, op=ALU.add), reads=[res("oh")], writes=[res("g8")])
                    s.op("vector", lambda h: h.tensor_scalar(out=d8[:], in0=d8[:], scalar1=-1.0, scalar2=None, op0=ALU.add), reads=[res("d8")], writes=[res("d8")])
                    s.op("vector", lambda h, gi=gi: h.tensor_copy(out=dstall[:, gi, :], in_=d8[:]), reads=[res("d8")], writes=[res("dstall")])
                    s.op("vector", lambda h, gi=gi: h.tensor_copy(out=gall[:, gi, :], in_=g8[:]), reads=[res("g8")], writes=[res("gall")])
                    if DEBUG:
                        s.dma("sync", lambda h, gi=gi: h.dma_start(out=dbg_sc[gi], in_=sc[:]), reads=[res("sc")])
                    for k in range(8):
                        s.dma("gpsimd", lambda h, k=k, gi=gi: h.indirect_dma_start(
                            out=xbuf[:, :], out_offset=bass.IndirectOffsetOnAxis(ap=dstall[:, gi, k:k + 1], axis=0),
                            in_=h2b[:], in_offset=None, bounds_check=None),
                            reads=[res("h2b"), res("dstall")])
                s.emit()

        def expert_phase():
            with ExitStack() as ph:
                def sbp(name, shape, dt=F32):
                    return ph.enter_context(nc.sbuf_tensor(f"{name}_e", list(shape), dt))
                wg = [sbp(f"wg{i}", [128, 16, 512], BF16) for i in range(2)]
                wu = [sbp(f"wu{i}", [128, 16, 512], BF16) for i in range(2)]
                wd = [sbp(f"wd{i}", [128, 4, D], BF16) for i in range(2)]
                xg = [sbp(f"xg{i}", [128, D], BF16) for i in range(2)]
                xT = sbp("xT", [128, 16, 512], BF16)
                sg = [sbp(f"sg{i}", [128, 512]) for i in range(2)]
                act = sbp("act", [128, 4, 512], BF16)
                ob = [sbp(f"ob{i}", [128, D], BF16) for i in range(2)]
                passes = [(e, e * CAP + q * 512) for e in range(NE) for q in range(CAP // 512)] + [(NE, q * 512) for q in range(4)]
                state = {"loaded": -1, "nx": 0, "nob": 0}

                def emit_loadx(idx):
                    e, r0 = passes[idx]
                    b = e % 2
                    if e != state["loaded"]:
                        state["loaded"] = e
                        gsrc = wge[e] if e < NE else wgs
                        usrc = wue[e] if e < NE else wus
                        dsrc = wde[e] if e < NE else wds
                        s.dma("gpsimd", lambda h, b=b, gsrc=gsrc: h.dma_start(out=wg[b][:], in_=gsrc.rearrange("(c p) n -> p c n", p=128)), writes=[res(f"wg{b}")])
                        s.dma("gpsimd", lambda h, b=b, usrc=usrc: h.dma_start(out=wu[b][:], in_=usrc.rearrange("(c p) n -> p c n", p=128)), writes=[res(f"wu{b}")])
                        s.dma("gpsimd", lambda h, b=b, dsrc=dsrc: h.dma_start(out=wd[b][:], in_=dsrc.rearrange("(c p) n -> p c n", p=128)), writes=[res(f"wd{b}")])
                    for sbk in range(4):
                        xb_ = state["nx"] % 2
                        state["nx"] += 1
                        s.dma("sync", lambda h, xb_=xb_, r0=r0, sbk=sbk, e=e: h.dma_start(out=xg[xb_][:], in_=(xbuf if e < NE else xbuf_s)[r0 + sbk * 128:r0 + (sbk + 1) * 128, :]),
                              writes=[res(f"xg{xb_}")])
                        transpose16(xg[xb_], res(f"xg{xb_}"), xT, res("xT"),
                                    lambda half, sbk=sbk: xT[:, half * 8:(half + 1) * 8, sbk * 128:(sbk + 1) * 128])

                def emit_gu(idx):
                    e, r0 = passes[idx]
                    b = e % 2
                    for fc in range(4):
                        gb, ub = fc % 2, 2 + fc % 2
                        for k in range(16):
                            s.op("tensor", lambda h, gb=gb, b=b, k=k, fc=fc: h.matmul(pp[gb][:], lhsT=wg[b][:, k, fc * 128:(fc + 1) * 128], rhs=xT[:, k, :],
                                                                                      start=(k == 0), stop=(k == 15)),
                                 reads=[res(f"wg{b}"), res("xT")], writes=[r_pp[gb]], signal=(k == 15))
                        for k in range(16):
                            s.op("tensor", lambda h, ub=ub, b=b, k=k, fc=fc: h.matmul(pp[ub][:], lhsT=wu[b][:, k, fc * 128:(fc + 1) * 128], rhs=xT[:, k, :],
                                                                                      start=(k == 0), stop=(k == 15)),
                                 reads=[res(f"wu{b}"), res("xT")], writes=[r_pp[ub]], signal=(k == 15))
                        s.op("scalar", lambda h, gb=gb, fc=fc: h.activation(out=sg[fc % 2][:], in_=pp[gb][:], func=AF.Silu),
                             reads=[r_pp[gb]], writes=[res(f"sg{fc % 2}")])
                        s.op("vector", lambda h, ub=ub, fc=fc: h.tensor_tensor(out=act[:, fc, :], in0=pp[ub][:], in1=sg[fc % 2][:], op=ALU.mult),
                             reads=[r_pp[ub], res(f"sg{fc % 2}")], writes=[res("act")])

                def emit_down(idx):
                    e, r0 = passes[idx]
                    b = e % 2
                    for sbk in range(4):
                        ob_ = state["nob"] % 2
                        state["nob"] += 1
                        for n in range(4):
                            bank = 4 + n % 2
                            for fc in range(4):
                                s.op("tensor", lambda h, bank=bank, fc=fc, sbk=sbk, n=n, b=b: h.matmul(
                                    pp[bank][:], lhsT=act[:, fc, sbk * 128:(sbk + 1) * 128], rhs=wd[b][:, fc, n * 512:(n + 1) * 512],
                                    start=(fc == 0), stop=(fc == 3)),
                                    reads=[res("act"), res(f"wd{b}")], writes=[r_pp[bank]], signal=(fc == 3))
                            if n % 2 == 0:
                                s.op("scalar", lambda h, bank=bank, ob_=ob_, n=n: h.copy(
                                    out=ob[ob_][:, n * 512:(n + 1) * 512], in_=pp[bank][:]),
                                    reads=[r_pp[bank]], writes=[res(f"ob{ob_}")])
                            else:
                                s.op("vector", lambda h, bank=bank, ob_=ob_, n=n: h.tensor_copy(
                                    out=ob[ob_][:, n * 512:(n + 1) * 512], in_=pp[bank][:]),
                                    reads=[r_pp[bank]], writes=[res(f"ob{ob_}")])
                        s.dma("sync", lambda h, ob_=ob_, r0=r0, sbk=sbk, e=e: h.dma_start(out=(obuf if e < NE else obuf_s)[r0 + sbk * 128:r0 + (sbk + 1) * 128, :], in_=ob[ob_][:]),
                              reads=[res(f"ob{ob_}")])

                emit_loadx(0)
                for idx in range(len(passes)):
                    emit_gu(idx)
                    if idx + 1 < len(passes):
                        emit_loadx(idx + 1)
                    emit_down(idx)
                s.emit()

        def combine_phase():
            with ExitStack() as ph:
                def sbp(name, shape, dt=F32):
                    return ph.enter_context(nc.sbuf_tensor(f"{name}_c", list(shape), dt))
                gk = [sbp(f"gk{i}", [128, D], BF16) for i in range(9)]
                accA = sbp("accA", [128, D]); accB = sbp("accB", [128, D])
                yt = sbp("yt", [128, D]); G2 = [sbp("G2a", [128, D]), sbp("G2b", [128, D])]
                st = sbp("st", [128, 4])
                for k in range(9):
                    s.op("gpsimd", lambda h, k=k: h.memset(gk[k][:], 0.0), writes=[res(f"gk{k}")])
                for p in range(2):
                    s.dma("sync", lambda h, p=p: h.dma_start(out=G2[p][:], in_=modsp[10 + p]), writes=[res(f"G2{p}")])
                for gi in range(16):
                    p, i = gi // 8, gi % 8
                    for k in range(8):
                        s.dma("gpsimd", lambda h, k=k, gi=gi: h.indirect_dma_start(
                            out=gk[k][:], out_offset=None, in_=obuf[:, :],
                            in_offset=bass.IndirectOffsetOnAxis(ap=dstall[:, gi, k:k + 1], axis=0),
                            bounds_check=None),
                            reads=[res("dstall")], writes=[res(f"gk{k}")])
                    s.dma("sync", lambda h, gi=gi: h.dma_start(out=gk[8][:], in_=obuf_s[gi * 128:(gi + 1) * 128, :]),
                          writes=[res("gk8")])
                    s.dma("sync", lambda h, gi=gi: h.dma_start(out=yt[:], in_=ysp[gi * 128:(gi + 1) * 128, :]), writes=[res("ytc")])
                    s.op("vector", lambda h, gi=gi: h.scalar_tensor_tensor(out=accA[:], in0=gk[0][:], scalar=gall[:, gi, 0:1], in1=gk[8][:], op0=ALU.mult, op1=ALU.add),
                         reads=[res("gk8"), res("gk0"), res("gall")], writes=[res("accA")])
                    for k in range(1, 8):
                        s.op("vector", lambda h, k=k, gi=gi: h.scalar_tensor_tensor(out=accA[:], in0=gk[k][:], scalar=gall[:, gi, k:k + 1], in1=accA[:], op0=ALU.mult, op1=ALU.add),
                             reads=[res("accA"), res(f"gk{k}"), res("gall")], writes=[res("accA")])
                    if DEBUG:
                        s.dma("sync", lambda h, gi=gi: h.dma_start(out=dbg_moe[gi * 128:(gi + 1) * 128, :], in_=accA[:]), reads=[res("accA")])
                    s.op("scalar", lambda h: h.activation(out=accB[:], in_=accA[:], func=AF.Square, accum_out=st[:, 0:1]),
                         reads=[res("accA")], writes=[res("accB"), res("stc")])
                    rstd_chain(st[:, 0:1], st[:, 1:2], D, res("stc"), res("stc"))
                    s.op("vector", lambda h, p=p: h.scalar_tensor_tensor(out=accB[:], in0=accA[:], scalar=st[:, 1:2], in1=G2[p][:], op0=ALU.mult, op1=ALU.mult),
                         reads=[res("accA"), res("stc"), res(f"G2{p}")], writes=[res("accB")])
                    s.op("gpsimd", lambda h: h.tensor_tensor(out=accB[:], in0=accB[:], in1=yt[:], op=ALU.add), reads=[res("accB"), res("ytc")], writes=[res("accB")])
                    s.dma("sync", lambda h, p=p, i=i: h.dma_start(out=youts[p][i * 128:(i + 1) * 128, :], in_=accB[:]),
                          reads=[res("accB")])
                s.emit()

        if DEBUG:
            dbg_dst = dscr("dbg_dst", [128, 16 * 8], I32)
            dbg_gate = dscr("dbg_gate", [128, 16 * 8])
            dbg_moe = dscr("dbg_moe", [2048, D])
            dbg_sc = dscr("dbg_sc", [16, 128, NE])
        for p in range(2):
            if STAGE >= 1:
                qkv_phase(p)
            if STAGE >= 2:
                with nc.sbuf_tensor(f"OT{p}", [128, 16, 1024], BF16) as OT:
                    attn_phase(p, OT)
                    if STAGE >= 3:
                        post_phase(p, OT)
        if STAGE >= 8:
            expert_phase()
            combine_phase()
        if DEBUG and STAGE >= 3:
            s.dma("sync", lambda h: h.dma_start(out=dbg_dst[:, :], in_=dstall[:].rearrange("p a b -> p (a b)")), reads=[res("dstall")])
            s.dma("sync", lambda h: h.dma_start(out=dbg_gate[:, :], in_=gall[:].rearrange("p a b -> p (a b)")), reads=[res("gall")])
        s.final_wait("sync", list(R.values()))
        s.emit()
    return nc


def _rope_tables():
    n = 2048
    rows = n // 64
    row = np.repeat(np.arange(rows, dtype=np.float32), 64)
    col = np.tile(np.arange(64, dtype=np.float32), rows)
    inv_freq = (10000.0 ** (-np.arange(0, 64, 2, dtype=np.float32) / 64)).astype(np.float32)
    ang_r = row[:, None] * inv_freq
    ang_c = col[:, None] * inv_freq
    ang = np.concatenate([ang_r, ang_r, ang_c, ang_c], axis=-1).astype(np.float32)
    cos = np.cos(ang).astype(np.float32)
    sin = np.sin(ang).astype(np.float32)
    sgn = np.ones(128, np.float32)
    sgn[0:32] = -1.0
    sgn[64:96] = -1.0
    return cos, sin * sgn[None, :]


def _consts(half):
    c = np.zeros((128, 7 * 128), np.float32)
    j = np.arange(128)[:, None]
    r = np.arange(128)[None, :]
    c[:, 0:128] = np.eye(128, dtype=np.float32)
    c[:, 128:256] = (j < r).astype(np.float32)
    c[:, 256:384] = 1.0
    band_prev = (j >= r).astype(np.float32)
    band_next = (j <= r).astype(np.float32)
    c[:, 384:512] = band_prev
    c[:, 512:640] = band_next
    c[:, 640:768] = band_prev if half == 1 else 0.0
    c[:, 768:896] = band_next if half == 0 else 0.0
    return c


def _local_order(half):
    own = np.arange(half * 1024, (half + 1) * 1024)
    if half == 0:
        other = np.arange(1024, 2048)
    else:
        other = np.concatenate([np.arange(896, 1024), np.arange(0, 896)])
    return np.concatenate([own, other])


_NC_CACHE = {}


def kernel(x_prompt, x_sample, cache_glob_k, cache_glob_v, cache_win_k, cache_win_v, c, c_ctx,
           w_ada, b_ada, attn_pre_g, attn_post_g, w_in, q_norm_g, k_norm_g, sink_logit, w_out,
           ffn_pre_g, ffn_post_g, w_router, router_bias, w_gate_e, w_up_e, w_down_e,
           w_gate_s, w_up_s, w_down_s):
    f = lambda a: np.ascontiguousarray(np.asarray(a, dtype=np.float32))
    x_prompt, x_sample = f(x_prompt), f(x_sample)
    cos, sins = _rope_tables()
    shared = {
        "w_ada": f(w_ada)[0], "b_ada": f(b_ada)[0][None, :],
        "gains": np.stack([f(attn_pre_g)[0], f(attn_post_g)[0], f(ffn_pre_g)[0], f(ffn_post_g)[0]]),
        "w_in": f(w_in)[0], "qkg": np.stack([f(q_norm_g)[0], f(k_norm_g)[0]]),
        "sink": f(sink_logit)[0][None, :], "w_out": f(w_out)[0], "w_router": f(w_router)[0],
        "rbias": f(router_bias)[0][None, :], "wge": f(w_gate_e)[0], "wue": f(w_up_e)[0], "wde": f(w_down_e)[0],
        "wgs": f(w_gate_s)[0], "wus": f(w_up_s)[0], "wds": f(w_down_s)[0],
    }
    caches = [f(cache_glob_k), f(cache_glob_v), f(cache_win_k), f(cache_win_v)]
    in_maps = []
    for core in range(NCORES):
        b, half = core // 2, core % 2
        order = _local_order(half)
        m = dict(shared)
        m["xc"] = x_prompt[4 * core:4 * core + 4].reshape(1024, D)
        m["xl"] = np.ascontiguousarray(x_sample[b][order])
        m["ropec"] = np.ascontiguousarray(cos[order])
        m["ropes"] = np.ascontiguousarray(sins[order])
        m["cache"] = np.stack([cc[b, 0].reshape(256, 256) for cc in caches])
        m["cond"] = np.stack([f(c_ctx), f(c)[b]])
        m["consts"] = _consts(half)
        in_maps.append(m)
    if "nc" not in _NC_CACHE:
        del INPUT_NAMES[:]
        _NC_CACHE["nc"] = build()
    nc = _NC_CACHE["nc"]
    in_maps = [{k: v for k, v in m.items() if k in INPUT_NAMES} for m in in_maps]
    r = run_bass_kernel_spmd(nc, in_maps, core_ids=list(range(NCORES))).results
    if DEBUG:
        _NC_CACHE["raw"] = r
    y_p = np.concatenate([r[i]["yc"].reshape(4, 256, D) for i in range(NCORES)], axis=0)
    y_s = np.stack([np.concatenate([r[2 * b]["yl"], r[2 * b + 1]["yl"]], axis=0) for b in range(4)])
    def kv(name):
        return np.concatenate([r[i][name].reshape(4, 1, 256, 2, 128) for i in range(NCORES)], axis=0)
    return (y_p.astype(np.float32), y_s.astype(np.float32), kv("ngk"), kv("ngv"), kv("nwk"), kv("nwv"))
```

```python
import numpy as np
from contextlib import ExitStack
import concourse.bass as bass
import concourse.mybir as mybir
from concourse.bass_utils import run_bass_kernel_spmd

F32 = mybir.dt.float32
BF16 = mybir.dt.bfloat16
I32 = mybir.dt.int32
U32 = mybir.dt.uint32
AF = mybir.ActivationFunctionType
ALU = mybir.AluOpType
AX = mybir.AxisListType

D = 2048
NCORES = 8
EPS = 1e-6
HD = 128
SCALE = HD ** -0.5
NE = 64
CAP = 1024
NSLOT = NE * CAP + 2048
STAGE = 99
DEBUG = False
SKIP_INPUTS = set()
INPUT_NAMES = []


class _Eng:
    def __init__(self, key):
        self.key = key
        self.sem = None
        self.count = 0
        self.thunks = []
        self.waited = {}


class Res:
    __slots__ = ("name", "w", "r")

    def __init__(self, name):
        self.name = name
        self.w = None
        self.r = []


class Sched:
    def __init__(self, nc, n_dma_sems=24):
        self.nc = nc
        self.eng = {k: _Eng(k) for k in ("tensor", "vector", "scalar", "gpsimd", "sync")}
        self.n_dma_sems = n_dma_sems
        self.dma_sems = {}
        self.dma_rr = {}
        self.sems = {}
        self.phase_id = 0

    def alloc_sems(self, stack):
        for k, e in self.eng.items():
            e.sem = stack.enter_context(self.nc.semaphore("s_" + k))
            self.sems[("e", k)] = e.sem
        for q in ("sync", "gpsimd"):
            lst = []
            for i in range(self.n_dma_sems):
                s = stack.enter_context(self.nc.semaphore(f"d_{q}_{i}"))
                self.sems[("d", q, i)] = s
                lst.append([("d", q, i), 0])
            self.dma_sems[q] = lst
            self.dma_rr[q] = 0

    def _deps(self, reads, writes):
        deps = []
        for r in reads:
            if r.w is not None:
                deps.append(r.w)
        for w in writes:
            if w.w is not None:
                deps.append(w.w)
            deps.extend(w.r)
        return deps

    def _waits(self, e, deps, skip_self=False):
        need = {}
        for src, val in deps:
            if skip_self and src == ("e", e.key):
                continue
            if e.waited.get(src, 0) >= val:
                continue
            if need.get(src, 0) < val:
                need[src] = val
        for src, val in need.items():
            e.waited[src] = val
        return list(need.items())

    def op(self, engine, fn, reads=(), writes=(), signal=True):
        e = self.eng[engine]
        waits = self._waits(e, self._deps(reads, writes), skip_self=(engine == "tensor"))
        if signal:
            e.count += 1
            tok = (("e", engine), e.count)
            for r in reads:
                r.r.append(tok)
            for w in writes:
                w.w = tok
                w.r = []
        sems = self.sems

        def thunk(h, waits=waits, fn=fn, signal=signal, sem=e.sem):
            for src, val in waits:
                h.wait_ge(sems[src], val)
            ins = fn(h)
            if signal:
                ins.then_inc(sem, 1)
        e.thunks.append(thunk)

    def dma(self, queue, fn, reads=(), writes=()):
        e = self.eng[queue]
        lst = self.dma_sems[queue]
        i = self.dma_rr[queue]
        self.dma_rr[queue] = (i + 1) % len(lst)
        slot = lst[i]
        deps = self._deps(reads, writes)
        if slot[1] > 0:
            deps.append((slot[0], slot[1]))
        waits = self._waits(e, deps)
        slot[1] += 16
        tok = (slot[0], slot[1])
        for r in reads:
            r.r.append(tok)
        for w in writes:
            w.w = tok
            w.r = []
        sems = self.sems

        def thunk(h, waits=waits, fn=fn, sem=sems[slot[0]]):
            for src, val in waits:
                h.wait_ge(sems[src], val)
            fn(h).then_inc(sem, 16)
        e.thunks.append(thunk)

    def final_wait(self, engine, resources):
        e = self.eng[engine]
        deps = [r.w for r in resources if r.w is not None]
        waits = self._waits(e, deps)
        sems = self.sems

        def thunk(h, waits=waits):
            for src, val in waits:
                h.wait_ge(sems[src], val)
        e.thunks.append(thunk)

    def drain(self):
        e = self.eng["sync"]
        deps = []
        for q, lst in self.dma_sems.items():
            for slot in lst:
                if slot[1] > 0:
                    deps.append((slot[0], slot[1]))
        for k, e2 in self.eng.items():
            if e2.count > 0 and k != "sync":
                deps.append((("e", k), e2.count))
        waits = self._waits(e, deps)
        sems = self.sems

        def thunk(h, waits=waits):
            for src, val in waits:
                h.wait_ge(sems[src], val)
        e.thunks.append(thunk)

    def emit(self):
        self.drain()
        self.phase_id = getattr(self, "phase_id", 0) + 1
        with self.nc.Block() as block:
            for k, e in self.eng.items():
                if not e.thunks:
                    continue

                def body(h, thunks=list(e.thunks)):
                    for t in thunks:
                        t(h)
                getattr(block, k)(body)
        for e in self.eng.values():
            e.thunks = []


def build():
    nc = bass.Bass("TRN2", target_bir_lowering=False)

    def din(name, shape, dt=F32):
        if name in SKIP_INPUTS:
            return None
        INPUT_NAMES.append(name)
        return nc.dram_tensor(name, list(shape), dt, kind="ExternalInput").ap()

    def dout(name, shape, dt=F32):
        return nc.dram_tensor(name, list(shape), dt, kind="ExternalOutput").ap()

    def dscr(name, shape, dt=F32):
        kind = "ExternalOutput" if (DEBUG and name in ("ysp", "dbg_dst", "dbg_gate", "dbg_moe", "dbg_sc")) else "Internal"
        return nc.dram_tensor(name, list(shape), dt, kind=kind).ap()

    xc = din("xc", [1024, D])
    xl = din("xl", [2048, D])
    ropec = din("ropec", [2048, 128])
    ropes = din("ropes", [2048, 128])
    cache = din("cache", [4, 256, 256])
    cond = din("cond", [2, D])
    w_ada = din("w_ada", [D, 6 * D])
    b_ada = din("b_ada", [1, 6 * D])
    gains = din("gains", [4, D])
    w_in = din("w_in", [D, 3072])
    qkg = din("qkg", [2, 128])
    sink = din("sink", [1, 8])
    w_out = din("w_out", [D, D])
    w_router = din("w_router", [D, NE])
    rbias = din("rbias", [1, NE])
    wge = din("wge", [NE, D, 512])
    wue = din("wue", [NE, D, 512])
    wde = din("wde", [NE, 512, D])
    wgs = din("wgs", [D, 512])
    wus = din("wus", [D, 512])
    wds = din("wds", [512, D])
    consts = din("consts", [128, 7 * 128])

    yc = dout("yc", [1024, D])
    yl = dout("yl", [1024, D])
    ngk = dout("ngk", [1024, 256])
    ngv = dout("ngv", [1024, 256])
    nwk = dout("nwk", [1024, 256])
    nwv = dout("nwv", [1024, 256])

    modsp = dscr("modsp", [12, 128, D])

    R = {}

    def res(name):
        if name not in R:
            R[name] = Res(name)
        return R[name]

    with ExitStack() as st:
        s = Sched(nc)
        s.alloc_sems(st)
        st.enter_context(nc.allow_non_contiguous_dma(reason="small strided loads"))
        st.enter_context(nc.allow_low_precision(reason="bf16 matmul operands"))

        def sb(name, shape, dt=F32):
            return st.enter_context(nc.sbuf_tensor(name, list(shape), dt))

        def ps(name, shape, dt=F32):
            return st.enter_context(nc.psum_tensor(name, list(shape), dt))

        pp = [ps(f"pp{i}", [128, 512], F32) for i in range(6)]
        pt = [ps(f"pt{i}", [128, 1024], BF16) for i in range(2)]
        r_pp = [res(f"pp{i}") for i in range(6)]
        r_pt = [res(f"pt{i}") for i in range(2)]

        cf = sb("cf", [128, 7 * 128], F32)
        cb = sb("cb", [128, 7 * 128], BF16)
        s.dma("sync", lambda h: h.dma_start(out=cf[:], in_=consts[:, :]), writes=[res("cf")])
        s.op("vector", lambda h: h.tensor_copy(out=cb[:], in_=cf[:]), reads=[res("cf")], writes=[res("cb")])
        identb = cb[:, 0:128]
        onesb = cb[:, 256:384]

        ph0 = ExitStack()

        def sb0(name, shape, dt=F32):
            return ph0.enter_context(nc.sbuf_tensor(name, list(shape), dt))
        condT = sb0("condT", [128, 16, 2], F32)
        crep = sb0("crep", [128, 2, 16, 128], BF16)
        gbc = sb0("gbc", [128, 4, D], F32)
        for p in range(2):
            s.dma("sync", lambda h, p=p: h.dma_start(
                out=condT[:, :, p], in_=cond[p:p + 1, :].rearrange("r (c p) -> p (r c)", p=128)),
                writes=[res("condT")])
        s.dma("sync", lambda h: h.dma_start(out=gbc[:], in_=gains.partition_broadcast(128)), writes=[res("gbc")])
        for p in range(2):
            s.op("scalar", lambda h, p=p: h.activation(
                out=crep[:, p, :, :], in_=condT[:, :, p:p + 1].broadcast_to([128, 16, 128]), func=AF.Silu),
                reads=[res("condT")], writes=[res("crep")])
        wa = [sb0(f"wa{i}", [128, 16, 512], BF16) for i in range(2)]
        bb = [sb0(f"bb{i}", [128, 512], F32) for i in range(2)]
        mt = [sb0(f"mt{i}", [128, 512], F32) for i in range(4)]
        gain_of = {1: 0, 2: 1, 4: 2, 5: 3}
        n_mt = 0
        for j in range(24):
            which, cc = j // 4, j % 4
            b = j % 2
            s.dma("gpsimd", lambda h, b=b, j=j: h.dma_start(
                out=wa[b][:], in_=w_ada[:, j * 512:(j + 1) * 512].rearrange("(c p) n -> p c n", p=128)),
                writes=[res(f"wa{b}")])
            s.dma("sync", lambda h, b=b, j=j: h.dma_start(
                out=bb[b][:], in_=b_ada[:, j * 512:(j + 1) * 512].partition_broadcast(128)),
                writes=[res(f"bb{b}")])
            for p in range(2):
                pi = (j * 2 + p) % 6
                for k in range(16):
                    s.op("tensor", lambda h, p=p, k=k, b=b, pi=pi: h.matmul(
                        pp[pi][:], lhsT=crep[:, p, k, :], rhs=wa[b][:, k, :], start=(k == 0), stop=(k == 15)),
                        reads=[res("crep"), res(f"wa{b}")], writes=[r_pp[pi]], signal=(k == 15))
                m = mt[n_mt % 4]
                rm = res(f"mt{n_mt % 4}")
                n_mt += 1
                if which in (0, 3):
                    s.op("vector", lambda h, m=m, pi=pi, b=b: h.tensor_tensor(
                        out=m[:], in0=pp[pi][:], in1=bb[b][:], op=ALU.add),
                        reads=[r_pp[pi], res(f"bb{b}")], writes=[rm])
                else:
                    gsl = gbc[:, gain_of[which], cc * 512:(cc + 1) * 512]
                    s.op("vector", lambda h, m=m, pi=pi, b=b: h.tensor_tensor(
                        out=m[:], in0=pp[pi][:], in1=bb[b][:], op=ALU.add),
                        reads=[r_pp[pi], res(f"bb{b}")], writes=[rm])
                    add1 = 1.0 if which in (1, 4) else 0.0
                    s.op("vector", lambda h, m=m, gsl=gsl, add1=add1: h.scalar_tensor_tensor(
                        out=m[:], in0=m[:], scalar=add1, in1=gsl, op0=ALU.add, op1=ALU.mult),
                        reads=[rm, res("gbc")], writes=[rm])
                idx = which * 2 + p
                s.dma("sync", lambda h, m=m, idx=idx, cc=cc: h.dma_start(
                    out=modsp[idx, :, cc * 512:(cc + 1) * 512], in_=m[:]),
                    reads=[rm])

        s.emit()
        ph0.close()

        qsp = [dscr("qsp0", [1024, 2560], BF16), dscr("qsp1", [2048, 2560], BF16)]
        vsp = [dscr("vsp0", [1024, 512], BF16), dscr("vsp1", [2048, 512], BF16)]
        ysp = dscr("ysp", [2048, D])
        xbuf = dscr("xbuf", [NE * CAP, D], BF16)
        xbuf_s = dscr("xbuf_s", [2048, D], BF16)
        obuf_s = dscr("obuf_s", [2048, D], BF16)
        obuf = dscr("obuf", [NE * CAP, D], BF16)
        xin = [xc, xl]
        youts = [yc, yl]
        identf = cf[:, 0:128]
        onesf = cf[:, 256:384]
        ustrb = cb[:, 128:256]
        _bc = {}

        def bc_reg(h):
            if _bc.get("phase") != s.phase_id:
                _bc["r"] = h.to_reg(NE * CAP - 1)
                _bc["phase"] = s.phase_id
            return _bc["r"]

        mx = sb("mx", [128, 2, 4], F32)
        negm = sb("negm", [128, 2, 2], F32)
        Mall = sb("Mall", [128, 16, NE], BF16)
        dstall = sb("dstall", [128, 16, 8], I32)
        gall = sb("gall", [128, 16, 8], F32)
        NHOT = 8
        hotidx = sb("hotidx", [128, NHOT, 24], I32)
        sinkb = sb("sinkb", [128, 8], F32)
        g10 = sb("g10", [128, 10, 128], F32)
        rbb = sb("rbb", [128, NE], F32)
        s.op("vector", lambda h: h.memset(mx[:], 0.0), writes=[res("mx")])
        s.dma("sync", lambda h: h.dma_start(out=sinkb[:], in_=sink.partition_broadcast(128)), writes=[res("sinkb")])
        s.dma("sync", lambda h: h.dma_start(out=rbb[:], in_=rbias.partition_broadcast(128)), writes=[res("rbb")])
        for hh in range(10):
            s.dma("sync", lambda h, hh=hh: h.dma_start(
                out=g10[:, hh, :], in_=qkg[(0 if hh < 8 else 1):(1 if hh < 8 else 2), :].partition_broadcast(128)),
                writes=[res("g10")])

        def rstd_chain(ssq_ap, rs_ap, n, r_in, r_out):
            s.op("vector", lambda h: h.tensor_scalar(out=rs_ap, in0=ssq_ap, scalar1=1.0 / n, scalar2=EPS,
                                                     op0=ALU.mult, op1=ALU.add), reads=[r_in], writes=[r_out])
            s.op("scalar", lambda h: h.activation(out=rs_ap, in_=rs_ap, func=AF.Sqrt), reads=[r_out], writes=[r_out])
            s.op("vector", lambda h: h.reciprocal(out=rs_ap, in_=rs_ap), reads=[r_out], writes=[r_out])

        def transpose16(src_bf, r_src, dst, r_dst, dst_slices):
            for half in range(2):
                for c in range(8):
                    cc = half * 8 + c
                    s.op("tensor", lambda h, half=half, c=c, cc=cc: h.transpose(
                        out=pt[half][:, c * 128:(c + 1) * 128], in_=src_bf[:, cc * 128:(cc + 1) * 128], identity=identb),
                        reads=[r_src, res("cb")], writes=[r_pt[half]], signal=(c == 7))
                eng = "scalar" if half == 0 else "vector"
                o = dst_slices(half)
                i_ = pt[half][:, :].rearrange("p (c t) -> p c t", t=128)
                if eng == "scalar":
                    s.op("scalar", lambda h, o=o, i_=i_: h.copy(out=o, in_=i_), reads=[r_pt[half]], writes=[r_dst])
                else:
                    s.op("vector", lambda h, o=o, i_=i_: h.tensor_copy(out=o, in_=i_), reads=[r_pt[half]], writes=[r_dst])

        def qkv_phase(p):
            nt = 8 if p == 0 else 16
            with ExitStack() as ph:
                def sbp(name, shape, dt=F32):
                    return ph.enter_context(nc.sbuf_tensor(f"{name}_{p}", list(shape), dt))
                win = sbp("win", [128, 16, 3072], BF16)
                A1 = sbp("A1", [128, D]); B1 = sbp("B1", [128, D])
                xt = [sbp(f"xt{i}", [128, D]) for i in range(2)]
                hb = sbp("hb", [128, D], BF16)
                hT = sbp("hT", [128, 16, 128], BF16)
                qkall = sbp("qkall", [128, 3072])
                qk20 = sbp("qk20", [128, 20, 128])
                t1 = sbp("t1", [128, 20, 128]); t2 = sbp("t2", [128, 20, 128])
                qkb = sbp("qkb", [128, 20, 128], BF16)
                vb = sbp("vb", [128, 512], BF16)
                rc = sbp("rc", [128, 128]); rs_ = sbp("rs", [128, 128])
                st1 = sbp("st1", [128, 8]); st10 = sbp("st10", [128, 10]); st20 = sbp("st20", [128, 20]); g4 = sbp("g4", [128, 4])
                for n in range(6):
                    s.dma("gpsimd", lambda h, n=n: h.dma_start(
                        out=win[:, :, n * 512:(n + 1) * 512],
                        in_=w_in[:, n * 512:(n + 1) * 512].rearrange("(c p) n -> p c n", p=128)), writes=[res("win")])
                s.dma("sync", lambda h: h.dma_start(out=A1[:], in_=modsp[2 + p]), writes=[res("A1")])
                s.dma("sync", lambda h: h.dma_start(out=B1[:], in_=modsp[0 + p]), writes=[res("B1")])
                for i in range(nt):
                    own = i < 8
                    b = i % 2
                    rx = res(f"xt{b}")
                    s.dma("sync", lambda h, b=b, i=i: h.dma_start(out=xt[b][:], in_=xin[p][i * 128:(i + 1) * 128, :]), writes=[rx])
                    s.op("scalar", lambda h, b=b: h.activation(out=t1[:].rearrange("p a b -> p (a b)")[:, 0:D], in_=xt[b][:],
                                                               func=AF.Square, accum_out=st1[:, 0:1]),
                         reads=[rx], writes=[res("t1"), res("st1")])
                    rstd_chain(st1[:, 0:1], st1[:, 1:2], D, res("st1"), res("st1b"))
                    t1f = t1[:].rearrange("p a b -> p (a b)")[:, 0:D]
                    s.op("vector", lambda h, b=b: h.scalar_tensor_tensor(out=t1f, in0=xt[b][:], scalar=st1[:, 1:2], in1=A1[:],
                                                                         op0=ALU.mult, op1=ALU.mult),
                         reads=[rx, res("st1b"), res("A1")], writes=[res("t1")])
                    s.op("gpsimd", lambda h: h.tensor_tensor(out=hb[:], in0=t1f, in1=B1[:], op=ALU.add),
                         reads=[res("t1"), res("B1")], writes=[res("hb")])
                    transpose16(hb, res("hb"), hT, res("hT"), lambda half: hT[:, half * 8:(half + 1) * 8, :])
                    chunks = range(6) if own else (2, 5)
                    for n in chunks:
                        for k in range(16):
                            s.op("tensor", lambda h, n=n, k=k: h.matmul(pp[n][:], lhsT=hT[:, k, :], rhs=win[:, k, n * 512:(n + 1) * 512],
                                                                        start=(k == 0), stop=(k == 15)),
                                 reads=[res("hT"), res("win")], writes=[r_pp[n]], signal=(k == 15))
                        eng = "scalar" if n % 2 == 0 else "vector"
                        if eng == "scalar":
                            s.op("scalar", lambda h, n=n: h.copy(out=qkall[:, n * 512:(n + 1) * 512], in_=pp[n][:]),
                                 reads=[r_pp[n]], writes=[res("qkall")])
                        else:
                            s.op("vector", lambda h, n=n: h.tensor_copy(out=qkall[:, n * 512:(n + 1) * 512], in_=pp[n][:]),
                                 reads=[r_pp[n]], writes=[res("qkall")])
                    h0 = 0 if own else 8
                    nn = 10 - h0
                    src_n = qkall[:, h0 * 128:1280].rearrange("p (a b) -> p a b", b=128)
                    s.op("scalar", lambda h, src_n=src_n, h0=h0: h.activation(out=t2[:, h0:10, :], in_=src_n, func=AF.Square),
                         reads=[res("qkall")], writes=[res("t2")])
                    s.op("vector", lambda h, h0=h0: h.tensor_reduce(out=st10[:, h0:10], in_=t2[:, h0:10, :], axis=AX.X, op=ALU.add),
                         reads=[res("t2")], writes=[res("st10")])
                    rstd_chain(st10[:, h0:10], st10[:, h0:10], 128, res("st10"), res("st10"))
                    s.op("vector", lambda h, src_n=src_n, h0=h0, nn=nn: h.tensor_tensor(
                        out=qk20[:, h0:10, :], in0=src_n, in1=st10[:, h0:10].unsqueeze(2).broadcast_to([128, nn, 128]), op=ALU.mult),
                        reads=[res("qkall"), res("st10")], writes=[res("qk20")])
                    s.op("vector", lambda h, h0=h0: h.tensor_tensor(out=qk20[:, h0:10, :], in0=qk20[:, h0:10, :], in1=g10[:, h0:10, :], op=ALU.mult),
                         reads=[res("qk20"), res("g10")], writes=[res("qk20")])
                    w0 = 10 if own else 18
                    c0 = 1536 + (w0 - 10) * 128
                    s.op("scalar", lambda h, w0=w0, c0=c0: h.copy(out=qk20[:, w0:20, :], in_=qkall[:, c0:2816].rearrange("p (a b) -> p a b", b=128)),
                         reads=[res("qkall")], writes=[res("qk20")])
                    rows = slice(i * 128, (i + 1) * 128)
                    if p == 0:
                        for (dst, src_ap) in ((ngk, qk20[:, 8:10, :].rearrange("p a b -> p (a b)")), (ngv, qkall[:, 1280:1536]),
                                              (nwk, qkall[:, 2560:2816]), (nwv, qkall[:, 2816:3072])):
                            s.dma("sync", lambda h, dst=dst, src_ap=src_ap, rows=rows: h.dma_start(out=dst[rows, :], in_=src_ap),
                                  reads=[res("qk20"), res("qkall")])
                        s.op("scalar", lambda h: h.copy(out=qkb[:], in_=qk20[:]), reads=[res("qk20")], writes=[res("qkb")])
                    else:
                        s.dma("sync", lambda h, rows=rows: h.dma_start(out=rc[:], in_=ropec[rows, :]), writes=[res("rc")])
                        s.dma("sync", lambda h, rows=rows: h.dma_start(out=rs_[:], in_=ropes[rows, :]), writes=[res("rs")])
                        groups = [(0, 20)] if own else [(8, 10), (18, 20)]
                        for (a, bnd) in groups:
                            nh = bnd - a
                            s.op("vector", lambda h, a=a, bnd=bnd, nh=nh: h.tensor_tensor(
                                out=t1[:, a:bnd, :], in0=qk20[:, a:bnd, :], in1=rc[:].unsqueeze(1).broadcast_to([128, nh, 128]), op=ALU.mult),
                                reads=[res("qk20"), res("rc")], writes=[res("t1")])
                            for pr in range(2):
                                for hf in range(2):
                                    o_ = t2[:, a:bnd, pr * 64 + hf * 32: pr * 64 + hf * 32 + 32]
                                    i_ = qk20[:, a:bnd, pr * 64 + (1 - hf) * 32: pr * 64 + (1 - hf) * 32 + 32]
                                    sn = rs_[:, pr * 64 + hf * 32: pr * 64 + hf * 32 + 32].unsqueeze(1).broadcast_to([128, nh, 32])
                                    s.op("gpsimd", lambda h, o_=o_, i_=i_, sn=sn: h.tensor_tensor(out=o_, in0=i_, in1=sn, op=ALU.mult),
                                         reads=[res("qk20"), res("rs")], writes=[res("t2")])
                            s.op("vector", lambda h, a=a, bnd=bnd: h.tensor_tensor(out=qkb[:, a:bnd, :], in0=t1[:, a:bnd, :], in1=t2[:, a:bnd, :], op=ALU.add),
                                 reads=[res("t1"), res("t2")], writes=[res("qkb")])
                    hs = [(0, 20)] if own else [(8, 10), (18, 20)]
                    for (a, bnd) in hs:
                        s.op("scalar", lambda h, a=a, bnd=bnd: h.activation(out=t1[:, a:bnd, :], in_=qkb[:, a:bnd, :], func=AF.Square),
                             reads=[res("qkb")], writes=[res("t1")])
                        s.op("vector", lambda h, a=a, bnd=bnd: h.tensor_reduce(out=st20[:, a:bnd], in_=t1[:, a:bnd, :], axis=AX.X, op=ALU.add),
                             reads=[res("t1")], writes=[res("st20")])
                    grp = [(0, 0, 8), (1, 8, 10), (2, 10, 18), (3, 18, 20)] if own else [(1, 8, 10), (3, 18, 20)]
                    for (gi_, a, bnd) in grp:
                        s.op("vector", lambda h, gi_=gi_, a=a, bnd=bnd: h.tensor_reduce(out=g4[:, gi_:gi_ + 1], in_=st20[:, a:bnd], axis=AX.X, op=ALU.max),
                             reads=[res("st20")], writes=[res("g4")])
                        s.op("vector", lambda h, gi_=gi_: h.tensor_tensor(out=mx[:, p, gi_:gi_ + 1], in0=mx[:, p, gi_:gi_ + 1], in1=g4[:, gi_:gi_ + 1], op=ALU.max),
                             reads=[res("g4"), res("mx")], writes=[res("mx")])
                    s.op("scalar", lambda h: h.copy(out=vb[:, 0:256], in_=qkall[:, 1280:1536]), reads=[res("qkall")], writes=[res("vb")])
                    s.op("scalar", lambda h: h.copy(out=vb[:, 256:512], in_=qkall[:, 2816:3072]), reads=[res("qkall")], writes=[res("vb")])
                    s.dma("sync", lambda h, rows=rows: h.dma_start(out=qsp[p][rows, :], in_=qkb[:].rearrange("p a b -> p (a b)")),
                          reads=[res("qkb")])
                    s.dma("sync", lambda h, rows=rows: h.dma_start(out=vsp[p][rows, :], in_=vb[:]),
                          reads=[res("vb")])
                s.emit()

        def attn_phase(p, OT):
            nq_t = 8
            nloc = 8 if p == 0 else 16
            nkt = 8 if p == 0 else 18
            koff = 0 if p == 0 else 2
            with ExitStack() as ph:
                def sbp(name, shape, dt=F32):
                    return ph.enter_context(nc.sbuf_tensor(f"{name}_a{p}", list(shape), dt))
                QT = [sbp("QTg", [128, 8, 1024], BF16), sbp("QTw", [128, 8, 1024], BF16)]
                KT = [sbp("KTg", [128, 2, nkt * 128], BF16), sbp("KTw", [128, 2, nkt * 128], BF16)]
                V = sbp("V", [128, nkt, 512], BF16)
                qt = [sbp(f"qt{i}", [128, 2560], BF16) for i in range(2)]
                pb = [sbp(f"pb{i}", [128, 512], BF16) for i in range(4)]
                rec = [sbp(f"rec{i}", [128, 512]) for i in range(2)]
                SE = sbp("SE", [128, 8])
                m4 = sbp("m4", [128, 4]); dg = sbp("dg", [128, 4]); mb4 = sbp("mb4", [128, 4])
                cft = sbp("cft", [128, 512]); cbt = sbp("cbt", [128, 512], BF16); st4 = sbp("st4", [128, 4])
                if p == 1:
                    for kt in range(2):
                        for which, kind in ((0, 0), (2, 1)):
                            s.dma("sync", lambda h, kt=kt, which=which: h.dma_start(out=cft[:, 0:256], in_=cache[which, kt * 128:(kt + 1) * 128, :]),
                                  writes=[res("cft")])
                            s.op("vector", lambda h: h.tensor_copy(out=cbt[:, 0:256], in_=cft[:, 0:256]), reads=[res("cft")], writes=[res("cbt")])
                            s.op("scalar", lambda h: h.activation(out=cft[:, 256:512], in_=cbt[:, 0:256], func=AF.Square),
                                 reads=[res("cbt")], writes=[res("cft2")])
                            s.op("vector", lambda h: h.tensor_reduce(out=st4[:, 0:2], in_=cft[:, 256:512].rearrange("p (a b) -> p a b", b=128), axis=AX.X, op=ALU.add),
                                 reads=[res("cft2")], writes=[res("st4")])
                            s.op("vector", lambda h: h.tensor_reduce(out=st4[:, 2:3], in_=st4[:, 0:2], axis=AX.X, op=ALU.max),
                                 reads=[res("st4")], writes=[res("st4")])
                            col = 1 if kind == 0 else 3
                            s.op("vector", lambda h, col=col: h.tensor_tensor(out=mx[:, 1, col:col + 1], in0=mx[:, 1, col:col + 1], in1=st4[:, 2:3], op=ALU.max),
                                 reads=[res("st4"), res("mx")], writes=[res("mx")])
                            for n in range(2):
                                s.op("tensor", lambda h, n=n: h.transpose(out=pt[0][:, n * 128:(n + 1) * 128], in_=cbt[:, n * 128:(n + 1) * 128], identity=identb),
                                     reads=[res("cbt"), res("cb")], writes=[r_pt[0]], signal=(n == 1))
                            s.op("vector", lambda h, kt=kt, kind=kind: h.tensor_copy(
                                out=KT[kind][:, :, kt * 128:(kt + 1) * 128], in_=pt[0][:, 0:256].rearrange("p (a b) -> p a b", b=128)),
                                reads=[r_pt[0]], writes=[res(f"KT{kind}")])
                        for which, off in ((1, 0), (3, 256)):
                            s.dma("sync", lambda h, kt=kt, which=which: h.dma_start(out=cft[:, 0:256], in_=cache[which, kt * 128:(kt + 1) * 128, :]),
                                  writes=[res("cft")])
                            s.op("vector", lambda h, kt=kt, off=off: h.tensor_copy(out=V[:, kt, off:off + 256], in_=cft[:, 0:256]),
                                 reads=[res("cft")], writes=[res("V")])
                for i in range(nloc):
                    b = i % 2
                    rows = slice(i * 128, (i + 1) * 128)
                    s.dma("sync", lambda h, b=b, rows=rows: h.dma_start(out=qt[b][:], in_=qsp[p][rows, :]),
                          writes=[res(f"qt{b}")])
                    s.dma("sync", lambda h, i=i, rows=rows: h.dma_start(out=V[:, koff + i, :], in_=vsp[p][rows, :]),
                          writes=[res("V")])
                    if i < 8:
                        for kind in range(2):
                            c0 = 0 if kind == 0 else 1280
                            for hh in range(8):
                                s.op("tensor", lambda h, b=b, hh=hh, c0=c0, kind=kind: h.transpose(
                                    out=pt[kind][:, hh * 128:(hh + 1) * 128], in_=qt[b][:, c0 + hh * 128:c0 + (hh + 1) * 128], identity=identb),
                                    reads=[res(f"qt{b}"), res("cb")], writes=[r_pt[kind]], signal=(hh == 7))
                            o_ = QT[kind][:, :, i * 128:(i + 1) * 128]
                            i_ = pt[kind][:, :].rearrange("p (a b) -> p a b", b=128)
                            if kind == 0:
                                s.op("scalar", lambda h, o_=o_, i_=i_: h.copy(out=o_, in_=i_), reads=[r_pt[kind]], writes=[res(f"QT{kind}")])
                            else:
                                s.op("vector", lambda h, o_=o_, i_=i_: h.tensor_copy(out=o_, in_=i_), reads=[r_pt[kind]], writes=[res(f"QT{kind}")])
                    for kind in range(2):
                        c0 = 1024 if kind == 0 else 2304
                        for n in range(2):
                            s.op("tensor", lambda h, b=b, n=n, c0=c0, kind=kind: h.transpose(
                                out=pt[kind][:, n * 128:(n + 1) * 128], in_=qt[b][:, c0 + n * 128:c0 + (n + 1) * 128], identity=identb),
                                reads=[res(f"qt{b}"), res("cb")], writes=[r_pt[kind]], signal=(n == 1))
                        kk = koff + i
                        s.op("vector", lambda h, kk=kk, kind=kind: h.tensor_copy(
                            out=KT[kind][:, :, kk * 128:(kk + 1) * 128], in_=pt[kind][:, 0:256].rearrange("p (a b) -> p a b", b=128)),
                            reads=[r_pt[kind]], writes=[res(f"KT{kind}")])
                s.op("tensor", lambda h: h.transpose(out=pp[0][0:4, 0:128], in_=mx[:, p, :], identity=identf),
                     reads=[res("mx"), res("cf")], writes=[r_pp[0]])
                s.op("vector", lambda h: h.tensor_reduce(out=m4[0:4, 0:1], in_=pp[0][0:4, 0:128], axis=AX.X, op=ALU.max),
                     reads=[r_pp[0]], writes=[res("m4")])
                s.op("vector", lambda h: h.tensor_scalar(out=dg[0:4, 0:4], in0=identf[0:4, 0:4], scalar1=m4[0:4, 0:1], scalar2=None, op0=ALU.mult),
                     reads=[res("m4"), res("cf")], writes=[res("dg")])
                s.op("tensor", lambda h: h.matmul(pp[1][:, 0:4], lhsT=onesf[0:4, 0:128], rhs=dg[0:4, 0:4], start=True, stop=True),
                     reads=[res("dg"), res("cf")], writes=[r_pp[1]])
                s.op("vector", lambda h: h.tensor_copy(out=mb4[:], in_=pp[1][:, 0:4]), reads=[r_pp[1]], writes=[res("mb4")])
                for kind in range(2):
                    s.op("vector", lambda h, kind=kind: h.tensor_tensor(out=negm[:, p, kind:kind + 1], in0=mb4[:, 2 * kind:2 * kind + 1],
                                                                        in1=mb4[:, 2 * kind + 1:2 * kind + 2], op=ALU.mult),
                         reads=[res("mb4")], writes=[res("negm")])
                s.op("scalar", lambda h: h.activation(out=negm[:, p, :], in_=negm[:, p, :], func=AF.Sqrt), reads=[res("negm")], writes=[res("negm")])
                s.op("vector", lambda h: h.tensor_scalar(out=negm[:, p, :], in0=negm[:, p, :], scalar1=-SCALE, scalar2=None, op0=ALU.mult),
                     reads=[res("negm")], writes=[res("negm")])
                s.op("scalar", lambda h: h.activation(out=SE[:], in_=sinkb[:], func=AF.Exp, bias=negm[:, p, 1:2], scale=1.0),
                     reads=[res("negm"), res("sinkb")], writes=[res("SE")])

                jobs = []
                if p == 0:
                    for sq in range(4):
                        for kind in range(2):
                            for n in range(2):
                                for qc in range(2):
                                    h0 = 4 * n + 2 * qc
                                    q_ap = QT[kind][:, h0:h0 + 2, sq * 256:(sq + 1) * 256]
                                    keys = [(sq * 2 + kt, None) for kt in range(2)]
                                    o_ap = OT[:, kind * 8 + h0:kind * 8 + h0 + 2, sq * 256:(sq + 1) * 256]
                                    jobs.append((kind, n, q_ap, keys, o_ap, (h0, 2, 256)))
                else:
                    for n in range(2):
                        for qb in range(8):
                            q_ap = QT[0][:, 4 * n:4 * n + 4, qb * 128:(qb + 1) * 128]
                            o_ap = OT[:, 4 * n:4 * n + 4, qb * 128:(qb + 1) * 128]
                            jobs.append((0, n, q_ap, [(kt, None) for kt in range(18)], o_ap, (4 * n, 4, 128)))
                    for n in range(2):
                        for qb in range(8):
                            q_ap = QT[1][:, 4 * n:4 * n + 4, qb * 128:(qb + 1) * 128]
                            o_ap = OT[:, 8 + 4 * n:8 + 4 * n + 4, qb * 128:(qb + 1) * 128]
                            prev = (2 + qb - 1, 384) if qb > 0 else (2 + 8, 640)
                            nxt = (2 + qb + 1, 512) if qb < 7 else (2 + 8, 768)
                            keys = [(0, None), (1, None), prev, (2 + qb, None), nxt]
                            jobs.append((1, n, q_ap, keys, o_ap, (4 * n, 4, 128)))
                npb = 0
                for ji, (kind, n, q_ap, keys, o_ap, (h0, nh, nqq)) in enumerate(jobs):
                    po, psm = 2 + (ji % 2), 4 + (ji % 2)
                    def emit_S(ki):
                        kt = keys[ki][0]
                        sbank = ki % 2
                        s.op("tensor", lambda h, sbank=sbank, kind=kind, n=n, kt=kt, q_ap=q_ap: h.matmul(
                            pp[sbank][:], lhsT=KT[kind][:, n, kt * 128:(kt + 1) * 128], rhs=q_ap, start=True, stop=True),
                            reads=[res(f"KT{kind}"), res(f"QT{kind}")], writes=[r_pp[sbank]])
                    emit_S(0)
                    for ki, (kt, moff) in enumerate(keys):
                        sbank = ki % 2
                        pi = npb % 4
                        npb += 1
                        s.op("scalar", lambda h, pi=pi, sbank=sbank, kind=kind: h.activation(
                            out=pb[pi][:], in_=pp[sbank][:], func=AF.Exp, bias=negm[:, p, kind:kind + 1], scale=SCALE),
                            reads=[r_pp[sbank], res("negm")], writes=[res(f"pb{pi}")])
                        if ki + 1 < len(keys):
                            emit_S(ki + 1)
                        if moff is not None:
                            mk = cb[:, moff:moff + 128].unsqueeze(1).broadcast_to([128, 4, 128])
                            s.op("gpsimd", lambda h, pi=pi, mk=mk: h.tensor_tensor(
                                out=pb[pi][:].rearrange("p (a b) -> p a b", b=128), in0=pb[pi][:].rearrange("p (a b) -> p a b", b=128), in1=mk, op=ALU.mult),
                                reads=[res(f"pb{pi}"), res("cb")], writes=[res(f"pb{pi}")])
                        vs = V[:, kt, kind * 256 + n * 128: kind * 256 + (n + 1) * 128]
                        last = ki == len(keys) - 1
                        s.op("tensor", lambda h, po=po, vs=vs, pi=pi, ki=ki, last=last: h.matmul(
                            pp[po][:], lhsT=vs, rhs=pb[pi][:], start=(ki == 0), stop=last),
                            reads=[res("V"), res(f"pb{pi}")], writes=[r_pp[po]], signal=last)
                        s.op("tensor", lambda h, psm=psm, pi=pi, ki=ki, last=last: h.matmul(
                            pp[psm][:], lhsT=onesb, rhs=pb[pi][:], start=(ki == 0), stop=last),
                            reads=[res("cb"), res(f"pb{pi}")], writes=[r_pp[psm]], signal=True)
                    rb = ji % 2
                    if kind == 1:
                        se = SE[:, h0:h0 + nh].unsqueeze(2).broadcast_to([128, nh, nqq])
                        s.op("vector", lambda h, rb=rb, psm=psm, se=se, nqq=nqq: h.tensor_tensor(
                            out=rec[rb][:].rearrange("p (a b) -> p a b", b=nqq), in0=pp[psm][:].rearrange("p (a b) -> p a b", b=nqq), in1=se, op=ALU.add),
                            reads=[r_pp[psm], res("SE")], writes=[res(f"rec{rb}")])
                        s.op("vector", lambda h, rb=rb: h.reciprocal(out=rec[rb][:], in_=rec[rb][:]), reads=[res(f"rec{rb}")], writes=[res(f"rec{rb}")])
                    else:
                        s.op("vector", lambda h, rb=rb, psm=psm: h.reciprocal(out=rec[rb][:], in_=pp[psm][:]), reads=[r_pp[psm]], writes=[res(f"rec{rb}")])
                    s.op("vector", lambda h, rb=rb, po=po, o_ap=o_ap, nqq=nqq: h.tensor_tensor(
                        out=o_ap, in0=pp[po][:].rearrange("p (a b) -> p a b", b=nqq), in1=rec[rb][:].rearrange("p (a b) -> p a b", b=nqq), op=ALU.mult),
                        reads=[r_pp[po], res(f"rec{rb}")], writes=[res("OT")])
                s.emit()

        def post_phase(p, OT):
            with ExitStack() as ph:
                def sbp(name, shape, dt=F32):
                    return ph.enter_context(nc.sbuf_tensor(f"{name}_p{p}", list(shape), dt))
                wo = sbp("wo", [128, 16, D], BF16)
                wr = sbp("wr", [128, 16, NE], BF16)
                G1 = sbp("G1", [128, D]); A2 = sbp("A2", [128, D]); B2 = sbp("B2", [128, D])
                xt = sbp("xt", [128, D]); yt = sbp("yt", [128, D]); tf = sbp("tf", [128, D])
                h2b = sbp("h2b", [128, D], BF16); h2T = sbp("h2T", [128, 16, 128], BF16)
                st = sbp("st", [128, 8])
                sc = sbp("sc", [128, NE]); sel = sbp("sel", [128, NE]); srt = sbp("srt", [128, 8, 8]); gs = sbp("gs", [128, 8])
                gs8 = sbp("gs8", [128, 8]); gm = sbp("gm", [128, 8]); gneg = sbp("gneg", [128, 8]); selm = sbp("selm", [128, NE])
                top8 = sbp("top8", [128, 8]); Mf = sbp("Mf", [128, NE]); wsel = sbp("wsel", [128, NE]); den = sbp("den", [128, 2])
                posf = sbp("posf", [128, NE]); vv = sbp("vv", [128, NE]); d8 = sbp("d8", [128, 8]); oh = sbp("oh", [128, NE])
                g8 = sbp("g8", [128, 8]); eoff = sbp("eoff", [128, NE])
                for n in range(4):
                    s.dma("gpsimd", lambda h, n=n: h.dma_start(out=wo[:, :, n * 512:(n + 1) * 512],
                                                              in_=w_out[:, n * 512:(n + 1) * 512].rearrange("(c p) n -> p c n", p=128)), writes=[res("wo")])
                s.dma("gpsimd", lambda h: h.dma_start(out=wr[:], in_=w_router.rearrange("(c p) n -> p c n", p=128)), writes=[res("wr")])
                s.dma("sync", lambda h: h.dma_start(out=G1[:], in_=modsp[4 + p]), writes=[res("G1")])
                s.dma("sync", lambda h: h.dma_start(out=A2[:], in_=modsp[8 + p]), writes=[res("A2")])
                s.dma("sync", lambda h: h.dma_start(out=B2[:], in_=modsp[6 + p]), writes=[res("B2")])
                s.op("gpsimd", lambda h: h.iota(eoff[:], pattern=[[CAP, NE]], base=1, channel_multiplier=0, allow_small_or_imprecise_dtypes=True),
                     writes=[res("eoff")])
                for i in range(8):
                    gi = p * 8 + i
                    rows = slice(i * 128, (i + 1) * 128)
                    grow = slice(gi * 128, (gi + 1) * 128)
                    for n in range(4):
                        for mh in range(16):
                            s.op("tensor", lambda h, n=n, mh=mh, i=i: h.matmul(pp[n][:], lhsT=OT[:, mh, i * 128:(i + 1) * 128], rhs=wo[:, mh, n * 512:(n + 1) * 512],
                                                                              start=(mh == 0), stop=(mh == 15)),
                                 reads=[res("OT"), res("wo")], writes=[r_pp[n]], signal=(mh == 15))
                        s.op("scalar", lambda h, n=n: h.activation(out=tf[:, n * 512:(n + 1) * 512], in_=pp[n][:], func=AF.Square, accum_out=st[:, n:n + 1]),
                             reads=[r_pp[n]], writes=[res("tf"), res("st")])
                    s.op("vector", lambda h: h.tensor_reduce(out=st[:, 4:5], in_=st[:, 0:4], axis=AX.X, op=ALU.add), reads=[res("st")], writes=[res("st")])
                    rstd_chain(st[:, 4:5], st[:, 5:6], D, res("st"), res("st"))
                    s.dma("sync", lambda h, rows=rows: h.dma_start(out=xt[:], in_=xin[p][rows, :]), writes=[res("xt")])
                    for n in range(4):
                        cs = slice(n * 512, (n + 1) * 512)
                        s.op("vector", lambda h, n=n, cs=cs: h.scalar_tensor_tensor(out=tf[:, cs], in0=pp[n][:], scalar=st[:, 5:6], in1=G1[:, cs],
                                                                                    op0=ALU.mult, op1=ALU.mult),
                             reads=[r_pp[n], res("st"), res("G1")], writes=[res("tf")])
                    s.op("gpsimd", lambda h: h.tensor_tensor(out=yt[:], in0=tf[:], in1=xt[:], op=ALU.add), reads=[res("tf"), res("xt")], writes=[res("yt")])
                    s.dma("sync", lambda h, grow=grow: h.dma_start(out=ysp[grow, :], in_=yt[:]), reads=[res("yt")])
                    s.op("scalar", lambda h: h.activation(out=tf[:], in_=yt[:], func=AF.Square, accum_out=st[:, 6:7]),
                         reads=[res("yt")], writes=[res("tf"), res("st")])
                    rstd_chain(st[:, 6:7], st[:, 7:8], D, res("st"), res("st"))
                    s.op("vector", lambda h: h.scalar_tensor_tensor(out=tf[:], in0=yt[:], scalar=st[:, 7:8], in1=A2[:], op0=ALU.mult, op1=ALU.mult),
                         reads=[res("yt"), res("st"), res("A2")], writes=[res("tf")])
                    s.op("gpsimd", lambda h: h.tensor_tensor(out=h2b[:], in0=tf[:], in1=B2[:], op=ALU.add), reads=[res("tf"), res("B2")], writes=[res("h2b")])
                    s.dma("sync", lambda h, gi=gi: h.dma_start(out=xbuf_s[gi * 128:(gi + 1) * 128, :], in_=h2b[:]),
                          reads=[res("h2b")])
                    transpose16(h2b, res("h2b"), h2T, res("h2T"), lambda half: h2T[:, half * 8:(half + 1) * 8, :])
                    for k in range(16):
                        s.op("tensor", lambda h, k=k: h.matmul(pp[4][:, 0:NE], lhsT=h2T[:, k, :], rhs=wr[:, k, :], start=(k == 0), stop=(k == 15)),
                             reads=[res("h2T"), res("wr")], writes=[r_pp[4]], signal=(k == 15))
                    s.op("scalar", lambda h: h.activation(out=sc[:], in_=pp[4][:, 0:NE], func=AF.Sigmoid), reads=[r_pp[4]], writes=[res("sc")])
                    s.op("vector", lambda h: h.tensor_tensor(out=sel[:], in0=sc[:], in1=rbb[:], op=ALU.add), reads=[res("sc"), res("rbb")], writes=[res("sel")])
                    for g in range(8):
                        s.op("vector", lambda h, g=g: h.max(out=srt[:, g, :], in_=sel[:, g * 8:(g + 1) * 8]), reads=[res("sel")], writes=[res("srt")])
                    s.op("vector", lambda h: h.tensor_tensor(out=gs[:], in0=srt[:, :, 0], in1=srt[:, :, 1], op=ALU.add), reads=[res("srt")], writes=[res("gs")])
                    s.op("vector", lambda h: h.max(out=gs8[:], in_=gs[:]), reads=[res("gs")], writes=[res("gs8")])
                    s.op("vector", lambda h: h.tensor_scalar(out=gm[:], in0=gs[:], scalar1=gs8[:, 3:4], scalar2=None, op0=ALU.is_ge),
                         reads=[res("gs"), res("gs8")], writes=[res("gm")])
                    s.op("vector", lambda h: h.tensor_scalar(out=gneg[:], in0=gm[:], scalar1=-1.0, scalar2=1e9, op0=ALU.add, op1=ALU.mult),
                         reads=[res("gm")], writes=[res("gneg")])
                    for g in range(8):
                        s.op("vector", lambda h, g=g: h.tensor_scalar(out=selm[:, g * 8:(g + 1) * 8], in0=sel[:, g * 8:(g + 1) * 8],
                                                                      scalar1=gm[:, g:g + 1], scalar2=gneg[:, g:g + 1], op0=ALU.mult, op1=ALU.add),
                             reads=[res("sel"), res("gm"), res("gneg")], writes=[res("selm")])
                    s.op("vector", lambda h: h.max(out=top8[:], in_=selm[:]), reads=[res("selm")], writes=[res("top8")])
                    s.op("vector", lambda h: h.tensor_scalar(out=Mf[:], in0=selm[:], scalar1=top8[:, 7:8], scalar2=None, op0=ALU.is_ge),
                         reads=[res("selm"), res("top8")], writes=[res("Mf")])
                    s.op("vector", lambda h, gi=gi: h.tensor_copy(out=Mall[:, gi, :], in_=Mf[:]), reads=[res("Mf")], writes=[res("Mall")])
                    s.op("vector", lambda h: h.tensor_tensor(out=wsel[:], in0=sc[:], in1=Mf[:], op=ALU.mult), reads=[res("sc"), res("Mf")], writes=[res("wsel")])
                    s.op("vector", lambda h: h.tensor_reduce(out=den[:, 0:1], in_=wsel[:], axis=AX.X, op=ALU.add), reads=[res("wsel")], writes=[res("den")])
                    s.op("vector", lambda h: h.reciprocal(out=den[:, 1:2], in_=den[:, 0:1]), reads=[res("den")], writes=[res("den")])
                    s.op("vector", lambda h: h.tensor_scalar(out=wsel[:], in0=wsel[:], scalar1=den[:, 1:2], scalar2=2.5, op0=ALU.mult, op1=ALU.mult),
                         reads=[res("wsel"), res("den")], writes=[res("wsel")])
                    s.op("tensor", lambda h, gi=gi: h.matmul(pp[5][:, 0:NE], lhsT=ustrb, rhs=Mall[:, gi, :], start=True, stop=(gi == 0)),
                         reads=[res("Mall"), res("cb")], writes=[r_pp[5]], signal=(gi == 0))
                    for j in range(gi):
                        s.op("tensor", lambda h, j=j, gi=gi: h.matmul(pp[5][:, 0:NE], lhsT=onesb, rhs=Mall[:, j, :], start=False, stop=(j == gi - 1)),
                             reads=[res("Mall"), res("cb")], writes=[r_pp[5]], signal=(j == gi - 1))
                    s.op("vector", lambda h: h.tensor_scalar(out=posf[:], in0=pp[5][:, 0:NE], scalar1=float(CAP - 1), scalar2=None, op0=ALU.min),
                         reads=[r_pp[5]], writes=[res("posf")])
                    s.op("vector", lambda h: h.tensor_tensor(out=vv[:], in0=posf[:], in1=eoff[:], op=ALU.add), reads=[res("posf"), res("eoff")], writes=[res("vv")])
                    s.op("vector", lambda h: h.tensor_tensor(out=vv[:], in0=vv[:], in1=Mf[:], op=ALU.mult), reads=[res("vv"), res("Mf")], writes=[res("vv")])
                    s.op("vector", lambda h: h.max(out=d8[:], in_=vv[:]), reads=[res("vv")], writes=[res("d8")])
                    for k in range(8):
                        s.op("vector", lambda h, k=k: h.tensor_scalar(out=oh[:], in0=vv[:], scalar1=d8[:, k:k + 1], scalar2=None, op0=ALU.is_equal),
                             reads=[res("vv"), res("d8")], writes=[res("oh")])
                        s.op("vector", lambda h, k=k: h.tensor_tensor(out=oh[:], in0=oh[:], in1=wsel[:], op=ALU.mult), reads=[res("oh"), res("wsel")], writes=[res("oh")])
                        s.op("vector", lambda h, k=k: h.tensor_reduce(out=g8[:, k:k + 1], in_=oh[:], axis=AX.X, op=ALU.add), reads=[res("oh")], writes=[res("g8")])
                    s.op("vector", lambda h: h.tensor_scalar(out=d8[:], in0=d8[:], scalar1=-1.0, scalar2=None, op0=ALU.add), reads=[res("d8")], writes=[res("d8")])
                    s.op("vector", lambda h, gi=gi: h.tensor_copy(out=dstall[:, gi, :], in_=d8[:]), reads=[res("d8")], writes=[res("dstall")])
                    s.op("vector", lambda h, gi=gi: h.tensor_copy(out=gall[:, gi, :], in_=g8[:]), reads=[res("g8")], writes=[res("gall")])
                    if DEBUG:
                        s.dma("sync", lambda h, gi=gi: h.dma_start(out=dbg_sc[gi], in_=sc[:]), reads=[res("sc")])
                    for k in range(8):
                        s.dma("gpsimd", lambda h, k=k, gi=gi: h.indirect_dma_start(
                            out=xbuf[:, :], out_offset=bass.IndirectOffsetOnAxis(ap=dstall[:, gi, k:k + 1], axis=0),
                            in_=h2b[:], in_offset=None, bounds_check=None),
                            reads=[res("h2b"), res("dstall")])
                if p == 1:
                    cntb = sbp("cntb", [128, NE]); flg = sbp("flg", [128, NE]); cum = sbp("cum", [128, NE]); one64 = sbp("one64", [128, NE])
                    eix = sbp("eix", [128, NE]); selj = sbp("selj", [128, NE]); ev = sbp("ev", [128, NHOT])
                    mult24 = sbp("mult24", [128, 24]); offs24 = sbp("offs24", [128, 24]); idxf = sbp("idxf", [128, NHOT, 24])
                    for j in range(16):
                        s.op("tensor", lambda h, j=j: h.matmul(pp[5][:, 0:NE], lhsT=onesb, rhs=Mall[:, j, :], start=(j == 0), stop=(j == 15)),
                             reads=[res("Mall"), res("cb")], writes=[r_pp[5]], signal=(j == 15))
                    s.op("vector", lambda h: h.tensor_scalar(out=flg[:], in0=pp[5][:, 0:NE], scalar1=512.0, scalar2=None, op0=ALU.is_gt),
                         reads=[r_pp[5]], writes=[res("flg")])
                    s.op("vector", lambda h: h.memset(one64[:], 1.0), writes=[res("one64")])
                    s.op("vector", lambda h: h.tensor_tensor_scan(out=cum[:], data0=one64[:], data1=flg[:], initial=0.0, op0=ALU.mult, op1=ALU.add),
                         reads=[res("one64"), res("flg")], writes=[res("cum")])
                    s.op("vector", lambda h: h.tensor_tensor(out=cum[:], in0=cum[:], in1=flg[:], op=ALU.subtract), reads=[res("cum"), res("flg")], writes=[res("cum")])
                    s.op("gpsimd", lambda h: h.iota(eix[:], pattern=[[1, NE]], base=0, channel_multiplier=0, allow_small_or_imprecise_dtypes=True), writes=[res("eix")])
                    s.op("gpsimd", lambda h: h.iota(offs24[:, 0:4], pattern=[[128, 4]], base=512, channel_multiplier=1, allow_small_or_imprecise_dtypes=True), writes=[res("offs24")])
                    s.op("gpsimd", lambda h: h.iota(offs24[:, 4:20], pattern=[[128, 16]], base=0, channel_multiplier=1, allow_small_or_imprecise_dtypes=True), writes=[res("offs24")])
                    s.op("gpsimd", lambda h: h.iota(offs24[:, 20:24], pattern=[[128, 4]], base=0, channel_multiplier=1, allow_small_or_imprecise_dtypes=True), writes=[res("offs24")])
                    s.op("vector", lambda h: h.memset(mult24[:, 0:4], float(CAP)), writes=[res("mult24")])
                    s.op("vector", lambda h: h.memset(mult24[:, 4:20], 2048.0), writes=[res("mult24")])
                    s.op("vector", lambda h: h.memset(mult24[:, 20:24], 512.0), writes=[res("mult24")])
                    for j in range(NHOT):
                        s.op("vector", lambda h, j=j: h.tensor_scalar(out=selj[:], in0=cum[:], scalar1=float(j), scalar2=None, op0=ALU.is_equal),
                             reads=[res("cum")], writes=[res("selj")])
                        s.op("vector", lambda h: h.tensor_tensor(out=selj[:], in0=selj[:], in1=flg[:], op=ALU.mult), reads=[res("selj"), res("flg")], writes=[res("selj")])
                        s.op("vector", lambda h: h.tensor_tensor(out=selj[:], in0=selj[:], in1=eix[:], op=ALU.mult), reads=[res("selj"), res("eix")], writes=[res("selj")])
                        s.op("vector", lambda h, j=j: h.tensor_reduce(out=ev[:, j:j + 1], in_=selj[:], axis=AX.X, op=ALU.add), reads=[res("selj")], writes=[res("ev")])
                        s.op("vector", lambda h, j=j: h.scalar_tensor_tensor(out=idxf[:, j, :], in0=mult24[:], scalar=ev[:, j:j + 1], in1=offs24[:], op0=ALU.mult, op1=ALU.add),
                             reads=[res("mult24"), res("ev"), res("offs24")], writes=[res("idxf")])
                    s.op("vector", lambda h: h.tensor_copy(out=hotidx[:], in_=idxf[:]), reads=[res("idxf")], writes=[res("hotidx")])
                s.emit()

        def expert_phase():
            with ExitStack() as ph:
                def sbp(name, shape, dt=F32):
                    return ph.enter_context(nc.sbuf_tensor(f"{name}_e", list(shape), dt))
                wg = [sbp(f"wg{i}", [128, 16, 512], BF16) for i in range(2)]
                wu = [sbp(f"wu{i}", [128, 16, 512], BF16) for i in range(2)]
                wd = [sbp(f"wd{i}", [128, 4, D], BF16) for i in range(2)]
                xg = [sbp(f"xg{i}", [128, D], BF16) for i in range(2)]
                xT = sbp("xT", [128, 16, 512], BF16)
                sg = [sbp(f"sg{i}", [128, 512]) for i in range(2)]
                act = sbp("act", [128, 4, 512], BF16)
                ob = [sbp(f"ob{i}", [128, D], BF16) for i in range(2)]
                passes = [("s", e, e * CAP, e % 2) for e in range(NE)] + [("d", j, 0, (NE + j) % 2) for j in range(NHOT)] \
                    + [("s", NE, q * 512, (NE + NHOT) % 2) for q in range(4)]
                state = {"loaded": None, "nx": 0, "nob": 0, "nstg": 0}
                stg = [sbp(f"stg{i}", [128, D]) for i in range(4)]
                wge_rows = wge.rearrange("e k n -> (e k) n")
                wue_rows = wue.rearrange("e k n -> (e k) n")
                wde_rows = wde.rearrange("e k n -> (e k) n")

                def gather_cast(dst_ap, rdst, src_rows, j, col, width):
                    si = state["nstg"] % 4
                    state["nstg"] += 1
                    s.dma("gpsimd", lambda h, si=si, j=j, col=col: h.indirect_dma_start(
                        out=stg[si][:, 0:width], out_offset=None, in_=src_rows[:, :],
                        in_offset=bass.IndirectOffsetOnAxis(ap=hotidx[:, j, col:col + 1], axis=0), bounds_check=None),
                        reads=[res("hotidx")], writes=[res(f"stg{si}")])
                    if si % 2 == 0:
                        s.op("vector", lambda h, si=si: h.tensor_copy(out=dst_ap, in_=stg[si][:, 0:width]), reads=[res(f"stg{si}")], writes=[rdst])
                    else:
                        s.op("scalar", lambda h, si=si: h.copy(out=dst_ap, in_=stg[si][:, 0:width]), reads=[res(f"stg{si}")], writes=[rdst])

                def emit_loadx(idx):
                    kind_, e, r0, b = passes[idx]
                    if kind_ == "d":
                        j = e
                        for c in range(16):
                            gather_cast(wg[b][:, c, :], res(f"wg{b}"), wge_rows, j, 4 + c, 512)
                        for c in range(16):
                            gather_cast(wu[b][:, c, :], res(f"wu{b}"), wue_rows, j, 4 + c, 512)
                        for c in range(4):
                            gather_cast(wd[b][:, c, :], res(f"wd{b}"), wde_rows, j, 20 + c, D)
                        state["loaded"] = ("d", j)
                        for sbk in range(4):
                            xb_ = state["nx"] % 2
                            state["nx"] += 1
                            s.dma("gpsimd", lambda h, xb_=xb_, j=j, sbk=sbk: h.indirect_dma_start(
                                out=xg[xb_][:], out_offset=None, in_=xbuf[:, :],
                                in_offset=bass.IndirectOffsetOnAxis(ap=hotidx[:, j, sbk:sbk + 1], axis=0), bounds_check=None),
                                reads=[res("hotidx")], writes=[res(f"xg{xb_}")])
                            transpose16(xg[xb_], res(f"xg{xb_}"), xT, res("xT"),
                                        lambda half, sbk=sbk: xT[:, half * 8:(half + 1) * 8, sbk * 128:(sbk + 1) * 128])
                        return
                    if ("s", e) != state["loaded"]:
                        state["loaded"] = ("s", e)
                        gsrc = wge[e] if e < NE else wgs
                        usrc = wue[e] if e < NE else wus
                        dsrc = wde[e] if e < NE else wds
                        s.dma("gpsimd", lambda h, b=b, gsrc=gsrc: h.dma_start(out=wg[b][:], in_=gsrc.rearrange("(c p) n -> p c n", p=128)), writes=[res(f"wg{b}")])
                        s.dma("gpsimd", lambda h, b=b, usrc=usrc: h.dma_start(out=wu[b][:], in_=usrc.rearrange("(c p) n -> p c n", p=128)), writes=[res(f"wu{b}")])
                        s.dma("gpsimd", lambda h, b=b, dsrc=dsrc: h.dma_start(out=wd[b][:], in_=dsrc.rearrange("(c p) n -> p c n", p=128)), writes=[res(f"wd{b}")])
                    for sbk in range(4):
                        xb_ = state["nx"] % 2
                        state["nx"] += 1
                        s.dma("sync", lambda h, xb_=xb_, r0=r0, sbk=sbk, e=e: h.dma_start(out=xg[xb_][:], in_=(xbuf if e < NE else xbuf_s)[r0 + sbk * 128:r0 + (sbk + 1) * 128, :]),
                              writes=[res(f"xg{xb_}")])
                        transpose16(xg[xb_], res(f"xg{xb_}"), xT, res("xT"),
                                    lambda half, sbk=sbk: xT[:, half * 8:(half + 1) * 8, sbk * 128:(sbk + 1) * 128])

                def emit_gu(idx):
                    kind_, e, r0, b = passes[idx]
                    for fc in range(4):
                        gb, ub = fc % 2, 2 + fc % 2
                        for k in range(16):
                            s.op("tensor", lambda h, gb=gb, b=b, k=k, fc=fc: h.matmul(pp[gb][:], lhsT=wg[b][:, k, fc * 128:(fc + 1) * 128], rhs=xT[:, k, :],
                                                                                      start=(k == 0), stop=(k == 15)),
                                 reads=[res(f"wg{b}"), res("xT")], writes=[r_pp[gb]], signal=(k == 15))
                        for k in range(16):
                            s.op("tensor", lambda h, ub=ub, b=b, k=k, fc=fc: h.matmul(pp[ub][:], lhsT=wu[b][:, k, fc * 128:(fc + 1) * 128], rhs=xT[:, k, :],
                                                                                      start=(k == 0), stop=(k == 15)),
                                 reads=[res(f"wu{b}"), res("xT")], writes=[r_pp[ub]], signal=(k == 15))
                        s.op("scalar", lambda h, gb=gb, fc=fc: h.activation(out=sg[fc % 2][:], in_=pp[gb][:], func=AF.Silu),
                             reads=[r_pp[gb]], writes=[res(f"sg{fc % 2}")])
                        s.op("vector", lambda h, ub=ub, fc=fc: h.tensor_tensor(out=act[:, fc, :], in0=pp[ub][:], in1=sg[fc % 2][:], op=ALU.mult),
                             reads=[r_pp[ub], res(f"sg{fc % 2}")], writes=[res("act")])

                def emit_down(idx):
                    kind_, e, r0, b = passes[idx]
                    for sbk in range(4):
                        ob_ = state["nob"] % 2
                        state["nob"] += 1
                        for n in range(4):
                            bank = 4 + n % 2
                            for fc in range(4):
                                s.op("tensor", lambda h, bank=bank, fc=fc, sbk=sbk, n=n, b=b: h.matmul(
                                    pp[bank][:], lhsT=act[:, fc, sbk * 128:(sbk + 1) * 128], rhs=wd[b][:, fc, n * 512:(n + 1) * 512],
                                    start=(fc == 0), stop=(fc == 3)),
                                    reads=[res("act"), res(f"wd{b}")], writes=[r_pp[bank]], signal=(fc == 3))
                            if n % 2 == 0:
                                s.op("scalar", lambda h, bank=bank, ob_=ob_, n=n: h.copy(
                                    out=ob[ob_][:, n * 512:(n + 1) * 512], in_=pp[bank][:]),
                                    reads=[r_pp[bank]], writes=[res(f"ob{ob_}")])
                            else:
                                s.op("vector", lambda h, bank=bank, ob_=ob_, n=n: h.tensor_copy(
                                    out=ob[ob_][:, n * 512:(n + 1) * 512], in_=pp[bank][:]),
                                    reads=[r_pp[bank]], writes=[res(f"ob{ob_}")])
                        if kind_ == "d":
                            s.dma("gpsimd", lambda h, ob_=ob_, e=e, sbk=sbk: h.indirect_dma_start(
                                out=obuf[:, :], out_offset=bass.IndirectOffsetOnAxis(ap=hotidx[:, e, sbk:sbk + 1], axis=0),
                                in_=ob[ob_][:], in_offset=None, bounds_check=None),
                                reads=[res(f"ob{ob_}"), res("hotidx")])
                        else:
                            s.dma("sync", lambda h, ob_=ob_, r0=r0, sbk=sbk, e=e: h.dma_start(out=(obuf if e < NE else obuf_s)[r0 + sbk * 128:r0 + (sbk + 1) * 128, :], in_=ob[ob_][:]),
                                  reads=[res(f"ob{ob_}")])

                emit_loadx(0)
                for idx in range(len(passes)):
                    emit_gu(idx)
                    if idx + 1 < len(passes):
                        emit_loadx(idx + 1)
                    emit_down(idx)
                s.emit()

        def combine_phase():
            with ExitStack() as ph:
                def sbp(name, shape, dt=F32):
                    return ph.enter_context(nc.sbuf_tensor(f"{name}_c", list(shape), dt))
                gk = [sbp(f"gk{i}", [128, D], BF16) for i in range(9)]
                accA = sbp("accA", [128, D]); accB = sbp("accB", [128, D])
                yt = sbp("yt", [128, D]); G2 = [sbp("G2a", [128, D]), sbp("G2b", [128, D])]
                st = sbp("st", [128, 4])
                for k in range(9):
                    s.op("gpsimd", lambda h, k=k: h.memset(gk[k][:], 0.0), writes=[res(f"gk{k}")])
                for p in range(2):
                    s.dma("sync", lambda h, p=p: h.dma_start(out=G2[p][:], in_=modsp[10 + p]), writes=[res(f"G2{p}")])
                for gi in range(16):
                    p, i = gi // 8, gi % 8
                    for k in range(8):
                        s.dma("gpsimd", lambda h, k=k, gi=gi: h.indirect_dma_start(
                            out=gk[k][:], out_offset=None, in_=obuf[:, :],
                            in_offset=bass.IndirectOffsetOnAxis(ap=dstall[:, gi, k:k + 1], axis=0),
                            bounds_check=None),
                            reads=[res("dstall")], writes=[res(f"gk{k}")])
                    s.dma("sync", lambda h, gi=gi: h.dma_start(out=gk[8][:], in_=obuf_s[gi * 128:(gi + 1) * 128, :]),
                          writes=[res("gk8")])
                    s.dma("sync", lambda h, gi=gi: h.dma_start(out=yt[:], in_=ysp[gi * 128:(gi + 1) * 128, :]), writes=[res("ytc")])
                    s.op("vector", lambda h, gi=gi: h.scalar_tensor_tensor(out=accA[:], in0=gk[0][:], scalar=gall[:, gi, 0:1], in1=gk[8][:], op0=ALU.mult, op1=ALU.add),
                         reads=[res("gk8"), res("gk0"), res("gall")], writes=[res("accA")])
                    for k in range(1, 8):
                        s.op("vector", lambda h, k=k, gi=gi: h.scalar_tensor_tensor(out=accA[:], in0=gk[k][:], scalar=gall[:, gi, k:k + 1], in1=accA[:], op0=ALU.mult, op1=ALU.add),
                             reads=[res("accA"), res(f"gk{k}"), res("gall")], writes=[res("accA")])
                    if DEBUG:
                        s.dma("sync", lambda h, gi=gi: h.dma_start(out=dbg_moe[gi * 128:(gi + 1) * 128, :], in_=accA[:]), reads=[res("accA")])
                    s.op("scalar", lambda h: h.activation(out=accB[:], in_=accA[:], func=AF.Square, accum_out=st[:, 0:1]),
                         reads=[res("accA")], writes=[res("accB"), res("stc")])
                    rstd_chain(st[:, 0:1], st[:, 1:2], D, res("stc"), res("stc"))
                    s.op("vector", lambda h, p=p: h.scalar_tensor_tensor(out=accB[:], in0=accA[:], scalar=st[:, 1:2], in1=G2[p][:], op0=ALU.mult, op1=ALU.mult),
                         reads=[res("accA"), res("stc"), res(f"G2{p}")], writes=[res("accB")])
                    s.op("gpsimd", lambda h: h.tensor_tensor(out=accB[:], in0=accB[:], in1=yt[:], op=ALU.add), reads=[res("accB"), res("ytc")], writes=[res("accB")])
                    s.dma("sync", lambda h, p=p, i=i: h.dma_start(out=youts[p][i * 128:(i + 1) * 128, :], in_=accB[:]),
                          reads=[res("accB")])
                s.emit()

        if DEBUG:
            dbg_dst = dscr("dbg_dst", [128, 16 * 8], I32)
            dbg_gate = dscr("dbg_gate", [128, 16 * 8])
            dbg_moe = dscr("dbg_moe", [2048, D])
            dbg_sc = dscr("dbg_sc", [16, 128, NE])
        for p in range(2):
            if STAGE >= 1:
                qkv_phase(p)
            if STAGE >= 2:
                with nc.sbuf_tensor(f"OT{p}", [128, 16, 1024], BF16) as OT:
                    attn_phase(p, OT)
                    if STAGE >= 3:
                        post_phase(p, OT)
        if STAGE >= 8:
            expert_phase()
            combine_phase()
        if DEBUG and STAGE >= 3:
            s.dma("sync", lambda h: h.dma_start(out=dbg_dst[:, :], in_=dstall[:].rearrange("p a b -> p (a b)")), reads=[res("dstall")])
            s.dma("sync", lambda h: h.dma_start(out=dbg_gate[:, :], in_=gall[:].rearrange("p a b -> p (a b)")), reads=[res("gall")])
        s.final_wait("sync", list(R.values()))
        s.emit()
    return nc


def _rope_tables():
    n = 2048
    rows = n // 64
    row = np.repeat(np.arange(rows, dtype=np.float32), 64)
    col = np.tile(np.arange(64, dtype=np.float32), rows)
    inv_freq = (10000.0 ** (-np.arange(0, 64, 2, dtype=np.float32) / 64)).astype(np.float32)
    ang_r = row[:, None] * inv_freq
    ang_c = col[:, None] * inv_freq
    ang = np.concatenate([ang_r, ang_r, ang_c, ang_c], axis=-1).astype(np.float32)
    cos = np.cos(ang).astype(np.float32)
    sin = np.sin(ang).astype(np.float32)
    sgn = np.ones(128, np.float32)
    sgn[0:32] = -1.0
    sgn[64:96] = -1.0
    return cos, sin * sgn[None, :]


def _consts(half):
    c = np.zeros((128, 7 * 128), np.float32)
    j = np.arange(128)[:, None]
    r = np.arange(128)[None, :]
    c[:, 0:128] = np.eye(128, dtype=np.float32)
    c[:, 128:256] = (j < r).astype(np.float32)
    c[:, 256:384] = 1.0
    band_prev = (j >= r).astype(np.float32)
    band_next = (j <= r).astype(np.float32)
    c[:, 384:512] = band_prev
    c[:, 512:640] = band_next
    c[:, 640:768] = band_prev if half == 1 else 0.0
    c[:, 768:896] = band_next if half == 0 else 0.0
    return c


def _local_order(half):
    own = np.arange(half * 1024, (half + 1) * 1024)
    if half == 0:
        other = np.arange(1024, 2048)
    else:
        other = np.concatenate([np.arange(896, 1024), np.arange(0, 896)])
    return np.concatenate([own, other])


_NC_CACHE = {}


def kernel(x_prompt, x_sample, cache_glob_k, cache_glob_v, cache_win_k, cache_win_v, c, c_ctx,
           w_ada, b_ada, attn_pre_g, attn_post_g, w_in, q_norm_g, k_norm_g, sink_logit, w_out,
           ffn_pre_g, ffn_post_g, w_router, router_bias, w_gate_e, w_up_e, w_down_e,
           w_gate_s, w_up_s, w_down_s):
    f = lambda a: np.ascontiguousarray(np.asarray(a, dtype=np.float32))
    x_prompt, x_sample = f(x_prompt), f(x_sample)
    cos, sins = _rope_tables()
    shared = {
        "w_ada": f(w_ada)[0], "b_ada": f(b_ada)[0][None, :],
        "gains": np.stack([f(attn_pre_g)[0], f(attn_post_g)[0], f(ffn_pre_g)[0], f(ffn_post_g)[0]]),
        "w_in": f(w_in)[0], "qkg": np.stack([f(q_norm_g)[0], f(k_norm_g)[0]]),
        "sink": f(sink_logit)[0][None, :], "w_out": f(w_out)[0], "w_router": f(w_router)[0],
        "rbias": f(router_bias)[0][None, :], "wge": f(w_gate_e)[0], "wue": f(w_up_e)[0], "wde": f(w_down_e)[0],
        "wgs": f(w_gate_s)[0], "wus": f(w_up_s)[0], "wds": f(w_down_s)[0],
    }
    caches = [f(cache_glob_k), f(cache_glob_v), f(cache_win_k), f(cache_win_v)]
    in_maps = []
    for core in range(NCORES):
        b, half = core // 2, core % 2
        order = _local_order(half)
        m = dict(shared)
        m["xc"] = x_prompt[4 * core:4 * core + 4].reshape(1024, D)
        m["xl"] = np.ascontiguousarray(x_sample[b][order])
        m["ropec"] = np.ascontiguousarray(cos[order])
        m["ropes"] = np.ascontiguousarray(sins[order])
        m["cache"] = np.stack([cc[b, 0].reshape(256, 256) for cc in caches])
        m["cond"] = np.stack([f(c_ctx), f(c)[b]])
        m["consts"] = _consts(half)
        in_maps.append(m)
    if "nc" not in _NC_CACHE:
        del INPUT_NAMES[:]
        _NC_CACHE["nc"] = build()
    nc = _NC_CACHE["nc"]
    in_maps = [{k: v for k, v in m.items() if k in INPUT_NAMES} for m in in_maps]
    r = run_bass_kernel_spmd(nc, in_maps, core_ids=list(range(NCORES))).results
    if DEBUG:
        _NC_CACHE["raw"] = r
    y_p = np.concatenate([r[i]["yc"].reshape(4, 256, D) for i in range(NCORES)], axis=0)
    y_s = np.stack([np.concatenate([r[2 * b]["yl"], r[2 * b + 1]["yl"]], axis=0) for b in range(4)])
    def kv(name):
        return np.concatenate([r[i][name].reshape(4, 1, 256, 2, 128) for i in range(NCORES)], axis=0)
    return (y_p.astype(np.float32), y_s.astype(np.float32), kv("ngk"), kv("ngv"), kv("nwk"), kv("nwv"))
```

```python
import numpy as np
from contextlib import ExitStack
import concourse.bass as bass
import concourse.mybir as mybir
from concourse.bass_utils import run_bass_kernel_spmd

F32 = mybir.dt.float32
BF16 = mybir.dt.bfloat16
I32 = mybir.dt.int32
U32 = mybir.dt.uint32
AF = mybir.ActivationFunctionType
ALU = mybir.AluOpType
AX = mybir.AxisListType

D = 2048
NCORES = 8
EPS = 1e-6
HD = 128
SCALE = HD ** -0.5
NE = 64
CAP = 1024
NSLOT = NE * CAP + 2048
STAGE = 99
DEBUG = False
SKIP_INPUTS = set()
INPUT_NAMES = []


class _Eng:
    def __init__(self, key):
        self.key = key
        self.sem = None
        self.count = 0
        self.thunks = []
        self.waited = {}


class Res:
    __slots__ = ("name", "w", "r")

    def __init__(self, name):
        self.name = name
        self.w = None
        self.r = []


class Sched:
    def __init__(self, nc, n_dma_sems=24):
        self.nc = nc
        self.eng = {k: _Eng(k) for k in ("tensor", "vector", "scalar", "gpsimd", "sync")}
        self.n_dma_sems = n_dma_sems
        self.dma_sems = {}
        self.dma_rr = {}
        self.sems = {}
        self.phase_id = 0

    def alloc_sems(self, stack):
        for k, e in self.eng.items():
            e.sem = stack.enter_context(self.nc.semaphore("s_" + k))
            self.sems[("e", k)] = e.sem
        for q in ("sync", "gpsimd"):
            lst = []
            for i in range(self.n_dma_sems):
                s = stack.enter_context(self.nc.semaphore(f"d_{q}_{i}"))
                self.sems[("d", q, i)] = s
                lst.append([("d", q, i), 0])
            self.dma_sems[q] = lst
            self.dma_rr[q] = 0

    def _deps(self, reads, writes):
        deps = []
        for r in reads:
            if r.w is not None:
                deps.append(r.w)
        for w in writes:
            if w.w is not None:
                deps.append(w.w)
            deps.extend(w.r)
        return deps

    def _waits(self, e, deps, skip_self=False):
        need = {}
        for src, val in deps:
            if skip_self and src == ("e", e.key):
                continue
            if e.waited.get(src, 0) >= val:
                continue
            if need.get(src, 0) < val:
                need[src] = val
        for src, val in need.items():
            e.waited[src] = val
        return list(need.items())

    def op(self, engine, fn, reads=(), writes=(), signal=True):
        e = self.eng[engine]
        waits = self._waits(e, self._deps(reads, writes), skip_self=(engine == "tensor"))
        if signal:
            e.count += 1
            tok = (("e", engine), e.count)
            for r in reads:
                r.r.append(tok)
            for w in writes:
                w.w = tok
                w.r = []
        sems = self.sems

        def thunk(h, waits=waits, fn=fn, signal=signal, sem=e.sem):
            for src, val in waits:
                h.wait_ge(sems[src], val)
            ins = fn(h)
            if signal:
                ins.then_inc(sem, 1)
        e.thunks.append(thunk)

    def dma(self, queue, fn, reads=(), writes=()):
        e = self.eng[queue]
        lst = self.dma_sems[queue]
        i = self.dma_rr[queue]
        self.dma_rr[queue] = (i + 1) % len(lst)
        slot = lst[i]
        deps = self._deps(reads, writes)
        if slot[1] > 0:
            deps.append((slot[0], slot[1]))
        waits = self._waits(e, deps)
        slot[1] += 16
        tok = (slot[0], slot[1])
        for r in reads:
            r.r.append(tok)
        for w in writes:
            w.w = tok
            w.r = []
        sems = self.sems

        def thunk(h, waits=waits, fn=fn, sem=sems[slot[0]]):
            for src, val in waits:
                h.wait_ge(sems[src], val)
            fn(h).then_inc(sem, 16)
        e.thunks.append(thunk)

    def final_wait(self, engine, resources):
        e = self.eng[engine]
        deps = [r.w for r in resources if r.w is not None]
        waits = self._waits(e, deps)
        sems = self.sems

        def thunk(h, waits=waits):
            for src, val in waits:
                h.wait_ge(sems[src], val)
        e.thunks.append(thunk)

    def drain(self):
        e = self.eng["sync"]
        deps = []
        for q, lst in self.dma_sems.items():
            for slot in lst:
                if slot[1] > 0:
                    deps.append((slot[0], slot[1]))
        for k, e2 in self.eng.items():
            if e2.count > 0 and k != "sync":
                deps.append((("e", k), e2.count))
        waits = self._waits(e, deps)
        sems = self.sems

        def thunk(h, waits=waits):
            for src, val in waits:
                h.wait_ge(sems[src], val)
        e.thunks.append(thunk)

    def emit(self):
        self.drain()
        self.phase_id = getattr(self, "phase_id", 0) + 1
        with self.nc.Block() as block:
            for k, e in self.eng.items():
                if not e.thunks:
                    continue

                def body(h, thunks=list(e.thunks)):
                    for t in thunks:
                        t(h)
                getattr(block, k)(body)
        for e in self.eng.values():
            e.thunks = []


def build():
    nc = bass.Bass("TRN2", target_bir_lowering=False)

    def din(name, shape, dt=F32):
        if name in SKIP_INPUTS:
            return None
        INPUT_NAMES.append(name)
        return nc.dram_tensor(name, list(shape), dt, kind="ExternalInput").ap()

    def dout(name, shape, dt=F32):
        return nc.dram_tensor(name, list(shape), dt, kind="ExternalOutput").ap()

    def dscr(name, shape, dt=F32):
        kind = "ExternalOutput" if (DEBUG and name in ("ysp", "dbg_dst", "dbg_gate", "dbg_moe", "dbg_sc")) else "Internal"
        return nc.dram_tensor(name, list(shape), dt, kind=kind).ap()

    xc = din("xc", [1024, D])
    xl = din("xl", [2048, D])
    ropec = din("ropec", [2048, 128])
    ropes = din("ropes", [2048, 128])
    cache = din("cache", [4, 256, 256])
    cond = din("cond", [2, D])
    w_ada = din("w_ada", [D, 6 * D])
    b_ada = din("b_ada", [1, 6 * D])
    gains = din("gains", [4, D])
    w_in = din("w_in", [D, 3072])
    qkg = din("qkg", [2, 128])
    sink = din("sink", [1, 8])
    w_out = din("w_out", [D, D])
    w_router = din("w_router", [D, NE])
    rbias = din("rbias", [1, NE])
    wge = din("wge", [NE, D, 512])
    wue = din("wue", [NE, D, 512])
    wde = din("wde", [NE, 512, D])
    wgs = din("wgs", [D, 512])
    wus = din("wus", [D, 512])
    wds = din("wds", [512, D])
    consts = din("consts", [128, 7 * 128])

    yc = dout("yc", [1024, D])
    yl = dout("yl", [1024, D])
    ngk = dout("ngk", [1024, 256])
    ngv = dout("ngv", [1024, 256])
    nwk = dout("nwk", [1024, 256])
    nwv = dout("nwv", [1024, 256])

    modsp = dscr("modsp", [12, 128, D])

    R = {}

    def res(name):
        if name not in R:
            R[name] = Res(name)
        return R[name]

    with ExitStack() as st:
        s = Sched(nc)
        s.alloc_sems(st)
        st.enter_context(nc.allow_non_contiguous_dma(reason="small strided loads"))
        st.enter_context(nc.allow_low_precision(reason="bf16 matmul operands"))

        def sb(name, shape, dt=F32):
            return st.enter_context(nc.sbuf_tensor(name, list(shape), dt))

        def ps(name, shape, dt=F32):
            return st.enter_context(nc.psum_tensor(name, list(shape), dt))

        pp = [ps(f"pp{i}", [128, 512], F32) for i in range(6)]
        pt = [ps(f"pt{i}", [128, 1024], BF16) for i in range(2)]
        r_pp = [res(f"pp{i}") for i in range(6)]
        r_pt = [res(f"pt{i}") for i in range(2)]

        cf = sb("cf", [128, 7 * 128], F32)
        cb = sb("cb", [128, 7 * 128], BF16)
        s.dma("sync", lambda h: h.dma_start(out=cf[:], in_=consts[:, :]), writes=[res("cf")])
        s.op("vector", lambda h: h.tensor_copy(out=cb[:], in_=cf[:]), reads=[res("cf")], writes=[res("cb")])
        identb = cb[:, 0:128]
        onesb = cb[:, 256:384]

        ph0 = ExitStack()

        def sb0(name, shape, dt=F32):
            return ph0.enter_context(nc.sbuf_tensor(name, list(shape), dt))
        condT = sb0("condT", [128, 16, 2], F32)
        crep = sb0("crep", [128, 2, 16, 128], BF16)
        gbc = sb0("gbc", [128, 4, D], F32)
        for p in range(2):
            s.dma("sync", lambda h, p=p: h.dma_start(
                out=condT[:, :, p], in_=cond[p:p + 1, :].rearrange("r (c p) -> p (r c)", p=128)),
                writes=[res("condT")])
        s.dma("sync", lambda h: h.dma_start(out=gbc[:], in_=gains.partition_broadcast(128)), writes=[res("gbc")])
        for p in range(2):
            s.op("scalar", lambda h, p=p: h.activation(
                out=crep[:, p, :, :], in_=condT[:, :, p:p + 1].broadcast_to([128, 16, 128]), func=AF.Silu),
                reads=[res("condT")], writes=[res("crep")])
        wa = [sb0(f"wa{i}", [128, 16, 512], BF16) for i in range(2)]
        bb = [sb0(f"bb{i}", [128, 512], F32) for i in range(2)]
        mt = [sb0(f"mt{i}", [128, 512], F32) for i in range(4)]
        gain_of = {1: 0, 2: 1, 4: 2, 5: 3}
        n_mt = 0
        for j in range(24):
            which, cc = j // 4, j % 4
            b = j % 2
            s.dma("gpsimd", lambda h, b=b, j=j: h.dma_start(
                out=wa[b][:], in_=w_ada[:, j * 512:(j + 1) * 512].rearrange("(c p) n -> p c n", p=128)),
                writes=[res(f"wa{b}")])
            s.dma("sync", lambda h, b=b, j=j: h.dma_start(
                out=bb[b][:], in_=b_ada[:, j * 512:(j + 1) * 512].partition_broadcast(128)),
                writes=[res(f"bb{b}")])
            for p in range(2):
                pi = (j * 2 + p) % 6
                for k in range(16):
                    s.op("tensor", lambda h, p=p, k=k, b=b, pi=pi: h.matmul(
                        pp[pi][:], lhsT=crep[:, p, k, :], rhs=wa[b][:, k, :], start=(k == 0), stop=(k == 15)),
                        reads=[res("crep"), res(f"wa{b}")], writes=[r_pp[pi]], signal=(k == 15))
                m = mt[n_mt % 4]
                rm = res(f"mt{n_mt % 4}")
                n_mt += 1
                if which in (0, 3):
                    s.op("vector", lambda h, m=m, pi=pi, b=b: h.tensor_tensor(
                        out=m[:], in0=pp[pi][:], in1=bb[b][:], op=ALU.add),
                        reads=[r_pp[pi], res(f"bb{b}")], writes=[rm])
                else:
                    gsl = gbc[:, gain_of[which], cc * 512:(cc + 1) * 512]
                    s.op("vector", lambda h, m=m, pi=pi, b=b: h.tensor_tensor(
                        out=m[:], in0=pp[pi][:], in1=bb[b][:], op=ALU.add),
                        reads=[r_pp[pi], res(f"bb{b}")], writes=[rm])
                    add1 = 1.0 if which in (1, 4) else 0.0
                    s.op("vector", lambda h, m=m, gsl=gsl, add1=add1: h.scalar_tensor_tensor(
                        out=m[:], in0=m[:], scalar=add1, in1=gsl, op0=ALU.add, op1=ALU.mult),
                        reads=[rm, res("gbc")], writes=[rm])
                idx = which * 2 + p
                s.dma("sync", lambda h, m=m, idx=idx, cc=cc: h.dma_start(
                    out=modsp[idx, :, cc * 512:(cc + 1) * 512], in_=m[:]),
                    reads=[rm])

        s.emit()
        ph0.close()

        qsp = [dscr("qsp0", [1024, 2560], BF16), dscr("qsp1", [2048, 2560], BF16)]
        vsp = [dscr("vsp0", [1024, 512], BF16), dscr("vsp1", [2048, 512], BF16)]
        ysp = dscr("ysp", [2048, D])
        xbuf = dscr("xbuf", [NE * CAP, D], BF16)
        xbuf_s = dscr("xbuf_s", [2048, D], BF16)
        obuf_s = dscr("obuf_s", [2048, D], BF16)
        obuf = dscr("obuf", [NE * CAP, D], BF16)
        xin = [xc, xl]
        youts = [yc, yl]
        identf = cf[:, 0:128]
        onesf = cf[:, 256:384]
        ustrb = cb[:, 128:256]
        _bc = {}

        def bc_reg(h):
            if _bc.get("phase") != s.phase_id:
                _bc["r"] = h.to_reg(NE * CAP - 1)
                _bc["phase"] = s.phase_id
            return _bc["r"]

        mx = sb("mx", [128, 2, 4], F32)
        negm = sb("negm", [128, 2, 2], F32)
        Mall = sb("Mall", [128, 16, NE], BF16)
        dstall = sb("dstall", [128, 16, 8], I32)
        gall = sb("gall", [128, 16, 8], F32)
        NHOT = 8
        hotidx = sb("hotidx", [128, NHOT, 24], I32)
        sinkb = sb("sinkb", [128, 8], F32)
        g10 = sb("g10", [128, 10, 128], F32)
        rbb = sb("rbb", [128, NE], F32)
        s.op("vector", lambda h: h.memset(mx[:], 0.0), writes=[res("mx")])
        s.dma("sync", lambda h: h.dma_start(out=sinkb[:], in_=sink.partition_broadcast(128)), writes=[res("sinkb")])
        s.dma("sync", lambda h: h.dma_start(out=rbb[:], in_=rbias.partition_broadcast(128)), writes=[res("rbb")])
        for hh in range(10):
            s.dma("sync", lambda h, hh=hh: h.dma_start(
                out=g10[:, hh, :], in_=qkg[(0 if hh < 8 else 1):(1 if hh < 8 else 2), :].partition_broadcast(128)),
                writes=[res("g10")])

        def rstd_chain(ssq_ap, rs_ap, n, r_in, r_out):
            s.op("vector", lambda h: h.tensor_scalar(out=rs_ap, in0=ssq_ap, scalar1=1.0 / n, scalar2=EPS,
                                                     op0=ALU.mult, op1=ALU.add), reads=[r_in], writes=[r_out])
            s.op("scalar", lambda h: h.activation(out=rs_ap, in_=rs_ap, func=AF.Sqrt), reads=[r_out], writes=[r_out])
            s.op("vector", lambda h: h.reciprocal(out=rs_ap, in_=rs_ap), reads=[r_out], writes=[r_out])

        def transpose16(src_bf, r_src, dst, r_dst, dst_slices):
            for half in range(2):
                for c in range(8):
                    cc = half * 8 + c
                    s.op("tensor", lambda h, half=half, c=c, cc=cc: h.transpose(
                        out=pt[half][:, c * 128:(c + 1) * 128], in_=src_bf[:, cc * 128:(cc + 1) * 128], identity=identb),
                        reads=[r_src, res("cb")], writes=[r_pt[half]], signal=(c == 7))
                eng = "scalar" if half == 0 else "vector"
                o = dst_slices(half)
                i_ = pt[half][:, :].rearrange("p (c t) -> p c t", t=128)
                if eng == "scalar":
                    s.op("scalar", lambda h, o=o, i_=i_: h.copy(out=o, in_=i_), reads=[r_pt[half]], writes=[r_dst])
                else:
                    s.op("vector", lambda h, o=o, i_=i_: h.tensor_copy(out=o, in_=i_), reads=[r_pt[half]], writes=[r_dst])

        def qkv_phase(p):
            nt = 8 if p == 0 else 16
            with ExitStack() as ph:
                def sbp(name, shape, dt=F32):
                    return ph.enter_context(nc.sbuf_tensor(f"{name}_{p}", list(shape), dt))
                win = sbp("win", [128, 16, 3072], BF16)
                A1 = sbp("A1", [128, D]); B1 = sbp("B1", [128, D])
                xt = [sbp(f"xt{i}", [128, D]) for i in range(2)]
                hb = sbp("hb", [128, D], BF16)
                hT = sbp("hT", [128, 16, 128], BF16)
                qkall = sbp("qkall", [128, 3072])
                qk20 = sbp("qk20", [128, 20, 128])
                t1 = sbp("t1", [128, 20, 128]); t2 = sbp("t2", [128, 20, 128])
                qkb = sbp("qkb", [128, 20, 128], BF16)
                vb = sbp("vb", [128, 512], BF16)
                rc = sbp("rc", [128, 128]); rs_ = sbp("rs", [128, 128])
                st1 = sbp("st1", [128, 8]); st10 = sbp("st10", [128, 10]); st20 = sbp("st20", [128, 20]); g4 = sbp("g4", [128, 4])
                for n in range(6):
                    s.dma("gpsimd", lambda h, n=n: h.dma_start(
                        out=win[:, :, n * 512:(n + 1) * 512],
                        in_=w_in[:, n * 512:(n + 1) * 512].rearrange("(c p) n -> p c n", p=128)), writes=[res("win")])
                s.dma("sync", lambda h: h.dma_start(out=A1[:], in_=modsp[2 + p]), writes=[res("A1")])
                s.dma("sync", lambda h: h.dma_start(out=B1[:], in_=modsp[0 + p]), writes=[res("B1")])
                for i in range(nt):
                    own = i < 8
                    b = i % 2
                    rx = res(f"xt{b}")
                    s.dma("sync", lambda h, b=b, i=i: h.dma_start(out=xt[b][:], in_=xin[p][i * 128:(i + 1) * 128, :]), writes=[rx])
                    s.op("scalar", lambda h, b=b: h.activation(out=t1[:].rearrange("p a b -> p (a b)")[:, 0:D], in_=xt[b][:],
                                                               func=AF.Square, accum_out=st1[:, 0:1]),
                         reads=[rx], writes=[res("t1"), res("st1")])
                    rstd_chain(st1[:, 0:1], st1[:, 1:2], D, res("st1"), res("st1b"))
                    t1f = t1[:].rearrange("p a b -> p (a b)")[:, 0:D]
                    s.op("vector", lambda h, b=b: h.scalar_tensor_tensor(out=t1f, in0=xt[b][:], scalar=st1[:, 1:2], in1=A1[:],
                                                                         op0=ALU.mult, op1=ALU.mult),
                         reads=[rx, res("st1b"), res("A1")], writes=[res("t1")])
                    s.op("gpsimd", lambda h: h.tensor_tensor(out=hb[:], in0=t1f, in1=B1[:], op=ALU.add),
                         reads=[res("t1"), res("B1")], writes=[res("hb")])
                    transpose16(hb, res("hb"), hT, res("hT"), lambda half: hT[:, half * 8:(half + 1) * 8, :])
                    chunks = range(6) if own else (2, 5)
                    for n in chunks:
                        for k in range(16):
                            s.op("tensor", lambda h, n=n, k=k: h.matmul(pp[n][:], lhsT=hT[:, k, :], rhs=win[:, k, n * 512:(n + 1) * 512],
                                                                        start=(k == 0), stop=(k == 15)),
                                 reads=[res("hT"), res("win")], writes=[r_pp[n]], signal=(k == 15))
                        eng = "scalar" if n % 2 == 0 else "vector"
                        if eng == "scalar":
                            s.op("scalar", lambda h, n=n: h.copy(out=qkall[:, n * 512:(n + 1) * 512], in_=pp[n][:]),
                                 reads=[r_pp[n]], writes=[res("qkall")])
                        else:
                            s.op("vector", lambda h, n=n: h.tensor_copy(out=qkall[:, n * 512:(n + 1) * 512], in_=pp[n][:]),
                                 reads=[r_pp[n]], writes=[res("qkall")])
                    h0 = 0 if own else 8
                    nn = 10 - h0
                    src_n = qkall[:, h0 * 128:1280].rearrange("p (a b) -> p a b", b=128)
                    s.op("scalar", lambda h, src_n=src_n, h0=h0: h.activation(out=t2[:, h0:10, :], in_=src_n, func=AF.Square),
                         reads=[res("qkall")], writes=[res("t2")])
                    s.op("vector", lambda h, h0=h0: h.tensor_reduce(out=st10[:, h0:10], in_=t2[:, h0:10, :], axis=AX.X, op=ALU.add),
                         reads=[res("t2")], writes=[res("st10")])
                    rstd_chain(st10[:, h0:10], st10[:, h0:10], 128, res("st10"), res("st10"))
                    s.op("vector", lambda h, src_n=src_n, h0=h0, nn=nn: h.tensor_tensor(
                        out=qk20[:, h0:10, :], in0=src_n, in1=st10[:, h0:10].unsqueeze(2).broadcast_to([128, nn, 128]), op=ALU.mult),
                        reads=[res("qkall"), res("st10")], writes=[res("qk20")])
                    s.op("vector", lambda h, h0=h0: h.tensor_tensor(out=qk20[:, h0:10, :], in0=qk20[:, h0:10, :], in1=g10[:, h0:10, :], op=ALU.mult),
                         reads=[res("qk20"), res("g10")], writes=[res("qk20")])
                    w0 = 10 if own else 18
                    c0 = 1536 + (w0 - 10) * 128
                    s.op("scalar", lambda h, w0=w0, c0=c0: h.copy(out=qk20[:, w0:20, :], in_=qkall[:, c0:2816].rearrange("p (a b) -> p a b", b=128)),
                         reads=[res("qkall")], writes=[res("qk20")])
                    rows = slice(i * 128, (i + 1) * 128)
                    if p == 0:
                        for (dst, src_ap) in ((ngk, qk20[:, 8:10, :].rearrange("p a b -> p (a b)")), (ngv, qkall[:, 1280:1536]),
                                              (nwk, qkall[:, 2560:2816]), (nwv, qkall[:, 2816:3072])):
                            s.dma("sync", lambda h, dst=dst, src_ap=src_ap, rows=rows: h.dma_start(out=dst[rows, :], in_=src_ap),
                                  reads=[res("qk20"), res("qkall")])
                        s.op("scalar", lambda h: h.copy(out=qkb[:], in_=qk20[:]), reads=[res("qk20")], writes=[res("qkb")])
                    else:
                        s.dma("sync", lambda h, rows=rows: h.dma_start(out=rc[:], in_=ropec[rows, :]), writes=[res("rc")])
                        s.dma("sync", lambda h, rows=rows: h.dma_start(out=rs_[:], in_=ropes[rows, :]), writes=[res("rs")])
                        groups = [(0, 20)] if own else [(8, 10), (18, 20)]
                        for (a, bnd) in groups:
                            nh = bnd - a
                            s.op("vector", lambda h, a=a, bnd=bnd, nh=nh: h.tensor_tensor(
                                out=t1[:, a:bnd, :], in0=qk20[:, a:bnd, :], in1=rc[:].unsqueeze(1).broadcast_to([128, nh, 128]), op=ALU.mult),
                                reads=[res("qk20"), res("rc")], writes=[res("t1")])
                            for pr in range(2):
                                for hf in range(2):
                                    o_ = t2[:, a:bnd, pr * 64 + hf * 32: pr * 64 + hf * 32 + 32]
                                    i_ = qk20[:, a:bnd, pr * 64 + (1 - hf) * 32: pr * 64 + (1 - hf) * 32 + 32]
                                    sn = rs_[:, pr * 64 + hf * 32: pr * 64 + hf * 32 + 32].unsqueeze(1).broadcast_to([128, nh, 32])
                                    s.op("gpsimd", lambda h, o_=o_, i_=i_, sn=sn: h.tensor_tensor(out=o_, in0=i_, in1=sn, op=ALU.mult),
                                         reads=[res("qk20"), res("rs")], writes=[res("t2")])
                            s.op("vector", lambda h, a=a, bnd=bnd: h.tensor_tensor(out=qkb[:, a:bnd, :], in0=t1[:, a:bnd, :], in1=t2[:, a:bnd, :], op=ALU.add),
                                 reads=[res("t1"), res("t2")], writes=[res("qkb")])
                    hs = [(0, 20)] if own else [(8, 10), (18, 20)]
                    for (a, bnd) in hs:
                        s.op("scalar", lambda h, a=a, bnd=bnd: h.activation(out=t1[:, a:bnd, :], in_=qkb[:, a:bnd, :], func=AF.Square),
                             reads=[res("qkb")], writes=[res("t1")])
                        s.op("vector", lambda h, a=a, bnd=bnd: h.tensor_reduce(out=st20[:, a:bnd], in_=t1[:, a:bnd, :], axis=AX.X, op=ALU.add),
                             reads=[res("t1")], writes=[res("st20")])
                    grp = [(0, 0, 8), (1, 8, 10), (2, 10, 18), (3, 18, 20)] if own else [(1, 8, 10), (3, 18, 20)]
                    for (gi_, a, bnd) in grp:
                        s.op("vector", lambda h, gi_=gi_, a=a, bnd=bnd: h.tensor_reduce(out=g4[:, gi_:gi_ + 1], in_=st20[:, a:bnd], axis=AX.X, op=ALU.max),
                             reads=[res("st20")], writes=[res("g4")])
                        s.op("vector", lambda h, gi_=gi_: h.tensor_tensor(out=mx[:, p, gi_:gi_ + 1], in0=mx[:, p, gi_:gi_ + 1], in1=g4[:, gi_:gi_ + 1], op=ALU.max),
                             reads=[res("g4"), res("mx")], writes=[res("mx")])
                    s.op("scalar", lambda h: h.copy(out=vb[:, 0:256], in_=qkall[:, 1280:1536]), reads=[res("qkall")], writes=[res("vb")])
                    s.op("scalar", lambda h: h.copy(out=vb[:, 256:512], in_=qkall[:, 2816:3072]), reads=[res("qkall")], writes=[res("vb")])
                    s.dma("sync", lambda h, rows=rows: h.dma_start(out=qsp[p][rows, :], in_=qkb[:].rearrange("p a b -> p (a b)")),
                          reads=[res("qkb")])
                    s.dma("sync", lambda h, rows=rows: h.dma_start(out=vsp[p][rows, :], in_=vb[:]),
                          reads=[res("vb")])
                s.emit()

        def attn_phase(p, OT):
            nq_t = 8
            nloc = 8 if p == 0 else 16
            nkt = 8 if p == 0 else 18
            koff = 0 if p == 0 else 2
            with ExitStack() as ph:
                def sbp(name, shape, dt=F32):
                    return ph.enter_context(nc.sbuf_tensor(f"{name}_a{p}", list(shape), dt))
                QT = [sbp("QTg", [128, 8, 1024], BF16), sbp("QTw", [128, 8, 1024], BF16)]
                KT = [sbp("KTg", [128, 2, nkt * 128], BF16), sbp("KTw", [128, 2, nkt * 128], BF16)]
                V = sbp("V", [128, nkt, 512], BF16)
                qt = [sbp(f"qt{i}", [128, 2560], BF16) for i in range(2)]
                pb = [sbp(f"pb{i}", [128, 512], BF16) for i in range(4)]
                rec = [sbp(f"rec{i}", [128, 512]) for i in range(2)]
                SE = sbp("SE", [128, 8])
                m4 = sbp("m4", [128, 4]); dg = sbp("dg", [128, 4]); mb4 = sbp("mb4", [128, 4])
                cft = sbp("cft", [128, 512]); cbt = sbp("cbt", [128, 512], BF16); st4 = sbp("st4", [128, 4])
                if p == 1:
                    for kt in range(2):
                        for which, kind in ((0, 0), (2, 1)):
                            s.dma("sync", lambda h, kt=kt, which=which: h.dma_start(out=cft[:, 0:256], in_=cache[which, kt * 128:(kt + 1) * 128, :]),
                                  writes=[res("cft")])
                            s.op("vector", lambda h: h.tensor_copy(out=cbt[:, 0:256], in_=cft[:, 0:256]), reads=[res("cft")], writes=[res("cbt")])
                            s.op("scalar", lambda h: h.activation(out=cft[:, 256:512], in_=cbt[:, 0:256], func=AF.Square),
                                 reads=[res("cbt")], writes=[res("cft2")])
                            s.op("vector", lambda h: h.tensor_reduce(out=st4[:, 0:2], in_=cft[:, 256:512].rearrange("p (a b) -> p a b", b=128), axis=AX.X, op=ALU.add),
                                 reads=[res("cft2")], writes=[res("st4")])
                            s.op("vector", lambda h: h.tensor_reduce(out=st4[:, 2:3], in_=st4[:, 0:2], axis=AX.X, op=ALU.max),
                                 reads=[res("st4")], writes=[res("st4")])
                            col = 1 if kind == 0 else 3
                            s.op("vector", lambda h, col=col: h.tensor_tensor(out=mx[:, 1, col:col + 1], in0=mx[:, 1, col:col + 1], in1=st4[:, 2:3], op=ALU.max),
                                 reads=[res("st4"), res("mx")], writes=[res("mx")])
                            for n in range(2):
                                s.op("tensor", lambda h, n=n: h.transpose(out=pt[0][:, n * 128:(n + 1) * 128], in_=cbt[:, n * 128:(n + 1) * 128], identity=identb),
                                     reads=[res("cbt"), res("cb")], writes=[r_pt[0]], signal=(n == 1))
                            s.op("vector", lambda h, kt=kt, kind=kind: h.tensor_copy(
                                out=KT[kind][:, :, kt * 128:(kt + 1) * 128], in_=pt[0][:, 0:256].rearrange("p (a b) -> p a b", b=128)),
                                reads=[r_pt[0]], writes=[res(f"KT{kind}")])
                        for which, off in ((1, 0), (3, 256)):
                            s.dma("sync", lambda h, kt=kt, which=which: h.dma_start(out=cft[:, 0:256], in_=cache[which, kt * 128:(kt + 1) * 128, :]),
                                  writes=[res("cft")])
                            s.op("vector", lambda h, kt=kt, off=off: h.tensor_copy(out=V[:, kt, off:off + 256], in_=cft[:, 0:256]),
                                 reads=[res("cft")], writes=[res("V")])
                for i in range(nloc):
                    b = i % 2
                    rows = slice(i * 128, (i + 1) * 128)
                    s.dma("sync", lambda h, b=b, rows=rows: h.dma_start(out=qt[b][:], in_=qsp[p][rows, :]),
                          writes=[res(f"qt{b}")])
                    s.dma("sync", lambda h, i=i, rows=rows: h.dma_start(out=V[:, koff + i, :], in_=vsp[p][rows, :]),
                          writes=[res("V")])
                    if i < 8:
                        for kind in range(2):
                            c0 = 0 if kind == 0 else 1280
                            for hh in range(8):
                                s.op("tensor", lambda h, b=b, hh=hh, c0=c0, kind=kind: h.transpose(
                                    out=pt[kind][:, hh * 128:(hh + 1) * 128], in_=qt[b][:, c0 + hh * 128:c0 + (hh + 1) * 128], identity=identb),
                                    reads=[res(f"qt{b}"), res("cb")], writes=[r_pt[kind]], signal=(hh == 7))
                            o_ = QT[kind][:, :, i * 128:(i + 1) * 128]
                            i_ = pt[kind][:, :].rearrange("p (a b) -> p a b", b=128)
                            if kind == 0:
                                s.op("scalar", lambda h, o_=o_, i_=i_: h.copy(out=o_, in_=i_), reads=[r_pt[kind]], writes=[res(f"QT{kind}")])
                            else:
                                s.op("vector", lambda h, o_=o_, i_=i_: h.tensor_copy(out=o_, in_=i_), reads=[r_pt[kind]], writes=[res(f"QT{kind}")])
                    for kind in range(2):
                        c0 = 1024 if kind == 0 else 2304
                        for n in range(2):
                            s.op("tensor", lambda h, b=b, n=n, c0=c0, kind=kind: h.transpose(
                                out=pt[kind][:, n * 128:(n + 1) * 128], in_=qt[b][:, c0 + n * 128:c0 + (n + 1) * 128], identity=identb),
                                reads=[res(f"qt{b}"), res("cb")], writes=[r_pt[kind]], signal=(n == 1))
                        kk = koff + i
                        s.op("vector", lambda h, kk=kk, kind=kind: h.tensor_copy(
                            out=KT[kind][:, :, kk * 128:(kk + 1) * 128], in_=pt[kind][:, 0:256].rearrange("p (a b) -> p a b", b=128)),
                            reads=[r_pt[kind]], writes=[res(f"KT{kind}")])
                if p == 1:
                    zt = sbp("zt", [128, 8192], BF16)
                    s.op("gpsimd", lambda h: h.memset(zt[:], 0.0), writes=[res("zt")])
                    for e in range(NE):
                        s.dma("sync", lambda h, e=e: h.dma_start(
                            out=obuf[e * CAP + 512:(e + 1) * CAP, :].rearrange("(p r) d -> p (r d)", p=128), in_=zt[:]),
                            reads=[res("zt")])
                s.op("tensor", lambda h: h.transpose(out=pp[0][0:4, 0:128], in_=mx[:, p, :], identity=identf),
                     reads=[res("mx"), res("cf")], writes=[r_pp[0]])
                s.op("vector", lambda h: h.tensor_reduce(out=m4[0:4, 0:1], in_=pp[0][0:4, 0:128], axis=AX.X, op=ALU.max),
                     reads=[r_pp[0]], writes=[res("m4")])
                s.op("vector", lambda h: h.tensor_scalar(out=dg[0:4, 0:4], in0=identf[0:4, 0:4], scalar1=m4[0:4, 0:1], scalar2=None, op0=ALU.mult),
                     reads=[res("m4"), res("cf")], writes=[res("dg")])
                s.op("tensor", lambda h: h.matmul(pp[1][:, 0:4], lhsT=onesf[0:4, 0:128], rhs=dg[0:4, 0:4], start=True, stop=True),
                     reads=[res("dg"), res("cf")], writes=[r_pp[1]])
                s.op("vector", lambda h: h.tensor_copy(out=mb4[:], in_=pp[1][:, 0:4]), reads=[r_pp[1]], writes=[res("mb4")])
                for kind in range(2):
                    s.op("vector", lambda h, kind=kind: h.tensor_tensor(out=negm[:, p, kind:kind + 1], in0=mb4[:, 2 * kind:2 * kind + 1],
                                                                        in1=mb4[:, 2 * kind + 1:2 * kind + 2], op=ALU.mult),
                         reads=[res("mb4")], writes=[res("negm")])
                s.op("scalar", lambda h: h.activation(out=negm[:, p, :], in_=negm[:, p, :], func=AF.Sqrt), reads=[res("negm")], writes=[res("negm")])
                s.op("vector", lambda h: h.tensor_scalar(out=negm[:, p, :], in0=negm[:, p, :], scalar1=-SCALE, scalar2=None, op0=ALU.mult),
                     reads=[res("negm")], writes=[res("negm")])
                s.op("scalar", lambda h: h.activation(out=SE[:], in_=sinkb[:], func=AF.Exp, bias=negm[:, p, 1:2], scale=1.0),
                     reads=[res("negm"), res("sinkb")], writes=[res("SE")])

                jobs = []
                if p == 0:
                    for sq in range(4):
                        for kind in range(2):
                            for n in range(2):
                                for qc in range(2):
                                    h0 = 4 * n + 2 * qc
                                    q_ap = QT[kind][:, h0:h0 + 2, sq * 256:(sq + 1) * 256]
                                    keys = [(sq * 2 + kt, None) for kt in range(2)]
                                    o_ap = OT[:, kind * 8 + h0:kind * 8 + h0 + 2, sq * 256:(sq + 1) * 256]
                                    jobs.append((kind, n, q_ap, keys, o_ap, (h0, 2, 256)))
                else:
                    for n in range(2):
                        for qb in range(8):
                            q_ap = QT[0][:, 4 * n:4 * n + 4, qb * 128:(qb + 1) * 128]
                            o_ap = OT[:, 4 * n:4 * n + 4, qb * 128:(qb + 1) * 128]
                            jobs.append((0, n, q_ap, [(kt, None) for kt in range(18)], o_ap, (4 * n, 4, 128)))
                    for n in range(2):
                        for qb in range(8):
                            q_ap = QT[1][:, 4 * n:4 * n + 4, qb * 128:(qb + 1) * 128]
                            o_ap = OT[:, 8 + 4 * n:8 + 4 * n + 4, qb * 128:(qb + 1) * 128]
                            prev = (2 + qb - 1, 384) if qb > 0 else (2 + 8, 640)
                            nxt = (2 + qb + 1, 512) if qb < 7 else (2 + 8, 768)
                            keys = [(0, None), (1, None), prev, (2 + qb, None), nxt]
                            jobs.append((1, n, q_ap, keys, o_ap, (4 * n, 4, 128)))
                npb = 0
                for ji, (kind, n, q_ap, keys, o_ap, (h0, nh, nqq)) in enumerate(jobs):
                    po, psm = 2 + (ji % 2), 4 + (ji % 2)
                    def emit_S(ki):
                        kt = keys[ki][0]
                        sbank = ki % 2
                        s.op("tensor", lambda h, sbank=sbank, kind=kind, n=n, kt=kt, q_ap=q_ap: h.matmul(
                            pp[sbank][:], lhsT=KT[kind][:, n, kt * 128:(kt + 1) * 128], rhs=q_ap, start=True, stop=True),
                            reads=[res(f"KT{kind}"), res(f"QT{kind}")], writes=[r_pp[sbank]])
                    emit_S(0)
                    for ki, (kt, moff) in enumerate(keys):
                        sbank = ki % 2
                        pi = npb % 4
                        npb += 1
                        s.op("scalar", lambda h, pi=pi, sbank=sbank, kind=kind: h.activation(
                            out=pb[pi][:], in_=pp[sbank][:], func=AF.Exp, bias=negm[:, p, kind:kind + 1], scale=SCALE),
                            reads=[r_pp[sbank], res("negm")], writes=[res(f"pb{pi}")])
                        if ki + 1 < len(keys):
                            emit_S(ki + 1)
                        if moff is not None:
                            mk = cb[:, moff:moff + 128].unsqueeze(1).broadcast_to([128, 4, 128])
                            s.op("gpsimd", lambda h, pi=pi, mk=mk: h.tensor_tensor(
                                out=pb[pi][:].rearrange("p (a b) -> p a b", b=128), in0=pb[pi][:].rearrange("p (a b) -> p a b", b=128), in1=mk, op=ALU.mult),
                                reads=[res(f"pb{pi}"), res("cb")], writes=[res(f"pb{pi}")])
                        vs = V[:, kt, kind * 256 + n * 128: kind * 256 + (n + 1) * 128]
                        last = ki == len(keys) - 1
                        s.op("tensor", lambda h, po=po, vs=vs, pi=pi, ki=ki, last=last: h.matmul(
                            pp[po][:], lhsT=vs, rhs=pb[pi][:], start=(ki == 0), stop=last),
                            reads=[res("V"), res(f"pb{pi}")], writes=[r_pp[po]], signal=last)
                        s.op("tensor", lambda h, psm=psm, pi=pi, ki=ki, last=last: h.matmul(
                            pp[psm][:], lhsT=onesb, rhs=pb[pi][:], start=(ki == 0), stop=last),
                            reads=[res("cb"), res(f"pb{pi}")], writes=[r_pp[psm]], signal=True)
                    rb = ji % 2
                    if kind == 1:
                        se = SE[:, h0:h0 + nh].unsqueeze(2).broadcast_to([128, nh, nqq])
                        s.op("vector", lambda h, rb=rb, psm=psm, se=se, nqq=nqq: h.tensor_tensor(
                            out=rec[rb][:].rearrange("p (a b) -> p a b", b=nqq), in0=pp[psm][:].rearrange("p (a b) -> p a b", b=nqq), in1=se, op=ALU.add),
                            reads=[r_pp[psm], res("SE")], writes=[res(f"rec{rb}")])
                        s.op("vector", lambda h, rb=rb: h.reciprocal(out=rec[rb][:], in_=rec[rb][:]), reads=[res(f"rec{rb}")], writes=[res(f"rec{rb}")])
                    else:
                        s.op("vector", lambda h, rb=rb, psm=psm: h.reciprocal(out=rec[rb][:], in_=pp[psm][:]), reads=[r_pp[psm]], writes=[res(f"rec{rb}")])
                    s.op("vector", lambda h, rb=rb, po=po, o_ap=o_ap, nqq=nqq: h.tensor_tensor(
                        out=o_ap, in0=pp[po][:].rearrange("p (a b) -> p a b", b=nqq), in1=rec[rb][:].rearrange("p (a b) -> p a b", b=nqq), op=ALU.mult),
                        reads=[r_pp[po], res(f"rec{rb}")], writes=[res("OT")])
                s.emit()

        def post_phase(p, OT):
            with ExitStack() as ph:
                def sbp(name, shape, dt=F32):
                    return ph.enter_context(nc.sbuf_tensor(f"{name}_p{p}", list(shape), dt))
                wo = sbp("wo", [128, 16, D], BF16)
                wr = sbp("wr", [128, 16, NE], BF16)
                G1 = sbp("G1", [128, D]); A2 = sbp("A2", [128, D]); B2 = sbp("B2", [128, D])
                xt = sbp("xt", [128, D]); yt = sbp("yt", [128, D]); tf = sbp("tf", [128, D])
                h2b = sbp("h2b", [128, D], BF16); h2T = sbp("h2T", [128, 16, 128], BF16)
                st = sbp("st", [128, 8])
                sc = sbp("sc", [128, NE]); sel = sbp("sel", [128, NE]); srt = sbp("srt", [128, 8, 8]); gs = sbp("gs", [128, 8])
                gs8 = sbp("gs8", [128, 8]); gm = sbp("gm", [128, 8]); gneg = sbp("gneg", [128, 8]); selm = sbp("selm", [128, NE])
                top8 = sbp("top8", [128, 8]); Mf = sbp("Mf", [128, NE]); wsel = sbp("wsel", [128, NE]); den = sbp("den", [128, 2])
                posf = sbp("posf", [128, NE]); vv = sbp("vv", [128, NE]); d8 = sbp("d8", [128, 8]); oh = sbp("oh", [128, NE])
                g8 = sbp("g8", [128, 8]); eoff = sbp("eoff", [128, NE])
                for n in range(4):
                    s.dma("gpsimd", lambda h, n=n: h.dma_start(out=wo[:, :, n * 512:(n + 1) * 512],
                                                              in_=w_out[:, n * 512:(n + 1) * 512].rearrange("(c p) n -> p c n", p=128)), writes=[res("wo")])
                s.dma("gpsimd", lambda h: h.dma_start(out=wr[:], in_=w_router.rearrange("(c p) n -> p c n", p=128)), writes=[res("wr")])
                s.dma("sync", lambda h: h.dma_start(out=G1[:], in_=modsp[4 + p]), writes=[res("G1")])
                s.dma("sync", lambda h: h.dma_start(out=A2[:], in_=modsp[8 + p]), writes=[res("A2")])
                s.dma("sync", lambda h: h.dma_start(out=B2[:], in_=modsp[6 + p]), writes=[res("B2")])
                s.op("gpsimd", lambda h: h.iota(eoff[:], pattern=[[CAP, NE]], base=1, channel_multiplier=0, allow_small_or_imprecise_dtypes=True),
                     writes=[res("eoff")])
                for i in range(8):
                    gi = p * 8 + i
                    rows = slice(i * 128, (i + 1) * 128)
                    grow = slice(gi * 128, (gi + 1) * 128)
                    for n in range(4):
                        for mh in range(16):
                            s.op("tensor", lambda h, n=n, mh=mh, i=i: h.matmul(pp[n][:], lhsT=OT[:, mh, i * 128:(i + 1) * 128], rhs=wo[:, mh, n * 512:(n + 1) * 512],
                                                                              start=(mh == 0), stop=(mh == 15)),
                                 reads=[res("OT"), res("wo")], writes=[r_pp[n]], signal=(mh == 15))
                        s.op("scalar", lambda h, n=n: h.activation(out=tf[:, n * 512:(n + 1) * 512], in_=pp[n][:], func=AF.Square, accum_out=st[:, n:n + 1]),
                             reads=[r_pp[n]], writes=[res("tf"), res("st")])
                    s.op("vector", lambda h: h.tensor_reduce(out=st[:, 4:5], in_=st[:, 0:4], axis=AX.X, op=ALU.add), reads=[res("st")], writes=[res("st")])
                    rstd_chain(st[:, 4:5], st[:, 5:6], D, res("st"), res("st"))
                    s.dma("sync", lambda h, rows=rows: h.dma_start(out=xt[:], in_=xin[p][rows, :]), writes=[res("xt")])
                    for n in range(4):
                        cs = slice(n * 512, (n + 1) * 512)
                        s.op("vector", lambda h, n=n, cs=cs: h.scalar_tensor_tensor(out=tf[:, cs], in0=pp[n][:], scalar=st[:, 5:6], in1=G1[:, cs],
                                                                                    op0=ALU.mult, op1=ALU.mult),
                             reads=[r_pp[n], res("st"), res("G1")], writes=[res("tf")])
                    s.op("gpsimd", lambda h: h.tensor_tensor(out=yt[:], in0=tf[:], in1=xt[:], op=ALU.add), reads=[res("tf"), res("xt")], writes=[res("yt")])
                    s.dma("sync", lambda h, grow=grow: h.dma_start(out=ysp[grow, :], in_=yt[:]), reads=[res("yt")])
                    s.op("scalar", lambda h: h.activation(out=tf[:], in_=yt[:], func=AF.Square, accum_out=st[:, 6:7]),
                         reads=[res("yt")], writes=[res("tf"), res("st")])
                    rstd_chain(st[:, 6:7], st[:, 7:8], D, res("st"), res("st"))
                    s.op("vector", lambda h: h.scalar_tensor_tensor(out=tf[:], in0=yt[:], scalar=st[:, 7:8], in1=A2[:], op0=ALU.mult, op1=ALU.mult),
                         reads=[res("yt"), res("st"), res("A2")], writes=[res("tf")])
                    s.op("gpsimd", lambda h: h.tensor_tensor(out=h2b[:], in0=tf[:], in1=B2[:], op=ALU.add), reads=[res("tf"), res("B2")], writes=[res("h2b")])
                    s.dma("sync", lambda h, gi=gi: h.dma_start(out=xbuf_s[gi * 128:(gi + 1) * 128, :], in_=h2b[:]),
                          reads=[res("h2b")])
                    transpose16(h2b, res("h2b"), h2T, res("h2T"), lambda half: h2T[:, half * 8:(half + 1) * 8, :])
                    for k in range(16):
                        s.op("tensor", lambda h, k=k: h.matmul(pp[4][:, 0:NE], lhsT=h2T[:, k, :], rhs=wr[:, k, :], start=(k == 0), stop=(k == 15)),
                             reads=[res("h2T"), res("wr")], writes=[r_pp[4]], signal=(k == 15))
                    s.op("scalar", lambda h: h.activation(out=sc[:], in_=pp[4][:, 0:NE], func=AF.Sigmoid), reads=[r_pp[4]], writes=[res("sc")])
                    s.op("vector", lambda h: h.tensor_tensor(out=sel[:], in0=sc[:], in1=rbb[:], op=ALU.add), reads=[res("sc"), res("rbb")], writes=[res("sel")])
                    for g in range(8):
                        s.op("vector", lambda h, g=g: h.max(out=srt[:, g, :], in_=sel[:, g * 8:(g + 1) * 8]), reads=[res("sel")], writes=[res("srt")])
                    s.op("vector", lambda h: h.tensor_tensor(out=gs[:], in0=srt[:, :, 0], in1=srt[:, :, 1], op=ALU.add), reads=[res("srt")], writes=[res("gs")])
                    s.op("vector", lambda h: h.max(out=gs8[:], in_=gs[:]), reads=[res("gs")], writes=[res("gs8")])
                    s.op("vector", lambda h: h.tensor_scalar(out=gm[:], in0=gs[:], scalar1=gs8[:, 3:4], scalar2=None, op0=ALU.is_ge),
                         reads=[res("gs"), res("gs8")], writes=[res("gm")])
                    s.op("vector", lambda h: h.tensor_scalar(out=gneg[:], in0=gm[:], scalar1=-1.0, scalar2=1e9, op0=ALU.add, op1=ALU.mult),
                         reads=[res("gm")], writes=[res("gneg")])
                    for g in range(8):
                        s.op("vector", lambda h, g=g: h.tensor_scalar(out=selm[:, g * 8:(g + 1) * 8], in0=sel[:, g * 8:(g + 1) * 8],
                                                                      scalar1=gm[:, g:g + 1], scalar2=gneg[:, g:g + 1], op0=ALU.mult, op1=ALU.add),
                             reads=[res("sel"), res("gm"), res("gneg")], writes=[res("selm")])
                    s.op("vector", lambda h: h.max(out=top8[:], in_=selm[:]), reads=[res("selm")], writes=[res("top8")])
                    s.op("vector", lambda h: h.tensor_scalar(out=Mf[:], in0=selm[:], scalar1=top8[:, 7:8], scalar2=None, op0=ALU.is_ge),
                         reads=[res("selm"), res("top8")], writes=[res("Mf")])
                    s.op("vector", lambda h, gi=gi: h.tensor_copy(out=Mall[:, gi, :], in_=Mf[:]), reads=[res("Mf")], writes=[res("Mall")])
                    s.op("vector", lambda h: h.tensor_tensor(out=wsel[:], in0=sc[:], in1=Mf[:], op=ALU.mult), reads=[res("sc"), res("Mf")], writes=[res("wsel")])
                    s.op("vector", lambda h: h.tensor_reduce(out=den[:, 0:1], in_=wsel[:], axis=AX.X, op=ALU.add), reads=[res("wsel")], writes=[res("den")])
                    s.op("vector", lambda h: h.reciprocal(out=den[:, 1:2], in_=den[:, 0:1]), reads=[res("den")], writes=[res("den")])
                    s.op("vector", lambda h: h.tensor_scalar(out=wsel[:], in0=wsel[:], scalar1=den[:, 1:2], scalar2=2.5, op0=ALU.mult, op1=ALU.mult),
                         reads=[res("wsel"), res("den")], writes=[res("wsel")])
                    s.op("tensor", lambda h, gi=gi: h.matmul(pp[5][:, 0:NE], lhsT=ustrb, rhs=Mall[:, gi, :], start=True, stop=(gi == 0)),
                         reads=[res("Mall"), res("cb")], writes=[r_pp[5]], signal=(gi == 0))
                    for j in range(gi):
                        s.op("tensor", lambda h, j=j, gi=gi: h.matmul(pp[5][:, 0:NE], lhsT=onesb, rhs=Mall[:, j, :], start=False, stop=(j == gi - 1)),
                             reads=[res("Mall"), res("cb")], writes=[r_pp[5]], signal=(j == gi - 1))
                    s.op("vector", lambda h: h.tensor_scalar(out=posf[:], in0=pp[5][:, 0:NE], scalar1=float(CAP - 1), scalar2=None, op0=ALU.min),
                         reads=[r_pp[5]], writes=[res("posf")])
                    s.op("vector", lambda h: h.tensor_tensor(out=vv[:], in0=posf[:], in1=eoff[:], op=ALU.add), reads=[res("posf"), res("eoff")], writes=[res("vv")])
                    s.op("vector", lambda h: h.tensor_tensor(out=vv[:], in0=vv[:], in1=Mf[:], op=ALU.mult), reads=[res("vv"), res("Mf")], writes=[res("vv")])
                    s.op("vector", lambda h: h.max(out=d8[:], in_=vv[:]), reads=[res("vv")], writes=[res("d8")])
                    for k in range(8):
                        s.op("vector", lambda h, k=k: h.tensor_scalar(out=oh[:], in0=vv[:], scalar1=d8[:, k:k + 1], scalar2=None, op0=ALU.is_equal),
                             reads=[res("vv"), res("d8")], writes=[res("oh")])
                        s.op("vector", lambda h, k=k: h.tensor_tensor(out=oh[:], in0=oh[:], in1=wsel[:], op=ALU.mult), reads=[res("oh"), res("wsel")], writes=[res("oh")])
                        s.op("vector", lambda h, k=k: h.tensor_reduce(out=g8[:, k:k + 1], in_=oh[:], axis=AX.X, op=ALU.add), reads=[res("oh")], writes=[res("g8")])
                    s.op("vector", lambda h: h.tensor_scalar(out=d8[:], in0=d8[:], scalar1=-1.0, scalar2=None, op0=ALU.add), reads=[res("d8")], writes=[res("d8")])
                    s.op("vector", lambda h, gi=gi: h.tensor_copy(out=dstall[:, gi, :], in_=d8[:]), reads=[res("d8")], writes=[res("dstall")])
                    s.op("vector", lambda h, gi=gi: h.tensor_copy(out=gall[:, gi, :], in_=g8[:]), reads=[res("g8")], writes=[res("gall")])
                    if DEBUG:
                        s.dma("sync", lambda h, gi=gi: h.dma_start(out=dbg_sc[gi], in_=sc[:]), reads=[res("sc")])
                    for k in range(8):
                        s.dma("gpsimd", lambda h, k=k, gi=gi: h.indirect_dma_start(
                            out=xbuf[:, :], out_offset=bass.IndirectOffsetOnAxis(ap=dstall[:, gi, k:k + 1], axis=0),
                            in_=h2b[:], in_offset=None, bounds_check=None),
                            reads=[res("h2b"), res("dstall")])
                if p == 1:
                    cntb = sbp("cntb", [128, NE]); flg = sbp("flg", [128, NE]); cum = sbp("cum", [128, NE]); one64 = sbp("one64", [128, NE])
                    eix = sbp("eix", [128, NE]); selj = sbp("selj", [128, NE]); ev = sbp("ev", [128, NHOT])
                    mult24 = sbp("mult24", [128, 24]); offs24 = sbp("offs24", [128, 24]); idxf = sbp("idxf", [128, NHOT, 24])
                    for j in range(16):
                        s.op("tensor", lambda h, j=j: h.matmul(pp[5][:, 0:NE], lhsT=onesb, rhs=Mall[:, j, :], start=(j == 0), stop=(j == 15)),
                             reads=[res("Mall"), res("cb")], writes=[r_pp[5]], signal=(j == 15))
                    s.op("vector", lambda h: h.tensor_scalar(out=flg[:], in0=pp[5][:, 0:NE], scalar1=512.0, scalar2=None, op0=ALU.is_gt),
                         reads=[r_pp[5]], writes=[res("flg")])
                    s.op("vector", lambda h: h.memset(one64[:], 1.0), writes=[res("one64")])
                    s.op("vector", lambda h: h.tensor_tensor_scan(out=cum[:], data0=one64[:], data1=flg[:], initial=0.0, op0=ALU.mult, op1=ALU.add),
                         reads=[res("one64"), res("flg")], writes=[res("cum")])
                    s.op("vector", lambda h: h.tensor_tensor(out=cum[:], in0=cum[:], in1=flg[:], op=ALU.subtract), reads=[res("cum"), res("flg")], writes=[res("cum")])
                    s.op("gpsimd", lambda h: h.iota(eix[:], pattern=[[1, NE]], base=0, channel_multiplier=0, allow_small_or_imprecise_dtypes=True), writes=[res("eix")])
                    s.op("gpsimd", lambda h: h.iota(offs24[:, 0:4], pattern=[[128, 4]], base=512, channel_multiplier=1, allow_small_or_imprecise_dtypes=True), writes=[res("offs24")])
                    s.op("gpsimd", lambda h: h.iota(offs24[:, 4:20], pattern=[[128, 16]], base=0, channel_multiplier=1, allow_small_or_imprecise_dtypes=True), writes=[res("offs24")])
                    s.op("gpsimd", lambda h: h.iota(offs24[:, 20:24], pattern=[[128, 4]], base=0, channel_multiplier=1, allow_small_or_imprecise_dtypes=True), writes=[res("offs24")])
                    s.op("vector", lambda h: h.memset(mult24[:, 0:4], float(CAP)), writes=[res("mult24")])
                    s.op("vector", lambda h: h.memset(mult24[:, 4:20], 2048.0), writes=[res("mult24")])
                    s.op("vector", lambda h: h.memset(mult24[:, 20:24], 512.0), writes=[res("mult24")])
                    for j in range(NHOT):
                        s.op("vector", lambda h, j=j: h.tensor_scalar(out=selj[:], in0=cum[:], scalar1=float(j), scalar2=None, op0=ALU.is_equal),
                             reads=[res("cum")], writes=[res("selj")])
                        s.op("vector", lambda h: h.tensor_tensor(out=selj[:], in0=selj[:], in1=flg[:], op=ALU.mult), reads=[res("selj"), res("flg")], writes=[res("selj")])
                        s.op("vector", lambda h: h.tensor_tensor(out=selj[:], in0=selj[:], in1=eix[:], op=ALU.mult), reads=[res("selj"), res("eix")], writes=[res("selj")])
                        s.op("vector", lambda h, j=j: h.tensor_reduce(out=ev[:, j:j + 1], in_=selj[:], axis=AX.X, op=ALU.add), reads=[res("selj")], writes=[res("ev")])
                        s.op("vector", lambda h, j=j: h.scalar_tensor_tensor(out=idxf[:, j, :], in0=mult24[:], scalar=ev[:, j:j + 1], in1=offs24[:], op0=ALU.mult, op1=ALU.add),
                             reads=[res("mult24"), res("ev"), res("offs24")], writes=[res("idxf")])
                    s.op("vector", lambda h: h.tensor_copy(out=hotidx[:], in_=idxf[:]), reads=[res("idxf")], writes=[res("hotidx")])
                s.emit()

        def expert_phase():
            with ExitStack() as ph:
                def sbp(name, shape, dt=F32):
                    return ph.enter_context(nc.sbuf_tensor(f"{name}_e", list(shape), dt))
                wg = [sbp(f"wg{i}", [128, 16, 512], BF16) for i in range(2)]
                wu = [sbp(f"wu{i}", [128, 16, 512], BF16) for i in range(2)]
                wd = [sbp(f"wd{i}", [128, 4, D], BF16) for i in range(2)]
                xg = [sbp(f"xg{i}", [128, D], BF16) for i in range(2)]
                xT = sbp("xT", [128, 16, 512], BF16)
                sg = [sbp(f"sg{i}", [128, 512]) for i in range(2)]
                act = sbp("act", [128, 4, 512], BF16)
                ob = [sbp(f"ob{i}", [128, D], BF16) for i in range(2)]
                passes = [("s", e, e * CAP, e % 2) for e in range(NE)] + [("d", j, 0, (NE + j) % 2) for j in range(NHOT)] \
                    + [("s", NE, q * 512, (NE + NHOT) % 2) for q in range(4)]
                state = {"loaded": None, "nx": 0, "nob": 0, "nstg": 0}
                stg = [sbp(f"stg{i}", [128, D]) for i in range(4)]
                wge_rows = wge.rearrange("e k n -> (e k) n")
                wue_rows = wue.rearrange("e k n -> (e k) n")
                wde_rows = wde.rearrange("e k n -> (e k) n")

                def gather_cast(dst_ap, rdst, src_rows, j, col, width):
                    si = state["nstg"] % 4
                    state["nstg"] += 1
                    s.dma("gpsimd", lambda h, si=si, j=j, col=col: h.indirect_dma_start(
                        out=stg[si][:, 0:width], out_offset=None, in_=src_rows[:, :],
                        in_offset=bass.IndirectOffsetOnAxis(ap=hotidx[:, j, col:col + 1], axis=0), bounds_check=None),
                        reads=[res("hotidx")], writes=[res(f"stg{si}")])
                    if si % 2 == 0:
                        s.op("vector", lambda h, si=si: h.tensor_copy(out=dst_ap, in_=stg[si][:, 0:width]), reads=[res(f"stg{si}")], writes=[rdst])
                    else:
                        s.op("scalar", lambda h, si=si: h.copy(out=dst_ap, in_=stg[si][:, 0:width]), reads=[res(f"stg{si}")], writes=[rdst])

                def emit_loadx(idx):
                    kind_, e, r0, b = passes[idx]
                    if kind_ == "d":
                        j = e
                        for c in range(16):
                            gather_cast(wg[b][:, c, :], res(f"wg{b}"), wge_rows, j, 4 + c, 512)
                        for c in range(16):
                            gather_cast(wu[b][:, c, :], res(f"wu{b}"), wue_rows, j, 4 + c, 512)
                        for c in range(4):
                            gather_cast(wd[b][:, c, :], res(f"wd{b}"), wde_rows, j, 20 + c, D)
                        state["loaded"] = ("d", j)
                        for sbk in range(4):
                            xb_ = state["nx"] % 2
                            state["nx"] += 1
                            s.dma("gpsimd", lambda h, xb_=xb_, j=j, sbk=sbk: h.indirect_dma_start(
                                out=xg[xb_][:], out_offset=None, in_=xbuf[:, :],
                                in_offset=bass.IndirectOffsetOnAxis(ap=hotidx[:, j, sbk:sbk + 1], axis=0), bounds_check=None),
                                reads=[res("hotidx")], writes=[res(f"xg{xb_}")])
                            transpose16(xg[xb_], res(f"xg{xb_}"), xT, res("xT"),
                                        lambda half, sbk=sbk: xT[:, half * 8:(half + 1) * 8, sbk * 128:(sbk + 1) * 128])
                        return
                    if ("s", e) != state["loaded"]:
                        state["loaded"] = ("s", e)
                        gsrc = wge[e] if e < NE else wgs
                        usrc = wue[e] if e < NE else wus
                        dsrc = wde[e] if e < NE else wds
                        s.dma("gpsimd", lambda h, b=b, gsrc=gsrc: h.dma_start(out=wg[b][:], in_=gsrc.rearrange("(c p) n -> p c n", p=128)), writes=[res(f"wg{b}")])
                        s.dma("gpsimd", lambda h, b=b, usrc=usrc: h.dma_start(out=wu[b][:], in_=usrc.rearrange("(c p) n -> p c n", p=128)), writes=[res(f"wu{b}")])
                        s.dma("gpsimd", lambda h, b=b, dsrc=dsrc: h.dma_start(out=wd[b][:], in_=dsrc.rearrange("(c p) n -> p c n", p=128)), writes=[res(f"wd{b}")])
                    for sbk in range(4):
                        xb_ = state["nx"] % 2
                        state["nx"] += 1
                        s.dma("sync", lambda h, xb_=xb_, r0=r0, sbk=sbk, e=e: h.dma_start(out=xg[xb_][:], in_=(xbuf if e < NE else xbuf_s)[r0 + sbk * 128:r0 + (sbk + 1) * 128, :]),
                              writes=[res(f"xg{xb_}")])
                        transpose16(xg[xb_], res(f"xg{xb_}"), xT, res("xT"),
                                    lambda half, sbk=sbk: xT[:, half * 8:(half + 1) * 8, sbk * 128:(sbk + 1) * 128])

                def emit_gu(idx):
                    kind_, e, r0, b = passes[idx]
                    for fc in range(4):
                        gb, ub = fc % 2, 2 + fc % 2
                        for k in range(16):
                            s.op("tensor", lambda h, gb=gb, b=b, k=k, fc=fc: h.matmul(pp[gb][:], lhsT=wg[b][:, k, fc * 128:(fc + 1) * 128], rhs=xT[:, k, :],
                                                                                      start=(k == 0), stop=(k == 15)),
                                 reads=[res(f"wg{b}"), res("xT")], writes=[r_pp[gb]], signal=(k == 15))
                        for k in range(16):
                            s.op("tensor", lambda h, ub=ub, b=b, k=k, fc=fc: h.matmul(pp[ub][:], lhsT=wu[b][:, k, fc * 128:(fc + 1) * 128], rhs=xT[:, k, :],
                                                                                      start=(k == 0), stop=(k == 15)),
                                 reads=[res(f"wu{b}"), res("xT")], writes=[r_pp[ub]], signal=(k == 15))
                        s.op("scalar", lambda h, gb=gb, fc=fc: h.activation(out=sg[fc % 2][:], in_=pp[gb][:], func=AF.Silu),
                             reads=[r_pp[gb]], writes=[res(f"sg{fc % 2}")])
                        s.op("vector", lambda h, ub=ub, fc=fc: h.tensor_tensor(out=act[:, fc, :], in0=pp[ub][:], in1=sg[fc % 2][:], op=ALU.mult),
                             reads=[r_pp[ub], res(f"sg{fc % 2}")], writes=[res("act")])

                def emit_down(idx):
                    kind_, e, r0, b = passes[idx]
                    for sbk in range(4):
                        ob_ = state["nob"] % 2
                        state["nob"] += 1
                        for n in range(4):
                            bank = 4 + n % 2
                            for fc in range(4):
                                s.op("tensor", lambda h, bank=bank, fc=fc, sbk=sbk, n=n, b=b: h.matmul(
                                    pp[bank][:], lhsT=act[:, fc, sbk * 128:(sbk + 1) * 128], rhs=wd[b][:, fc, n * 512:(n + 1) * 512],
                                    start=(fc == 0), stop=(fc == 3)),
                                    reads=[res("act"), res(f"wd{b}")], writes=[r_pp[bank]], signal=(fc == 3))
                            if n % 2 == 0:
                                s.op("scalar", lambda h, bank=bank, ob_=ob_, n=n: h.copy(
                                    out=ob[ob_][:, n * 512:(n + 1) * 512], in_=pp[bank][:]),
                                    reads=[r_pp[bank]], writes=[res(f"ob{ob_}")])
                            else:
                                s.op("vector", lambda h, bank=bank, ob_=ob_, n=n: h.tensor_copy(
                                    out=ob[ob_][:, n * 512:(n + 1) * 512], in_=pp[bank][:]),
                                    reads=[r_pp[bank]], writes=[res(f"ob{ob_}")])
                        if kind_ == "d":
                            s.dma("gpsimd", lambda h, ob_=ob_, e=e, sbk=sbk: h.indirect_dma_start(
                                out=obuf[:, :], out_offset=bass.IndirectOffsetOnAxis(ap=hotidx[:, e, sbk:sbk + 1], axis=0),
                                in_=ob[ob_][:], in_offset=None, bounds_check=None),
                                reads=[res(f"ob{ob_}"), res("hotidx")])
                        else:
                            s.dma("sync", lambda h, ob_=ob_, r0=r0, sbk=sbk, e=e: h.dma_start(out=(obuf if e < NE else obuf_s)[r0 + sbk * 128:r0 + (sbk + 1) * 128, :], in_=ob[ob_][:]),
                                  reads=[res(f"ob{ob_}")])

                emit_loadx(0)
                for idx in range(len(passes)):
                    emit_gu(idx)
                    if idx + 1 < len(passes):
                        emit_loadx(idx + 1)
                    emit_down(idx)
                s.emit()

        def combine_phase():
            with ExitStack() as ph:
                def sbp(name, shape, dt=F32):
                    return ph.enter_context(nc.sbuf_tensor(f"{name}_c", list(shape), dt))
                gk = [sbp(f"gk{i}", [128, D], BF16) for i in range(9)]
                accA = sbp("accA", [128, D]); accB = sbp("accB", [128, D])
                yt = sbp("yt", [128, D]); G2 = [sbp("G2a", [128, D]), sbp("G2b", [128, D])]
                st = sbp("st", [128, 4])
                for k in range(9):
                    s.op("gpsimd", lambda h, k=k: h.memset(gk[k][:], 0.0), writes=[res(f"gk{k}")])
                for p in range(2):
                    s.dma("sync", lambda h, p=p: h.dma_start(out=G2[p][:], in_=modsp[10 + p]), writes=[res(f"G2{p}")])
                for gi in range(16):
                    p, i = gi // 8, gi % 8
                    for k in range(8):
                        s.dma("gpsimd", lambda h, k=k, gi=gi: h.indirect_dma_start(
                            out=gk[k][:], out_offset=None, in_=obuf[:, :],
                            in_offset=bass.IndirectOffsetOnAxis(ap=dstall[:, gi, k:k + 1], axis=0),
                            bounds_check=None),
                            reads=[res("dstall")], writes=[res(f"gk{k}")])
                    s.dma("sync", lambda h, gi=gi: h.dma_start(out=gk[8][:], in_=obuf_s[gi * 128:(gi + 1) * 128, :]),
                          writes=[res("gk8")])
                    s.dma("sync", lambda h, gi=gi: h.dma_start(out=yt[:], in_=ysp[gi * 128:(gi + 1) * 128, :]), writes=[res("ytc")])
                    s.op("vector", lambda h, gi=gi: h.scalar_tensor_tensor(out=accA[:], in0=gk[0][:], scalar=gall[:, gi, 0:1], in1=gk[8][:], op0=ALU.mult, op1=ALU.add),
                         reads=[res("gk8"), res("gk0"), res("gall")], writes=[res("accA")])
                    for k in range(1, 8):
                        s.op("vector", lambda h, k=k, gi=gi: h.scalar_tensor_tensor(out=accA[:], in0=gk[k][:], scalar=gall[:, gi, k:k + 1], in1=accA[:], op0=ALU.mult, op1=ALU.add),
                             reads=[res("accA"), res(f"gk{k}"), res("gall")], writes=[res("accA")])
                    if DEBUG:
                        s.dma("sync", lambda h, gi=gi: h.dma_start(out=dbg_moe[gi * 128:(gi + 1) * 128, :], in_=accA[:]), reads=[res("accA")])
                    s.op("scalar", lambda h: h.activation(out=accB[:], in_=accA[:], func=AF.Square, accum_out=st[:, 0:1]),
                         reads=[res("accA")], writes=[res("accB"), res("stc")])
                    rstd_chain(st[:, 0:1], st[:, 1:2], D, res("stc"), res("stc"))
                    s.op("vector", lambda h, p=p: h.scalar_tensor_tensor(out=accB[:], in0=accA[:], scalar=st[:, 1:2], in1=G2[p][:], op0=ALU.mult, op1=ALU.mult),
                         reads=[res("accA"), res("stc"), res(f"G2{p}")], writes=[res("accB")])
                    s.op("gpsimd", lambda h: h.tensor_tensor(out=accB[:], in0=accB[:], in1=yt[:], op=ALU.add), reads=[res("accB"), res("ytc")], writes=[res("accB")])
                    s.dma("sync", lambda h, p=p, i=i: h.dma_start(out=youts[p][i * 128:(i + 1) * 128, :], in_=accB[:]),
                          reads=[res("accB")])
                s.emit()

        if DEBUG:
            dbg_dst = dscr("dbg_dst", [128, 16 * 8], I32)
            dbg_gate = dscr("dbg_gate", [128, 16 * 8])
            dbg_moe = dscr("dbg_moe", [2048, D])
            dbg_sc = dscr("dbg_sc", [16, 128, NE])
        for p in range(2):
            if STAGE >= 1:
                qkv_phase(p)
            if STAGE >= 2:
                with nc.sbuf_tensor(f"OT{p}", [128, 16, 1024], BF16) as OT:
                    attn_phase(p, OT)
                    if STAGE >= 3:
                        post_phase(p, OT)
        if STAGE >= 8:
            expert_phase()
            combine_phase()
        if DEBUG and STAGE >= 3:
            s.dma("sync", lambda h: h.dma_start(out=dbg_dst[:, :], in_=dstall[:].rearrange("p a b -> p (a b)")), reads=[res("dstall")])
            s.dma("sync", lambda h: h.dma_start(out=dbg_gate[:, :], in_=gall[:].rearrange("p a b -> p (a b)")), reads=[res("gall")])
        s.final_wait("sync", list(R.values()))
        s.emit()
    return nc


def _rope_tables():
    n = 2048
    rows = n // 64
    row = np.repeat(np.arange(rows, dtype=np.float32), 64)
    col = np.tile(np.arange(64, dtype=np.float32), rows)
    inv_freq = (10000.0 ** (-np.arange(0, 64, 2, dtype=np.float32) / 64)).astype(np.float32)
    ang_r = row[:, None] * inv_freq
    ang_c = col[:, None] * inv_freq
    ang = np.concatenate([ang_r, ang_r, ang_c, ang_c], axis=-1).astype(np.float32)
    cos = np.cos(ang).astype(np.float32)
    sin = np.sin(ang).astype(np.float32)
    sgn = np.ones(128, np.float32)
    sgn[0:32] = -1.0
    sgn[64:96] = -1.0
    return cos, sin * sgn[None, :]


def _consts(half):
    c = np.zeros((128, 7 * 128), np.float32)
    j = np.arange(128)[:, None]
    r = np.arange(128)[None, :]
    c[:, 0:128] = np.eye(128, dtype=np.float32)
    c[:, 128:256] = (j < r).astype(np.float32)
    c[:, 256:384] = 1.0
    band_prev = (j >= r).astype(np.float32)
    band_next = (j <= r).astype(np.float32)
    c[:, 384:512] = band_prev
    c[:, 512:640] = band_next
    c[:, 640:768] = band_prev if half == 1 else 0.0
    c[:, 768:896] = band_next if half == 0 else 0.0
    return c


def _local_order(half):
    own = np.arange(half * 1024, (half + 1) * 1024)
    if half == 0:
        other = np.arange(1024, 2048)
    else:
        other = np.concatenate([np.arange(896, 1024), np.arange(0, 896)])
    return np.concatenate([own, other])


_NC_CACHE = {}


def kernel(x_prompt, x_sample, cache_glob_k, cache_glob_v, cache_win_k, cache_win_v, c, c_ctx,
           w_ada, b_ada, attn_pre_g, attn_post_g, w_in, q_norm_g, k_norm_g, sink_logit, w_out,
           ffn_pre_g, ffn_post_g, w_router, router_bias, w_gate_e, w_up_e, w_down_e,
           w_gate_s, w_up_s, w_down_s):
    f = lambda a: np.ascontiguousarray(np.asarray(a, dtype=np.float32))
    x_prompt, x_sample = f(x_prompt), f(x_sample)
    cos, sins = _rope_tables()
    shared = {
        "w_ada": f(w_ada)[0], "b_ada": f(b_ada)[0][None, :],
        "gains": np.stack([f(attn_pre_g)[0], f(attn_post_g)[0], f(ffn_pre_g)[0], f(ffn_post_g)[0]]),
        "w_in": f(w_in)[0], "qkg": np.stack([f(q_norm_g)[0], f(k_norm_g)[0]]),
        "sink": f(sink_logit)[0][None, :], "w_out": f(w_out)[0], "w_router": f(w_router)[0],
        "rbias": f(router_bias)[0][None, :], "wge": f(w_gate_e)[0], "wue": f(w_up_e)[0], "wde": f(w_down_e)[0],
        "wgs": f(w_gate_s)[0], "wus": f(w_up_s)[0], "wds": f(w_down_s)[0],
    }
    caches = [f(cache_glob_k), f(cache_glob_v), f(cache_win_k), f(cache_win_v)]
    in_maps = []
    for core in range(NCORES):
        b, half = core // 2, core % 2
        order = _local_order(half)
        m = dict(shared)
        m["xc"] = x_prompt[4 * core:4 * core + 4].reshape(1024, D)
        m["xl"] = np.ascontiguousarray(x_sample[b][order])
        m["ropec"] = np.ascontiguousarray(cos[order])
        m["ropes"] = np.ascontiguousarray(sins[order])
        m["cache"] = np.stack([cc[b, 0].reshape(256, 256) for cc in caches])
        m["cond"] = np.stack([f(c_ctx), f(c)[b]])
        m["consts"] = _consts(half)
        in_maps.append(m)
    if "nc" not in _NC_CACHE:
        del INPUT_NAMES[:]
        _NC_CACHE["nc"] = build()
    nc = _NC_CACHE["nc"]
    in_maps = [{k: v for k, v in m.items() if k in INPUT_NAMES} for m in in_maps]
    r = run_bass_kernel_spmd(nc, in_maps, core_ids=list(range(NCORES))).results
    if DEBUG:
        _NC_CACHE["raw"] = r
    y_p = np.concatenate([r[i]["yc"].reshape(4, 256, D) for i in range(NCORES)], axis=0)
    y_s = np.stack([np.concatenate([r[2 * b]["yl"], r[2 * b + 1]["yl"]], axis=0) for b in range(4)])
    def kv(name):
        return np.concatenate([r[i][name].reshape(4, 1, 256, 2, 128) for i in range(NCORES)], axis=0)
    return (y_p.astype(np.float32), y_s.astype(np.float32), kv("ngk"), kv("ngv"), kv("nwk"), kv("nwv"))
```

```python
import numpy as np
from contextlib import ExitStack
import concourse.bass as bass
import concourse.mybir as mybir
from concourse.bass_utils import run_bass_kernel_spmd

F32 = mybir.dt.float32
BF16 = mybir.dt.bfloat16
I32 = mybir.dt.int32
U32 = mybir.dt.uint32
AF = mybir.ActivationFunctionType
ALU = mybir.AluOpType
AX = mybir.AxisListType

D = 2048
NCORES = 8
EPS = 1e-6
HD = 128
SCALE = HD ** -0.5
NE = 64
CAP = 1024
NSLOT = NE * CAP + 2048
STAGE = 99
DEBUG = False
SKIP_INPUTS = set()
INPUT_NAMES = []


class _Eng:
    def __init__(self, key):
        self.key = key
        self.sem = None
        self.count = 0
        self.thunks = []
        self.waited = {}


class Res:
    __slots__ = ("name", "w", "r")

    def __init__(self, name):
        self.name = name
        self.w = None
        self.r = []


class Sched:
    def __init__(self, nc, n_dma_sems=24):
        self.nc = nc
        self.eng = {k: _Eng(k) for k in ("tensor", "vector", "scalar", "gpsimd", "sync")}
        self.n_dma_sems = n_dma_sems
        self.dma_sems = {}
        self.dma_rr = {}
        self.sems = {}
        self.phase_id = 0

    def alloc_sems(self, stack):
        for k, e in self.eng.items():
            e.sem = stack.enter_context(self.nc.semaphore("s_" + k))
            self.sems[("e", k)] = e.sem
        for q in ("sync", "gpsimd"):
            lst = []
            for i in range(self.n_dma_sems):
                s = stack.enter_context(self.nc.semaphore(f"d_{q}_{i}"))
                self.sems[("d", q, i)] = s
                lst.append([("d", q, i), 0])
            self.dma_sems[q] = lst
            self.dma_rr[q] = 0

    def _deps(self, reads, writes):
        deps = []
        for r in reads:
            if r.w is not None:
                deps.append(r.w)
        for w in writes:
            if w.w is not None:
                deps.append(w.w)
            deps.extend(w.r)
        return deps

    def _waits(self, e, deps, skip_self=False):
        need = {}
        for src, val in deps:
            if skip_self and src == ("e", e.key):
                continue
            if e.waited.get(src, 0) >= val:
                continue
            if need.get(src, 0) < val:
                need[src] = val
        for src, val in need.items():
            e.waited[src] = val
        return list(need.items())

    def op(self, engine, fn, reads=(), writes=(), signal=True):
        e = self.eng[engine]
        waits = self._waits(e, self._deps(reads, writes), skip_self=(engine == "tensor"))
        if signal:
            e.count += 1
            tok = (("e", engine), e.count)
            for r in reads:
                r.r.append(tok)
            for w in writes:
                w.w = tok
                w.r = []
        sems = self.sems

        def thunk(h, waits=waits, fn=fn, signal=signal, sem=e.sem):
            for src, val in waits:
                h.wait_ge(sems[src], val)
            ins = fn(h)
            if signal:
                ins.then_inc(sem, 1)
        e.thunks.append(thunk)

    def dma(self, queue, fn, reads=(), writes=()):
        e = self.eng[queue]
        lst = self.dma_sems[queue]
        i = self.dma_rr[queue]
        self.dma_rr[queue] = (i + 1) % len(lst)
        slot = lst[i]
        deps = self._deps(reads, writes)
        if slot[1] > 0:
            deps.append((slot[0], slot[1]))
        waits = self._waits(e, deps)
        slot[1] += 16
        tok = (slot[0], slot[1])
        for r in reads:
            r.r.append(tok)
        for w in writes:
            w.w = tok
            w.r = []
        sems = self.sems

        def thunk(h, waits=waits, fn=fn, sem=sems[slot[0]]):
            for src, val in waits:
                h.wait_ge(sems[src], val)
            fn(h).then_inc(sem, 16)
        e.thunks.append(thunk)

    def final_wait(self, engine, resources):
        e = self.eng[engine]
        deps = [r.w for r in resources if r.w is not None]
        waits = self._waits(e, deps)
        sems = self.sems

        def thunk(h, waits=waits):
            for src, val in waits:
                h.wait_ge(sems[src], val)
        e.thunks.append(thunk)

    def drain(self):
        e = self.eng["sync"]
        deps = []
        for q, lst in self.dma_sems.items():
            for slot in lst:
                if slot[1] > 0:
                    deps.append((slot[0], slot[1]))
        for k, e2 in self.eng.items():
            if e2.count > 0 and k != "sync":
                deps.append((("e", k), e2.count))
        waits = self._waits(e, deps)
        sems = self.sems

        def thunk(h, waits=waits):
            for src, val in waits:
                h.wait_ge(sems[src], val)
        e.thunks.append(thunk)

    def emit(self):
        self.drain()
        self.phase_id = getattr(self, "phase_id", 0) + 1
        with self.nc.Block() as block:
            for k, e in self.eng.items():
                if not e.thunks:
                    continue

                def body(h, thunks=list(e.thunks)):
                    for t in thunks:
                        t(h)
                getattr(block, k)(body)
        for e in self.eng.values():
            e.thunks = []


def build():
    nc = bass.Bass("TRN2", target_bir_lowering=False)

    def din(name, shape, dt=F32):
        if name in SKIP_INPUTS:
            return None
        INPUT_NAMES.append(name)
        return nc.dram_tensor(name, list(shape), dt, kind="ExternalInput").ap()

    def dout(name, shape, dt=F32):
        return nc.dram_tensor(name, list(shape), dt, kind="ExternalOutput").ap()

    def dscr(name, shape, dt=F32):
        kind = "ExternalOutput" if (DEBUG and name in ("ysp", "dbg_dst", "dbg_gate", "dbg_moe", "dbg_sc")) else "Internal"
        return nc.dram_tensor(name, list(shape), dt, kind=kind).ap()

    xc = din("xc", [1024, D])
    xl = din("xl", [2048, D])
    ropec = din("ropec", [2048, 128])
    ropes = din("ropes", [2048, 128])
    cache = din("cache", [4, 256, 256])
    cond = din("cond", [2, D])
    w_ada = din("w_ada", [D, 6 * D])
    b_ada = din("b_ada", [1, 6 * D])
    gains = din("gains", [4, D])
    w_in = din("w_in", [D, 3072])
    qkg = din("qkg", [2, 128])
    sink = din("sink", [1, 8])
    w_out = din("w_out", [D, D])
    w_router = din("w_router", [D, NE])
    rbias = din("rbias", [1, NE])
    wge = din("wge", [NE, D, 512])
    wue = din("wue", [NE, D, 512])
    wde = din("wde", [NE, 512, D])
    wgs = din("wgs", [D, 512])
    wus = din("wus", [D, 512])
    wds = din("wds", [512, D])
    consts = din("consts", [128, 7 * 128])

    yc = dout("yc", [1024, D])
    yl = dout("yl", [1024, D])
    ngk = dout("ngk", [1024, 256])
    ngv = dout("ngv", [1024, 256])
    nwk = dout("nwk", [1024, 256])
    nwv = dout("nwv", [1024, 256])

    modsp = dscr("modsp", [12, 128, D])

    R = {}

    def res(name):
        if name not in R:
            R[name] = Res(name)
        return R[name]

    with ExitStack() as st:
        s = Sched(nc)
        s.alloc_sems(st)
        st.enter_context(nc.allow_non_contiguous_dma(reason="small strided loads"))
        st.enter_context(nc.allow_low_precision(reason="bf16 matmul operands"))

        def sb(name, shape, dt=F32):
            return st.enter_context(nc.sbuf_tensor(name, list(shape), dt))

        def ps(name, shape, dt=F32):
            return st.enter_context(nc.psum_tensor(name, list(shape), dt))

        pp = [ps(f"pp{i}", [128, 512], F32) for i in range(6)]
        pt = [ps(f"pt{i}", [128, 1024], BF16) for i in range(2)]
        r_pp = [res(f"pp{i}") for i in range(6)]
        r_pt = [res(f"pt{i}") for i in range(2)]

        cf = sb("cf", [128, 7 * 128], F32)
        cb = sb("cb", [128, 7 * 128], BF16)
        s.dma("sync", lambda h: h.dma_start(out=cf[:], in_=consts[:, :]), writes=[res("cf")])
        s.op("vector", lambda h: h.tensor_copy(out=cb[:], in_=cf[:]), reads=[res("cf")], writes=[res("cb")])
        identb = cb[:, 0:128]
        onesb = cb[:, 256:384]

        ph0 = ExitStack()

        def sb0(name, shape, dt=F32):
            return ph0.enter_context(nc.sbuf_tensor(name, list(shape), dt))
        condT = sb0("condT", [128, 16, 2], F32)
        crep = sb0("crep", [128, 2, 16, 128], BF16)
        gbc = sb0("gbc", [128, 4, D], F32)
        for p in range(2):
            s.dma("sync", lambda h, p=p: h.dma_start(
                out=condT[:, :, p], in_=cond[p:p + 1, :].rearrange("r (c p) -> p (r c)", p=128)),
                writes=[res("condT")])
        s.dma("sync", lambda h: h.dma_start(out=gbc[:], in_=gains.partition_broadcast(128)), writes=[res("gbc")])
        for p in range(2):
            s.op("scalar", lambda h, p=p: h.activation(
                out=crep[:, p, :, :], in_=condT[:, :, p:p + 1].broadcast_to([128, 16, 128]), func=AF.Silu),
                reads=[res("condT")], writes=[res("crep")])
        wa = [sb0(f"wa{i}", [128, 16, 512], BF16) for i in range(2)]
        bb = [sb0(f"bb{i}", [128, 512], F32) for i in range(2)]
        mt = [sb0(f"mt{i}", [128, 512], F32) for i in range(4)]
        gain_of = {1: 0, 2: 1, 4: 2, 5: 3}
        n_mt = 0
        for j in range(24):
            which, cc = j // 4, j % 4
            b = j % 2
            s.dma("gpsimd", lambda h, b=b, j=j: h.dma_start(
                out=wa[b][:], in_=w_ada[:, j * 512:(j + 1) * 512].rearrange("(c p) n -> p c n", p=128)),
                writes=[res(f"wa{b}")])
            s.dma("sync", lambda h, b=b, j=j: h.dma_start(
                out=bb[b][:], in_=b_ada[:, j * 512:(j + 1) * 512].partition_broadcast(128)),
                writes=[res(f"bb{b}")])
            for p in range(2):
                pi = (j * 2 + p) % 6
                for k in range(16):
                    s.op("tensor", lambda h, p=p, k=k, b=b, pi=pi: h.matmul(
                        pp[pi][:], lhsT=crep[:, p, k, :], rhs=wa[b][:, k, :], start=(k == 0), stop=(k == 15)),
                        reads=[res("crep"), res(f"wa{b}")], writes=[r_pp[pi]], signal=(k == 15))
                m = mt[n_mt % 4]
                rm = res(f"mt{n_mt % 4}")
                n_mt += 1
                if which in (0, 3):
                    s.op("vector", lambda h, m=m, pi=pi, b=b: h.tensor_tensor(
                        out=m[:], in0=pp[pi][:], in1=bb[b][:], op=ALU.add),
                        reads=[r_pp[pi], res(f"bb{b}")], writes=[rm])
                else:
                    gsl = gbc[:, gain_of[which], cc * 512:(cc + 1) * 512]
                    s.op("vector", lambda h, m=m, pi=pi, b=b: h.tensor_tensor(
                        out=m[:], in0=pp[pi][:], in1=bb[b][:], op=ALU.add),
                        reads=[r_pp[pi], res(f"bb{b}")], writes=[rm])
                    add1 = 1.0 if which in (1, 4) else 0.0
                    s.op("vector", lambda h, m=m, gsl=gsl, add1=add1: h.scalar_tensor_tensor(
                        out=m[:], in0=m[:], scalar=add1, in1=gsl, op0=ALU.add, op1=ALU.mult),
                        reads=[rm, res("gbc")], writes=[rm])
                idx = which * 2 + p
                s.dma("sync", lambda h, m=m, idx=idx, cc=cc: h.dma_start(
                    out=modsp[idx, :, cc * 512:(cc + 1) * 512], in_=m[:]),
                    reads=[rm])

        s.emit()
        ph0.close()

        qsp = [dscr("qsp0", [1024, 2560], BF16), dscr("qsp1", [2048, 2560], BF16)]
        vsp = [dscr("vsp0", [1024, 512], BF16), dscr("vsp1", [2048, 512], BF16)]
        ysp = dscr("ysp", [2048, D])
        xbuf = dscr("xbuf", [NE * CAP, D], BF16)
        xbuf_s = dscr("xbuf_s", [2048, D], BF16)
        obuf_s = dscr("obuf_s", [2048, D], BF16)
        obuf = dscr("obuf", [NE * CAP, D], BF16)
        xin = [xc, xl]
        youts = [yc, yl]
        identf = cf[:, 0:128]
        onesf = cf[:, 256:384]
        ustrb = cb[:, 128:256]
        _bc = {}

        def bc_reg(h):
            if _bc.get("phase") != s.phase_id:
                _bc["r"] = h.to_reg(NE * CAP - 1)
                _bc["phase"] = s.phase_id
            return _bc["r"]

        mx = sb("mx", [128, 2, 4], F32)
        negm = sb("negm", [128, 2, 2], F32)
        Mall = sb("Mall", [128, 16, NE], BF16)
        dstall = sb("dstall", [128, 16, 8], I32)
        gall = sb("gall", [128, 16, 8], F32)
        NHOT = 4
        hotidx = sb("hotidx", [128, NHOT, 24], I32)
        sinkb = sb("sinkb", [128, 8], F32)
        g10 = sb("g10", [128, 10, 128], F32)
        rbb = sb("rbb", [128, NE], F32)
        s.op("vector", lambda h: h.memset(mx[:], 0.0), writes=[res("mx")])
        s.dma("sync", lambda h: h.dma_start(out=sinkb[:], in_=sink.partition_broadcast(128)), writes=[res("sinkb")])
        s.dma("sync", lambda h: h.dma_start(out=rbb[:], in_=rbias.partition_broadcast(128)), writes=[res("rbb")])
        for hh in range(10):
            s.dma("sync", lambda h, hh=hh: h.dma_start(
                out=g10[:, hh, :], in_=qkg[(0 if hh < 8 else 1):(1 if hh < 8 else 2), :].partition_broadcast(128)),
                writes=[res("g10")])

        def rstd_chain(ssq_ap, rs_ap, n, r_in, r_out):
            s.op("vector", lambda h: h.tensor_scalar(out=rs_ap, in0=ssq_ap, scalar1=1.0 / n, scalar2=EPS,
                                                     op0=ALU.mult, op1=ALU.add), reads=[r_in], writes=[r_out])
            s.op("scalar", lambda h: h.activation(out=rs_ap, in_=rs_ap, func=AF.Sqrt), reads=[r_out], writes=[r_out])
            s.op("vector", lambda h: h.reciprocal(out=rs_ap, in_=rs_ap), reads=[r_out], writes=[r_out])

        def transpose16(src_bf, r_src, dst, r_dst, dst_slices):
            for half in range(2):
                for c in range(8):
                    cc = half * 8 + c
                    s.op("tensor", lambda h, half=half, c=c, cc=cc: h.transpose(
                        out=pt[half][:, c * 128:(c + 1) * 128], in_=src_bf[:, cc * 128:(cc + 1) * 128], identity=identb),
                        reads=[r_src, res("cb")], writes=[r_pt[half]], signal=(c == 7))
                eng = "scalar" if half == 0 else "vector"
                o = dst_slices(half)
                i_ = pt[half][:, :].rearrange("p (c t) -> p c t", t=128)
                if eng == "scalar":
                    s.op("scalar", lambda h, o=o, i_=i_: h.copy(out=o, in_=i_), reads=[r_pt[half]], writes=[r_dst])
                else:
                    s.op("vector", lambda h, o=o, i_=i_: h.tensor_copy(out=o, in_=i_), reads=[r_pt[half]], writes=[r_dst])

        def qkv_phase(p):
            nt = 8 if p == 0 else 16
            with ExitStack() as ph:
                def sbp(name, shape, dt=F32):
                    return ph.enter_context(nc.sbuf_tensor(f"{name}_{p}", list(shape), dt))
                win = sbp("win", [128, 16, 3072], BF16)
                A1 = sbp("A1", [128, D]); B1 = sbp("B1", [128, D])
                xt = [sbp(f"xt{i}", [128, D]) for i in range(2)]
                hb = sbp("hb", [128, D], BF16)
                hT = sbp("hT", [128, 16, 128], BF16)
                qkall = sbp("qkall", [128, 3072])
                qk20 = sbp("qk20", [128, 20, 128])
                t1 = sbp("t1", [128, 20, 128]); t2 = sbp("t2", [128, 20, 128])
                qkb = sbp("qkb", [128, 20, 128], BF16)
                vb = sbp("vb", [128, 512], BF16)
                rc = sbp("rc", [128, 128]); rs_ = sbp("rs", [128, 128])
                st1 = sbp("st1", [128, 8]); st10 = sbp("st10", [128, 10]); st20 = sbp("st20", [128, 20]); g4 = sbp("g4", [128, 4])
                for n in range(6):
                    s.dma("gpsimd", lambda h, n=n: h.dma_start(
                        out=win[:, :, n * 512:(n + 1) * 512],
                        in_=w_in[:, n * 512:(n + 1) * 512].rearrange("(c p) n -> p c n", p=128)), writes=[res("win")])
                s.dma("sync", lambda h: h.dma_start(out=A1[:], in_=modsp[2 + p]), writes=[res("A1")])
                s.dma("sync", lambda h: h.dma_start(out=B1[:], in_=modsp[0 + p]), writes=[res("B1")])
                for i in range(nt):
                    own = i < 8
                    b = i % 2
                    rx = res(f"xt{b}")
                    s.dma("sync", lambda h, b=b, i=i: h.dma_start(out=xt[b][:], in_=xin[p][i * 128:(i + 1) * 128, :]), writes=[rx])
                    s.op("scalar", lambda h, b=b: h.activation(out=t1[:].rearrange("p a b -> p (a b)")[:, 0:D], in_=xt[b][:],
                                                               func=AF.Square, accum_out=st1[:, 0:1]),
                         reads=[rx], writes=[res("t1"), res("st1")])
                    rstd_chain(st1[:, 0:1], st1[:, 1:2], D, res("st1"), res("st1b"))
                    t1f = t1[:].rearrange("p a b -> p (a b)")[:, 0:D]
                    s.op("vector", lambda h, b=b: h.scalar_tensor_tensor(out=t1f, in0=xt[b][:], scalar=st1[:, 1:2], in1=A1[:],
                                                                         op0=ALU.mult, op1=ALU.mult),
                         reads=[rx, res("st1b"), res("A1")], writes=[res("t1")])
                    s.op("gpsimd", lambda h: h.tensor_tensor(out=hb[:], in0=t1f, in1=B1[:], op=ALU.add),
                         reads=[res("t1"), res("B1")], writes=[res("hb")])
                    transpose16(hb, res("hb"), hT, res("hT"), lambda half: hT[:, half * 8:(half + 1) * 8, :])
                    chunks = range(6) if own else (2, 5)
                    for n in chunks:
                        for k in range(16):
                            s.op("tensor", lambda h, n=n, k=k: h.matmul(pp[n][:], lhsT=hT[:, k, :], rhs=win[:, k, n * 512:(n + 1) * 512],
                                                                        start=(k == 0), stop=(k == 15)),
                                 reads=[res("hT"), res("win")], writes=[r_pp[n]], signal=(k == 15))
                        eng = "scalar" if n % 2 == 0 else "vector"
                        if eng == "scalar":
                            s.op("scalar", lambda h, n=n: h.copy(out=qkall[:, n * 512:(n + 1) * 512], in_=pp[n][:]),
                                 reads=[r_pp[n]], writes=[res("qkall")])
                        else:
                            s.op("vector", lambda h, n=n: h.tensor_copy(out=qkall[:, n * 512:(n + 1) * 512], in_=pp[n][:]),
                                 reads=[r_pp[n]], writes=[res("qkall")])
                    h0 = 0 if own else 8
                    nn = 10 - h0
                    src_n = qkall[:, h0 * 128:1280].rearrange("p (a b) -> p a b", b=128)
                    s.op("scalar", lambda h, src_n=src_n, h0=h0: h.activation(out=t2[:, h0:10, :], in_=src_n, func=AF.Square),
                         reads=[res("qkall")], writes=[res("t2")])
                    s.op("vector", lambda h, h0=h0: h.tensor_reduce(out=st10[:, h0:10], in_=t2[:, h0:10, :], axis=AX.X, op=ALU.add),
                         reads=[res("t2")], writes=[res("st10")])
                    rstd_chain(st10[:, h0:10], st10[:, h0:10], 128, res("st10"), res("st10"))
                    s.op("vector", lambda h, src_n=src_n, h0=h0, nn=nn: h.tensor_tensor(
                        out=qk20[:, h0:10, :], in0=src_n, in1=st10[:, h0:10].unsqueeze(2).broadcast_to([128, nn, 128]), op=ALU.mult),
                        reads=[res("qkall"), res("st10")], writes=[res("qk20")])
                    s.op("vector", lambda h, h0=h0: h.tensor_tensor(out=qk20[:, h0:10, :], in0=qk20[:, h0:10, :], in1=g10[:, h0:10, :], op=ALU.mult),
                         reads=[res("qk20"), res("g10")], writes=[res("qk20")])
                    w0 = 10 if own else 18
                    c0 = 1536 + (w0 - 10) * 128
                    s.op("scalar", lambda h, w0=w0, c0=c0: h.copy(out=qk20[:, w0:20, :], in_=qkall[:, c0:2816].rearrange("p (a b) -> p a b", b=128)),
                         reads=[res("qkall")], writes=[res("qk20")])
                    rows = slice(i * 128, (i + 1) * 128)
                    if p == 0:
                        for (dst, src_ap) in ((ngk, qk20[:, 8:10, :].rearrange("p a b -> p (a b)")), (ngv, qkall[:, 1280:1536]),
                                              (nwk, qkall[:, 2560:2816]), (nwv, qkall[:, 2816:3072])):
                            s.dma("sync", lambda h, dst=dst, src_ap=src_ap, rows=rows: h.dma_start(out=dst[rows, :], in_=src_ap),
                                  reads=[res("qk20"), res("qkall")])
                        s.op("scalar", lambda h: h.copy(out=qkb[:], in_=qk20[:]), reads=[res("qk20")], writes=[res("qkb")])
                    else:
                        s.dma("sync", lambda h, rows=rows: h.dma_start(out=rc[:], in_=ropec[rows, :]), writes=[res("rc")])
                        s.dma("sync", lambda h, rows=rows: h.dma_start(out=rs_[:], in_=ropes[rows, :]), writes=[res("rs")])
                        groups = [(0, 20)] if own else [(8, 10), (18, 20)]
                        for (a, bnd) in groups:
                            nh = bnd - a
                            s.op("vector", lambda h, a=a, bnd=bnd, nh=nh: h.tensor_tensor(
                                out=t1[:, a:bnd, :], in0=qk20[:, a:bnd, :], in1=rc[:].unsqueeze(1).broadcast_to([128, nh, 128]), op=ALU.mult),
                                reads=[res("qk20"), res("rc")], writes=[res("t1")])
                            for pr in range(2):
                                for hf in range(2):
                                    o_ = t2[:, a:bnd, pr * 64 + hf * 32: pr * 64 + hf * 32 + 32]
                                    i_ = qk20[:, a:bnd, pr * 64 + (1 - hf) * 32: pr * 64 + (1 - hf) * 32 + 32]
                                    sn = rs_[:, pr * 64 + hf * 32: pr * 64 + hf * 32 + 32].unsqueeze(1).broadcast_to([128, nh, 32])
                                    s.op("gpsimd", lambda h, o_=o_, i_=i_, sn=sn: h.tensor_tensor(out=o_, in0=i_, in1=sn, op=ALU.mult),
                                         reads=[res("qk20"), res("rs")], writes=[res("t2")])
                            s.op("vector", lambda h, a=a, bnd=bnd: h.tensor_tensor(out=qkb[:, a:bnd, :], in0=t1[:, a:bnd, :], in1=t2[:, a:bnd, :], op=ALU.add),
                                 reads=[res("t1"), res("t2")], writes=[res("qkb")])
                    hs = [(0, 20)] if own else [(8, 10), (18, 20)]
                    for (a, bnd) in hs:
                        s.op("scalar", lambda h, a=a, bnd=bnd: h.activation(out=t1[:, a:bnd, :], in_=qkb[:, a:bnd, :], func=AF.Square),
                             reads=[res("qkb")], writes=[res("t1")])
                        s.op("vector", lambda h, a=a, bnd=bnd: h.tensor_reduce(out=st20[:, a:bnd], in_=t1[:, a:bnd, :], axis=AX.X, op=ALU.add),
                             reads=[res("t1")], writes=[res("st20")])
                    grp = [(0, 0, 8), (1, 8, 10), (2, 10, 18), (3, 18, 20)] if own else [(1, 8, 10), (3, 18, 20)]
                    for (gi_, a, bnd) in grp:
                        s.op("vector", lambda h, gi_=gi_, a=a, bnd=bnd: h.tensor_reduce(out=g4[:, gi_:gi_ + 1], in_=st20[:, a:bnd], axis=AX.X, op=ALU.max),
                             reads=[res("st20")], writes=[res("g4")])
                        s.op("vector", lambda h, gi_=gi_: h.tensor_tensor(out=mx[:, p, gi_:gi_ + 1], in0=mx[:, p, gi_:gi_ + 1], in1=g4[:, gi_:gi_ + 1], op=ALU.max),
                             reads=[res("g4"), res("mx")], writes=[res("mx")])
                    s.op("scalar", lambda h: h.copy(out=vb[:, 0:256], in_=qkall[:, 1280:1536]), reads=[res("qkall")], writes=[res("vb")])
                    s.op("scalar", lambda h: h.copy(out=vb[:, 256:512], in_=qkall[:, 2816:3072]), reads=[res("qkall")], writes=[res("vb")])
                    s.dma("sync", lambda h, rows=rows: h.dma_start(out=qsp[p][rows, :], in_=qkb[:].rearrange("p a b -> p (a b)")),
                          reads=[res("qkb")])
                    s.dma("sync", lambda h, rows=rows: h.dma_start(out=vsp[p][rows, :], in_=vb[:]),
                          reads=[res("vb")])
                s.emit()

        def attn_phase(p, OT):
            nq_t = 8
            nloc = 8 if p == 0 else 16
            nkt = 8 if p == 0 else 18
            koff = 0 if p == 0 else 2
            with ExitStack() as ph:
                def sbp(name, shape, dt=F32):
                    return ph.enter_context(nc.sbuf_tensor(f"{name}_a{p}", list(shape), dt))
                QT = [sbp("QTg", [128, 8, 1024], BF16), sbp("QTw", [128, 8, 1024], BF16)]
                KT = [sbp("KTg", [128, 2, nkt * 128], BF16), sbp("KTw", [128, 2, nkt * 128], BF16)]
                V = sbp("V", [128, nkt, 512], BF16)
                qt = [sbp(f"qt{i}", [128, 2560], BF16) for i in range(2)]
                pb = [sbp(f"pb{i}", [128, 512], BF16) for i in range(4)]
                rec = [sbp(f"rec{i}", [128, 512]) for i in range(2)]
                SE = sbp("SE", [128, 8])
                m4 = sbp("m4", [128, 4]); dg = sbp("dg", [128, 4]); mb4 = sbp("mb4", [128, 4])
                cft = sbp("cft", [128, 512]); cbt = sbp("cbt", [128, 512], BF16); st4 = sbp("st4", [128, 4])
                if p == 1:
                    for kt in range(2):
                        for which, kind in ((0, 0), (2, 1)):
                            s.dma("sync", lambda h, kt=kt, which=which: h.dma_start(out=cft[:, 0:256], in_=cache[which, kt * 128:(kt + 1) * 128, :]),
                                  writes=[res("cft")])
                            s.op("vector", lambda h: h.tensor_copy(out=cbt[:, 0:256], in_=cft[:, 0:256]), reads=[res("cft")], writes=[res("cbt")])
                            s.op("scalar", lambda h: h.activation(out=cft[:, 256:512], in_=cbt[:, 0:256], func=AF.Square),
                                 reads=[res("cbt")], writes=[res("cft2")])
                            s.op("vector", lambda h: h.tensor_reduce(out=st4[:, 0:2], in_=cft[:, 256:512].rearrange("p (a b) -> p a b", b=128), axis=AX.X, op=ALU.add),
                                 reads=[res("cft2")], writes=[res("st4")])
                            s.op("vector", lambda h: h.tensor_reduce(out=st4[:, 2:3], in_=st4[:, 0:2], axis=AX.X, op=ALU.max),
                                 reads=[res("st4")], writes=[res("st4")])
                            col = 1 if kind == 0 else 3
                            s.op("vector", lambda h, col=col: h.tensor_tensor(out=mx[:, 1, col:col + 1], in0=mx[:, 1, col:col + 1], in1=st4[:, 2:3], op=ALU.max),
                                 reads=[res("st4"), res("mx")], writes=[res("mx")])
                            for n in range(2):
                                s.op("tensor", lambda h, n=n: h.transpose(out=pt[0][:, n * 128:(n + 1) * 128], in_=cbt[:, n * 128:(n + 1) * 128], identity=identb),
                                     reads=[res("cbt"), res("cb")], writes=[r_pt[0]], signal=(n == 1))
                            s.op("vector", lambda h, kt=kt, kind=kind: h.tensor_copy(
                                out=KT[kind][:, :, kt * 128:(kt + 1) * 128], in_=pt[0][:, 0:256].rearrange("p (a b) -> p a b", b=128)),
                                reads=[r_pt[0]], writes=[res(f"KT{kind}")])
                        for which, off in ((1, 0), (3, 256)):
                            s.dma("sync", lambda h, kt=kt, which=which: h.dma_start(out=cft[:, 0:256], in_=cache[which, kt * 128:(kt + 1) * 128, :]),
                                  writes=[res("cft")])
                            s.op("vector", lambda h, kt=kt, off=off: h.tensor_copy(out=V[:, kt, off:off + 256], in_=cft[:, 0:256]),
                                 reads=[res("cft")], writes=[res("V")])
                for i in range(nloc):
                    b = i % 2
                    rows = slice(i * 128, (i + 1) * 128)
                    s.dma("sync", lambda h, b=b, rows=rows: h.dma_start(out=qt[b][:], in_=qsp[p][rows, :]),
                          writes=[res(f"qt{b}")])
                    s.dma("sync", lambda h, i=i, rows=rows: h.dma_start(out=V[:, koff + i, :], in_=vsp[p][rows, :]),
                          writes=[res("V")])
                    if i < 8:
                        for kind in range(2):
                            c0 = 0 if kind == 0 else 1280
                            for hh in range(8):
                                s.op("tensor", lambda h, b=b, hh=hh, c0=c0, kind=kind: h.transpose(
                                    out=pt[kind][:, hh * 128:(hh + 1) * 128], in_=qt[b][:, c0 + hh * 128:c0 + (hh + 1) * 128], identity=identb),
                                    reads=[res(f"qt{b}"), res("cb")], writes=[r_pt[kind]], signal=(hh == 7))
                            o_ = QT[kind][:, :, i * 128:(i + 1) * 128]
                            i_ = pt[kind][:, :].rearrange("p (a b) -> p a b", b=128)
                            if kind == 0:
                                s.op("scalar", lambda h, o_=o_, i_=i_: h.copy(out=o_, in_=i_), reads=[r_pt[kind]], writes=[res(f"QT{kind}")])
                            else:
                                s.op("vector", lambda h, o_=o_, i_=i_: h.tensor_copy(out=o_, in_=i_), reads=[r_pt[kind]], writes=[res(f"QT{kind}")])
                    for kind in range(2):
                        c0 = 1024 if kind == 0 else 2304
                        for n in range(2):
                            s.op("tensor", lambda h, b=b, n=n, c0=c0, kind=kind: h.transpose(
                                out=pt[kind][:, n * 128:(n + 1) * 128], in_=qt[b][:, c0 + n * 128:c0 + (n + 1) * 128], identity=identb),
                                reads=[res(f"qt{b}"), res("cb")], writes=[r_pt[kind]], signal=(n == 1))
                        kk = koff + i
                        s.op("vector", lambda h, kk=kk, kind=kind: h.tensor_copy(
                            out=KT[kind][:, :, kk * 128:(kk + 1) * 128], in_=pt[kind][:, 0:256].rearrange("p (a b) -> p a b", b=128)),
                            reads=[r_pt[kind]], writes=[res(f"KT{kind}")])
                if p == 1:
                    zt = sbp("zt", [128, 8192], BF16)
                    s.op("gpsimd", lambda h: h.memset(zt[:], 0.0), writes=[res("zt")])
                    for e in range(NE):
                        s.dma("sync", lambda h, e=e: h.dma_start(
                            out=obuf[e * CAP + 512:(e + 1) * CAP, :].rearrange("(p r) d -> p (r d)", p=128), in_=zt[:]),
                            reads=[res("zt")])
                s.op("tensor", lambda h: h.transpose(out=pp[0][0:4, 0:128], in_=mx[:, p, :], identity=identf),
                     reads=[res("mx"), res("cf")], writes=[r_pp[0]])
                s.op("vector", lambda h: h.tensor_reduce(out=m4[0:4, 0:1], in_=pp[0][0:4, 0:128], axis=AX.X, op=ALU.max),
                     reads=[r_pp[0]], writes=[res("m4")])
                s.op("vector", lambda h: h.tensor_scalar(out=dg[0:4, 0:4], in0=identf[0:4, 0:4], scalar1=m4[0:4, 0:1], scalar2=None, op0=ALU.mult),
                     reads=[res("m4"), res("cf")], writes=[res("dg")])
                s.op("tensor", lambda h: h.matmul(pp[1][:, 0:4], lhsT=onesf[0:4, 0:128], rhs=dg[0:4, 0:4], start=True, stop=True),
                     reads=[res("dg"), res("cf")], writes=[r_pp[1]])
                s.op("vector", lambda h: h.tensor_copy(out=mb4[:], in_=pp[1][:, 0:4]), reads=[r_pp[1]], writes=[res("mb4")])
                for kind in range(2):
                    s.op("vector", lambda h, kind=kind: h.tensor_tensor(out=negm[:, p, kind:kind + 1], in0=mb4[:, 2 * kind:2 * kind + 1],
                                                                        in1=mb4[:, 2 * kind + 1:2 * kind + 2], op=ALU.mult),
                         reads=[res("mb4")], writes=[res("negm")])
                s.op("scalar", lambda h: h.activation(out=negm[:, p, :], in_=negm[:, p, :], func=AF.Sqrt), reads=[res("negm")], writes=[res("negm")])
                s.op("vector", lambda h: h.tensor_scalar(out=negm[:, p, :], in0=negm[:, p, :], scalar1=-SCALE, scalar2=None, op0=ALU.mult),
                     reads=[res("negm")], writes=[res("negm")])
                s.op("scalar", lambda h: h.activation(out=SE[:], in_=sinkb[:], func=AF.Exp, bias=negm[:, p, 1:2], scale=1.0),
                     reads=[res("negm"), res("sinkb")], writes=[res("SE")])

                jobs = []
                if p == 0:
                    for sq in range(4):
                        for kind in range(2):
                            for n in range(2):
                                for qc in range(2):
                                    h0 = 4 * n + 2 * qc
                                    q_ap = QT[kind][:, h0:h0 + 2, sq * 256:(sq + 1) * 256]
                                    keys = [(sq * 2 + kt, None) for kt in range(2)]
                                    o_ap = OT[:, kind * 8 + h0:kind * 8 + h0 + 2, sq * 256:(sq + 1) * 256]
                                    jobs.append((kind, n, q_ap, keys, o_ap, (h0, 2, 256)))
                else:
                    for n in range(2):
                        for qb in range(8):
                            q_ap = QT[0][:, 4 * n:4 * n + 4, qb * 128:(qb + 1) * 128]
                            o_ap = OT[:, 4 * n:4 * n + 4, qb * 128:(qb + 1) * 128]
                            jobs.append((0, n, q_ap, [(kt, None) for kt in range(18)], o_ap, (4 * n, 4, 128)))
                    for n in range(2):
                        for qb in range(8):
                            q_ap = QT[1][:, 4 * n:4 * n + 4, qb * 128:(qb + 1) * 128]
                            o_ap = OT[:, 8 + 4 * n:8 + 4 * n + 4, qb * 128:(qb + 1) * 128]
                            prev = (2 + qb - 1, 384) if qb > 0 else (2 + 8, 640)
                            nxt = (2 + qb + 1, 512) if qb < 7 else (2 + 8, 768)
                            keys = [(0, None), (1, None), prev, (2 + qb, None), nxt]
                            jobs.append((1, n, q_ap, keys, o_ap, (4 * n, 4, 128)))
                npb = 0
                for ji, (kind, n, q_ap, keys, o_ap, (h0, nh, nqq)) in enumerate(jobs):
                    po, psm = 2 + (ji % 2), 4 + (ji % 2)
                    def emit_S(ki):
                        kt = keys[ki][0]
                        sbank = ki % 2
                        s.op("tensor", lambda h, sbank=sbank, kind=kind, n=n, kt=kt, q_ap=q_ap: h.matmul(
                            pp[sbank][:], lhsT=KT[kind][:, n, kt * 128:(kt + 1) * 128], rhs=q_ap, start=True, stop=True),
                            reads=[res(f"KT{kind}"), res(f"QT{kind}")], writes=[r_pp[sbank]])
                    emit_S(0)
                    for ki, (kt, moff) in enumerate(keys):
                        sbank = ki % 2
                        pi = npb % 4
                        npb += 1
                        s.op("scalar", lambda h, pi=pi, sbank=sbank, kind=kind: h.activation(
                            out=pb[pi][:], in_=pp[sbank][:], func=AF.Exp, bias=negm[:, p, kind:kind + 1], scale=SCALE),
                            reads=[r_pp[sbank], res("negm")], writes=[res(f"pb{pi}")])
                        if ki + 1 < len(keys):
                            emit_S(ki + 1)
                        if moff is not None:
                            mk = cb[:, moff:moff + 128].unsqueeze(1).broadcast_to([128, 4, 128])
                            s.op("gpsimd", lambda h, pi=pi, mk=mk: h.tensor_tensor(
                                out=pb[pi][:].rearrange("p (a b) -> p a b", b=128), in0=pb[pi][:].rearrange("p (a b) -> p a b", b=128), in1=mk, op=ALU.mult),
                                reads=[res(f"pb{pi}"), res("cb")], writes=[res(f"pb{pi}")])
                        vs = V[:, kt, kind * 256 + n * 128: kind * 256 + (n + 1) * 128]
                        last = ki == len(keys) - 1
                        s.op("tensor", lambda h, po=po, vs=vs, pi=pi, ki=ki, last=last: h.matmul(
                            pp[po][:], lhsT=vs, rhs=pb[pi][:], start=(ki == 0), stop=last),
                            reads=[res("V"), res(f"pb{pi}")], writes=[r_pp[po]], signal=last)
                        s.op("tensor", lambda h, psm=psm, pi=pi, ki=ki, last=last: h.matmul(
                            pp[psm][:], lhsT=onesb, rhs=pb[pi][:], start=(ki == 0), stop=last),
                            reads=[res("cb"), res(f"pb{pi}")], writes=[r_pp[psm]], signal=True)
                    rb = ji % 2
                    if kind == 1:
                        se = SE[:, h0:h0 + nh].unsqueeze(2).broadcast_to([128, nh, nqq])
                        s.op("vector", lambda h, rb=rb, psm=psm, se=se, nqq=nqq: h.tensor_tensor(
                            out=rec[rb][:].rearrange("p (a b) -> p a b", b=nqq), in0=pp[psm][:].rearrange("p (a b) -> p a b", b=nqq), in1=se, op=ALU.add),
                            reads=[r_pp[psm], res("SE")], writes=[res(f"rec{rb}")])
                        s.op("vector", lambda h, rb=rb: h.reciprocal(out=rec[rb][:], in_=rec[rb][:]), reads=[res(f"rec{rb}")], writes=[res(f"rec{rb}")])
                    else:
                        s.op("vector", lambda h, rb=rb, psm=psm: h.reciprocal(out=rec[rb][:], in_=pp[psm][:]), reads=[r_pp[psm]], writes=[res(f"rec{rb}")])
                    s.op("vector", lambda h, rb=rb, po=po, o_ap=o_ap, nqq=nqq: h.tensor_tensor(
                        out=o_ap, in0=pp[po][:].rearrange("p (a b) -> p a b", b=nqq), in1=rec[rb][:].rearrange("p (a b) -> p a b", b=nqq), op=ALU.mult),
                        reads=[r_pp[po], res(f"rec{rb}")], writes=[res("OT")])
                s.emit()

        def post_phase(p, OT):
            with ExitStack() as ph:
                def sbp(name, shape, dt=F32):
                    return ph.enter_context(nc.sbuf_tensor(f"{name}_p{p}", list(shape), dt))
                wo = sbp("wo", [128, 16, D], BF16)
                wr = sbp("wr", [128, 16, NE], BF16)
                G1 = sbp("G1", [128, D]); A2 = sbp("A2", [128, D]); B2 = sbp("B2", [128, D])
                xt = sbp("xt", [128, D]); yt = sbp("yt", [128, D]); tf = sbp("tf", [128, D])
                h2b = sbp("h2b", [128, D], BF16); h2T = sbp("h2T", [128, 16, 128], BF16)
                st = sbp("st", [128, 8])
                sc = sbp("sc", [128, NE]); sel = sbp("sel", [128, NE]); srt = sbp("srt", [128, 8, 8]); gs = sbp("gs", [128, 8])
                gs8 = sbp("gs8", [128, 8]); gm = sbp("gm", [128, 8]); gneg = sbp("gneg", [128, 8]); selm = sbp("selm", [128, NE])
                top8 = sbp("top8", [128, 8]); Mf = sbp("Mf", [128, NE]); wsel = sbp("wsel", [128, NE]); den = sbp("den", [128, 2])
                posf = sbp("posf", [128, NE]); vv = sbp("vv", [128, NE]); d8 = sbp("d8", [128, 8]); oh = sbp("oh", [128, NE])
                g8 = sbp("g8", [128, 8]); eoff = sbp("eoff", [128, NE])
                for n in range(4):
                    s.dma("gpsimd", lambda h, n=n: h.dma_start(out=wo[:, :, n * 512:(n + 1) * 512],
                                                              in_=w_out[:, n * 512:(n + 1) * 512].rearrange("(c p) n -> p c n", p=128)), writes=[res("wo")])
                s.dma("gpsimd", lambda h: h.dma_start(out=wr[:], in_=w_router.rearrange("(c p) n -> p c n", p=128)), writes=[res("wr")])
                s.dma("sync", lambda h: h.dma_start(out=G1[:], in_=modsp[4 + p]), writes=[res("G1")])
                s.dma("sync", lambda h: h.dma_start(out=A2[:], in_=modsp[8 + p]), writes=[res("A2")])
                s.dma("sync", lambda h: h.dma_start(out=B2[:], in_=modsp[6 + p]), writes=[res("B2")])
                s.op("gpsimd", lambda h: h.iota(eoff[:], pattern=[[CAP, NE]], base=1, channel_multiplier=0, allow_small_or_imprecise_dtypes=True),
                     writes=[res("eoff")])
                for i in range(8):
                    gi = p * 8 + i
                    rows = slice(i * 128, (i + 1) * 128)
                    grow = slice(gi * 128, (gi + 1) * 128)
                    for n in range(4):
                        for mh in range(16):
                            s.op("tensor", lambda h, n=n, mh=mh, i=i: h.matmul(pp[n][:], lhsT=OT[:, mh, i * 128:(i + 1) * 128], rhs=wo[:, mh, n * 512:(n + 1) * 512],
                                                                              start=(mh == 0), stop=(mh == 15)),
                                 reads=[res("OT"), res("wo")], writes=[r_pp[n]], signal=(mh == 15))
                        s.op("scalar", lambda h, n=n: h.activation(out=tf[:, n * 512:(n + 1) * 512], in_=pp[n][:], func=AF.Square, accum_out=st[:, n:n + 1]),
                             reads=[r_pp[n]], writes=[res("tf"), res("st")])
                    s.op("vector", lambda h: h.tensor_reduce(out=st[:, 4:5], in_=st[:, 0:4], axis=AX.X, op=ALU.add), reads=[res("st")], writes=[res("st")])
                    rstd_chain(st[:, 4:5], st[:, 5:6], D, res("st"), res("st"))
                    s.dma("sync", lambda h, rows=rows: h.dma_start(out=xt[:], in_=xin[p][rows, :]), writes=[res("xt")])
                    for n in range(4):
                        cs = slice(n * 512, (n + 1) * 512)
                        s.op("vector", lambda h, n=n, cs=cs: h.scalar_tensor_tensor(out=tf[:, cs], in0=pp[n][:], scalar=st[:, 5:6], in1=G1[:, cs],
                                                                                    op0=ALU.mult, op1=ALU.mult),
                             reads=[r_pp[n], res("st"), res("G1")], writes=[res("tf")])
                    s.op("gpsimd", lambda h: h.tensor_tensor(out=yt[:], in0=tf[:], in1=xt[:], op=ALU.add), reads=[res("tf"), res("xt")], writes=[res("yt")])
                    s.dma("sync", lambda h, grow=grow: h.dma_start(out=ysp[grow, :], in_=yt[:]), reads=[res("yt")])
                    s.op("scalar", lambda h: h.activation(out=tf[:], in_=yt[:], func=AF.Square, accum_out=st[:, 6:7]),
                         reads=[res("yt")], writes=[res("tf"), res("st")])
                    rstd_chain(st[:, 6:7], st[:, 7:8], D, res("st"), res("st"))
                    s.op("vector", lambda h: h.scalar_tensor_tensor(out=tf[:], in0=yt[:], scalar=st[:, 7:8], in1=A2[:], op0=ALU.mult, op1=ALU.mult),
                         reads=[res("yt"), res("st"), res("A2")], writes=[res("tf")])
                    s.op("gpsimd", lambda h: h.tensor_tensor(out=h2b[:], in0=tf[:], in1=B2[:], op=ALU.add), reads=[res("tf"), res("B2")], writes=[res("h2b")])
                    s.dma("sync", lambda h, gi=gi: h.dma_start(out=xbuf_s[gi * 128:(gi + 1) * 128, :], in_=h2b[:]),
                          reads=[res("h2b")])
                    transpose16(h2b, res("h2b"), h2T, res("h2T"), lambda half: h2T[:, half * 8:(half + 1) * 8, :])
                    for k in range(16):
                        s.op("tensor", lambda h, k=k: h.matmul(pp[4][:, 0:NE], lhsT=h2T[:, k, :], rhs=wr[:, k, :], start=(k == 0), stop=(k == 15)),
                             reads=[res("h2T"), res("wr")], writes=[r_pp[4]], signal=(k == 15))
                    s.op("scalar", lambda h: h.activation(out=sc[:], in_=pp[4][:, 0:NE], func=AF.Sigmoid), reads=[r_pp[4]], writes=[res("sc")])
                    s.op("vector", lambda h: h.tensor_tensor(out=sel[:], in0=sc[:], in1=rbb[:], op=ALU.add), reads=[res("sc"), res("rbb")], writes=[res("sel")])
                    for g in range(8):
                        s.op("vector", lambda h, g=g: h.max(out=srt[:, g, :], in_=sel[:, g * 8:(g + 1) * 8]), reads=[res("sel")], writes=[res("srt")])
                    s.op("vector", lambda h: h.tensor_tensor(out=gs[:], in0=srt[:, :, 0], in1=srt[:, :, 1], op=ALU.add), reads=[res("srt")], writes=[res("gs")])
                    s.op("vector", lambda h: h.max(out=gs8[:], in_=gs[:]), reads=[res("gs")], writes=[res("gs8")])
                    s.op("vector", lambda h: h.tensor_scalar(out=gm[:], in0=gs[:], scalar1=gs8[:, 3:4], scalar2=None, op0=ALU.is_ge),
                         reads=[res("gs"), res("gs8")], writes=[res("gm")])
                    s.op("vector", lambda h: h.tensor_scalar(out=gneg[:], in0=gm[:], scalar1=-1.0, scalar2=1e9, op0=ALU.add, op1=ALU.mult),
                         reads=[res("gm")], writes=[res("gneg")])
                    for g in range(8):
                        s.op("vector", lambda h, g=g: h.tensor_scalar(out=selm[:, g * 8:(g + 1) * 8], in0=sel[:, g * 8:(g + 1) * 8],
                                                                      scalar1=gm[:, g:g + 1], scalar2=gneg[:, g:g + 1], op0=ALU.mult, op1=ALU.add),
                             reads=[res("sel"), res("gm"), res("gneg")], writes=[res("selm")])
                    s.op("vector", lambda h: h.max(out=top8[:], in_=selm[:]), reads=[res("selm")], writes=[res("top8")])
                    s.op("vector", lambda h: h.tensor_scalar(out=Mf[:], in0=selm[:], scalar1=top8[:, 7:8], scalar2=None, op0=ALU.is_ge),
                         reads=[res("selm"), res("top8")], writes=[res("Mf")])
                    s.op("vector", lambda h, gi=gi: h.tensor_copy(out=Mall[:, gi, :], in_=Mf[:]), reads=[res("Mf")], writes=[res("Mall")])
                    s.op("vector", lambda h: h.tensor_tensor(out=wsel[:], in0=sc[:], in1=Mf[:], op=ALU.mult), reads=[res("sc"), res("Mf")], writes=[res("wsel")])
                    s.op("vector", lambda h: h.tensor_reduce(out=den[:, 0:1], in_=wsel[:], axis=AX.X, op=ALU.add), reads=[res("wsel")], writes=[res("den")])
                    s.op("vector", lambda h: h.reciprocal(out=den[:, 1:2], in_=den[:, 0:1]), reads=[res("den")], writes=[res("den")])
                    s.op("vector", lambda h: h.tensor_scalar(out=wsel[:], in0=wsel[:], scalar1=den[:, 1:2], scalar2=2.5, op0=ALU.mult, op1=ALU.mult),
                         reads=[res("wsel"), res("den")], writes=[res("wsel")])
                    s.op("tensor", lambda h, gi=gi: h.matmul(pp[5][:, 0:NE], lhsT=ustrb, rhs=Mall[:, gi, :], start=True, stop=(gi == 0)),
                         reads=[res("Mall"), res("cb")], writes=[r_pp[5]], signal=(gi == 0))
                    for j in range(gi):
                        s.op("tensor", lambda h, j=j, gi=gi: h.matmul(pp[5][:, 0:NE], lhsT=onesb, rhs=Mall[:, j, :], start=False, stop=(j == gi - 1)),
                             reads=[res("Mall"), res("cb")], writes=[r_pp[5]], signal=(j == gi - 1))
                    s.op("vector", lambda h: h.tensor_scalar(out=posf[:], in0=pp[5][:, 0:NE], scalar1=float(CAP - 1), scalar2=None, op0=ALU.min),
                         reads=[r_pp[5]], writes=[res("posf")])
                    s.op("vector", lambda h: h.tensor_tensor(out=vv[:], in0=posf[:], in1=eoff[:], op=ALU.add), reads=[res("posf"), res("eoff")], writes=[res("vv")])
                    s.op("vector", lambda h: h.tensor_tensor(out=vv[:], in0=vv[:], in1=Mf[:], op=ALU.mult), reads=[res("vv"), res("Mf")], writes=[res("vv")])
                    s.op("vector", lambda h: h.max(out=d8[:], in_=vv[:]), reads=[res("vv")], writes=[res("d8")])
                    for k in range(8):
                        s.op("vector", lambda h, k=k: h.tensor_scalar(out=oh[:], in0=vv[:], scalar1=d8[:, k:k + 1], scalar2=None, op0=ALU.is_equal),
                             reads=[res("vv"), res("d8")], writes=[res("oh")])
                        s.op("vector", lambda h, k=k: h.tensor_tensor(out=oh[:], in0=oh[:], in1=wsel[:], op=ALU.mult), reads=[res("oh"), res("wsel")], writes=[res("oh")])
                        s.op("vector", lambda h, k=k: h.tensor_reduce(out=g8[:, k:k + 1], in_=oh[:], axis=AX.X, op=ALU.add), reads=[res("oh")], writes=[res("g8")])
                    s.op("vector", lambda h: h.tensor_scalar(out=d8[:], in0=d8[:], scalar1=-1.0, scalar2=None, op0=ALU.add), reads=[res("d8")], writes=[res("d8")])
                    s.op("vector", lambda h, gi=gi: h.tensor_copy(out=dstall[:, gi, :], in_=d8[:]), reads=[res("d8")], writes=[res("dstall")])
                    s.op("vector", lambda h, gi=gi: h.tensor_copy(out=gall[:, gi, :], in_=g8[:]), reads=[res("g8")], writes=[res("gall")])
                    if DEBUG:
                        s.dma("sync", lambda h, gi=gi: h.dma_start(out=dbg_sc[gi], in_=sc[:]), reads=[res("sc")])
                    for k in range(8):
                        s.dma("gpsimd", lambda h, k=k, gi=gi: h.indirect_dma_start(
                            out=xbuf[:, :], out_offset=bass.IndirectOffsetOnAxis(ap=dstall[:, gi, k:k + 1], axis=0),
                            in_=h2b[:], in_offset=None, bounds_check=None),
                            reads=[res("h2b"), res("dstall")])
                if p == 1:
                    cntb = sbp("cntb", [128, NE]); flg = sbp("flg", [128, NE]); cum = sbp("cum", [128, NE]); one64 = sbp("one64", [128, NE])
                    eix = sbp("eix", [128, NE]); selj = sbp("selj", [128, NE]); ev = sbp("ev", [128, NHOT])
                    mult24 = sbp("mult24", [128, 24]); offs24 = sbp("offs24", [128, 24]); idxf = sbp("idxf", [128, NHOT, 24])
                    for j in range(16):
                        s.op("tensor", lambda h, j=j: h.matmul(pp[5][:, 0:NE], lhsT=onesb, rhs=Mall[:, j, :], start=(j == 0), stop=(j == 15)),
                             reads=[res("Mall"), res("cb")], writes=[r_pp[5]], signal=(j == 15))
                    s.op("vector", lambda h: h.tensor_scalar(out=flg[:], in0=pp[5][:, 0:NE], scalar1=512.0, scalar2=None, op0=ALU.is_gt),
                         reads=[r_pp[5]], writes=[res("flg")])
                    s.op("vector", lambda h: h.memset(one64[:], 1.0), writes=[res("one64")])
                    s.op("vector", lambda h: h.tensor_tensor_scan(out=cum[:], data0=one64[:], data1=flg[:], initial=0.0, op0=ALU.mult, op1=ALU.add),
                         reads=[res("one64"), res("flg")], writes=[res("cum")])
                    s.op("vector", lambda h: h.tensor_tensor(out=cum[:], in0=cum[:], in1=flg[:], op=ALU.subtract), reads=[res("cum"), res("flg")], writes=[res("cum")])
                    s.op("gpsimd", lambda h: h.iota(eix[:], pattern=[[1, NE]], base=0, channel_multiplier=0, allow_small_or_imprecise_dtypes=True), writes=[res("eix")])
                    s.op("gpsimd", lambda h: h.iota(offs24[:, 0:4], pattern=[[128, 4]], base=512, channel_multiplier=1, allow_small_or_imprecise_dtypes=True), writes=[res("offs24")])
                    s.op("gpsimd", lambda h: h.iota(offs24[:, 4:20], pattern=[[128, 16]], base=0, channel_multiplier=1, allow_small_or_imprecise_dtypes=True), writes=[res("offs24")])
                    s.op("gpsimd", lambda h: h.iota(offs24[:, 20:24], pattern=[[128, 4]], base=0, channel_multiplier=1, allow_small_or_imprecise_dtypes=True), writes=[res("offs24")])
                    s.op("vector", lambda h: h.memset(mult24[:, 0:4], float(CAP)), writes=[res("mult24")])
                    s.op("vector", lambda h: h.memset(mult24[:, 4:20], 2048.0), writes=[res("mult24")])
                    s.op("vector", lambda h: h.memset(mult24[:, 20:24], 512.0), writes=[res("mult24")])
                    for j in range(NHOT):
                        s.op("vector", lambda h, j=j: h.tensor_scalar(out=selj[:], in0=cum[:], scalar1=float(j), scalar2=None, op0=ALU.is_equal),
                             reads=[res("cum")], writes=[res("selj")])
                        s.op("vector", lambda h: h.tensor_tensor(out=selj[:], in0=selj[:], in1=flg[:], op=ALU.mult), reads=[res("selj"), res("flg")], writes=[res("selj")])
                        s.op("vector", lambda h: h.tensor_tensor(out=selj[:], in0=selj[:], in1=eix[:], op=ALU.mult), reads=[res("selj"), res("eix")], writes=[res("selj")])
                        s.op("vector", lambda h, j=j: h.tensor_reduce(out=ev[:, j:j + 1], in_=selj[:], axis=AX.X, op=ALU.add), reads=[res("selj")], writes=[res("ev")])
                        s.op("vector", lambda h, j=j: h.scalar_tensor_tensor(out=idxf[:, j, :], in0=mult24[:], scalar=ev[:, j:j + 1], in1=offs24[:], op0=ALU.mult, op1=ALU.add),
                             reads=[res("mult24"), res("ev"), res("offs24")], writes=[res("idxf")])
                    s.op("vector", lambda h: h.tensor_copy(out=hotidx[:], in_=idxf[:]), reads=[res("idxf")], writes=[res("hotidx")])
                s.emit()

        def expert_phase():
            with ExitStack() as ph:
                def sbp(name, shape, dt=F32):
                    return ph.enter_context(nc.sbuf_tensor(f"{name}_e", list(shape), dt))
                wg = [sbp(f"wg{i}", [128, 16, 512], BF16) for i in range(2)]
                wu = [sbp(f"wu{i}", [128, 16, 512], BF16) for i in range(2)]
                wd = [sbp(f"wd{i}", [128, 4, D], BF16) for i in range(2)]
                xg = [sbp(f"xg{i}", [128, D], BF16) for i in range(2)]
                xT = sbp("xT", [128, 16, 512], BF16)
                sg = [sbp(f"sg{i}", [128, 512]) for i in range(2)]
                act = sbp("act", [128, 4, 512], BF16)
                ob = [sbp(f"ob{i}", [128, D], BF16) for i in range(2)]
                passes = [("s", e, e * CAP, e % 2) for e in range(NE)] + [("d", j, 0, (NE + j) % 2) for j in range(NHOT)] \
                    + [("s", NE, q * 512, (NE + NHOT) % 2) for q in range(4)]
                state = {"loaded": None, "nx": 0, "nob": 0, "nstg": 0}
                stg = [sbp(f"stg{i}", [128, D]) for i in range(4)]
                wge_rows = wge.rearrange("e k n -> (e k) n")
                wue_rows = wue.rearrange("e k n -> (e k) n")
                wde_rows = wde.rearrange("e k n -> (e k) n")

                def gather_cast(dst_ap, rdst, src_rows, j, col, width):
                    si = state["nstg"] % 4
                    state["nstg"] += 1
                    s.dma("gpsimd", lambda h, si=si, j=j, col=col: h.indirect_dma_start(
                        out=stg[si][:, 0:width], out_offset=None, in_=src_rows[:, :],
                        in_offset=bass.IndirectOffsetOnAxis(ap=hotidx[:, j, col:col + 1], axis=0), bounds_check=None),
                        reads=[res("hotidx")], writes=[res(f"stg{si}")])
                    if si % 2 == 0:
                        s.op("vector", lambda h, si=si: h.tensor_copy(out=dst_ap, in_=stg[si][:, 0:width]), reads=[res(f"stg{si}")], writes=[rdst])
                    else:
                        s.op("scalar", lambda h, si=si: h.copy(out=dst_ap, in_=stg[si][:, 0:width]), reads=[res(f"stg{si}")], writes=[rdst])

                def emit_loadx(idx):
                    kind_, e, r0, b = passes[idx]
                    if kind_ == "d":
                        j = e
                        for c in range(16):
                            gather_cast(wg[b][:, c, :], res(f"wg{b}"), wge_rows, j, 4 + c, 512)
                        for c in range(16):
                            gather_cast(wu[b][:, c, :], res(f"wu{b}"), wue_rows, j, 4 + c, 512)
                        for c in range(4):
                            gather_cast(wd[b][:, c, :], res(f"wd{b}"), wde_rows, j, 20 + c, D)
                        state["loaded"] = ("d", j)
                        for sbk in range(4):
                            xb_ = state["nx"] % 2
                            state["nx"] += 1
                            s.dma("gpsimd", lambda h, xb_=xb_, j=j, sbk=sbk: h.indirect_dma_start(
                                out=xg[xb_][:], out_offset=None, in_=xbuf[:, :],
                                in_offset=bass.IndirectOffsetOnAxis(ap=hotidx[:, j, sbk:sbk + 1], axis=0), bounds_check=None),
                                reads=[res("hotidx")], writes=[res(f"xg{xb_}")])
                            transpose16(xg[xb_], res(f"xg{xb_}"), xT, res("xT"),
                                        lambda half, sbk=sbk: xT[:, half * 8:(half + 1) * 8, sbk * 128:(sbk + 1) * 128])
                        return
                    if ("s", e) != state["loaded"]:
                        state["loaded"] = ("s", e)
                        gsrc = wge[e] if e < NE else wgs
                        usrc = wue[e] if e < NE else wus
                        dsrc = wde[e] if e < NE else wds
                        s.dma("gpsimd", lambda h, b=b, gsrc=gsrc: h.dma_start(out=wg[b][:], in_=gsrc.rearrange("(c p) n -> p c n", p=128)), writes=[res(f"wg{b}")])
                        s.dma("gpsimd", lambda h, b=b, usrc=usrc: h.dma_start(out=wu[b][:], in_=usrc.rearrange("(c p) n -> p c n", p=128)), writes=[res(f"wu{b}")])
                        s.dma("gpsimd", lambda h, b=b, dsrc=dsrc: h.dma_start(out=wd[b][:], in_=dsrc.rearrange("(c p) n -> p c n", p=128)), writes=[res(f"wd{b}")])
                    for sbk in range(4):
                        xb_ = state["nx"] % 2
                        state["nx"] += 1
                        s.dma("sync", lambda h, xb_=xb_, r0=r0, sbk=sbk, e=e: h.dma_start(out=xg[xb_][:], in_=(xbuf if e < NE else xbuf_s)[r0 + sbk * 128:r0 + (sbk + 1) * 128, :]),
                              writes=[res(f"xg{xb_}")])
                        transpose16(xg[xb_], res(f"xg{xb_}"), xT, res("xT"),
                                    lambda half, sbk=sbk: xT[:, half * 8:(half + 1) * 8, sbk * 128:(sbk + 1) * 128])

                def emit_gu(idx):
                    kind_, e, r0, b = passes[idx]
                    for fc in range(4):
                        gb, ub = fc % 2, 2 + fc % 2
                        for k in range(16):
                            s.op("tensor", lambda h, gb=gb, b=b, k=k, fc=fc: h.matmul(pp[gb][:], lhsT=wg[b][:, k, fc * 128:(fc + 1) * 128], rhs=xT[:, k, :],
                                                                                      start=(k == 0), stop=(k == 15)),
                                 reads=[res(f"wg{b}"), res("xT")], writes=[r_pp[gb]], signal=(k == 15))
                        for k in range(16):
                            s.op("tensor", lambda h, ub=ub, b=b, k=k, fc=fc: h.matmul(pp[ub][:], lhsT=wu[b][:, k, fc * 128:(fc + 1) * 128], rhs=xT[:, k, :],
                                                                                      start=(k == 0), stop=(k == 15)),
                                 reads=[res(f"wu{b}"), res("xT")], writes=[r_pp[ub]], signal=(k == 15))
                        s.op("scalar", lambda h, gb=gb, fc=fc: h.activation(out=sg[fc % 2][:], in_=pp[gb][:], func=AF.Silu),
                             reads=[r_pp[gb]], writes=[res(f"sg{fc % 2}")])
                        s.op("vector", lambda h, ub=ub, fc=fc: h.tensor_tensor(out=act[:, fc, :], in0=pp[ub][:], in1=sg[fc % 2][:], op=ALU.mult),
                             reads=[r_pp[ub], res(f"sg{fc % 2}")], writes=[res("act")])

                def emit_down(idx):
                    kind_, e, r0, b = passes[idx]
                    for sbk in range(4):
                        ob_ = state["nob"] % 2
                        state["nob"] += 1
                        for n in range(4):
                            bank = 4 + n % 2
                            for fc in range(4):
                                s.op("tensor", lambda h, bank=bank, fc=fc, sbk=sbk, n=n, b=b: h.matmul(
                                    pp[bank][:], lhsT=act[:, fc, sbk * 128:(sbk + 1) * 128], rhs=wd[b][:, fc, n * 512:(n + 1) * 512],
                                    start=(fc == 0), stop=(fc == 3)),
                                    reads=[res("act"), res(f"wd{b}")], writes=[r_pp[bank]], signal=(fc == 3))
                            if n % 2 == 0:
                                s.op("scalar", lambda h, bank=bank, ob_=ob_, n=n: h.copy(
                                    out=ob[ob_][:, n * 512:(n + 1) * 512], in_=pp[bank][:]),
                                    reads=[r_pp[bank]], writes=[res(f"ob{ob_}")])
                            else:
                                s.op("vector", lambda h, bank=bank, ob_=ob_, n=n: h.tensor_copy(
                                    out=ob[ob_][:, n * 512:(n + 1) * 512], in_=pp[bank][:]),
                                    reads=[r_pp[bank]], writes=[res(f"ob{ob_}")])
                        if kind_ == "d":
                            s.dma("gpsimd", lambda h, ob_=ob_, e=e, sbk=sbk: h.indirect_dma_start(
                                out=obuf[:, :], out_offset=bass.IndirectOffsetOnAxis(ap=hotidx[:, e, sbk:sbk + 1], axis=0),
                                in_=ob[ob_][:], in_offset=None, bounds_check=None),
                                reads=[res(f"ob{ob_}"), res("hotidx")])
                        else:
                            s.dma("sync", lambda h, ob_=ob_, r0=r0, sbk=sbk, e=e: h.dma_start(out=(obuf if e < NE else obuf_s)[r0 + sbk * 128:r0 + (sbk + 1) * 128, :], in_=ob[ob_][:]),
                                  reads=[res(f"ob{ob_}")])

                emit_loadx(0)
                for idx in range(len(passes)):
                    emit_gu(idx)
                    if idx + 1 < len(passes):
                        emit_loadx(idx + 1)
                    emit_down(idx)
                s.emit()

        def combine_phase():
            with ExitStack() as ph:
                def sbp(name, shape, dt=F32):
                    return ph.enter_context(nc.sbuf_tensor(f"{name}_c", list(shape), dt))
                gk = [sbp(f"gk{i}", [128, D], BF16) for i in range(9)]
                accA = sbp("accA", [128, D]); accB = sbp("accB", [128, D])
                yt = sbp("yt", [128, D]); G2 = [sbp("G2a", [128, D]), sbp("G2b", [128, D])]
                st = sbp("st", [128, 4])
                for k in range(9):
                    s.op("gpsimd", lambda h, k=k: h.memset(gk[k][:], 0.0), writes=[res(f"gk{k}")])
                for p in range(2):
                    s.dma("sync", lambda h, p=p: h.dma_start(out=G2[p][:], in_=modsp[10 + p]), writes=[res(f"G2{p}")])
                for gi in range(16):
                    p, i = gi // 8, gi % 8
                    for k in range(8):
                        s.dma("gpsimd", lambda h, k=k, gi=gi: h.indirect_dma_start(
                            out=gk[k][:], out_offset=None, in_=obuf[:, :],
                            in_offset=bass.IndirectOffsetOnAxis(ap=dstall[:, gi, k:k + 1], axis=0),
                            bounds_check=None),
                            reads=[res("dstall")], writes=[res(f"gk{k}")])
                    s.dma("sync", lambda h, gi=gi: h.dma_start(out=gk[8][:], in_=obuf_s[gi * 128:(gi + 1) * 128, :]),
                          writes=[res("gk8")])
                    s.dma("sync", lambda h, gi=gi: h.dma_start(out=yt[:], in_=ysp[gi * 128:(gi + 1) * 128, :]), writes=[res("ytc")])
                    s.op("vector", lambda h, gi=gi: h.scalar_tensor_tensor(out=accA[:], in0=gk[0][:], scalar=gall[:, gi, 0:1], in1=gk[8][:], op0=ALU.mult, op1=ALU.add),
                         reads=[res("gk8"), res("gk0"), res("gall")], writes=[res("accA")])
                    for k in range(1, 8):
                        s.op("vector", lambda h, k=k, gi=gi: h.scalar_tensor_tensor(out=accA[:], in0=gk[k][:], scalar=gall[:, gi, k:k + 1], in1=accA[:], op0=ALU.mult, op1=ALU.add),
                             reads=[res("accA"), res(f"gk{k}"), res("gall")], writes=[res("accA")])
                    if DEBUG:
                        s.dma("sync", lambda h, gi=gi: h.dma_start(out=dbg_moe[gi * 128:(gi + 1) * 128, :], in_=accA[:]), reads=[res("accA")])
                    s.op("scalar", lambda h: h.activation(out=accB[:], in_=accA[:], func=AF.Square, accum_out=st[:, 0:1]),
                         reads=[res("accA")], writes=[res("accB"), res("stc")])
                    rstd_chain(st[:, 0:1], st[:, 1:2], D, res("stc"), res("stc"))
                    s.op("vector", lambda h, p=p: h.scalar_tensor_tensor(out=accB[:], in0=accA[:], scalar=st[:, 1:2], in1=G2[p][:], op0=ALU.mult, op1=ALU.mult),
                         reads=[res("accA"), res("stc"), res(f"G2{p}")], writes=[res("accB")])
                    s.op("gpsimd", lambda h: h.tensor_tensor(out=accB[:], in0=accB[:], in1=yt[:], op=ALU.add), reads=[res("accB"), res("ytc")], writes=[res("accB")])
                    s.dma("sync", lambda h, p=p, i=i: h.dma_start(out=youts[p][i * 128:(i + 1) * 128, :], in_=accB[:]),
                          reads=[res("accB")])
                s.emit()

        if DEBUG:
            dbg_dst = dscr("dbg_dst", [128, 16 * 8], I32)
            dbg_gate = dscr("dbg_gate", [128, 16 * 8])
            dbg_moe = dscr("dbg_moe", [2048, D])
            dbg_sc = dscr("dbg_sc", [16, 128, NE])
        for p in range(2):
            if STAGE >= 1:
                qkv_phase(p)
            if STAGE >= 2:
                with nc.sbuf_tensor(f"OT{p}", [128, 16, 1024], BF16) as OT:
                    attn_phase(p, OT)
                    if STAGE >= 3:
                        post_phase(p, OT)
        if STAGE >= 8:
            expert_phase()
            combine_phase()
        if DEBUG and STAGE >= 3:
            s.dma("sync", lambda h: h.dma_start(out=dbg_dst[:, :], in_=dstall[:].rearrange("p a b -> p (a b)")), reads=[res("dstall")])
            s.dma("sync", lambda h: h.dma_start(out=dbg_gate[:, :], in_=gall[:].rearrange("p a b -> p (a b)")), reads=[res("gall")])
        s.final_wait("sync", list(R.values()))
        s.emit()
    return nc


def _rope_tables():
    n = 2048
    rows = n // 64
    row = np.repeat(np.arange(rows, dtype=np.float32), 64)
    col = np.tile(np.arange(64, dtype=np.float32), rows)
    inv_freq = (10000.0 ** (-np.arange(0, 64, 2, dtype=np.float32) / 64)).astype(np.float32)
    ang_r = row[:, None] * inv_freq
    ang_c = col[:, None] * inv_freq
    ang = np.concatenate([ang_r, ang_r, ang_c, ang_c], axis=-1).astype(np.float32)
    cos = np.cos(ang).astype(np.float32)
    sin = np.sin(ang).astype(np.float32)
    sgn = np.ones(128, np.float32)
    sgn[0:32] = -1.0
    sgn[64:96] = -1.0
    return cos, sin * sgn[None, :]


def _consts(half):
    c = np.zeros((128, 7 * 128), np.float32)
    j = np.arange(128)[:, None]
    r = np.arange(128)[None, :]
    c[:, 0:128] = np.eye(128, dtype=np.float32)
    c[:, 128:256] = (j < r).astype(np.float32)
    c[:, 256:384] = 1.0
    band_prev = (j >= r).astype(np.float32)
    band_next = (j <= r).astype(np.float32)
    c[:, 384:512] = band_prev
    c[:, 512:640] = band_next
    c[:, 640:768] = band_prev if half == 1 else 0.0
    c[:, 768:896] = band_next if half == 0 else 0.0
    return c


def _local_order(half):
    own = np.arange(half * 1024, (half + 1) * 1024)
    if half == 0:
        other = np.arange(1024, 2048)
    else:
        other = np.concatenate([np.arange(896, 1024), np.arange(0, 896)])
    return np.concatenate([own, other])


_NC_CACHE = {}


def kernel(x_prompt, x_sample, cache_glob_k, cache_glob_v, cache_win_k, cache_win_v, c, c_ctx,
           w_ada, b_ada, attn_pre_g, attn_post_g, w_in, q_norm_g, k_norm_g, sink_logit, w_out,
           ffn_pre_g, ffn_post_g, w_router, router_bias, w_gate_e, w_up_e, w_down_e,
           w_gate_s, w_up_s, w_down_s):
    f = lambda a: np.ascontiguousarray(np.asarray(a, dtype=np.float32))
    x_prompt, x_sample = f(x_prompt), f(x_sample)
    cos, sins = _rope_tables()
    shared = {
        "w_ada": f(w_ada)[0], "b_ada": f(b_ada)[0][None, :],
        "gains": np.stack([f(attn_pre_g)[0], f(attn_post_g)[0], f(ffn_pre_g)[0], f(ffn_post_g)[0]]),
        "w_in": f(w_in)[0], "qkg": np.stack([f(q_norm_g)[0], f(k_norm_g)[0]]),
        "sink": f(sink_logit)[0][None, :], "w_out": f(w_out)[0], "w_router": f(w_router)[0],
        "rbias": f(router_bias)[0][None, :], "wge": f(w_gate_e)[0], "wue": f(w_up_e)[0], "wde": f(w_down_e)[0],
        "wgs": f(w_gate_s)[0], "wus": f(w_up_s)[0], "wds": f(w_down_s)[0],
    }
    caches = [f(cache_glob_k), f(cache_glob_v), f(cache_win_k), f(cache_win_v)]
    in_maps = []
    for core in range(NCORES):
        b, half = core // 2, core % 2
        order = _local_order(half)
        m = dict(shared)
        m["xc"] = x_prompt[4 * core:4 * core + 4].reshape(1024, D)
        m["xl"] = np.ascontiguousarray(x_sample[b][order])
        m["ropec"] = np.ascontiguousarray(cos[order])
        m["ropes"] = np.ascontiguousarray(sins[order])
        m["cache"] = np.stack([cc[b, 0].reshape(256, 256) for cc in caches])
        m["cond"] = np.stack([f(c_ctx), f(c)[b]])
        m["consts"] = _consts(half)
        in_maps.append(m)
    if "nc" not in _NC_CACHE:
        del INPUT_NAMES[:]
        _NC_CACHE["nc"] = build()
    nc = _NC_CACHE["nc"]
    in_maps = [{k: v for k, v in m.items() if k in INPUT_NAMES} for m in in_maps]
    r = run_bass_kernel_spmd(nc, in_maps, core_ids=list(range(NCORES))).results
    if DEBUG:
        _NC_CACHE["raw"] = r
    y_p = np.concatenate([r[i]["yc"].reshape(4, 256, D) for i in range(NCORES)], axis=0)
    y_s = np.stack([np.concatenate([r[2 * b]["yl"], r[2 * b + 1]["yl"]], axis=0) for b in range(4)])
    def kv(name):
        return np.concatenate([r[i][name].reshape(4, 1, 256, 2, 128) for i in range(NCORES)], axis=0)
    return (y_p.astype(np.float32), y_s.astype(np.float32), kv("ngk"), kv("ngv"), kv("nwk"), kv("nwv"))
```

```python
import numpy as np
from contextlib import ExitStack
import concourse.bass as bass
import concourse.mybir as mybir
from concourse.bass_utils import run_bass_kernel_spmd

F32 = mybir.dt.float32
BF16 = mybir.dt.bfloat16
I32 = mybir.dt.int32
U32 = mybir.dt.uint32
AF = mybir.ActivationFunctionType
ALU = mybir.AluOpType
AX = mybir.AxisListType

D = 2048
NCORES = 8
EPS = 1e-6
HD = 128
SCALE = HD ** -0.5
NE = 64
CAP = 1024
NSLOT = NE * CAP + 2048
STAGE = 99
DEBUG = False
SKIP_INPUTS = set()
INPUT_NAMES = []


class _Eng:
    def __init__(self, key):
        self.key = key
        self.sem = None
        self.count = 0
        self.thunks = []
        self.waited = {}


class Res:
    __slots__ = ("name", "w", "r")

    def __init__(self, name):
        self.name = name
        self.w = None
        self.r = []


class Sched:
    def __init__(self, nc, n_dma_sems=24):
        self.nc = nc
        self.eng = {k: _Eng(k) for k in ("tensor", "vector", "scalar", "gpsimd", "sync")}
        self.n_dma_sems = n_dma_sems
        self.dma_sems = {}
        self.dma_rr = {}
        self.sems = {}
        self.phase_id = 0

    def alloc_sems(self, stack):
        for k, e in self.eng.items():
            e.sem = stack.enter_context(self.nc.semaphore("s_" + k))
            self.sems[("e", k)] = e.sem
        for q in ("sync", "gpsimd"):
            lst = []
            for i in range(self.n_dma_sems):
                s = stack.enter_context(self.nc.semaphore(f"d_{q}_{i}"))
                self.sems[("d", q, i)] = s
                lst.append([("d", q, i), 0])
            self.dma_sems[q] = lst
            self.dma_rr[q] = 0

    def _deps(self, reads, writes):
        deps = []
        for r in reads:
            if r.w is not None:
                deps.append(r.w)
        for w in writes:
            if w.w is not None:
                deps.append(w.w)
            deps.extend(w.r)
        return deps

    def _waits(self, e, deps, skip_self=False):
        need = {}
        for src, val in deps:
            if skip_self and src == ("e", e.key):
                continue
            if e.waited.get(src, 0) >= val:
                continue
            if need.get(src, 0) < val:
                need[src] = val
        for src, val in need.items():
            e.waited[src] = val
        return list(need.items())

    def op(self, engine, fn, reads=(), writes=(), signal=True):
        e = self.eng[engine]
        waits = self._waits(e, self._deps(reads, writes), skip_self=(engine == "tensor"))
        if signal:
            e.count += 1
            tok = (("e", engine), e.count)
            for r in reads:
                r.r.append(tok)
            for w in writes:
                w.w = tok
                w.r = []
        sems = self.sems

        def thunk(h, waits=waits, fn=fn, signal=signal, sem=e.sem):
            for src, val in waits:
                h.wait_ge(sems[src], val)
            ins = fn(h)
            if signal:
                ins.then_inc(sem, 1)
        e.thunks.append(thunk)

    def dma(self, queue, fn, reads=(), writes=()):
        e = self.eng[queue]
        lst = self.dma_sems[queue]
        i = self.dma_rr[queue]
        self.dma_rr[queue] = (i + 1) % len(lst)
        slot = lst[i]
        deps = self._deps(reads, writes)
        if slot[1] > 0:
            deps.append((slot[0], slot[1]))
        waits = self._waits(e, deps)
        slot[1] += 16
        tok = (slot[0], slot[1])
        for r in reads:
            r.r.append(tok)
        for w in writes:
            w.w = tok
            w.r = []
        sems = self.sems

        def thunk(h, waits=waits, fn=fn, sem=sems[slot[0]]):
            for src, val in waits:
                h.wait_ge(sems[src], val)
            fn(h).then_inc(sem, 16)
        e.thunks.append(thunk)

    def final_wait(self, engine, resources):
        e = self.eng[engine]
        deps = [r.w for r in resources if r.w is not None]
        waits = self._waits(e, deps)
        sems = self.sems

        def thunk(h, waits=waits):
            for src, val in waits:
                h.wait_ge(sems[src], val)
        e.thunks.append(thunk)

    def drain(self):
        e = self.eng["sync"]
        deps = []
        for q, lst in self.dma_sems.items():
            for slot in lst:
                if slot[1] > 0:
                    deps.append((slot[0], slot[1]))
        for k, e2 in self.eng.items():
            if e2.count > 0 and k != "sync":
                deps.append((("e", k), e2.count))
        waits = self._waits(e, deps)
        sems = self.sems

        def thunk(h, waits=waits):
            for src, val in waits:
                h.wait_ge(sems[src], val)
        e.thunks.append(thunk)

    def emit(self):
        self.drain()
        self.phase_id = getattr(self, "phase_id", 0) + 1
        with self.nc.Block() as block:
            for k, e in self.eng.items():
                if not e.thunks:
                    continue

                def body(h, thunks=list(e.thunks)):
                    for t in thunks:
                        t(h)
                getattr(block, k)(body)
        for e in self.eng.values():
            e.thunks = []


def build():
    nc = bass.Bass("TRN2", target_bir_lowering=False)

    def din(name, shape, dt=F32):
        if name in SKIP_INPUTS:
            return None
        INPUT_NAMES.append(name)
        return nc.dram_tensor(name, list(shape), dt, kind="ExternalInput").ap()

    def dout(name, shape, dt=F32):
        return nc.dram_tensor(name, list(shape), dt, kind="ExternalOutput").ap()

    def dscr(name, shape, dt=F32):
        kind = "ExternalOutput" if (DEBUG and name in ("ysp", "dbg_dst", "dbg_gate", "dbg_moe", "dbg_sc")) else "Internal"
        return nc.dram_tensor(name, list(shape), dt, kind=kind).ap()

    xc = din("xc", [1024, D])
    xl = din("xl", [2048, D])
    ropec = din("ropec", [2048, 128])
    ropes = din("ropes", [2048, 128])
    cache = din("cache", [4, 256, 256])
    cond = din("cond", [2, D])
    w_ada = din("w_ada", [D, 6 * D])
    b_ada = din("b_ada", [1, 6 * D])
    gains = din("gains", [4, D])
    w_in = din("w_in", [D, 3072])
    qkg = din("qkg", [2, 128])
    sink = din("sink", [1, 8])
    w_out = din("w_out", [D, D])
    w_router = din("w_router", [D, NE])
    rbias = din("rbias", [1, NE])
    wge = din("wge", [NE, D, 512])
    wue = din("wue", [NE, D, 512])
    wde = din("wde", [NE, 512, D])
    wgs = din("wgs", [D, 512])
    wus = din("wus", [D, 512])
    wds = din("wds", [512, D])
    consts = din("consts", [128, 7 * 128])

    yc = dout("yc", [1024, D])
    yl = dout("yl", [1024, D])
    ngk = dout("ngk", [1024, 256])
    ngv = dout("ngv", [1024, 256])
    nwk = dout("nwk", [1024, 256])
    nwv = dout("nwv", [1024, 256])

    modsp = dscr("modsp", [12, 128, D])

    R = {}

    def res(name):
        if name not in R:
            R[name] = Res(name)
        return R[name]

    with ExitStack() as st:
        s = Sched(nc)
        s.alloc_sems(st)
        st.enter_context(nc.allow_non_contiguous_dma(reason="small strided loads"))
        st.enter_context(nc.allow_low_precision(reason="bf16 matmul operands"))

        def sb(name, shape, dt=F32):
            return st.enter_context(nc.sbuf_tensor(name, list(shape), dt))

        def ps(name, shape, dt=F32):
            return st.enter_context(nc.psum_tensor(name, list(shape), dt))

        pp = [ps(f"pp{i}", [128, 512], F32) for i in range(6)]
        pt = [ps(f"pt{i}", [128, 1024], BF16) for i in range(2)]
        r_pp = [res(f"pp{i}") for i in range(6)]
        r_pt = [res(f"pt{i}") for i in range(2)]

        cf = sb("cf", [128, 7 * 128], F32)
        cb = sb("cb", [128, 7 * 128], BF16)
        s.dma("sync", lambda h: h.dma_start(out=cf[:], in_=consts[:, :]), writes=[res("cf")])
        s.op("vector", lambda h: h.tensor_copy(out=cb[:], in_=cf[:]), reads=[res("cf")], writes=[res("cb")])
        identb = cb[:, 0:128]
        onesb = cb[:, 256:384]

        ph0 = ExitStack()

        def sb0(name, shape, dt=F32):
            return ph0.enter_context(nc.sbuf_tensor(name, list(shape), dt))
        condT = sb0("condT", [128, 16, 2], F32)
        crep = sb0("crep", [128, 2, 16, 128], BF16)
        gbc = sb0("gbc", [128, 4, D], F32)
        for p in range(2):
            s.dma("sync", lambda h, p=p: h.dma_start(
                out=condT[:, :, p], in_=cond[p:p + 1, :].rearrange("r (c p) -> p (r c)", p=128)),
                writes=[res("condT")])
        s.dma("sync", lambda h: h.dma_start(out=gbc[:], in_=gains.partition_broadcast(128)), writes=[res("gbc")])
        for p in range(2):
            s.op("scalar", lambda h, p=p: h.activation(
                out=crep[:, p, :, :], in_=condT[:, :, p:p + 1].broadcast_to([128, 16, 128]), func=AF.Silu),
                reads=[res("condT")], writes=[res("crep")])
        wa = [sb0(f"wa{i}", [128, 16, 512], BF16) for i in range(2)]
        bb = [sb0(f"bb{i}", [128, 512], F32) for i in range(2)]
        mt = [sb0(f"mt{i}", [128, 512], F32) for i in range(4)]
        gain_of = {1: 0, 2: 1, 4: 2, 5: 3}
        n_mt = 0
        for j in range(24):
            which, cc = j // 4, j % 4
            b = j % 2
            s.dma("gpsimd", lambda h, b=b, j=j: h.dma_start(
                out=wa[b][:], in_=w_ada[:, j * 512:(j + 1) * 512].rearrange("(c p) n -> p c n", p=128)),
                writes=[res(f"wa{b}")])
            s.dma("sync", lambda h, b=b, j=j: h.dma_start(
                out=bb[b][:], in_=b_ada[:, j * 512:(j + 1) * 512].partition_broadcast(128)),
                writes=[res(f"bb{b}")])
            for p in range(2):
                pi = (j * 2 + p) % 6
                for k in range(16):
                    s.op("tensor", lambda h, p=p, k=k, b=b, pi=pi: h.matmul(
                        pp[pi][:], lhsT=crep[:, p, k, :], rhs=wa[b][:, k, :], start=(k == 0), stop=(k == 15)),
                        reads=[res("crep"), res(f"wa{b}")], writes=[r_pp[pi]], signal=(k == 15))
                m = mt[n_mt % 4]
                rm = res(f"mt{n_mt % 4}")
                n_mt += 1
                if which in (0, 3):
                    s.op("vector", lambda h, m=m, pi=pi, b=b: h.tensor_tensor(
                        out=m[:], in0=pp[pi][:], in1=bb[b][:], op=ALU.add),
                        reads=[r_pp[pi], res(f"bb{b}")], writes=[rm])
                else:
                    gsl = gbc[:, gain_of[which], cc * 512:(cc + 1) * 512]
                    s.op("vector", lambda h, m=m, pi=pi, b=b: h.tensor_tensor(
                        out=m[:], in0=pp[pi][:], in1=bb[b][:], op=ALU.add),
                        reads=[r_pp[pi], res(f"bb{b}")], writes=[rm])
                    add1 = 1.0 if which in (1, 4) else 0.0
                    s.op("vector", lambda h, m=m, gsl=gsl, add1=add1: h.scalar_tensor_tensor(
                        out=m[:], in0=m[:], scalar=add1, in1=gsl, op0=ALU.add, op1=ALU.mult),
                        reads=[rm, res("gbc")], writes=[rm])
                idx = which * 2 + p
                s.dma("sync", lambda h, m=m, idx=idx, cc=cc: h.dma_start(
                    out=modsp[idx, :, cc * 512:(cc + 1) * 512], in_=m[:]),
                    reads=[rm])

        s.emit()
        ph0.close()

        qsp = [dscr("qsp0", [1024, 2560], BF16), dscr("qsp1", [2048, 2560], BF16)]
        vsp = [dscr("vsp0", [1024, 512], BF16), dscr("vsp1", [2048, 512], BF16)]
        ysp = dscr("ysp", [2048, D])
        xbuf = dscr("xbuf", [NE * CAP, D], BF16)
        xbuf_s = dscr("xbuf_s", [2048, D], BF16)
        obuf_s = dscr("obuf_s", [2048, D], BF16)
        obuf = dscr("obuf", [NE * CAP, D], BF16)
        xin = [xc, xl]
        youts = [yc, yl]
        identf = cf[:, 0:128]
        onesf = cf[:, 256:384]
        ustrb = cb[:, 128:256]
        _bc = {}

        def bc_reg(h):
            if _bc.get("phase") != s.phase_id:
                _bc["r"] = h.to_reg(NE * CAP - 1)
                _bc["phase"] = s.phase_id
            return _bc["r"]

        mx = sb("mx", [128, 2, 4], F32)
        negm = sb("negm", [128, 2, 2], F32)
        Mall = sb("Mall", [128, 16, NE], BF16)
        dstall = sb("dstall", [128, 16, 8], I32)
        gall = sb("gall", [128, 16, 8], F32)
        NHOT = 4
        hotidx = sb("hotidx", [128, NHOT, 24], I32)
        sinkb = sb("sinkb", [128, 8], F32)
        g10 = sb("g10", [128, 10, 128], F32)
        rbb = sb("rbb", [128, NE], F32)
        s.op("vector", lambda h: h.memset(mx[:], 0.0), writes=[res("mx")])
        s.dma("sync", lambda h: h.dma_start(out=sinkb[:], in_=sink.partition_broadcast(128)), writes=[res("sinkb")])
        s.dma("sync", lambda h: h.dma_start(out=rbb[:], in_=rbias.partition_broadcast(128)), writes=[res("rbb")])
        for hh in range(10):
            s.dma("sync", lambda h, hh=hh: h.dma_start(
                out=g10[:, hh, :], in_=qkg[(0 if hh < 8 else 1):(1 if hh < 8 else 2), :].partition_broadcast(128)),
                writes=[res("g10")])

        def rstd_chain(ssq_ap, rs_ap, n, r_in, r_out):
            s.op("vector", lambda h: h.tensor_scalar(out=rs_ap, in0=ssq_ap, scalar1=1.0 / n, scalar2=EPS,
                                                     op0=ALU.mult, op1=ALU.add), reads=[r_in], writes=[r_out])
            s.op("scalar", lambda h: h.activation(out=rs_ap, in_=rs_ap, func=AF.Sqrt), reads=[r_out], writes=[r_out])
            s.op("vector", lambda h: h.reciprocal(out=rs_ap, in_=rs_ap), reads=[r_out], writes=[r_out])

        def transpose16(src_bf, r_src, dst, r_dst, dst_slices):
            for half in range(2):
                for c in range(8):
                    cc = half * 8 + c
                    s.op("tensor", lambda h, half=half, c=c, cc=cc: h.transpose(
                        out=pt[half][:, c * 128:(c + 1) * 128], in_=src_bf[:, cc * 128:(cc + 1) * 128], identity=identb),
                        reads=[r_src, res("cb")], writes=[r_pt[half]], signal=(c == 7))
                eng = "scalar" if half == 0 else "vector"
                o = dst_slices(half)
                i_ = pt[half][:, :].rearrange("p (c t) -> p c t", t=128)
                if eng == "scalar":
                    s.op("scalar", lambda h, o=o, i_=i_: h.copy(out=o, in_=i_), reads=[r_pt[half]], writes=[r_dst])
                else:
                    s.op("vector", lambda h, o=o, i_=i_: h.tensor_copy(out=o, in_=i_), reads=[r_pt[half]], writes=[r_dst])

        def qkv_phase(p):
            nt = 8 if p == 0 else 16
            with ExitStack() as ph:
                def sbp(name, shape, dt=F32):
                    return ph.enter_context(nc.sbuf_tensor(f"{name}_{p}", list(shape), dt))
                win = sbp("win", [128, 16, 3072], BF16)
                A1 = sbp("A1", [128, D]); B1 = sbp("B1", [128, D])
                xt = [sbp(f"xt{i}", [128, D]) for i in range(2)]
                hb = sbp("hb", [128, D], BF16)
                hT = sbp("hT", [128, 16, 128], BF16)
                qkall = sbp("qkall", [128, 3072])
                qk20 = sbp("qk20", [128, 20, 128])
                t1 = sbp("t1", [128, 20, 128]); t2 = sbp("t2", [128, 20, 128])
                qkb = sbp("qkb", [128, 20, 128], BF16)
                vb = sbp("vb", [128, 512], BF16)
                rc = sbp("rc", [128, 128]); rs_ = sbp("rs", [128, 128])
                st1 = sbp("st1", [128, 8]); st10 = sbp("st10", [128, 10]); st20 = sbp("st20", [128, 20]); g4 = sbp("g4", [128, 4])
                for n in range(6):
                    s.dma("gpsimd", lambda h, n=n: h.dma_start(
                        out=win[:, :, n * 512:(n + 1) * 512],
                        in_=w_in[:, n * 512:(n + 1) * 512].rearrange("(c p) n -> p c n", p=128)), writes=[res("win")])
                s.dma("sync", lambda h: h.dma_start(out=A1[:], in_=modsp[2 + p]), writes=[res("A1")])
                s.dma("sync", lambda h: h.dma_start(out=B1[:], in_=modsp[0 + p]), writes=[res("B1")])
                for i in range(nt):
                    own = i < 8
                    b = i % 2
                    rx = res(f"xt{b}")
                    s.dma("sync", lambda h, b=b, i=i: h.dma_start(out=xt[b][:], in_=xin[p][i * 128:(i + 1) * 128, :]), writes=[rx])
                    s.op("scalar", lambda h, b=b: h.activation(out=t1[:].rearrange("p a b -> p (a b)")[:, 0:D], in_=xt[b][:],
                                                               func=AF.Square, accum_out=st1[:, 0:1]),
                         reads=[rx], writes=[res("t1"), res("st1")])
                    rstd_chain(st1[:, 0:1], st1[:, 1:2], D, res("st1"), res("st1b"))
                    t1f = t1[:].rearrange("p a b -> p (a b)")[:, 0:D]
                    s.op("vector", lambda h, b=b: h.scalar_tensor_tensor(out=t1f, in0=xt[b][:], scalar=st1[:, 1:2], in1=A1[:],
                                                                         op0=ALU.mult, op1=ALU.mult),
                         reads=[rx, res("st1b"), res("A1")], writes=[res("t1")])
                    s.op("gpsimd", lambda h: h.tensor_tensor(out=hb[:], in0=t1f, in1=B1[:], op=ALU.add),
                         reads=[res("t1"), res("B1")], writes=[res("hb")])
                    transpose16(hb, res("hb"), hT, res("hT"), lambda half: hT[:, half * 8:(half + 1) * 8, :])
                    chunks = range(6) if own else (2, 5)
                    for n in chunks:
                        for k in range(16):
                            s.op("tensor", lambda h, n=n, k=k: h.matmul(pp[n][:], lhsT=hT[:, k, :], rhs=win[:, k, n * 512:(n + 1) * 512],
                                                                        start=(k == 0), stop=(k == 15)),
                                 reads=[res("hT"), res("win")], writes=[r_pp[n]], signal=(k == 15))
                        eng = "scalar" if n % 2 == 0 else "vector"
                        if eng == "scalar":
                            s.op("scalar", lambda h, n=n: h.copy(out=qkall[:, n * 512:(n + 1) * 512], in_=pp[n][:]),
                                 reads=[r_pp[n]], writes=[res("qkall")])
                        else:
                            s.op("vector", lambda h, n=n: h.tensor_copy(out=qkall[:, n * 512:(n + 1) * 512], in_=pp[n][:]),
                                 reads=[r_pp[n]], writes=[res("qkall")])
                    h0 = 0 if own else 8
                    nn = 10 - h0
                    src_n = qkall[:, h0 * 128:1280].rearrange("p (a b) -> p a b", b=128)
                    s.op("scalar", lambda h, src_n=src_n, h0=h0: h.activation(out=t2[:, h0:10, :], in_=src_n, func=AF.Square),
                         reads=[res("qkall")], writes=[res("t2")])
                    s.op("vector", lambda h, h0=h0: h.tensor_reduce(out=st10[:, h0:10], in_=t2[:, h0:10, :], axis=AX.X, op=ALU.add),
                         reads=[res("t2")], writes=[res("st10")])
                    rstd_chain(st10[:, h0:10], st10[:, h0:10], 128, res("st10"), res("st10"))
                    s.op("vector", lambda h, src_n=src_n, h0=h0, nn=nn: h.tensor_tensor(
                        out=qk20[:, h0:10, :], in0=src_n, in1=st10[:, h0:10].unsqueeze(2).broadcast_to([128, nn, 128]), op=ALU.mult),
                        reads=[res("qkall"), res("st10")], writes=[res("qk20")])
                    s.op("vector", lambda h, h0=h0: h.tensor_tensor(out=qk20[:, h0:10, :], in0=qk20[:, h0:10, :], in1=g10[:, h0:10, :], op=ALU.mult),
                         reads=[res("qk20"), res("g10")], writes=[res("qk20")])
                    w0 = 10 if own else 18
                    c0 = 1536 + (w0 - 10) * 128
                    s.op("scalar", lambda h, w0=w0, c0=c0: h.copy(out=qk20[:, w0:20, :], in_=qkall[:, c0:2816].rearrange("p (a b) -> p a b", b=128)),
                         reads=[res("qkall")], writes=[res("qk20")])
                    rows = slice(i * 128, (i + 1) * 128)
                    if p == 0:
                        for (dst, src_ap) in ((ngk, qk20[:, 8:10, :].rearrange("p a b -> p (a b)")), (ngv, qkall[:, 1280:1536]),
                                              (nwk, qkall[:, 2560:2816]), (nwv, qkall[:, 2816:3072])):
                            s.dma("sync", lambda h, dst=dst, src_ap=src_ap, rows=rows: h.dma_start(out=dst[rows, :], in_=src_ap),
                                  reads=[res("qk20"), res("qkall")])
                        s.op("scalar", lambda h: h.copy(out=qkb[:], in_=qk20[:]), reads=[res("qk20")], writes=[res("qkb")])
                    else:
                        s.dma("sync", lambda h, rows=rows: h.dma_start(out=rc[:], in_=ropec[rows, :]), writes=[res("rc")])
                        s.dma("sync", lambda h, rows=rows: h.dma_start(out=rs_[:], in_=ropes[rows, :]), writes=[res("rs")])
                        groups = [(0, 20)] if own else [(8, 10), (18, 20)]
                        for (a, bnd) in groups:
                            nh = bnd - a
                            s.op("vector", lambda h, a=a, bnd=bnd, nh=nh: h.tensor_tensor(
                                out=t1[:, a:bnd, :], in0=qk20[:, a:bnd, :], in1=rc[:].unsqueeze(1).broadcast_to([128, nh, 128]), op=ALU.mult),
                                reads=[res("qk20"), res("rc")], writes=[res("t1")])
                            for pr in range(2):
                                for hf in range(2):
                                    o_ = t2[:, a:bnd, pr * 64 + hf * 32: pr * 64 + hf * 32 + 32]
                                    i_ = qk20[:, a:bnd, pr * 64 + (1 - hf) * 32: pr * 64 + (1 - hf) * 32 + 32]
                                    sn = rs_[:, pr * 64 + hf * 32: pr * 64 + hf * 32 + 32].unsqueeze(1).broadcast_to([128, nh, 32])
                                    s.op("gpsimd", lambda h, o_=o_, i_=i_, sn=sn: h.tensor_tensor(out=o_, in0=i_, in1=sn, op=ALU.mult),
                                         reads=[res("qk20"), res("rs")], writes=[res("t2")])
                            s.op("vector", lambda h, a=a, bnd=bnd: h.tensor_tensor(out=qkb[:, a:bnd, :], in0=t1[:, a:bnd, :], in1=t2[:, a:bnd, :], op=ALU.add),
                                 reads=[res("t1"), res("t2")], writes=[res("qkb")])
                    hs = [(0, 20)] if own else [(8, 10), (18, 20)]
                    for (a, bnd) in hs:
                        s.op("scalar", lambda h, a=a, bnd=bnd: h.activation(out=t1[:, a:bnd, :], in_=qkb[:, a:bnd, :], func=AF.Square),
                             reads=[res("qkb")], writes=[res("t1")])
                        s.op("vector", lambda h, a=a, bnd=bnd: h.tensor_reduce(out=st20[:, a:bnd], in_=t1[:, a:bnd, :], axis=AX.X, op=ALU.add),
                             reads=[res("t1")], writes=[res("st20")])
                    grp = [(0, 0, 8), (1, 8, 10), (2, 10, 18), (3, 18, 20)] if own else [(1, 8, 10), (3, 18, 20)]
                    for (gi_, a, bnd) in grp:
                        s.op("vector", lambda h, gi_=gi_, a=a, bnd=bnd: h.tensor_reduce(out=g4[:, gi_:gi_ + 1], in_=st20[:, a:bnd], axis=AX.X, op=ALU.max),
                             reads=[res("st20")], writes=[res("g4")])
                        s.op("vector", lambda h, gi_=gi_: h.tensor_tensor(out=mx[:, p, gi_:gi_ + 1], in0=mx[:, p, gi_:gi_ + 1], in1=g4[:, gi_:gi_ + 1], op=ALU.max),
                             reads=[res("g4"), res("mx")], writes=[res("mx")])
                    s.op("scalar", lambda h: h.copy(out=vb[:, 0:256], in_=qkall[:, 1280:1536]), reads=[res("qkall")], writes=[res("vb")])
                    s.op("scalar", lambda h: h.copy(out=vb[:, 256:512], in_=qkall[:, 2816:3072]), reads=[res("qkall")], writes=[res("vb")])
                    s.dma("sync", lambda h, rows=rows: h.dma_start(out=qsp[p][rows, :], in_=qkb[:].rearrange("p a b -> p (a b)")),
                          reads=[res("qkb")])
                    s.dma("sync", lambda h, rows=rows: h.dma_start(out=vsp[p][rows, :], in_=vb[:]),
                          reads=[res("vb")])
                s.emit()

        def attn_phase(p, OT):
            nq_t = 8
            nloc = 8 if p == 0 else 16
            nkt = 8 if p == 0 else 18
            koff = 0 if p == 0 else 2
            with ExitStack() as ph:
                def sbp(name, shape, dt=F32):
                    return ph.enter_context(nc.sbuf_tensor(f"{name}_a{p}", list(shape), dt))
                QT = [sbp("QTg", [128, 8, 1024], BF16), sbp("QTw", [128, 8, 1024], BF16)]
                KT = [sbp("KTg", [128, 2, nkt * 128], BF16), sbp("KTw", [128, 2, nkt * 128], BF16)]
                V = sbp("V", [128, nkt, 512], BF16)
                qt = [sbp(f"qt{i}", [128, 2560], BF16) for i in range(2)]
                pb = [sbp(f"pb{i}", [128, 512], BF16) for i in range(4)]
                rec = [sbp(f"rec{i}", [128, 512]) for i in range(2)]
                SE = sbp("SE", [128, 8])
                m4 = sbp("m4", [128, 4]); dg = sbp("dg", [128, 4]); mb4 = sbp("mb4", [128, 4])
                cft = sbp("cft", [128, 512]); cbt = sbp("cbt", [128, 512], BF16); st4 = sbp("st4", [128, 4])
                if p == 1:
                    for kt in range(2):
                        for which, kind in ((0, 0), (2, 1)):
                            s.dma("sync", lambda h, kt=kt, which=which: h.dma_start(out=cft[:, 0:256], in_=cache[which, kt * 128:(kt + 1) * 128, :]),
                                  writes=[res("cft")])
                            s.op("vector", lambda h: h.tensor_copy(out=cbt[:, 0:256], in_=cft[:, 0:256]), reads=[res("cft")], writes=[res("cbt")])
                            s.op("scalar", lambda h: h.activation(out=cft[:, 256:512], in_=cbt[:, 0:256], func=AF.Square),
                                 reads=[res("cbt")], writes=[res("cft2")])
                            s.op("vector", lambda h: h.tensor_reduce(out=st4[:, 0:2], in_=cft[:, 256:512].rearrange("p (a b) -> p a b", b=128), axis=AX.X, op=ALU.add),
                                 reads=[res("cft2")], writes=[res("st4")])
                            s.op("vector", lambda h: h.tensor_reduce(out=st4[:, 2:3], in_=st4[:, 0:2], axis=AX.X, op=ALU.max),
                                 reads=[res("st4")], writes=[res("st4")])
                            col = 1 if kind == 0 else 3
                            s.op("vector", lambda h, col=col: h.tensor_tensor(out=mx[:, 1, col:col + 1], in0=mx[:, 1, col:col + 1], in1=st4[:, 2:3], op=ALU.max),
                                 reads=[res("st4"), res("mx")], writes=[res("mx")])
                            for n in range(2):
                                s.op("tensor", lambda h, n=n: h.transpose(out=pt[0][:, n * 128:(n + 1) * 128], in_=cbt[:, n * 128:(n + 1) * 128], identity=identb),
                                     reads=[res("cbt"), res("cb")], writes=[r_pt[0]], signal=(n == 1))
                            s.op("vector", lambda h, kt=kt, kind=kind: h.tensor_copy(
                                out=KT[kind][:, :, kt * 128:(kt + 1) * 128], in_=pt[0][:, 0:256].rearrange("p (a b) -> p a b", b=128)),
                                reads=[r_pt[0]], writes=[res(f"KT{kind}")])
                        for which, off in ((1, 0), (3, 256)):
                            s.dma("sync", lambda h, kt=kt, which=which: h.dma_start(out=cft[:, 0:256], in_=cache[which, kt * 128:(kt + 1) * 128, :]),
                                  writes=[res("cft")])
                            s.op("vector", lambda h, kt=kt, off=off: h.tensor_copy(out=V[:, kt, off:off + 256], in_=cft[:, 0:256]),
                                 reads=[res("cft")], writes=[res("V")])
                for i in range(nloc):
                    b = i % 2
                    rows = slice(i * 128, (i + 1) * 128)
                    s.dma("sync", lambda h, b=b, rows=rows: h.dma_start(out=qt[b][:], in_=qsp[p][rows, :]),
                          writes=[res(f"qt{b}")])
                    s.dma("sync", lambda h, i=i, rows=rows: h.dma_start(out=V[:, koff + i, :], in_=vsp[p][rows, :]),
                          writes=[res("V")])
                    if i < 8:
                        for kind in range(2):
                            c0 = 0 if kind == 0 else 1280
                            for hh in range(8):
                                s.op("tensor", lambda h, b=b, hh=hh, c0=c0, kind=kind: h.transpose(
                                    out=pt[kind][:, hh * 128:(hh + 1) * 128], in_=qt[b][:, c0 + hh * 128:c0 + (hh + 1) * 128], identity=identb),
                                    reads=[res(f"qt{b}"), res("cb")], writes=[r_pt[kind]], signal=(hh == 7))
                            o_ = QT[kind][:, :, i * 128:(i + 1) * 128]
                            i_ = pt[kind][:, :].rearrange("p (a b) -> p a b", b=128)
                            if kind == 0:
                                s.op("scalar", lambda h, o_=o_, i_=i_: h.copy(out=o_, in_=i_), reads=[r_pt[kind]], writes=[res(f"QT{kind}")])
                            else:
                                s.op("vector", lambda h, o_=o_, i_=i_: h.tensor_copy(out=o_, in_=i_), reads=[r_pt[kind]], writes=[res(f"QT{kind}")])
                    for kind in range(2):
                        c0 = 1024 if kind == 0 else 2304
                        for n in range(2):
                            s.op("tensor", lambda h, b=b, n=n, c0=c0, kind=kind: h.transpose(
                                out=pt[kind][:, n * 128:(n + 1) * 128], in_=qt[b][:, c0 + n * 128:c0 + (n + 1) * 128], identity=identb),
                                reads=[res(f"qt{b}"), res("cb")], writes=[r_pt[kind]], signal=(n == 1))
                        kk = koff + i
                        s.op("vector", lambda h, kk=kk, kind=kind: h.tensor_copy(
                            out=KT[kind][:, :, kk * 128:(kk + 1) * 128], in_=pt[kind][:, 0:256].rearrange("p (a b) -> p a b", b=128)),
                            reads=[r_pt[kind]], writes=[res(f"KT{kind}")])
                if p == 1:
                    zt = sbp("zt", [128, 8192], BF16)
                    s.op("gpsimd", lambda h: h.memset(zt[:], 0.0), writes=[res("zt")])
                    for e in range(NE):
                        s.dma("sync", lambda h, e=e: h.dma_start(
                            out=obuf[e * CAP + 512:(e + 1) * CAP, :].rearrange("(p r) d -> p (r d)", p=128), in_=zt[:]),
                            reads=[res("zt")])
                s.op("tensor", lambda h: h.transpose(out=pp[0][0:4, 0:128], in_=mx[:, p, :], identity=identf),
                     reads=[res("mx"), res("cf")], writes=[r_pp[0]])
                s.op("vector", lambda h: h.tensor_reduce(out=m4[0:4, 0:1], in_=pp[0][0:4, 0:128], axis=AX.X, op=ALU.max),
                     reads=[r_pp[0]], writes=[res("m4")])
                s.op("vector", lambda h: h.tensor_scalar(out=dg[0:4, 0:4], in0=identf[0:4, 0:4], scalar1=m4[0:4, 0:1], scalar2=None, op0=ALU.mult),
                     reads=[res("m4"), res("cf")], writes=[res("dg")])
                s.op("tensor", lambda h: h.matmul(pp[1][:, 0:4], lhsT=onesf[0:4, 0:128], rhs=dg[0:4, 0:4], start=True, stop=True),
                     reads=[res("dg"), res("cf")], writes=[r_pp[1]])
                s.op("vector", lambda h: h.tensor_copy(out=mb4[:], in_=pp[1][:, 0:4]), reads=[r_pp[1]], writes=[res("mb4")])
                for kind in range(2):
                    s.op("vector", lambda h, kind=kind: h.tensor_tensor(out=negm[:, p, kind:kind + 1], in0=mb4[:, 2 * kind:2 * kind + 1],
                                                                        in1=mb4[:, 2 * kind + 1:2 * kind + 2], op=ALU.mult),
                         reads=[res("mb4")], writes=[res("negm")])
                s.op("scalar", lambda h: h.activation(out=negm[:, p, :], in_=negm[:, p, :], func=AF.Sqrt), reads=[res("negm")], writes=[res("negm")])
                s.op("vector", lambda h: h.tensor_scalar(out=negm[:, p, :], in0=negm[:, p, :], scalar1=-SCALE, scalar2=None, op0=ALU.mult),
                     reads=[res("negm")], writes=[res("negm")])
                s.op("scalar", lambda h: h.activation(out=SE[:], in_=sinkb[:], func=AF.Exp, bias=negm[:, p, 1:2], scale=1.0),
                     reads=[res("negm"), res("sinkb")], writes=[res("SE")])

                jobs = []
                if p == 0:
                    for sq in range(4):
                        for kind in range(2):
                            for n in range(2):
                                for qc in range(2):
                                    h0 = 4 * n + 2 * qc
                                    q_ap = QT[kind][:, h0:h0 + 2, sq * 256:(sq + 1) * 256]
                                    keys = [(sq * 2 + kt, None) for kt in range(2)]
                                    o_ap = OT[:, kind * 8 + h0:kind * 8 + h0 + 2, sq * 256:(sq + 1) * 256]
                                    jobs.append((kind, n, q_ap, keys, o_ap, (h0, 2, 256)))
                else:
                    for n in range(2):
                        for qb in range(8):
                            q_ap = QT[0][:, 4 * n:4 * n + 4, qb * 128:(qb + 1) * 128]
                            o_ap = OT[:, 4 * n:4 * n + 4, qb * 128:(qb + 1) * 128]
                            jobs.append((0, n, q_ap, [(kt, None) for kt in range(18)], o_ap, (4 * n, 4, 128)))
                    for n in range(2):
                        for qb in range(8):
                            q_ap = QT[1][:, 4 * n:4 * n + 4, qb * 128:(qb + 1) * 128]
                            o_ap = OT[:, 8 + 4 * n:8 + 4 * n + 4, qb * 128:(qb + 1) * 128]
                            prev = (2 + qb - 1, 384) if qb > 0 else (2 + 8, 640)
                            nxt = (2 + qb + 1, 512) if qb < 7 else (2 + 8, 768)
                            keys = [(0, None), (1, None), prev, (2 + qb, None), nxt]
                            jobs.append((1, n, q_ap, keys, o_ap, (4 * n, 4, 128)))
                npb = 0
                for ji, (kind, n, q_ap, keys, o_ap, (h0, nh, nqq)) in enumerate(jobs):
                    po, psm = 2 + (ji % 2), 4 + (ji % 2)
                    def emit_S(ki):
                        kt = keys[ki][0]
                        sbank = ki % 2
                        s.op("tensor", lambda h, sbank=sbank, kind=kind, n=n, kt=kt, q_ap=q_ap: h.matmul(
                            pp[sbank][:], lhsT=KT[kind][:, n, kt * 128:(kt + 1) * 128], rhs=q_ap, start=True, stop=True),
                            reads=[res(f"KT{kind}"), res(f"QT{kind}")], writes=[r_pp[sbank]])
                    emit_S(0)
                    for ki, (kt, moff) in enumerate(keys):
                        sbank = ki % 2
                        pi = npb % 4
                        npb += 1
                        s.op("scalar", lambda h, pi=pi, sbank=sbank, kind=kind: h.activation(
                            out=pb[pi][:], in_=pp[sbank][:], func=AF.Exp, bias=negm[:, p, kind:kind + 1], scale=SCALE),
                            reads=[r_pp[sbank], res("negm")], writes=[res(f"pb{pi}")])
                        if ki + 1 < len(keys):
                            emit_S(ki + 1)
                        if moff is not None:
                            mk = cb[:, moff:moff + 128].unsqueeze(1).broadcast_to([128, 4, 128])
                            s.op("gpsimd", lambda h, pi=pi, mk=mk: h.tensor_tensor(
                                out=pb[pi][:].rearrange("p (a b) -> p a b", b=128), in0=pb[pi][:].rearrange("p (a b) -> p a b", b=128), in1=mk, op=ALU.mult),
                                reads=[res(f"pb{pi}"), res("cb")], writes=[res(f"pb{pi}")])
                        vs = V[:, kt, kind * 256 + n * 128: kind * 256 + (n + 1) * 128]
                        last = ki == len(keys) - 1
                        s.op("tensor", lambda h, po=po, vs=vs, pi=pi, ki=ki, last=last: h.matmul(
                            pp[po][:], lhsT=vs, rhs=pb[pi][:], start=(ki == 0), stop=last),
                            reads=[res("V"), res(f"pb{pi}")], writes=[r_pp[po]], signal=last)
                        s.op("tensor", lambda h, psm=psm, pi=pi, ki=ki, last=last: h.matmul(
                            pp[psm][:], lhsT=onesb, rhs=pb[pi][:], start=(ki == 0), stop=last),
                            reads=[res("cb"), res(f"pb{pi}")], writes=[r_pp[psm]], signal=True)
                    rb = ji % 2
                    if kind == 1:
                        se = SE[:, h0:h0 + nh].unsqueeze(2).broadcast_to([128, nh, nqq])
                        s.op("vector", lambda h, rb=rb, psm=psm, se=se, nqq=nqq: h.tensor_tensor(
                            out=rec[rb][:].rearrange("p (a b) -> p a b", b=nqq), in0=pp[psm][:].rearrange("p (a b) -> p a b", b=nqq), in1=se, op=ALU.add),
                            reads=[r_pp[psm], res("SE")], writes=[res(f"rec{rb}")])
                        s.op("vector", lambda h, rb=rb: h.reciprocal(out=rec[rb][:], in_=rec[rb][:]), reads=[res(f"rec{rb}")], writes=[res(f"rec{rb}")])
                    else:
                        s.op("vector", lambda h, rb=rb, psm=psm: h.reciprocal(out=rec[rb][:], in_=pp[psm][:]), reads=[r_pp[psm]], writes=[res(f"rec{rb}")])
                    s.op("vector", lambda h, rb=rb, po=po, o_ap=o_ap, nqq=nqq: h.tensor_tensor(
                        out=o_ap, in0=pp[po][:].rearrange("p (a b) -> p a b", b=nqq), in1=rec[rb][:].rearrange("p (a b) -> p a b", b=nqq), op=ALU.mult),
                        reads=[r_pp[po], res(f"rec{rb}")], writes=[res("OT")])
                s.emit()

        def post_phase(p, OT):
            with ExitStack() as ph:
                def sbp(name, shape, dt=F32):
                    return ph.enter_context(nc.sbuf_tensor(f"{name}_p{p}", list(shape), dt))
                wo = sbp("wo", [128, 16, D], BF16)
                wr = sbp("wr", [128, 16, NE], BF16)
                G1 = sbp("G1", [128, D]); A2 = sbp("A2", [128, D]); B2 = sbp("B2", [128, D])
                xt = sbp("xt", [128, D]); yt = sbp("yt", [128, D]); tf = sbp("tf", [128, D])
                h2b = sbp("h2b", [128, D], BF16); h2T = sbp("h2T", [128, 16, 128], BF16)
                st = sbp("st", [128, 8])
                sc = sbp("sc", [128, NE]); sel = sbp("sel", [128, NE]); srt = sbp("srt", [128, 8, 8]); gs = sbp("gs", [128, 8])
                gs8 = sbp("gs8", [128, 8]); gm = sbp("gm", [128, 8]); gneg = sbp("gneg", [128, 8]); selm = sbp("selm", [128, NE])
                top8 = sbp("top8", [128, 8]); Mf = sbp("Mf", [128, NE]); wsel = sbp("wsel", [128, NE]); den = sbp("den", [128, 2])
                posf = sbp("posf", [128, NE]); vv = sbp("vv", [128, NE]); d8 = sbp("d8", [128, 8]); oh = sbp("oh", [128, NE])
                g8 = sbp("g8", [128, 8]); eoff = sbp("eoff", [128, NE])
                for n in range(4):
                    s.dma("gpsimd", lambda h, n=n: h.dma_start(out=wo[:, :, n * 512:(n + 1) * 512],
                                                              in_=w_out[:, n * 512:(n + 1) * 512].rearrange("(c p) n -> p c n", p=128)), writes=[res("wo")])
                s.dma("gpsimd", lambda h: h.dma_start(out=wr[:], in_=w_router.rearrange("(c p) n -> p c n", p=128)), writes=[res("wr")])
                s.dma("sync", lambda h: h.dma_start(out=G1[:], in_=modsp[4 + p]), writes=[res("G1")])
                s.dma("sync", lambda h: h.dma_start(out=A2[:], in_=modsp[8 + p]), writes=[res("A2")])
                s.dma("sync", lambda h: h.dma_start(out=B2[:], in_=modsp[6 + p]), writes=[res("B2")])
                s.op("gpsimd", lambda h: h.iota(eoff[:], pattern=[[CAP, NE]], base=1, channel_multiplier=0, allow_small_or_imprecise_dtypes=True),
                     writes=[res("eoff")])
                for i in range(8):
                    gi = p * 8 + i
                    rows = slice(i * 128, (i + 1) * 128)
                    grow = slice(gi * 128, (gi + 1) * 128)
                    for n in range(4):
                        for mh in range(16):
                            s.op("tensor", lambda h, n=n, mh=mh, i=i: h.matmul(pp[n][:], lhsT=OT[:, mh, i * 128:(i + 1) * 128], rhs=wo[:, mh, n * 512:(n + 1) * 512],
                                                                              start=(mh == 0), stop=(mh == 15)),
                                 reads=[res("OT"), res("wo")], writes=[r_pp[n]], signal=(mh == 15))
                        s.op("scalar", lambda h, n=n: h.activation(out=tf[:, n * 512:(n + 1) * 512], in_=pp[n][:], func=AF.Square, accum_out=st[:, n:n + 1]),
                             reads=[r_pp[n]], writes=[res("tf"), res("st")])
                    s.op("vector", lambda h: h.tensor_reduce(out=st[:, 4:5], in_=st[:, 0:4], axis=AX.X, op=ALU.add), reads=[res("st")], writes=[res("st")])
                    rstd_chain(st[:, 4:5], st[:, 5:6], D, res("st"), res("st"))
                    s.dma("sync", lambda h, rows=rows: h.dma_start(out=xt[:], in_=xin[p][rows, :]), writes=[res("xt")])
                    for n in range(4):
                        cs = slice(n * 512, (n + 1) * 512)
                        s.op("vector", lambda h, n=n, cs=cs: h.scalar_tensor_tensor(out=tf[:, cs], in0=pp[n][:], scalar=st[:, 5:6], in1=G1[:, cs],
                                                                                    op0=ALU.mult, op1=ALU.mult),
                             reads=[r_pp[n], res("st"), res("G1")], writes=[res("tf")])
                    s.op("gpsimd", lambda h: h.tensor_tensor(out=yt[:], in0=tf[:], in1=xt[:], op=ALU.add), reads=[res("tf"), res("xt")], writes=[res("yt")])
                    s.dma("sync", lambda h, grow=grow: h.dma_start(out=ysp[grow, :], in_=yt[:]), reads=[res("yt")])
                    s.op("scalar", lambda h: h.activation(out=tf[:], in_=yt[:], func=AF.Square, accum_out=st[:, 6:7]),
                         reads=[res("yt")], writes=[res("tf"), res("st")])
                    rstd_chain(st[:, 6:7], st[:, 7:8], D, res("st"), res("st"))
                    s.op("vector", lambda h: h.scalar_tensor_tensor(out=tf[:], in0=yt[:], scalar=st[:, 7:8], in1=A2[:], op0=ALU.mult, op1=ALU.mult),
                         reads=[res("yt"), res("st"), res("A2")], writes=[res("tf")])
                    s.op("gpsimd", lambda h: h.tensor_tensor(out=h2b[:], in0=tf[:], in1=B2[:], op=ALU.add), reads=[res("tf"), res("B2")], writes=[res("h2b")])
                    s.dma("sync", lambda h, gi=gi: h.dma_start(out=xbuf_s[gi * 128:(gi + 1) * 128, :], in_=h2b[:]),
                          reads=[res("h2b")])
                    transpose16(h2b, res("h2b"), h2T, res("h2T"), lambda half: h2T[:, half * 8:(half + 1) * 8, :])
                    for k in range(16):
                        s.op("tensor", lambda h, k=k: h.matmul(pp[4][:, 0:NE], lhsT=h2T[:, k, :], rhs=wr[:, k, :], start=(k == 0), stop=(k == 15)),
                             reads=[res("h2T"), res("wr")], writes=[r_pp[4]], signal=(k == 15))
                    s.op("scalar", lambda h: h.activation(out=sc[:], in_=pp[4][:, 0:NE], func=AF.Sigmoid), reads=[r_pp[4]], writes=[res("sc")])
                    s.op("vector", lambda h: h.tensor_tensor(out=sel[:], in0=sc[:], in1=rbb[:], op=ALU.add), reads=[res("sc"), res("rbb")], writes=[res("sel")])
                    for g in range(8):
                        s.op("vector", lambda h, g=g: h.max(out=srt[:, g, :], in_=sel[:, g * 8:(g + 1) * 8]), reads=[res("sel")], writes=[res("srt")])
                    s.op("vector", lambda h: h.tensor_tensor(out=gs[:], in0=srt[:, :, 0], in1=srt[:, :, 1], op=ALU.add), reads=[res("srt")], writes=[res("gs")])
                    s.op("vector", lambda h: h.max(out=gs8[:], in_=gs[:]), reads=[res("gs")], writes=[res("gs8")])
                    s.op("vector", lambda h: h.tensor_scalar(out=gm[:], in0=gs[:], scalar1=gs8[:, 3:4], scalar2=None, op0=ALU.is_ge),
                         reads=[res("gs"), res("gs8")], writes=[res("gm")])
                    s.op("vector", lambda h: h.tensor_scalar(out=gneg[:], in0=gm[:], scalar1=-1.0, scalar2=1e9, op0=ALU.add, op1=ALU.mult),
                         reads=[res("gm")], writes=[res("gneg")])
                    for g in range(8):
                        s.op("vector", lambda h, g=g: h.tensor_scalar(out=selm[:, g * 8:(g + 1) * 8], in0=sel[:, g * 8:(g + 1) * 8],
                                                                      scalar1=gm[:, g:g + 1], scalar2=gneg[:, g:g + 1], op0=ALU.mult, op1=ALU.add),
                             reads=[res("sel"), res("gm"), res("gneg")], writes=[res("selm")])
                    s.op("vector", lambda h: h.max(out=top8[:], in_=selm[:]), reads=[res("selm")], writes=[res("top8")])
                    s.op("vector", lambda h: h.tensor_scalar(out=Mf[:], in0=selm[:], scalar1=top8[:, 7:8], scalar2=None, op0=ALU.is_ge),
                         reads=[res("selm"), res("top8")], writes=[res("Mf")])
                    s.op("vector", lambda h, gi=gi: h.tensor_copy(out=Mall[:, gi, :], in_=Mf[:]), reads=[res("Mf")], writes=[res("Mall")])
                    s.op("vector", lambda h: h.tensor_tensor(out=wsel[:], in0=sc[:], in1=Mf[:], op=ALU.mult), reads=[res("sc"), res("Mf")], writes=[res("wsel")])
                    s.op("vector", lambda h: h.tensor_reduce(out=den[:, 0:1], in_=wsel[:], axis=AX.X, op=ALU.add), reads=[res("wsel")], writes=[res("den")])
                    s.op("vector", lambda h: h.reciprocal(out=den[:, 1:2], in_=den[:, 0:1]), reads=[res("den")], writes=[res("den")])
                    s.op("vector", lambda h: h.tensor_scalar(out=wsel[:], in0=wsel[:], scalar1=den[:, 1:2], scalar2=2.5, op0=ALU.mult, op1=ALU.mult),
                         reads=[res("wsel"), res("den")], writes=[res("wsel")])
                    s.op("tensor", lambda h, gi=gi: h.matmul(pp[5][:, 0:NE], lhsT=ustrb, rhs=Mall[:, gi, :], start=True, stop=(gi == 0)),
                         reads=[res("Mall"), res("cb")], writes=[r_pp[5]], signal=(gi == 0))
                    for j in range(gi):
                        s.op("tensor", lambda h, j=j, gi=gi: h.matmul(pp[5][:, 0:NE], lhsT=onesb, rhs=Mall[:, j, :], start=False, stop=(j == gi - 1)),
                             reads=[res("Mall"), res("cb")], writes=[r_pp[5]], signal=(j == gi - 1))
                    s.op("vector", lambda h: h.tensor_scalar(out=posf[:], in0=pp[5][:, 0:NE], scalar1=float(CAP - 1), scalar2=None, op0=ALU.min),
                         reads=[r_pp[5]], writes=[res("posf")])
                    s.op("vector", lambda h: h.tensor_tensor(out=vv[:], in0=posf[:], in1=eoff[:], op=ALU.add), reads=[res("posf"), res("eoff")], writes=[res("vv")])
                    s.op("vector", lambda h: h.tensor_tensor(out=vv[:], in0=vv[:], in1=Mf[:], op=ALU.mult), reads=[res("vv"), res("Mf")], writes=[res("vv")])
                    s.op("vector", lambda h: h.max(out=d8[:], in_=vv[:]), reads=[res("vv")], writes=[res("d8")])
                    for k in range(8):
                        s.op("vector", lambda h, k=k: h.tensor_scalar(out=oh[:], in0=vv[:], scalar1=d8[:, k:k + 1], scalar2=None, op0=ALU.is_equal),
                             reads=[res("vv"), res("d8")], writes=[res("oh")])
                        s.op("vector", lambda h, k=k: h.tensor_tensor(out=oh[:], in0=oh[:], in1=wsel[:], op=ALU.mult), reads=[res("oh"), res("wsel")], writes=[res("oh")])
                        s.op("vector", lambda h, k=k: h.tensor_reduce(out=g8[:, k:k + 1], in_=oh[:], axis=AX.X, op=ALU.add), reads=[res("oh")], writes=[res("g8")])
                    s.op("vector", lambda h: h.tensor_scalar(out=d8[:], in0=d8[:], scalar1=-1.0, scalar2=None, op0=ALU.add), reads=[res("d8")], writes=[res("d8")])
                    s.op("vector", lambda h, gi=gi: h.tensor_copy(out=dstall[:, gi, :], in_=d8[:]), reads=[res("d8")], writes=[res("dstall")])
                    s.op("vector", lambda h, gi=gi: h.tensor_copy(out=gall[:, gi, :], in_=g8[:]), reads=[res("g8")], writes=[res("gall")])
                    if DEBUG:
                        s.dma("sync", lambda h, gi=gi: h.dma_start(out=dbg_sc[gi], in_=sc[:]), reads=[res("sc")])
                    for k in range(8):
                        s.dma("gpsimd", lambda h, k=k, gi=gi: h.indirect_dma_start(
                            out=xbuf[:, :], out_offset=bass.IndirectOffsetOnAxis(ap=dstall[:, gi, k:k + 1], axis=0),
                            in_=h2b[:], in_offset=None, bounds_check=None),
                            reads=[res("h2b"), res("dstall")])
                if p == 1:
                    cntb = sbp("cntb", [128, NE]); flg = sbp("flg", [128, NE]); cum = sbp("cum", [128, NE]); one64 = sbp("one64", [128, NE])
                    eix = sbp("eix", [128, NE]); selj = sbp("selj", [128, NE]); ev = sbp("ev", [128, NHOT])
                    mult24 = sbp("mult24", [128, 24]); offs24 = sbp("offs24", [128, 24]); idxf = sbp("idxf", [128, NHOT, 24])
                    for j in range(16):
                        s.op("tensor", lambda h, j=j: h.matmul(pp[5][:, 0:NE], lhsT=onesb, rhs=Mall[:, j, :], start=(j == 0), stop=(j == 15)),
                             reads=[res("Mall"), res("cb")], writes=[r_pp[5]], signal=(j == 15))
                    s.op("vector", lambda h: h.tensor_scalar(out=flg[:], in0=pp[5][:, 0:NE], scalar1=512.0, scalar2=None, op0=ALU.is_gt),
                         reads=[r_pp[5]], writes=[res("flg")])
                    s.op("vector", lambda h: h.memset(one64[:], 1.0), writes=[res("one64")])
                    s.op("vector", lambda h: h.tensor_tensor_scan(out=cum[:], data0=one64[:], data1=flg[:], initial=0.0, op0=ALU.mult, op1=ALU.add),
                         reads=[res("one64"), res("flg")], writes=[res("cum")])
                    s.op("vector", lambda h: h.tensor_tensor(out=cum[:], in0=cum[:], in1=flg[:], op=ALU.subtract), reads=[res("cum"), res("flg")], writes=[res("cum")])
                    s.op("gpsimd", lambda h: h.iota(eix[:], pattern=[[1, NE]], base=0, channel_multiplier=0, allow_small_or_imprecise_dtypes=True), writes=[res("eix")])
                    s.op("gpsimd", lambda h: h.iota(offs24[:, 0:4], pattern=[[128, 4]], base=512, channel_multiplier=1, allow_small_or_imprecise_dtypes=True), writes=[res("offs24")])
                    s.op("gpsimd", lambda h: h.iota(offs24[:, 4:20], pattern=[[128, 16]], base=0, channel_multiplier=1, allow_small_or_imprecise_dtypes=True), writes=[res("offs24")])
                    s.op("gpsimd", lambda h: h.iota(offs24[:, 20:24], pattern=[[128, 4]], base=0, channel_multiplier=1, allow_small_or_imprecise_dtypes=True), writes=[res("offs24")])
                    s.op("vector", lambda h: h.memset(mult24[:, 0:4], float(CAP)), writes=[res("mult24")])
                    s.op("vector", lambda h: h.memset(mult24[:, 4:20], 2048.0), writes=[res("mult24")])
                    s.op("vector", lambda h: h.memset(mult24[:, 20:24], 512.0), writes=[res("mult24")])
                    for j in range(NHOT):
                        s.op("vector", lambda h, j=j: h.tensor_scalar(out=selj[:], in0=cum[:], scalar1=float(j), scalar2=None, op0=ALU.is_equal),
                             reads=[res("cum")], writes=[res("selj")])
                        s.op("vector", lambda h: h.tensor_tensor(out=selj[:], in0=selj[:], in1=flg[:], op=ALU.mult), reads=[res("selj"), res("flg")], writes=[res("selj")])
                        s.op("vector", lambda h: h.tensor_tensor(out=selj[:], in0=selj[:], in1=eix[:], op=ALU.mult), reads=[res("selj"), res("eix")], writes=[res("selj")])
                        s.op("vector", lambda h, j=j: h.tensor_reduce(out=ev[:, j:j + 1], in_=selj[:], axis=AX.X, op=ALU.add), reads=[res("selj")], writes=[res("ev")])
                        s.op("vector", lambda h, j=j: h.scalar_tensor_tensor(out=idxf[:, j, :], in0=mult24[:], scalar=ev[:, j:j + 1], in1=offs24[:], op0=ALU.mult, op1=ALU.add),
                             reads=[res("mult24"), res("ev"), res("offs24")], writes=[res("idxf")])
                    s.op("vector", lambda h: h.tensor_copy(out=hotidx[:], in_=idxf[:]), reads=[res("idxf")], writes=[res("hotidx")])
                s.emit()

        def expert_phase():
            with ExitStack() as ph:
                def sbp(name, shape, dt=F32):
                    return ph.enter_context(nc.sbuf_tensor(f"{name}_e", list(shape), dt))
                wg = [sbp(f"wg{i}", [128, 16, 512], BF16) for i in range(2)]
                wu = [sbp(f"wu{i}", [128, 16, 512], BF16) for i in range(2)]
                wd = [sbp(f"wd{i}", [128, 4, D], BF16) for i in range(2)]
                xg = [sbp(f"xg{i}", [128, D], BF16) for i in range(2)]
                xT = sbp("xT", [128, 16, 512], BF16)
                sg = [sbp(f"sg{i}", [128, 512]) for i in range(2)]
                act = sbp("act", [128, 4, 512], BF16)
                ob = [sbp(f"ob{i}", [128, D], BF16) for i in range(2)]
                passes = [("s", e, e * CAP, e % 2) for e in range(NE)] + [("d", j, 0, (NE + j) % 2) for j in range(NHOT)] \
                    + [("s", NE, q * 512, (NE + NHOT) % 2) for q in range(4)]
                state = {"loaded": None, "nx": 0, "nob": 0, "nstg": 0}
                stgA = [sbp(f"stgA{i}", [128, 512]) for i in range(8)]
                stgD = [sbp(f"stgD{i}", [128, D]) for i in range(2)]
                wge_rows = wge.rearrange("e k n -> (e k) n")
                wue_rows = wue.rearrange("e k n -> (e k) n")
                wde_rows = wde.rearrange("e k n -> (e k) n")

                def gather_cast(dst_ap, rdst, src_rows, j, col, width):
                    ring, nm = (stgA, "stgA") if width == 512 else (stgD, "stgD")
                    si = state["nstg"] % len(ring)
                    state["nstg"] += 1
                    st_t = ring[si]
                    rst = res(f"{nm}{si}")
                    s.dma("gpsimd", lambda h, st_t=st_t, j=j, col=col: h.indirect_dma_start(
                        out=st_t[:, 0:width], out_offset=None, in_=src_rows[:, :],
                        in_offset=bass.IndirectOffsetOnAxis(ap=hotidx[:, j, col:col + 1], axis=0), bounds_check=None),
                        reads=[res("hotidx")], writes=[rst])
                    if state["nstg"] % 2 == 0:
                        s.op("vector", lambda h, st_t=st_t: h.tensor_copy(out=dst_ap, in_=st_t[:, 0:width]), reads=[rst], writes=[rdst])
                    else:
                        s.op("scalar", lambda h, st_t=st_t: h.copy(out=dst_ap, in_=st_t[:, 0:width]), reads=[rst], writes=[rdst])

                def emit_loadx(idx):
                    kind_, e, r0, b = passes[idx]
                    if kind_ == "d":
                        j = e
                        for c in range(16):
                            gather_cast(wg[b][:, c, :], res(f"wg{b}"), wge_rows, j, 4 + c, 512)
                        for c in range(16):
                            gather_cast(wu[b][:, c, :], res(f"wu{b}"), wue_rows, j, 4 + c, 512)
                        for c in range(4):
                            gather_cast(wd[b][:, c, :], res(f"wd{b}"), wde_rows, j, 20 + c, D)
                        state["loaded"] = ("d", j)
                        for sbk in range(4):
                            xb_ = state["nx"] % 2
                            state["nx"] += 1
                            s.dma("gpsimd", lambda h, xb_=xb_, j=j, sbk=sbk: h.indirect_dma_start(
                                out=xg[xb_][:], out_offset=None, in_=xbuf[:, :],
                                in_offset=bass.IndirectOffsetOnAxis(ap=hotidx[:, j, sbk:sbk + 1], axis=0), bounds_check=None),
                                reads=[res("hotidx")], writes=[res(f"xg{xb_}")])
                            transpose16(xg[xb_], res(f"xg{xb_}"), xT, res("xT"),
                                        lambda half, sbk=sbk: xT[:, half * 8:(half + 1) * 8, sbk * 128:(sbk + 1) * 128])
                        return
                    if ("s", e) != state["loaded"]:
                        state["loaded"] = ("s", e)
                        gsrc = wge[e] if e < NE else wgs
                        usrc = wue[e] if e < NE else wus
                        dsrc = wde[e] if e < NE else wds
                        s.dma("gpsimd", lambda h, b=b, gsrc=gsrc: h.dma_start(out=wg[b][:], in_=gsrc.rearrange("(c p) n -> p c n", p=128)), writes=[res(f"wg{b}")])
                        s.dma("gpsimd", lambda h, b=b, usrc=usrc: h.dma_start(out=wu[b][:], in_=usrc.rearrange("(c p) n -> p c n", p=128)), writes=[res(f"wu{b}")])
                        s.dma("gpsimd", lambda h, b=b, dsrc=dsrc: h.dma_start(out=wd[b][:], in_=dsrc.rearrange("(c p) n -> p c n", p=128)), writes=[res(f"wd{b}")])
                    for sbk in range(4):
                        xb_ = state["nx"] % 2
                        state["nx"] += 1
                        s.dma("sync", lambda h, xb_=xb_, r0=r0, sbk=sbk, e=e: h.dma_start(out=xg[xb_][:], in_=(xbuf if e < NE else xbuf_s)[r0 + sbk * 128:r0 + (sbk + 1) * 128, :]),
                              writes=[res(f"xg{xb_}")])
                        transpose16(xg[xb_], res(f"xg{xb_}"), xT, res("xT"),
                                    lambda half, sbk=sbk: xT[:, half * 8:(half + 1) * 8, sbk * 128:(sbk + 1) * 128])

                def emit_gu(idx):
                    kind_, e, r0, b = passes[idx]
                    for fc in range(4):
                        gb, ub = fc % 2, 2 + fc % 2
                        for k in range(16):
                            s.op("tensor", lambda h, gb=gb, b=b, k=k, fc=fc: h.matmul(pp[gb][:], lhsT=wg[b][:, k, fc * 128:(fc + 1) * 128], rhs=xT[:, k, :],
                                                                                      start=(k == 0), stop=(k == 15)),
                                 reads=[res(f"wg{b}"), res("xT")], writes=[r_pp[gb]], signal=(k == 15))
                        for k in range(16):
                            s.op("tensor", lambda h, ub=ub, b=b, k=k, fc=fc: h.matmul(pp[ub][:], lhsT=wu[b][:, k, fc * 128:(fc + 1) * 128], rhs=xT[:, k, :],
                                                                                      start=(k == 0), stop=(k == 15)),
                                 reads=[res(f"wu{b}"), res("xT")], writes=[r_pp[ub]], signal=(k == 15))
                        s.op("scalar", lambda h, gb=gb, fc=fc: h.activation(out=sg[fc % 2][:], in_=pp[gb][:], func=AF.Silu),
                             reads=[r_pp[gb]], writes=[res(f"sg{fc % 2}")])
                        s.op("vector", lambda h, ub=ub, fc=fc: h.tensor_tensor(out=act[:, fc, :], in0=pp[ub][:], in1=sg[fc % 2][:], op=ALU.mult),
                             reads=[r_pp[ub], res(f"sg{fc % 2}")], writes=[res("act")])

                def emit_down(idx):
                    kind_, e, r0, b = passes[idx]
                    for sbk in range(4):
                        ob_ = state["nob"] % 2
                        state["nob"] += 1
                        for n in range(4):
                            bank = 4 + n % 2
                            for fc in range(4):
                                s.op("tensor", lambda h, bank=bank, fc=fc, sbk=sbk, n=n, b=b: h.matmul(
                                    pp[bank][:], lhsT=act[:, fc, sbk * 128:(sbk + 1) * 128], rhs=wd[b][:, fc, n * 512:(n + 1) * 512],
                                    start=(fc == 0), stop=(fc == 3)),
                                    reads=[res("act"), res(f"wd{b}")], writes=[r_pp[bank]], signal=(fc == 3))
                            if n % 2 == 0:
                                s.op("scalar", lambda h, bank=bank, ob_=ob_, n=n: h.copy(
                                    out=ob[ob_][:, n * 512:(n + 1) * 512], in_=pp[bank][:]),
                                    reads=[r_pp[bank]], writes=[res(f"ob{ob_}")])
                            else:
                                s.op("vector", lambda h, bank=bank, ob_=ob_, n=n: h.tensor_copy(
                                    out=ob[ob_][:, n * 512:(n + 1) * 512], in_=pp[bank][:]),
                                    reads=[r_pp[bank]], writes=[res(f"ob{ob_}")])
                        if kind_ == "d":
                            s.dma("gpsimd", lambda h, ob_=ob_, e=e, sbk=sbk: h.indirect_dma_start(
                                out=obuf[:, :], out_offset=bass.IndirectOffsetOnAxis(ap=hotidx[:, e, sbk:sbk + 1], axis=0),
                                in_=ob[ob_][:], in_offset=None, bounds_check=None),
                                reads=[res(f"ob{ob_}"), res("hotidx")])
                        else:
                            s.dma("sync", lambda h, ob_=ob_, r0=r0, sbk=sbk, e=e: h.dma_start(out=(obuf if e < NE else obuf_s)[r0 + sbk * 128:r0 + (sbk + 1) * 128, :], in_=ob[ob_][:]),
                                  reads=[res(f"ob{ob_}")])

                emit_loadx(0)
                for idx in range(len(passes)):
                    emit_gu(idx)
                    if idx + 1 < len(passes):
                        emit_loadx(idx + 1)
                    emit_down(idx)
                s.emit()

        def combine_phase():
            with ExitStack() as ph:
                def sbp(name, shape, dt=F32):
                    return ph.enter_context(nc.sbuf_tensor(f"{name}_c", list(shape), dt))
                gk = [sbp(f"gk{i}", [128, D], BF16) for i in range(9)]
                accA = sbp("accA", [128, D]); accB = sbp("accB", [128, D])
                yt = sbp("yt", [128, D]); G2 = [sbp("G2a", [128, D]), sbp("G2b", [128, D])]
                st = sbp("st", [128, 4])
                for k in range(9):
                    s.op("gpsimd", lambda h, k=k: h.memset(gk[k][:], 0.0), writes=[res(f"gk{k}")])
                for p in range(2):
                    s.dma("sync", lambda h, p=p: h.dma_start(out=G2[p][:], in_=modsp[10 + p]), writes=[res(f"G2{p}")])
                for gi in range(16):
                    p, i = gi // 8, gi % 8
                    for k in range(8):
                        s.dma("gpsimd", lambda h, k=k, gi=gi: h.indirect_dma_start(
                            out=gk[k][:], out_offset=None, in_=obuf[:, :],
                            in_offset=bass.IndirectOffsetOnAxis(ap=dstall[:, gi, k:k + 1], axis=0),
                            bounds_check=None),
                            reads=[res("dstall")], writes=[res(f"gk{k}")])
                    s.dma("sync", lambda h, gi=gi: h.dma_start(out=gk[8][:], in_=obuf_s[gi * 128:(gi + 1) * 128, :]),
                          writes=[res("gk8")])
                    s.dma("sync", lambda h, gi=gi: h.dma_start(out=yt[:], in_=ysp[gi * 128:(gi + 1) * 128, :]), writes=[res("ytc")])
                    s.op("vector", lambda h, gi=gi: h.scalar_tensor_tensor(out=accA[:], in0=gk[0][:], scalar=gall[:, gi, 0:1], in1=gk[8][:], op0=ALU.mult, op1=ALU.add),
                         reads=[res("gk8"), res("gk0"), res("gall")], writes=[res("accA")])
                    for k in range(1, 8):
                        s.op("vector", lambda h, k=k, gi=gi: h.scalar_tensor_tensor(out=accA[:], in0=gk[k][:], scalar=gall[:, gi, k:k + 1], in1=accA[:], op0=ALU.mult, op1=ALU.add),
                             reads=[res("accA"), res(f"gk{k}"), res("gall")], writes=[res("accA")])
                    if DEBUG:
                        s.dma("sync", lambda h, gi=gi: h.dma_start(out=dbg_moe[gi * 128:(gi + 1) * 128, :], in_=accA[:]), reads=[res("accA")])
                    s.op("scalar", lambda h: h.activation(out=accB[:], in_=accA[:], func=AF.Square, accum_out=st[:, 0:1]),
                         reads=[res("accA")], writes=[res("accB"), res("stc")])
                    rstd_chain(st[:, 0:1], st[:, 1:2], D, res("stc"), res("stc"))
                    s.op("vector", lambda h, p=p: h.scalar_tensor_tensor(out=accB[:], in0=accA[:], scalar=st[:, 1:2], in1=G2[p][:], op0=ALU.mult, op1=ALU.mult),
                         reads=[res("accA"), res("stc"), res(f"G2{p}")], writes=[res("accB")])
                    s.op("gpsimd", lambda h: h.tensor_tensor(out=accB[:], in0=accB[:], in1=yt[:], op=ALU.add), reads=[res("accB"), res("ytc")], writes=[res("accB")])
                    s.dma("sync", lambda h, p=p, i=i: h.dma_start(out=youts[p][i * 128:(i + 1) * 128, :], in_=accB[:]),
                          reads=[res("accB")])
                s.emit()

        if DEBUG:
            dbg_dst = dscr("dbg_dst", [128, 16 * 8], I32)
            dbg_gate = dscr("dbg_gate", [128, 16 * 8])
            dbg_moe = dscr("dbg_moe", [2048, D])
            dbg_sc = dscr("dbg_sc", [16, 128, NE])
        for p in range(2):
            if STAGE >= 1:
                qkv_phase(p)
            if STAGE >= 2:
                with nc.sbuf_tensor(f"OT{p}", [128, 16, 1024], BF16) as OT:
                    attn_phase(p, OT)
                    if STAGE >= 3:
                        post_phase(p, OT)
        if STAGE >= 8:
            expert_phase()
            combine_phase()
        if DEBUG and STAGE >= 3:
            s.dma("sync", lambda h: h.dma_start(out=dbg_dst[:, :], in_=dstall[:].rearrange("p a b -> p (a b)")), reads=[res("dstall")])
            s.dma("sync", lambda h: h.dma_start(out=dbg_gate[:, :], in_=gall[:].rearrange("p a b -> p (a b)")), reads=[res("gall")])
        s.final_wait("sync", list(R.values()))
        s.emit()
    return nc


def _rope_tables():
    n = 2048
    rows = n // 64
    row = np.repeat(np.arange(rows, dtype=np.float32), 64)
    col = np.tile(np.arange(64, dtype=np.float32), rows)
    inv_freq = (10000.0 ** (-np.arange(0, 64, 2, dtype=np.float32) / 64)).astype(np.float32)
    ang_r = row[:, None] * inv_freq
    ang_c = col[:, None] * inv_freq
    ang = np.concatenate([ang_r, ang_r, ang_c, ang_c], axis=-1).astype(np.float32)
    cos = np.cos(ang).astype(np.float32)
    sin = np.sin(ang).astype(np.float32)
    sgn = np.ones(128, np.float32)
    sgn[0:32] = -1.0
    sgn[64:96] = -1.0
    return cos, sin * sgn[None, :]


def _consts(half):
    c = np.zeros((128, 7 * 128), np.float32)
    j = np.arange(128)[:, None]
    r = np.arange(128)[None, :]
    c[:, 0:128] = np.eye(128, dtype=np.float32)
    c[:, 128:256] = (j < r).astype(np.float32)
    c[:, 256:384] = 1.0
    band_prev = (j >= r).astype(np.float32)
    band_next = (j <= r).astype(np.float32)
    c[:, 384:512] = band_prev
    c[:, 512:640] = band_next
    c[:, 640:768] = band_prev if half == 1 else 0.0
    c[:, 768:896] = band_next if half == 0 else 0.0
    return c


def _local_order(half):
    own = np.arange(half * 1024, (half + 1) * 1024)
    if half == 0:
        other = np.arange(1024, 2048)
    else:
        other = np.concatenate([np.arange(896, 1024), np.arange(0, 896)])
    return np.concatenate([own, other])


_NC_CACHE = {}


def kernel(x_prompt, x_sample, cache_glob_k, cache_glob_v, cache_win_k, cache_win_v, c, c_ctx,
           w_ada, b_ada, attn_pre_g, attn_post_g, w_in, q_norm_g, k_norm_g, sink_logit, w_out,
           ffn_pre_g, ffn_post_g, w_router, router_bias, w_gate_e, w_up_e, w_down_e,
           w_gate_s, w_up_s, w_down_s):
    f = lambda a: np.ascontiguousarray(np.asarray(a, dtype=np.float32))
    x_prompt, x_sample = f(x_prompt), f(x_sample)
    cos, sins = _rope_tables()
    shared = {
        "w_ada": f(w_ada)[0], "b_ada": f(b_ada)[0][None, :],
        "gains": np.stack([f(attn_pre_g)[0], f(attn_post_g)[0], f(ffn_pre_g)[0], f(ffn_post_g)[0]]),
        "w_in": f(w_in)[0], "qkg": np.stack([f(q_norm_g)[0], f(k_norm_g)[0]]),
        "sink": f(sink_logit)[0][None, :], "w_out": f(w_out)[0], "w_router": f(w_router)[0],
        "rbias": f(router_bias)[0][None, :], "wge": f(w_gate_e)[0], "wue": f(w_up_e)[0], "wde": f(w_down_e)[0],
        "wgs": f(w_gate_s)[0], "wus": f(w_up_s)[0], "wds": f(w_down_s)[0],
    }
    caches = [f(cache_glob_k), f(cache_glob_v), f(cache_win_k), f(cache_win_v)]
    in_maps = []
    for core in range(NCORES):
        b, half = core // 2, core % 2
        order = _local_order(half)
        m = dict(shared)
        m["xc"] = x_prompt[4 * core:4 * core + 4].reshape(1024, D)
        m["xl"] = np.ascontiguousarray(x_sample[b][order])
        m["ropec"] = np.ascontiguousarray(cos[order])
        m["ropes"] = np.ascontiguousarray(sins[order])
        m["cache"] = np.stack([cc[b, 0].reshape(256, 256) for cc in caches])
        m["cond"] = np.stack([f(c_ctx), f(c)[b]])
        m["consts"] = _consts(half)
        in_maps.append(m)
    if "nc" not in _NC_CACHE:
        del INPUT_NAMES[:]
        _NC_CACHE["nc"] = build()
    nc = _NC_CACHE["nc"]
    in_maps = [{k: v for k, v in m.items() if k in INPUT_NAMES} for m in in_maps]
    r = run_bass_kernel_spmd(nc, in_maps, core_ids=list(range(NCORES))).results
    if DEBUG:
        _NC_CACHE["raw"] = r
    y_p = np.concatenate([r[i]["yc"].reshape(4, 256, D) for i in range(NCORES)], axis=0)
    y_s = np.stack([np.concatenate([r[2 * b]["yl"], r[2 * b + 1]["yl"]], axis=0) for b in range(4)])
    def kv(name):
        return np.concatenate([r[i][name].reshape(4, 1, 256, 2, 128) for i in range(NCORES)], axis=0)
    return (y_p.astype(np.float32), y_s.astype(np.float32), kv("ngk"), kv("ngv"), kv("nwk"), kv("nwv"))
```
